# Optimizing a Trainium2 kernel written in Bass

```python
import math
import jax
import jax.numpy as jnp
from jax import lax
import numpy as np

D_MODEL = 2048
BATCH = 4
SEQ = 2048
DEPTH = 2

CTX_LEN = 256
GRID_W = 64

N_EVEN = (DEPTH + 1) // 2
N_ODD = DEPTH // 2
LAST_ATTN_LAYER = ((DEPTH - 1) // 2) * 2
DEEPNORM_ALPHA = (2.0 * DEPTH) ** 0.25
DEEPNORM_BETA = (8.0 * DEPTH) ** -0.25
LN_EPS = 1e-6
NEG_INF = -1e30
Q_BLOCK = 128

MLA_HEADS = 8
MLA_NOPE = 128
MLA_ROPE = 64
MLA_QK = MLA_NOPE + MLA_ROPE
MLA_V = 128
MLA_KV_RANK = 512
ROPE_THETA = 10000.0

NA_HEADS = 8
NA_HEAD_DIM = 128
NA_KH = 8
NA_KW = 16
NA_QC = 16
NA_KC = NA_QC + NA_KW

OFF_CKV = MLA_HEADS * MLA_QK
OFF_KPE = OFF_CKV + MLA_KV_RANK
OFF_QNA = OFF_KPE + MLA_ROPE
OFF_KNA = OFF_QNA + NA_HEADS * NA_HEAD_DIM
OFF_VNA = OFF_KNA + NA_HEADS * NA_HEAD_DIM
IN_AB_W = OFF_VNA + NA_HEADS * NA_HEAD_DIM
MIX_AB_W = MLA_HEADS * MLA_V + NA_HEADS * NA_HEAD_DIM

HY_W = 1024
HY_ORDER = 2
HY_IN_W = (HY_ORDER + 1) * HY_W
HY_SHORT = 3
HY_BANDS = 16
HY_EMB = 1 + 2 * HY_BANDS
HY_FILT_HID = 64
HY_FILT_OUT = HY_ORDER * 2 * HY_W
HY_DECAY_TARGET = 1e-2
HY_FAST_DECAY_PCT = 0.3
HY_SLOW_DECAY_PCT = 1.5
HY_MIN_DECAY = math.log(HY_DECAY_TARGET) / HY_SLOW_DECAY_PCT
HY_MAX_DECAY = math.log(HY_DECAY_TARGET) / HY_FAST_DECAY_PCT

FN_W = 1024
FN_GROUPS = 4
FN_GROUP_W = FN_W // FN_GROUPS
IN_CD_W = HY_IN_W + FN_W
MIX_CD_W = HY_W + FN_W

N_EXPERTS = 16
EC_CAPACITY_FACTOR = 2
EXPERT_FF = 1408

kernel_name = 'hybrid_mla_natten_hyena_fnet_ecmoe_dit'


def layer_norm(x, g, b):
    xf = x.astype(jnp.float32)
    mu = jnp.mean(xf, -1, keepdims=True)
    var = jnp.mean(jnp.square(xf - mu), -1, keepdims=True)
    y = (xf - mu) * lax.rsqrt(var + LN_EPS) * g.astype(jnp.float32) + b.astype(jnp.float32)
    return y.astype(x.dtype)


def rms_norm(x, g):
    xf = x.astype(jnp.float32)
    y = xf * lax.rsqrt(jnp.mean(jnp.square(xf), -1, keepdims=True) + LN_EPS) * g.astype(jnp.float32)
    return y.astype(x.dtype)


def ada_params(cvec, w, b):
    return jax.nn.silu(cvec) @ w + b


def modulate(h, shift, scale):
    return h * (1 + scale) + shift


def post_norm(h, y, gate, g, b):
    return layer_norm(DEEPNORM_ALPHA * h + gate * y, g, b)


def axial_rope_tables(n_tok):
    t = np.arange(n_tok)
    row = (t // GRID_W).astype(np.float32)
    col = (t % GRID_W).astype(np.float32)
    n_freq = MLA_ROPE // 4
    inv = (ROPE_THETA ** (-np.arange(n_freq, dtype=np.float32) / n_freq)).astype(np.float32)
    ang = np.concatenate([row[:, None] * inv, col[:, None] * inv], axis=1)
    return np.cos(ang).astype(np.float32), np.sin(ang).astype(np.float32)


def apply_rope(t, cos, sin):
    cos = jnp.asarray(cos, t.dtype)
    sin = jnp.asarray(sin, t.dtype)
    t1, t2 = jnp.split(t, 2, axis=-1)
    return jnp.concatenate([t1 * cos - t2 * sin, t2 * cos + t1 * sin], axis=-1)


def mla_up(ckv, kv_norm, w_ukv):
    B, L, _ = ckv.shape
    kv = (rms_norm(ckv, kv_norm) @ w_ukv).reshape(B, L, MLA_HEADS, MLA_NOPE + MLA_V)
    return kv[..., :MLA_NOPE], kv[..., MLA_NOPE:]


def mla_attend_latent(q_nope, q_pe, k_nope, k_pe, v, kc_nope, kc_pe, vc):
    B, S, H, _ = q_nope.shape
    nb = S // Q_BLOCK
    scale = MLA_QK ** -0.5

    def to_blocks(t):
        return jnp.moveaxis(t.reshape(B, nb, Q_BLOCK, *t.shape[2:]), 1, 0)

    def one_block(qs):
        qn, qp = qs
        s_lat = jnp.einsum('bqhd,bkhd->bhqk', qn, k_nope) + jnp.einsum('bqhr,bkr->bhqk', qp, k_pe)
        s_ctx = jnp.einsum('bqhd,bkhd->bhqk', qn, kc_nope) + jnp.einsum('bqhr,bkr->bhqk', qp, kc_pe)
        logits = jnp.concatenate([s_lat, s_ctx], -1).astype(jnp.float32) * scale
        p = jax.nn.softmax(logits, axis=-1).astype(v.dtype)
        return (jnp.einsum('bhqk,bkhd->bqhd', p[..., :S], v)
                + jnp.einsum('bhqk,bkhd->bqhd', p[..., S:], vc))

    out = lax.map(one_block, (to_blocks(q_nope), to_blocks(q_pe)))
    return jnp.moveaxis(out, 0, 1).reshape(B, S, H * MLA_V)


def na_tables(rows, kh):
    r = np.arange(rows)
    rs = np.clip(r - kh // 2, 0, rows - kh)
    key_rows = rs[:, None] + np.arange(kh)[None, :]
    ncb = GRID_W // NA_QC
    m = np.arange(ncb)
    cb = np.clip(m * NA_QC - NA_KW // 2, 0, GRID_W - NA_KC)
    key_cols = cb[:, None] + np.arange(NA_KC)[None, :]
    idx = key_rows[:, None, :, None] * GRID_W + key_cols[None, :, None, :]
    qcol = m[:, None] * NA_QC + np.arange(NA_QC)[None, :]
    cs = np.clip(qcol - NA_KW // 2, 0, GRID_W - NA_KW)
    kcol = key_cols[:, None, :]
    col_ok = (kcol >= cs[..., None]) & (kcol < cs[..., None] + NA_KW)
    mask = np.broadcast_to(col_ok[:, :, None, :], (ncb, NA_QC, kh, NA_KC)).reshape(ncb, NA_QC, kh * NA_KC)
    dr = key_rows - r[:, None] + (NA_KH - 1)
    dc = np.clip(kcol - qcol[..., None], -(NA_KW - 1), NA_KW - 1) + (NA_KW - 1)
    return (idx.reshape(rows, ncb * kh * NA_KC).astype(np.int32), dr.astype(np.int32),
            dc.astype(np.int32), mask)


def na_attend_latent(q, k, v, kc, vc, rpb):
    B, S, H, d = q.shape
    rows = S // GRID_W
    kh = min(NA_KH, rows)
    ncb = GRID_W // NA_QC
    nk = kh * NA_KC
    idx, dr, dc, mask = na_tables(rows, kh)
    dc = jnp.asarray(dc)
    mask = jnp.asarray(mask)
    scale = d ** -0.5
    q_rows = jnp.moveaxis(q.reshape(B, rows, ncb, NA_QC, H, d), 1, 0)

    def one_row(args):
        q_r, idx_r, dr_r = args
        k_r = jnp.take(k, idx_r, axis=1).reshape(B, ncb, nk, H, d)
        v_r = jnp.take(v, idx_r, axis=1).reshape(B, ncb, nk, H, d)
        bias = rpb[:, dr_r[None, None, :, None], dc[:, :, None, :]].reshape(H, ncb, NA_QC, nk)
        s_win = jnp.einsum('bmqhd,bmkhd->bhmqk', q_r, k_r).astype(jnp.float32) * scale + bias.astype(jnp.float32)
        s_win = jnp.where(mask, s_win, NEG_INF)
        s_ctx = jnp.einsum('bmqhd,bchd->bhmqc', q_r, kc).astype(jnp.float32) * scale
        p = jax.nn.softmax(jnp.concatenate([s_win, s_ctx], -1), axis=-1).astype(v.dtype)
        return (jnp.einsum('bhmqk,bmkhd->bmqhd', p[..., :nk], v_r)
                + jnp.einsum('bhmqc,bchd->bmqhd', p[..., nk:], vc))

    out = lax.map(one_row, (q_rows, jnp.asarray(idx), jnp.asarray(dr)))
    return jnp.moveaxis(out, 0, 1).reshape(B, S, H * d)


def mixer_ab(u, uc, w_in, kv_norm, w_ukv, rpb, w_out, ctx_queries):
    B, S, _ = u.shape
    Lc = uc.shape[1]
    cos, sin = axial_rope_tables(S)
    p = u @ w_in
    q_mla = p[..., :OFF_CKV].reshape(B, S, MLA_HEADS, MLA_QK)
    q_nope = q_mla[..., :MLA_NOPE]
    q_pe = apply_rope(q_mla[..., MLA_NOPE:], cos[:, None, :], sin[:, None, :])
    k_nope, v_mla = mla_up(p[..., OFF_CKV:OFF_KPE], kv_norm, w_ukv)
    k_pe = apply_rope(p[..., OFF_KPE:OFF_QNA], cos, sin)
    q_na = p[..., OFF_QNA:OFF_KNA].reshape(B, S, NA_HEADS, NA_HEAD_DIM)
    k_na = p[..., OFF_KNA:OFF_VNA].reshape(B, S, NA_HEADS, NA_HEAD_DIM)
    v_na = p[..., OFF_VNA:].reshape(B, S, NA_HEADS, NA_HEAD_DIM)
    pc_mla = uc @ w_in[:, OFF_CKV:OFF_QNA]
    kc_nope, vc_mla = mla_up(pc_mla[..., :MLA_KV_RANK], kv_norm, w_ukv)
    kc_pe = pc_mla[..., MLA_KV_RANK:]
    pc_na = uc @ w_in[:, OFF_KNA:]
    kc_na = pc_na[..., :NA_HEADS * NA_HEAD_DIM].reshape(B, Lc, NA_HEADS, NA_HEAD_DIM)
    vc_na = pc_na[..., NA_HEADS * NA_HEAD_DIM:].reshape(B, Lc, NA_HEADS, NA_HEAD_DIM)

    a_out = mla_attend_latent(q_nope, q_pe, k_nope, k_pe, v_mla, kc_nope, kc_pe, vc_mla)
    b_out = na_attend_latent(q_na, k_na, v_na, kc_na, vc_na, rpb)
    y = jnp.concatenate([a_out, b_out], -1) @ w_out

    yc = None
    if ctx_queries:
        qc = (uc @ w_in[:, :OFF_CKV]).reshape(B, Lc, MLA_HEADS, MLA_QK)
        s = (jnp.einsum('bqhd,bkhd->bhqk', qc[..., :MLA_NOPE], kc_nope)
             + jnp.einsum('bqhr,bkr->bhqk', qc[..., MLA_NOPE:], kc_pe)).astype(jnp.float32) * MLA_QK ** -0.5
        ac = jnp.einsum('bhqk,bkhd->bqhd', jax.nn.softmax(s, -1).astype(vc_mla.dtype), vc_mla).reshape(B, Lc, -1)
        qn = (uc @ w_in[:, OFF_QNA:OFF_KNA]).reshape(B, Lc, NA_HEADS, NA_HEAD_DIM)
        s = jnp.einsum('bqhd,bkhd->bhqk', qn, kc_na).astype(jnp.float32) * NA_HEAD_DIM ** -0.5
        bc = jnp.einsum('bhqk,bkhd->bqhd', jax.nn.softmax(s, -1).astype(vc_na.dtype), vc_na).reshape(B, Lc, -1)
        yc = jnp.concatenate([ac, bc], -1) @ w_out
    return y, yc


def hyena_filters(L, fw1, fb1, ff1, fw2, fb2, ff2, fw3):
    f32 = jnp.float32
    t01 = jnp.linspace(0.0, 1.0, L, dtype=f32)
    w = 2.0 * math.pi * jnp.arange(L, dtype=f32) / L
    bands = jnp.linspace(1e-4, HY_BANDS - 1, HY_BANDS, dtype=f32)
    z = jnp.concatenate([t01[:, None], jnp.cos(w[:, None] * bands), -jnp.sin(w[:, None] * bands)], -1)
    hdn = jnp.sin(ff1.astype(f32) * (z @ fw1.astype(f32) + fb1.astype(f32)))
    hdn = jnp.sin(ff2.astype(f32) * (hdn @ fw2.astype(f32) + fb2.astype(f32)))
    filt = hdn @ fw3.astype(f32)
    deltas = jnp.abs(jnp.linspace(HY_MIN_DECAY, HY_MAX_DECAY, HY_W, dtype=f32))
    decay = jnp.exp(-t01[:, None] * deltas)
    return filt.reshape(L, HY_ORDER, 2, HY_W) * decay[:, None, None, :]


def bidir_fftconv(z, hf, hb, skip):
    L = z.shape[1]
    zf = z.astype(jnp.float32)
    h_circ = jnp.concatenate([hf, jnp.zeros_like(hf[:1]), hb[:0:-1]], axis=0)
    spec = jnp.fft.rfft(zf, n=2 * L, axis=1) * jnp.fft.rfft(h_circ, n=2 * L, axis=0)[None]
    y = jnp.fft.irfft(spec, n=2 * L, axis=1)[:, :L]
    return (y + zf * skip.astype(jnp.float32)).astype(z.dtype)


def hyena_mix(p, conv_w, conv_b, filt, skip):
    L = p.shape[1]
    half = HY_SHORT // 2
    pad = jnp.pad(p, ((0, 0), (half, half), (0, 0)))
    s = conv_b + sum(pad[:, k:k + L] * conv_w[k] for k in range(HY_SHORT))
    parts = jnp.split(s, HY_ORDER + 1, axis=-1)
    z = parts[0]
    for o in range(HY_ORDER):
        z = parts[o + 1] * bidir_fftconv(z, filt[:, o, 0], filt[:, o, 1], skip[o])
    return z


def fnet_mix(p):
    B, L, _ = p.shape
    g = p.astype(jnp.float32).reshape(B, L, FN_GROUPS, FN_GROUP_W)
    y = jnp.fft.fft2(g, axes=(1, 3), norm='ortho').real
    return y.reshape(B, L, FN_W).astype(p.dtype)


def mixer_cd(u, w_in, conv_w, conv_b, fw1, fb1, ff1, fw2, fb2, ff2, fw3, skip, w_out):
    p = u @ w_in
    filt = hyena_filters(u.shape[1], fw1, fb1, ff1, fw2, fb2, ff2, fw3)
    y_hy = hyena_mix(p[..., :HY_IN_W], conv_w, conv_b, filt, skip)
    y_fn = fnet_mix(p[..., HY_IN_W:])
    return jnp.concatenate([y_hy, y_fn], -1) @ w_out


def ec_moe(u, router, w1, w3, w2):
    B, L, D = u.shape
    cap = EC_CAPACITY_FACTOR * L // N_EXPERTS
    aff = jax.nn.softmax((u @ router).astype(jnp.float32), axis=-1)
    gate, idx = lax.top_k(jnp.swapaxes(aff, 1, 2), cap)
    xe = jax.vmap(lambda ub, ib: ub[ib])(u, idx)
    hdn = jax.nn.silu(jnp.einsum('becd,edf->becf', xe, w1)) * jnp.einsum('becd,edf->becf', xe, w3)
    ye = jnp.einsum('becf,efd->becd', hdn, w2) * gate[..., None].astype(u.dtype)
    return jax.vmap(lambda yb, ib: jnp.zeros((L, D), yb.dtype).at[ib.reshape(-1)].add(yb.reshape(-1, D)))(ye, idx)


def setup_inputs(seed: int = 0) -> dict:
    key = jax.random.key(seed)
    ks = iter(jax.random.split(key, 40))
    D = D_MODEL

    def nrm(shape, std):
        return std * jax.random.normal(next(ks), shape, jnp.float32)

    return {
        'x': nrm((BATCH, SEQ, D), 1.0),
        'c': nrm((BATCH, D), 1.0),
        'ctx': nrm((BATCH, CTX_LEN, D), 1.0),
        'c_ctx': nrm((D,), 1.0),
        'ada_w': nrm((DEPTH, D, 6 * D), D ** -0.5),
        'ada_b': nrm((DEPTH, 6 * D), 0.01),
        'ln1_g': 1.0 + nrm((DEPTH, D), 0.02),
        'ln1_b': nrm((DEPTH, D), 0.02),
        'ln2_g': 1.0 + nrm((DEPTH, D), 0.02),
        'ln2_b': nrm((DEPTH, D), 0.02),
        'router': nrm((DEPTH, D, N_EXPERTS), D ** -0.5),
        'exp_w1': nrm((DEPTH, N_EXPERTS, D, EXPERT_FF), D ** -0.5),
        'exp_w3': nrm((DEPTH, N_EXPERTS, D, EXPERT_FF), D ** -0.5),
        'exp_w2': nrm((DEPTH, N_EXPERTS, EXPERT_FF, D), EXPERT_FF ** -0.5 * DEEPNORM_BETA),
        'ab_w_in': nrm((N_EVEN, D, IN_AB_W), D ** -0.5),
        'ab_kv_norm': 1.0 + nrm((N_EVEN, MLA_KV_RANK), 0.02),
        'ab_w_ukv': nrm((N_EVEN, MLA_KV_RANK, MLA_HEADS * (MLA_NOPE + MLA_V)), MLA_KV_RANK ** -0.5),
        'ab_rpb': nrm((N_EVEN, NA_HEADS, 2 * NA_KH - 1, 2 * NA_KW - 1), 0.1),
        'ab_w_out': nrm((N_EVEN, MIX_AB_W, D), MIX_AB_W ** -0.5 * DEEPNORM_BETA),
        'cd_w_in': nrm((N_ODD, D, IN_CD_W), D ** -0.5),
        'cd_conv_w': nrm((N_ODD, HY_SHORT, HY_IN_W), HY_SHORT ** -0.5),
        'cd_conv_b': nrm((N_ODD, HY_IN_W), 0.02),
        'cd_filt_w1': nrm((N_ODD, HY_EMB, HY_FILT_HID), HY_EMB ** -0.5),
        'cd_filt_b1': nrm((N_ODD, HY_FILT_HID), 0.02),
        'cd_filt_freq1': 1.0 + nrm((N_ODD, HY_FILT_HID), 0.02),
        'cd_filt_w2': nrm((N_ODD, HY_FILT_HID, HY_FILT_HID), HY_FILT_HID ** -0.5),
        'cd_filt_b2': nrm((N_ODD, HY_FILT_HID), 0.02),
        'cd_filt_freq2': 1.0 + nrm((N_ODD, HY_FILT_HID), 0.02),
        'cd_filt_w3': nrm((N_ODD, HY_FILT_HID, HY_FILT_OUT), 0.01),
        'cd_skip': nrm((N_ODD, HY_ORDER, HY_W), 0.1),
        'cd_w_out': nrm((N_ODD, MIX_CD_W, D), MIX_CD_W ** -0.5 * DEEPNORM_BETA),
    }


def reference(x, c, ctx, c_ctx, ada_w, ada_b, ln1_g, ln1_b, ln2_g, ln2_b, router, exp_w1, exp_w3, exp_w2,
              ab_w_in, ab_kv_norm, ab_w_ukv, ab_rpb, ab_w_out,
              cd_w_in, cd_conv_w, cd_conv_b, cd_filt_w1, cd_filt_b1, cd_filt_freq1, cd_filt_w2, cd_filt_b2,
              cd_filt_freq2, cd_filt_w3, cd_skip, cd_w_out):
    h = x
    hc = ctx
    for i in range(DEPTH):
        j = i // 2
        attn_layer = (i % 2 == 0)
        ctx_continues = i < LAST_ATTN_LAYER
        m = ada_params(c, ada_w[i], ada_b[i])[:, None, :]
        sh1, sc1, g1, sh2, sc2, g2 = jnp.split(m, 6, axis=-1)
        if ctx_continues:
            mc = jnp.split(ada_params(c_ctx, ada_w[i], ada_b[i]), 6, axis=-1)
        elif attn_layer:
            mc = jnp.split(ada_params(c_ctx, ada_w[i][:, :2 * D_MODEL], ada_b[i][:2 * D_MODEL]), 2, axis=-1)
        if attn_layer or ctx_continues:
            uc = modulate(hc, mc[0], mc[1])
        u = modulate(h, sh1, sc1)
        if attn_layer:
            y, yc = mixer_ab(u, uc, ab_w_in[j], ab_kv_norm[j], ab_w_ukv[j], ab_rpb[j], ab_w_out[j], ctx_continues)
        else:
            cd_args = (cd_w_in[j], cd_conv_w[j], cd_conv_b[j], cd_filt_w1[j], cd_filt_b1[j], cd_filt_freq1[j],
                       cd_filt_w2[j], cd_filt_b2[j], cd_filt_freq2[j], cd_filt_w3[j], cd_skip[j], cd_w_out[j])
            y = mixer_cd(u, *cd_args)
            yc = mixer_cd(uc, *cd_args) if ctx_continues else None
        h = post_norm(h, y, g1, ln1_g[i], ln1_b[i])
        h = post_norm(h, ec_moe(modulate(h, sh2, sc2), router[i], exp_w1[i], exp_w3[i], exp_w2[i]),
                      g2, ln2_g[i], ln2_b[i])
        if ctx_continues:
            hc = post_norm(hc, yc, mc[2], ln1_g[i], ln1_b[i])
            hc = post_norm(hc, ec_moe(modulate(hc, mc[3], mc[4]), router[i], exp_w1[i], exp_w3[i], exp_w2[i]),
                           mc[5], ln2_g[i], ln2_b[i])
    return h
```

```python
import numpy as np
from contextlib import ExitStack
import ml_dtypes
import concourse.bass as bass
import concourse.mybir as mybir
from concourse.bass_utils import run_bass_kernel_spmd

F32 = mybir.dt.float32
BF16 = mybir.dt.bfloat16
AF = mybir.ActivationFunctionType
ALU = mybir.AluOpType
AX = mybir.AxisListType

D = 2048
S = 2048
NB = 4
LC = 256
NE = 16
FF = 1408
CAP = 256
ALPHA = (2.0 * 2) ** 0.25
LN_EPS = 1e-6


_cnt = [0]


def SB(nc, name, shape, dt):
    _cnt[0] += 1
    return nc.sbuf_tensor("%s_u%d" % (name, _cnt[0]), shape, dt)


def PSM(nc, name, shape, dt):
    _cnt[0] += 1
    return nc.psum_tensor("%s_u%d" % (name, _cnt[0]), shape, dt)


def load_pieces(P, dst, pieces, w):
    if not isinstance(pieces, (list, tuple)):
        pieces = [pieces]
    off = 0
    for ap in pieces:
        n = ap.shape[-1]
        P.dma("sp", dst[:, off:off + n], ap, w=w)
        off += n


class Tr:
    __slots__ = ("w", "r")

    def __init__(self):
        self.w = None
        self.r = {}


class Prog:
    ENG = ("pe", "act", "dve", "pool", "sp")

    def __init__(self, nc, es):
        self.nc = nc
        self.eobj = {"pe": nc.tensor, "act": nc.scalar, "dve": nc.vector, "pool": nc.gpsimd, "sp": nc.sync}
        self.es = es
        self.gen = {e: 0 for e in ("pe", "act", "dve", "pool")}
        self.esem = {(e, 0): es.enter_context(nc.semaphore("s_" + e)) for e in ("pe", "act", "dve", "pool")}
        self.cnt = {e: 0 for e in self.gen}
        nds = {"sp": 40, "pool": 32, "act": 1}
        self.dsem = {e: [es.enter_context(nc.semaphore("d_%s%d" % (e, i))) for i in range(n)] for e, n in nds.items()}
        self.dcnt = {e: [0] * n for e, n in nds.items()}
        self.dnext = {e: 0 for e in nds}
        self.seen = {e: {} for e in self.ENG}

    def _sem(self, k):
        return self.esem[(k[1], k[2])] if k[0] == "e" else self.dsem[k[1]][k[2]]

    def _sync(self, eng, r, w, mykey, myval):
        need = {}
        for t in r:
            if t.w is not None and need.get(t.w[0], 0) < t.w[1]:
                need[t.w[0]] = t.w[1]
        for t in w:
            if t.w is not None and need.get(t.w[0], 0) < t.w[1]:
                need[t.w[0]] = t.w[1]
            for k, v in t.r.items():
                if need.get(k, 0) < v:
                    need[k] = v
        seen = self.seen[eng]
        e = self.eobj[eng]
        for k, v in need.items():
            if eng == "pe" and k[0] == "e" and k[1] == "pe":
                continue
            if seen.get(k, 0) < v:
                seen[k] = v
                e.wait_ge(self._sem(k), v)
        for t in r:
            if t.r.get(mykey, 0) < myval:
                t.r[mykey] = myval
        for t in w:
            t.w = (mykey, myval)
            t.r = {}

    def op(self, eng, fn, r=(), w=()):
        self.cnt[eng] += 1
        g = self.gen[eng]
        self._sync(eng, r, w, ("e", eng, g), self.cnt[eng])
        fn(self.eobj[eng]).then_inc(self.esem[(eng, g)], 1)

    def dma(self, eng, out, in_, r=(), w=()):
        i = self.dnext[eng]
        self.dnext[eng] = (i + 1) % len(self.dsem[eng])
        key = ("d", eng, i)
        prev = self.dcnt[eng][i]
        self.dcnt[eng][i] = prev + 16
        if prev > 0 and self.seen[eng].get(key, 0) < prev:
            self.seen[eng][key] = prev
            self.eobj[eng].wait_ge(self.dsem[eng][i], prev)
        self._sync(eng, r, w, key, prev + 16)
        self.eobj[eng].dma_start(out=out, in_=in_).then_inc(self.dsem[eng][i], 16)

    def idma(self, out, in_, idx, r=(), w=()):
        eng = "pool"
        i = self.dnext[eng]
        self.dnext[eng] = (i + 1) % len(self.dsem[eng])
        key = ("d", eng, i)
        prev = self.dcnt[eng][i]
        self.dcnt[eng][i] = prev + 16
        if prev > 0 and self.seen[eng].get(key, 0) < prev:
            self.seen[eng][key] = prev
            self.eobj[eng].wait_ge(self.dsem[eng][i], prev)
        self._sync(eng, r, w, key, prev + 16)
        self.nc.gpsimd.indirect_dma_start(out=out, out_offset=None, in_=in_,
                                          in_offset=bass.IndirectOffsetOnAxis(ap=idx, axis=0)).then_inc(self.dsem[eng][i], 16)

    def allgather(self, src, dst, r=(), w=()):
        eng = "pool"
        i = self.dnext[eng]
        self.dnext[eng] = (i + 1) % len(self.dsem[eng])
        key = ("d", eng, i)
        prev = self.dcnt[eng][i]
        self.dcnt[eng][i] = prev + 16
        if prev > 0 and self.seen[eng].get(key, 0) < prev:
            self.seen[eng][key] = prev
            self.eobj[eng].wait_ge(self.dsem[eng][i], prev)
        self._sync(eng, r, w, key, prev + 16)
        self.nc.gpsimd.collective_compute("AllGather", ALU.bypass, replica_groups=[list(range(8))], ins=[src],
                                          outs=[dst]).then_inc(self.dsem[eng][i], 16)

    def barrier(self):
        for eng in self.ENG:
            seen = self.seen[eng]
            e = self.eobj[eng]
            for k2, c in self.cnt.items():
                k = ("e", k2, self.gen[k2])
                if c > 0 and seen.get(k, 0) < c:
                    seen[k] = c
                    e.wait_ge(self.esem[(k2, self.gen[k2])], c)
            for q, lst in self.dcnt.items():
                for i, c in enumerate(lst):
                    k = ("d", q, i)
                    if c > 0 and seen.get(k, 0) < c:
                        seen[k] = c
                        e.wait_ge(self.dsem[q][i], c)
        for k2 in self.cnt:
            if self.cnt[k2] > 24000:
                self.gen[k2] += 1
                self.esem[(k2, self.gen[k2])] = self.es.enter_context(self.nc.semaphore("s_%s_g%d" % (k2, self.gen[k2])))
                self.cnt[k2] = 0


def make_consts():
    c = {}
    c["iota_row"] = np.tile(np.arange(256, dtype=np.float32)[None, :], (128, 1))
    pid = np.arange(128, dtype=np.float32)
    c["pidx"] = np.stack([pid, pid + 128], axis=1).astype(np.float32)
    c["ident_f"] = np.eye(128, dtype=np.float32)
    c["ident_b"] = np.eye(128, dtype=np.float32).astype(ml_dtypes.bfloat16)
    c["ones_f"] = np.ones((128, 128), dtype=np.float32)
    sel = np.zeros((16, 8, 128), dtype=np.float32)
    for e in range(8):
        sel[e, e, :] = 1.0
    c["selrow"] = sel
    sel16 = np.zeros((16, 16, 128), dtype=np.float32)
    for e in range(16):
        sel16[e, e, :] = 1.0
    c["selrow16"] = sel16
    return c


def stage_moe_route(P, cst, hT, modv, router, xe_out, pos_out, gate_out, t_in, t_out, load_h=None):
    nc = P.nc
    if load_h is None:
        def load_h(dst, c, w):
            P.dma("sp", dst, hT[c * 128:(c + 1) * 128, :], r=[t_in], w=w)
    NDC = D // 128
    NTT = S // 128
    NFT = FF // 128
    with ExitStack() as es:
        def sb(name, shape, dt):
            return es.enter_context(SB(nc, "m_" + name, shape, dt))

        def pst(name, shape, dt):
            return es.enter_context(PSM(nc, "mp_" + name, shape, dt))

        regA = sb("regA", [128, NTT * D], BF16)
        modv_sb = sb("modv", [128, 32], F32)
        sc1p = sb("sc1p", [128, 16], F32)
        router_sb = sb("router", [128, NDC * 16], F32)
        iota_row = sb("iota_row", [128, 256], F32)
        pidx = sb("pidx", [128, 2], F32)
        ident_f = sb("ident_f", [128, 128], F32)
        ident_b = sb("ident_b", [128, 128], BF16)
        ones_f = sb("ones_f", [128, 128], F32)
        selrow = sb("selrow", [16, 8 * 128], F32)
        t_c = Tr()
        load_pieces(P, modv_sb, modv, [t_c])
        P.dma("sp", router_sb[:].rearrange("p (k e) -> p k e", e=16), router.rearrange("(k p) e -> p k e", p=128), w=[t_c])
        P.dma("sp", iota_row[:], cst["iota_row"][:, :], w=[t_c])
        P.dma("sp", pidx[:], cst["pidx"][:, :], w=[t_c])
        P.dma("sp", ident_f[:], cst["ident_f"][:, :], w=[t_c])
        P.dma("sp", ident_b[:], cst["ident_b"][:, :], w=[t_c])
        P.dma("sp", ones_f[:], cst["ones_f"][:, :], w=[t_c])
        P.dma("sp", selrow[:].rearrange("p (e m) -> p e m", m=128), cst["selrow"][:, :, :], w=[t_c])
        P.op("dve", lambda e: e.tensor_scalar(out=sc1p[:], in0=modv_sb[:, 16:32], scalar1=1.0, scalar2=None, op0=ALU.add),
             r=[t_c], w=[t_c])

        u2tok = regA[:].rearrange("p (t d) -> p t d", d=D)
        pos = sb("pos", [16, S], F32)
        gate = sb("gate", [16, S], F32)
        posT = sb("posT", [128, NTT * 16], F32)
        with ExitStack() as es1:
            hbuf = [es1.enter_context(SB(nc, "m_hbuf%d" % i, [128, S], F32)) for i in range(2)]
            u2f = [es1.enter_context(SB(nc, "m_u2f%d" % i, [128, S], F32)) for i in range(2)]
            u2b = [es1.enter_context(SB(nc, "m_u2b%d" % i, [128, S], BF16)) for i in range(2)]
            psL = [es1.enter_context(PSM(nc, "mp_L%d" % i, [128, 512], F32)) for i in range(4)]
            psT = [es1.enter_context(PSM(nc, "mp_T%d" % i, [128, 1024], BF16)) for i in range(2)]
            max8 = es1.enter_context(SB(nc, "m_max8", [16, 8], F32))
            t_h = [Tr(), Tr()]
            t_uf = [Tr(), Tr()]
            t_ub = [Tr(), Tr()]
            t_L = Tr()
            t_T = [Tr(), Tr()]
            t_u2tok = Tr()
            nT = 0
            for c in range(NDC):
                i = c % 2
                load_h(hbuf[i][:], c, [t_h[i]])
                P.op("act", lambda e, i=i, c=c: e.activation(out=u2f[i][:], in_=hbuf[i][:], func=AF.Identity,
                                                             bias=modv_sb[:, c:c + 1], scale=sc1p[:, c:c + 1]),
                     r=[t_h[i], t_c], w=[t_uf[i]])
                for tq in range(4):
                    P.op("pe", lambda e, i=i, c=c, tq=tq: e.matmul(psL[tq][0:16, :], router_sb[:, c * 16:(c + 1) * 16],
                                                                   u2f[i][:, tq * 512:(tq + 1) * 512],
                                                                   start=(c == 0), stop=(c == NDC - 1)),
                         r=[t_uf[i], t_c], w=[t_L])
                P.op("dve", lambda e, i=i: e.tensor_copy(out=u2b[i][:], in_=u2f[i][:]), r=[t_uf[i]], w=[t_ub[i]])
                for g in range(4):
                    j = nT % 2
                    nT += 1
                    for k in range(4):
                        t = g * 4 + k
                        P.op("pe", lambda e, i=i, j=j, k=k, t=t: e.transpose(psT[j][:, k * 128:(k + 1) * 128],
                                                                             u2b[i][:, t * 128:(t + 1) * 128], ident_b[:]),
                             r=[t_ub[i], t_c], w=[t_T[j]])
                    P.op("dve" if g % 2 == 0 else "act",
                         (lambda e, j=j, g=g, c=c: e.tensor_copy(out=u2tok[:, g * 4:(g + 1) * 4, c * 128:(c + 1) * 128],
                                                                 in_=psT[j][:, 0:512].rearrange("p (k d) -> p k d", d=128)))
                         if g % 2 == 0 else
                         (lambda e, j=j, g=g, c=c: e.copy(out=u2tok[:, g * 4:(g + 1) * 4, c * 128:(c + 1) * 128],
                                                          in_=psT[j][:, 0:512].rearrange("p (k d) -> p k d", d=128))),
                         r=[t_T[j]], w=[t_u2tok])
            ex = hbuf[0]
            work = hbuf[1]
            t_ex = t_h[0]
            t_work = t_h[1]
            aff = u2f[0][0:16, :]
            mask = u2f[1][0:16, :]
            onesr = ex[0:16, :]
            t_aff = t_uf[0]
            t_mask = t_uf[1]
            t_ones = t_ex
            for tq in range(4):
                P.op("act", lambda e, tq=tq: e.activation(out=ex[0:16, tq * 512:(tq + 1) * 512], in_=psL[tq][0:16, :], func=AF.Exp),
                     r=[t_L], w=[t_ex])
            t_L2 = Tr()
            for tq in range(4):
                P.op("pe", lambda e, tq=tq: e.matmul(psL[tq][0:16, :], ones_f[0:16, 0:16], ex[0:16, tq * 512:(tq + 1) * 512],
                                                     start=True, stop=True), r=[t_ex, t_c, t_L], w=[t_L])
            for tq in range(4):
                P.op("dve", lambda e, tq=tq: e.reciprocal(out=work[0:16, tq * 512:(tq + 1) * 512], in_=psL[tq][0:16, :]),
                     r=[t_L], w=[t_work])
            P.op("dve", lambda e: e.tensor_tensor(out=aff, in0=ex[0:16, :], in1=work[0:16, :], op=ALU.mult),
                 r=[t_ex, t_work], w=[t_aff])
            P.op("dve", lambda e: e.tensor_copy(out=work[0:16, :], in_=aff), r=[t_aff], w=[t_work])
            t_m8 = Tr()
            for it in range(CAP // 8):
                P.op("dve", lambda e: e.max(out=max8[:], in_=work[0:16, :]), r=[t_work], w=[t_m8])
                P.op("dve", lambda e: e.match_replace(out=work[0:16, :], in_to_replace=max8[:], in_values=work[0:16, :],
                                                      imm_value=-1.0), r=[t_m8, t_work], w=[t_work])
            t_pos = Tr(); t_gate = Tr()
            P.op("pool", lambda e: e.memset(onesr, 1.0), w=[t_ones])
            P.op("dve", lambda e: e.tensor_tensor(out=mask, in0=work[0:16, :], in1=aff, op=ALU.not_equal),
                 r=[t_work, t_aff], w=[t_mask])
            P.op("dve", lambda e: e.tensor_tensor_scan(out=pos[:], data0=onesr, data1=mask, initial=0.0,
                                                       op0=ALU.mult, op1=ALU.add), r=[t_ones, t_mask], w=[t_pos])
            P.op("dve", lambda e: e.tensor_tensor(out=pos[:], in0=pos[:], in1=mask, op=ALU.mult), r=[t_mask, t_pos], w=[t_pos])
            P.op("dve", lambda e: e.tensor_scalar(out=pos[:], in0=pos[:], scalar1=-1.0, scalar2=None, op0=ALU.add),
                 r=[t_pos], w=[t_pos])
            P.op("dve", lambda e: e.tensor_tensor(out=gate[:], in0=aff, in1=mask, op=ALU.mult), r=[t_aff, t_mask], w=[t_gate])
            t_posT = Tr()
            psP = psL[0]
            for t in range(NTT):
                P.op("pe", lambda e, t=t: e.transpose(psP[:, t * 16:(t + 1) * 16], pos[:, t * 128:(t + 1) * 128], ident_f[0:16, 0:16]),
                     r=[t_pos, t_c, t_L], w=[t_L])
            P.op("dve", lambda e: e.tensor_copy(out=posT[:], in_=psP[:, 0:NTT * 16]), r=[t_L], w=[t_posT])
            P.barrier()
        regB = sb("regB", [128, 8 * NDC * CAP], BF16)
        xeT = regB[:].rearrange("p (e c s) -> p e c s", c=NDC, s=CAP)
        t_xeT = [Tr() for _ in range(8)]
        with ExitStack() as es2:
            selb = [es2.enter_context(SB(nc, "m_sel%d" % i, [128, NTT * CAP], BF16)) for i in range(2)]
            psG = [es2.enter_context(PSM(nc, "mp_G%d" % i, [128, 512], F32)) for i in range(4)]
            t_sel = [Tr(), Tr()]
            t_G = [Tr() for _ in range(4)]
            nG = 0
            for ex_i in range(8):
                i = ex_i % 2
                for t in range(NTT):
                    P.op("dve" if t % 2 == 0 else "pool",
                         lambda e, i=i, t=t, ex_i=ex_i: e.tensor_scalar(out=selb[i][:, t * CAP:(t + 1) * CAP], in0=iota_row[:],
                                                                        scalar1=posT[:, t * 16 + ex_i:t * 16 + ex_i + 1],
                                                                        scalar2=None, op0=ALU.is_equal),
                         r=[t_posT, t_c], w=[t_sel[i]])
                for dc in range(NDC):
                    g = nG % 4
                    nG += 1
                    for t in range(NTT):
                        P.op("pe", lambda e, g=g, t=t, dc=dc, i=i: e.matmul(psG[g][:, 0:CAP], u2tok[:, t, dc * 128:(dc + 1) * 128],
                                                                            selb[i][:, t * CAP:(t + 1) * CAP],
                                                                            start=(t == 0), stop=(t == NTT - 1)),
                             r=[t_u2tok, t_sel[i]], w=[t_G[g]])
                    if dc % 2 == 0:
                        P.op("act", lambda e, g=g, dc=dc, ex_i=ex_i: e.copy(out=xeT[:, ex_i, dc, :], in_=psG[g][:, 0:CAP]),
                             r=[t_G[g]], w=[t_xeT[ex_i]])
                    else:
                        P.op("dve", lambda e, g=g, dc=dc, ex_i=ex_i: e.tensor_copy(out=xeT[:, ex_i, dc, :], in_=psG[g][:, 0:CAP]),
                             r=[t_G[g]], w=[t_xeT[ex_i]])
            P.barrier()
        for ex_i in range(8):
            P.dma("sp", xe_out[ex_i], regB[:, ex_i * NDC * CAP:(ex_i + 1) * NDC * CAP], r=[t_xeT[ex_i]], w=[t_out])
        P.dma("sp", pos_out[:, :], pos[:], r=[t_pos], w=[t_out])
        P.dma("sp", gate_out[:, :], gate[:], r=[t_gate], w=[t_out])
        P.barrier()


def stage_moe_route16(P, cst, hT, modv, router, xe_out, pos_out, gate_out, t_in, t_out, load_h=None):
    nc = P.nc
    if load_h is None:
        def load_h(dst, c, w):
            P.dma("sp", dst, hT[c * 128:(c + 1) * 128, :], r=[t_in], w=w)
    NDC = D // 128
    NTT = S // 128
    NFT = FF // 128
    with ExitStack() as es:
        def sb(name, shape, dt):
            return es.enter_context(SB(nc, "m_" + name, shape, dt))

        def pst(name, shape, dt):
            return es.enter_context(PSM(nc, "mp_" + name, shape, dt))

        regA = sb("regA", [128, NTT * D], BF16)
        modv_sb = sb("modv", [128, 32], F32)
        sc1p = sb("sc1p", [128, 16], F32)
        router_sb = sb("router", [128, NDC * 16], F32)
        iota_row = sb("iota_row", [128, 256], F32)
        pidx = sb("pidx", [128, 2], F32)
        ident_f = sb("ident_f", [128, 128], F32)
        ident_b = sb("ident_b", [128, 128], BF16)
        ones_f = sb("ones_f", [128, 128], F32)
        selrow = sb("selrow", [16, 8 * 128], F32)
        t_c = Tr()
        load_pieces(P, modv_sb, modv, [t_c])
        P.dma("sp", router_sb[:].rearrange("p (k e) -> p k e", e=16), router.rearrange("(k p) e -> p k e", p=128), w=[t_c])
        P.dma("sp", iota_row[:], cst["iota_row"][:, :], w=[t_c])
        P.dma("sp", pidx[:], cst["pidx"][:, :], w=[t_c])
        P.dma("sp", ident_f[:], cst["ident_f"][:, :], w=[t_c])
        P.dma("sp", ident_b[:], cst["ident_b"][:, :], w=[t_c])
        P.dma("sp", ones_f[:], cst["ones_f"][:, :], w=[t_c])
        P.dma("sp", selrow[:].rearrange("p (e m) -> p e m", m=128), cst["selrow"][:, :, :], w=[t_c])
        P.op("dve", lambda e: e.tensor_scalar(out=sc1p[:], in0=modv_sb[:, 16:32], scalar1=1.0, scalar2=None, op0=ALU.add),
             r=[t_c], w=[t_c])

        u2tok = regA[:].rearrange("p (t d) -> p t d", d=D)
        pos = sb("pos", [16, S], F32)
        gate = sb("gate", [16, S], F32)
        posT = sb("posT", [128, NTT * 16], F32)
        with ExitStack() as es1:
            hbuf = [es1.enter_context(SB(nc, "m_hbuf%d" % i, [128, S], F32)) for i in range(2)]
            u2f = [es1.enter_context(SB(nc, "m_u2f%d" % i, [128, S], F32)) for i in range(2)]
            u2b = [es1.enter_context(SB(nc, "m_u2b%d" % i, [128, S], BF16)) for i in range(2)]
            psL = [es1.enter_context(PSM(nc, "mp_L%d" % i, [128, 512], F32)) for i in range(4)]
            psT = [es1.enter_context(PSM(nc, "mp_T%d" % i, [128, 1024], BF16)) for i in range(2)]
            max8 = es1.enter_context(SB(nc, "m_max8", [16, 8], F32))
            t_h = [Tr(), Tr()]
            t_uf = [Tr(), Tr()]
            t_ub = [Tr(), Tr()]
            t_L = Tr()
            t_T = [Tr(), Tr()]
            t_u2tok = Tr()
            nT = 0
            for c in range(NDC):
                i = c % 2
                load_h(hbuf[i][:], c, [t_h[i]])
                P.op("act", lambda e, i=i, c=c: e.activation(out=u2f[i][:], in_=hbuf[i][:], func=AF.Identity,
                                                             bias=modv_sb[:, c:c + 1], scale=sc1p[:, c:c + 1]),
                     r=[t_h[i], t_c], w=[t_uf[i]])
                for tq in range(4):
                    P.op("pe", lambda e, i=i, c=c, tq=tq: e.matmul(psL[tq][0:16, :], router_sb[:, c * 16:(c + 1) * 16],
                                                                   u2f[i][:, tq * 512:(tq + 1) * 512],
                                                                   start=(c == 0), stop=(c == NDC - 1)),
                         r=[t_uf[i], t_c], w=[t_L])
                P.op("dve", lambda e, i=i: e.tensor_copy(out=u2b[i][:], in_=u2f[i][:]), r=[t_uf[i]], w=[t_ub[i]])
                for g in range(4):
                    j = nT % 2
                    nT += 1
                    for k in range(4):
                        t = g * 4 + k
                        P.op("pe", lambda e, i=i, j=j, k=k, t=t: e.transpose(psT[j][:, k * 128:(k + 1) * 128],
                                                                             u2b[i][:, t * 128:(t + 1) * 128], ident_b[:]),
                             r=[t_ub[i], t_c], w=[t_T[j]])
                    P.op("dve" if g % 2 == 0 else "act",
                         (lambda e, j=j, g=g, c=c: e.tensor_copy(out=u2tok[:, g * 4:(g + 1) * 4, c * 128:(c + 1) * 128],
                                                                 in_=psT[j][:, 0:512].rearrange("p (k d) -> p k d", d=128)))
                         if g % 2 == 0 else
                         (lambda e, j=j, g=g, c=c: e.copy(out=u2tok[:, g * 4:(g + 1) * 4, c * 128:(c + 1) * 128],
                                                          in_=psT[j][:, 0:512].rearrange("p (k d) -> p k d", d=128))),
                         r=[t_T[j]], w=[t_u2tok])
            ex = hbuf[0]
            work = hbuf[1]
            t_ex = t_h[0]
            t_work = t_h[1]
            aff = u2f[0][0:16, :]
            mask = u2f[1][0:16, :]
            onesr = ex[0:16, :]
            t_aff = t_uf[0]
            t_mask = t_uf[1]
            t_ones = t_ex
            for tq in range(4):
                P.op("act", lambda e, tq=tq: e.activation(out=ex[0:16, tq * 512:(tq + 1) * 512], in_=psL[tq][0:16, :], func=AF.Exp),
                     r=[t_L], w=[t_ex])
            t_L2 = Tr()
            for tq in range(4):
                P.op("pe", lambda e, tq=tq: e.matmul(psL[tq][0:16, :], ones_f[0:16, 0:16], ex[0:16, tq * 512:(tq + 1) * 512],
                                                     start=True, stop=True), r=[t_ex, t_c, t_L], w=[t_L])
            for tq in range(4):
                P.op("dve", lambda e, tq=tq: e.reciprocal(out=work[0:16, tq * 512:(tq + 1) * 512], in_=psL[tq][0:16, :]),
                     r=[t_L], w=[t_work])
            P.op("dve", lambda e: e.tensor_tensor(out=aff, in0=ex[0:16, :], in1=work[0:16, :], op=ALU.mult),
                 r=[t_ex, t_work], w=[t_aff])
            P.op("dve", lambda e: e.tensor_copy(out=work[0:16, :], in_=aff), r=[t_aff], w=[t_work])
            t_m8 = Tr()
            for it in range(CAP // 8):
                P.op("dve", lambda e: e.max(out=max8[:], in_=work[0:16, :]), r=[t_work], w=[t_m8])
                P.op("dve", lambda e: e.match_replace(out=work[0:16, :], in_to_replace=max8[:], in_values=work[0:16, :],
                                                      imm_value=-1.0), r=[t_m8, t_work], w=[t_work])
            t_pos = Tr(); t_gate = Tr()
            P.op("pool", lambda e: e.memset(onesr, 1.0), w=[t_ones])
            P.op("dve", lambda e: e.tensor_tensor(out=mask, in0=work[0:16, :], in1=aff, op=ALU.not_equal),
                 r=[t_work, t_aff], w=[t_mask])
            P.op("dve", lambda e: e.tensor_tensor_scan(out=pos[:], data0=onesr, data1=mask, initial=0.0,
                                                       op0=ALU.mult, op1=ALU.add), r=[t_ones, t_mask], w=[t_pos])
            P.op("dve", lambda e: e.tensor_tensor(out=pos[:], in0=pos[:], in1=mask, op=ALU.mult), r=[t_mask, t_pos], w=[t_pos])
            P.op("dve", lambda e: e.tensor_scalar(out=pos[:], in0=pos[:], scalar1=-1.0, scalar2=None, op0=ALU.add),
                 r=[t_pos], w=[t_pos])
            P.op("dve", lambda e: e.tensor_tensor(out=gate[:], in0=aff, in1=mask, op=ALU.mult), r=[t_aff, t_mask], w=[t_gate])
            t_posT = Tr()
            psP = psL[0]
            for t in range(NTT):
                P.op("pe", lambda e, t=t: e.transpose(psP[:, t * 16:(t + 1) * 16], pos[:, t * 128:(t + 1) * 128], ident_f[0:16, 0:16]),
                     r=[t_pos, t_c, t_L], w=[t_L])
            P.op("dve", lambda e: e.tensor_copy(out=posT[:], in_=psP[:, 0:NTT * 16]), r=[t_L], w=[t_posT])
            P.barrier()
        with ExitStack() as es2:
            selb = [es2.enter_context(SB(nc, "m_sel%d" % i, [128, NTT * CAP], BF16)) for i in range(2)]
            psG = [es2.enter_context(PSM(nc, "mp_G%d" % i, [128, 512], F32)) for i in range(4)]
            xst = Pool(nc, es2, "sb", 2, [128, NDC * CAP], BF16, "m_xst")
            t_sel = [Tr(), Tr()]
            t_G = [Tr() for _ in range(4)]
            nG = 0
            posTv = posT[:].rearrange("p (t e) -> p t e", e=16)
            for ex_i in range(16):
                i = ex_i % 2
                P.op("dve", lambda e, i=i, ex_i=ex_i: e.tensor_tensor(
                    out=selb[i][:].rearrange("p (t s) -> p t s", s=CAP), in0=iota_row[:].unsqueeze(1).to_broadcast([128, NTT, CAP]),
                    in1=posTv[:, :, ex_i:ex_i + 1].to_broadcast([128, NTT, CAP]), op=ALU.is_equal), r=[t_posT, t_c], w=[t_sel[i]])
                xs_, xs_t = xst.get()
                for dc in range(NDC):
                    g = nG % 4
                    nG += 1
                    for t in range(NTT):
                        P.op("pe", lambda e, g=g, t=t, dc=dc, i=i: e.matmul(psG[g][:, 0:CAP], u2tok[:, t, dc * 128:(dc + 1) * 128],
                                                                            selb[i][:, t * CAP:(t + 1) * CAP],
                                                                            start=(t == 0), stop=(t == NTT - 1)),
                             r=[t_u2tok, t_sel[i]], w=[t_G[g]])
                    evac_copy(P, xs_[:, dc * CAP:(dc + 1) * CAP], psG[g][:, 0:CAP], [t_G[g]], [xs_t])
                P.dma("sp", xe_out[ex_i], xs_[:], r=[xs_t], w=[t_out])
            P.barrier()
        P.dma("sp", pos_out[:, :], pos[:], r=[t_pos], w=[t_out])
        P.dma("sp", gate_out[:, :], gate[:], r=[t_gate], w=[t_out])
        P.barrier()


def dram_in(nc, name, shape, dt=F32):
    return nc.dram_tensor(name, list(shape), dt, kind="ExternalInput").ap()


def dram_out(nc, name, shape, dt=F32):
    return nc.dram_tensor(name, list(shape), dt, kind="ExternalOutput").ap()


def declare_consts(nc, consts):
    out = {}
    for k, v in consts.items():
        dt = BF16 if v.dtype == ml_dtypes.bfloat16 else F32
        out[k] = dram_in(nc, "c_" + k, v.shape, dt)
    return out


_uid = [0]


class Pool:
    def __init__(self, nc, es, kind, n, shape, dt, name):
        mk = (lambda *a: SB(nc, *a)) if kind == "sb" else (lambda *a: PSM(nc, *a))
        _uid[0] += 1
        self.t = [es.enter_context(mk("%s_%d_%d" % (name, _uid[0], i), shape, dt)) for i in range(n)]
        self.tr = [Tr() for _ in range(n)]
        self.i = 0

    def get(self):
        i = self.i
        self.i = (i + 1) % len(self.t)
        return self.t[i], self.tr[i]


def dram_scratch(nc, name, shape, dt):
    _cnt[0] += 1
    return nc.dram_tensor("%s_u%d" % (name, _cnt[0]), list(shape), dt, kind="Internal").ap()


_alt = [0]


def evac_copy(P, out, in_, r, w):
    _alt[0] ^= 1
    if _alt[0]:
        P.op("act", lambda e: e.copy(out=out, in_=in_), r=r, w=w)
    else:
        P.op("dve", lambda e: e.tensor_copy(out=out, in_=in_), r=r, w=w)


def load_wgroup(P, wpool, wsrc, c0, ncols, nk=16):
    wt, wtr = wpool.get()
    view = wt[:, 0:nk * ncols].rearrange("p (k c) -> p k c", c=ncols)
    P.dma("pool", view, wsrc[:, c0:c0 + ncols].rearrange("(k p) c -> p k c", p=128), w=[wtr])
    return view, wtr


def proj_fm(P, pspool, wv, wtr, ft, xT, t_x, t0, n, nk=16):
    ps, ptr = pspool.get()
    for k in range(nk):
        P.op("pe", lambda e, k=k: e.matmul(ps[:, 0:n], wv[:, k, ft * 128:(ft + 1) * 128], xT[:, k, t0:t0 + n],
                                           start=(k == 0), stop=(k == nk - 1)), r=[wtr, t_x], w=[ptr])
    return ps, ptr


def proj_tm(P, pspool, wv, wtr, c0, ncols, xT, t_x, tt, nk=16):
    ps, ptr = pspool.get()
    for k in range(nk):
        P.op("pe", lambda e, k=k: e.matmul(ps[:, 0:ncols], xT[:, k, tt * 128:(tt + 1) * 128], wv[:, k, c0:c0 + ncols],
                                           start=(k == 0), stop=(k == nk - 1)), r=[wtr, t_x], w=[ptr])
    return ps, ptr


def ln_apply(P, es, z, t_z, T, g_sb, b_sb, t_gb, ones_f, t_c, outT, t_out, pspool, name):
    nc = P.nc
    sq = Pool(nc, es, "sb", 3, [128, 512], BF16, name + "_sq")
    zb = Pool(nc, es, "sb", 3, [128, 512], BF16, name + "_zb")
    ones_b = es.enter_context(SB(nc, name + "_1b", [128, 128], BF16))
    P.op("dve", lambda e: e.tensor_copy(out=ones_b[:], in_=ones_f[:]), r=[t_c], w=[t_c])
    st = Pool(nc, es, "sb", 2, [128, 4 * 512], F32, name + "_st")
    ot = Pool(nc, es, "sb", 3, [128, 512], F32, name + "_ot")
    for t0 in range(0, T, 512):
        p1, p1t = pspool.get()
        p2, p2t = pspool.get()
        for c in range(16):
            s, s_t = sq.get()
            P.op("act", lambda e, c=c, s=s: e.activation(out=s[:], in_=z[:, c, t0:t0 + 512], func=AF.Square), r=[t_z], w=[s_t])
            b_, b_t = zb.get()
            P.op("pool" if c % 2 else "dve", lambda e, c=c, b_=b_: e.tensor_copy(out=b_[:], in_=z[:, c, t0:t0 + 512]), r=[t_z], w=[b_t])
            P.op("pe", lambda e, c=c, b_=b_: e.matmul(p1[:, :], ones_b[:, :], b_[:], start=(c == 0), stop=(c == 15)),
                 r=[b_t, t_c], w=[p1t])
            P.op("pe", lambda e, c=c, s=s: e.matmul(p2[:, :], ones_b[:, :], s[:], start=(c == 0), stop=(c == 15)),
                 r=[s_t, t_c], w=[p2t])
        stt, st_t = st.get()
        mean = stt[:, 0:512]
        rstd = stt[:, 512:1024]
        tmp = stt[:, 1024:1536]
        P.op("dve", lambda e: e.tensor_scalar(out=mean, in0=p1[:, :], scalar1=1.0 / D, scalar2=None, op0=ALU.mult), r=[p1t], w=[st_t])
        P.op("dve", lambda e: e.tensor_tensor(out=tmp, in0=mean, in1=mean, op=ALU.mult), r=[st_t], w=[st_t])
        P.op("dve", lambda e: e.scalar_tensor_tensor(out=tmp, in0=p2[:, :], scalar=1.0 / D, in1=tmp, op0=ALU.mult, op1=ALU.subtract),
             r=[p2t, st_t], w=[st_t])
        P.op("dve", lambda e: e.tensor_scalar(out=tmp, in0=tmp, scalar1=LN_EPS, scalar2=None, op0=ALU.add), r=[st_t], w=[st_t])
        P.op("act", lambda e: e.activation(out=tmp, in_=tmp, func=AF.Sqrt), r=[st_t], w=[st_t])
        P.op("dve", lambda e: e.reciprocal(out=rstd, in_=tmp), r=[st_t], w=[st_t])
        for c in range(16):
            o, o_t = ot.get()
            P.op("dve", lambda e, c=c, o=o: e.tensor_tensor(out=o[:], in0=z[:, c, t0:t0 + 512], in1=mean, op=ALU.subtract),
                 r=[t_z, st_t], w=[o_t])
            P.op("dve", lambda e, o=o: e.tensor_tensor(out=o[:], in0=o[:], in1=rstd, op=ALU.mult), r=[st_t, o_t], w=[o_t])
            P.op("act", lambda e, c=c, o=o: e.activation(out=o[:], in_=o[:], func=AF.Identity, bias=b_sb[:, c:c + 1],
                                                         scale=g_sb[:, c:c + 1]), r=[o_t, t_gb], w=[o_t])
            P.dma("sp", outT[c * 128:(c + 1) * 128, t0:t0 + 512], o[:], r=[o_t], w=[t_out])


NA_J = [(0, 6), (2, 10), (6, 14), (10, 16)]
TB_OFF = 3
TB_N = 22
LT = S + LC


def stage_attn(P, cst, xT, ctxT, modv, w_in_r, kvn, w_ukv_r, cosT, sinT, na_T, na_RM, aoT, t_out):
    nc = P.nc
    C_QN, C_QP, C_QPS, C_CKV, C_KP, C_KPS, C_QA, C_KA, C_VA = 0, 512, 768, 1024, 1536, 1664, 1792, 2304, 2816
    qn_d = dram_scratch(nc, "a_qn", [4, 128, S], BF16)
    qp_d = dram_scratch(nc, "a_qp", [2, 128, S], BF16)
    kn_d = dram_scratch(nc, "a_kn", [4, 128, LT], BF16)
    kp_d = dram_scratch(nc, "a_kp", [128, LT], BF16)
    qa_d = dram_scratch(nc, "a_qa", [4, 128, S], BF16)
    ka_d = dram_scratch(nc, "a_ka", [4, 128, LT], BF16)
    vm_d = dram_scratch(nc, "a_vm", [LT, 512], BF16)
    va_d = dram_scratch(nc, "a_va", [LT, 512], BF16)
    t_d = {k: Tr() for k in ("qn", "qp", "kn", "kp", "qa", "ka", "vm", "va")}
    with ExitStack() as es:
        def sb(name, shape, dt):
            return es.enter_context(SB(nc, "a_" + name, shape, dt))
        modv_sb = sb("modv", [128, 64], F32)
        sc1p = sb("sc1p", [128, 32], F32)
        kvn_sb = sb("kvn", [128, 4], F32)
        ones_f = sb("ones_f", [128, 128], F32)
        ones_b = sb("ones_b", [128, 128], BF16)
        t_c = Tr()
        load_pieces(P, modv_sb, modv, [t_c])
        P.dma("sp", kvn_sb[:], kvn[:, :], w=[t_c])
        P.dma("sp", ones_f[:], cst["ones_f"][:, :], w=[t_c])
        P.op("dve", lambda e: e.tensor_scalar(out=sc1p[:, 0:16], in0=modv_sb[:, 16:32], scalar1=1.0, scalar2=None, op0=ALU.add),
             r=[t_c], w=[t_c])
        P.op("dve", lambda e: e.tensor_scalar(out=sc1p[:, 16:32], in0=modv_sb[:, 48:64], scalar1=1.0, scalar2=None, op0=ALU.add),
             r=[t_c], w=[t_c])
        P.op("dve", lambda e: e.tensor_copy(out=ones_b[:], in_=ones_f[:]), r=[t_c], w=[t_c])
        with ExitStack() as es1:
            uall_t = es1.enter_context(SB(nc, "a_uall", [128, 16 * LT], BF16))
            uall = uall_t[:].rearrange("p (k t) -> p k t", t=LT)
            t_u = Tr()
            es_x = ExitStack()
            xin = Pool(nc, es_x, "sb", 2, [128, S], F32, "a_xin")
            for c in range(16):
                xt, xtr = xin.get()
                P.dma("sp", xt[:], xT[c * 128:(c + 1) * 128, :], w=[xtr])
                P.op("act", lambda e, c=c, xt=xt: e.activation(out=uall[:, c, 0:S], in_=xt[:], func=AF.Identity,
                                                               bias=modv_sb[:, c:c + 1], scale=sc1p[:, c:c + 1]),
                     r=[xtr, t_c], w=[t_u])
                xt2, xtr2 = xin.get()
                P.dma("sp", xt2[:, 0:LC], ctxT[c * 128:(c + 1) * 128, :], w=[xtr2])
                P.op("act", lambda e, c=c, xt2=xt2: e.activation(out=uall[:, c, S:LT], in_=xt2[:, 0:LC], func=AF.Identity,
                                                                 bias=modv_sb[:, 32 + c:33 + c], scale=sc1p[:, 16 + c:17 + c]),
                     r=[xtr2, t_c], w=[t_u])
            P.barrier()
            es_x.close()
            wpool = Pool(nc, es1, "sb", 2, [128, 16 * 512], BF16, "a_w")
            pspool = Pool(nc, es1, "ps", 4, [128, 512], F32, "ap_p")
            stg = Pool(nc, es1, "sb", 4, [128, 512], BF16, "a_stg")
            cs = es1.enter_context(SB(nc, "a_cos", [128, S], F32))
            sn = es1.enter_context(SB(nc, "a_sin", [128, S], F32))
            P.dma("sp", cs[:], cosT[:, :], w=[t_c])
            P.dma("sp", sn[:], sinT[:, :], w=[t_c])

            def plain(c0, ntile, ntok, dst, t_dst):
                wv, wtr = load_wgroup(P, wpool, w_in_r, c0, ntile * 128)
                for ft in range(ntile):
                    for t0 in range(0, ntok, 512):
                        ps, ptr = proj_fm(P, pspool, wv, wtr, ft, uall, t_u, t0, 512)
                        s_, s_t = stg.get()
                        evac_copy(P, s_[:], ps[:, :], [ptr], [s_t])
                        P.dma("sp", dst[ft, :, t0:t0 + 512], s_[:], r=[s_t], w=[t_dst])
            plain(C_QN, 4, S, qn_d, t_d["qn"])
            plain(C_QA, 4, S, qa_d, t_d["qa"])
            plain(C_KA, 4, LT // 512 * 512, ka_d, t_d["ka"])
            wv, wtr = load_wgroup(P, wpool, w_in_r, C_KA, 512)
            for ft in range(4):
                ps, ptr = proj_fm(P, pspool, wv, wtr, ft, uall, t_u, S, LC)
                s_, s_t = stg.get()
                evac_copy(P, s_[:, 0:LC], ps[:, 0:LC], [ptr], [s_t])
                P.dma("sp", ka_d[ft, :, S:LT], s_[:, 0:LC], r=[s_t], w=[t_d["ka"]])
            rtmp = Pool(nc, es1, "sb", 2, [128, 512], F32, "a_rtmp")

            def roped(c_p, c_s, ntile, dst_of, t_dst, with_ctx):
                wv, wtr = load_wgroup(P, wpool, w_in_r, c_p, ntile * 128)
                wv2, wtr2 = load_wgroup(P, wpool, w_in_r, c_s, ntile * 128)
                for ft in range(ntile):
                    for t0 in range(0, S, 512):
                        ps, ptr = proj_fm(P, pspool, wv, wtr, ft, uall, t_u, t0, 512)
                        ps2, ptr2 = proj_fm(P, pspool, wv2, wtr2, ft, uall, t_u, t0, 512)
                        r1, r1t = rtmp.get()
                        r2, r2t = rtmp.get()
                        P.op("dve", lambda e, r1=r1, ps=ps, t0=t0: e.tensor_tensor(out=r1[:], in0=ps[:, :], in1=cs[:, t0:t0 + 512], op=ALU.mult),
                             r=[ptr, t_c], w=[r1t])
                        P.op("dve", lambda e, r2=r2, ps2=ps2, t0=t0: e.tensor_tensor(out=r2[:], in0=ps2[:, :], in1=sn[:, t0:t0 + 512], op=ALU.mult),
                             r=[ptr2, t_c], w=[r2t])
                        s_, s_t = stg.get()
                        P.op("pool", lambda e, r1=r1, r2=r2, s_=s_: e.tensor_tensor(out=s_[:], in0=r1[:], in1=r2[:], op=ALU.add),
                             r=[r1t, r2t], w=[s_t])
                        P.dma("sp", dst_of(ft)[:, t0:t0 + 512], s_[:], r=[s_t], w=[t_dst])
                    if with_ctx:
                        ps, ptr = proj_fm(P, pspool, wv, wtr, ft, uall, t_u, S, LC)
                        s_, s_t = stg.get()
                        evac_copy(P, s_[:, 0:LC], ps[:, 0:LC], [ptr], [s_t])
                        P.dma("sp", dst_of(ft)[:, S:LT], s_[:, 0:LC], r=[s_t], w=[t_dst])
            roped(C_QP, C_QPS, 2, lambda ft: qp_d[ft], t_d["qp"], False)
            roped(C_KP, C_KPS, 1, lambda ft: kp_d, t_d["kp"], True)
            wv, wtr = load_wgroup(P, wpool, w_in_r, C_VA, 512)
            for tt in range(LT // 128):
                ps, ptr = proj_tm(P, pspool, wv, wtr, 0, 512, uall, t_u, tt)
                s_, s_t = stg.get()
                evac_copy(P, s_[:], ps[:, :], [ptr], [s_t])
                P.dma("sp", va_d[tt * 128:(tt + 1) * 128, :], s_[:], r=[s_t], w=[t_d["va"]])
            ckf_t = es1.enter_context(SB(nc, "a_ckf", [128, 4 * LT], F32))
            ckf = ckf_t[:].rearrange("p (k t) -> p k t", t=LT)
            ckn_t = es1.enter_context(SB(nc, "a_ckn", [128, 4 * LT], BF16))
            ckn = ckn_t[:].rearrange("p (k t) -> p k t", t=LT)
            t_ckf = Tr()
            t_ckn = Tr()
            wv, wtr = load_wgroup(P, wpool, w_in_r, C_CKV, 512)
            chunks = [(t0, 512) for t0 in range(0, S, 512)] + [(S, LC)]
            for ft in range(4):
                for (t0, n) in chunks:
                    ps, ptr = proj_fm(P, pspool, wv, wtr, ft, uall, t_u, t0, n)
                    evac_copy(P, ckf[:, ft, t0:t0 + n], ps[:, 0:n], [ptr], [t_ckf])
            rs = Pool(nc, es1, "sb", 2, [128, 512], F32, "a_rs")
            for (t0, n) in chunks:
                pss, psst = pspool.get()
                for k in range(4):
                    q_, q_t = rtmp.get()
                    P.op("act", lambda e, q_=q_, k=k, t0=t0, n=n: e.activation(out=q_[:, 0:n], in_=ckf[:, k, t0:t0 + n], func=AF.Square),
                         r=[t_ckf], w=[q_t])
                    P.op("pe", lambda e, q_=q_, k=k, n=n: e.matmul(pss[:, 0:n], ones_f[:, :], q_[:, 0:n], start=(k == 0), stop=(k == 3)),
                         r=[q_t, t_c], w=[psst])
                r_, r_t = rs.get()
                P.op("dve", lambda e, r_=r_, n=n: e.tensor_scalar(out=r_[:, 0:n], in0=pss[:, 0:n], scalar1=1.0 / 512, scalar2=LN_EPS,
                                                                  op0=ALU.mult, op1=ALU.add), r=[psst], w=[r_t])
                P.op("act", lambda e, r_=r_, n=n: e.activation(out=r_[:, 0:n], in_=r_[:, 0:n], func=AF.Sqrt), r=[r_t], w=[r_t])
                P.op("dve", lambda e, r_=r_, n=n: e.reciprocal(out=r_[:, 0:n], in_=r_[:, 0:n]), r=[r_t], w=[r_t])
                for k in range(4):
                    P.op("dve", lambda e, r_=r_, k=k, t0=t0, n=n: e.scalar_tensor_tensor(
                        out=ckn[:, k, t0:t0 + n], in0=ckf[:, k, t0:t0 + n], scalar=kvn_sb[:, k:k + 1], in1=r_[:, 0:n],
                        op0=ALU.mult, op1=ALU.mult), r=[t_ckf, r_t, t_c], w=[t_ckn])
            wv, wtr = load_wgroup(P, wpool, w_ukv_r, 0, 512, nk=4)
            for ft in range(4):
                for (t0, n) in chunks:
                    ps, ptr = proj_fm(P, pspool, wv, wtr, ft, ckn, t_ckn, t0, n, nk=4)
                    s_, s_t = stg.get()
                    evac_copy(P, s_[:, 0:n], ps[:, 0:n], [ptr], [s_t])
                    P.dma("sp", kn_d[ft, :, t0:t0 + n], s_[:, 0:n], r=[s_t], w=[t_d["kn"]])
            wv, wtr = load_wgroup(P, wpool, w_ukv_r, 512, 512, nk=4)
            for tt in range(LT // 128):
                ps, ptr = proj_tm(P, pspool, wv, wtr, 0, 512, ckn, t_ckn, tt, nk=4)
                s_, s_t = stg.get()
                evac_copy(P, s_[:], ps[:, :], [ptr], [s_t])
                P.dma("sp", vm_d[tt * 128:(tt + 1) * 128, :], s_[:], r=[s_t], w=[t_d["vm"]])
            P.barrier()
        with ExitStack() as es2:
            NKT = LT // 128
            qb = Pool(nc, es2, "sb", 2, [128, S], BF16, "a_q")
            qpb = Pool(nc, es2, "sb", 2, [128, S], BF16, "a_qp")
            kb = Pool(nc, es2, "sb", 2, [128, LT], BF16, "a_k")
            vb = Pool(nc, es2, "sb", 2, [128, NKT * 128], BF16, "a_v")
            kpb = es2.enter_context(SB(nc, "a_kpb", [128, LT], BF16))
            t_kpb = Tr()
            P.dma("sp", kpb[:], kp_d[:, :], r=[t_d["kp"]], w=[t_kpb])
            naT = es2.enter_context(SB(nc, "a_naT", [128, 4 * TB_N * 64], F32))
            naRM = es2.enter_context(SB(nc, "a_naRM", [128, 4 * 8 * 8], F32))
            P.dma("sp", naT[:], na_T[:, :], w=[t_c])
            P.dma("sp", naRM[:], na_RM[:, :], w=[t_c])
            naTv = naT[:].rearrange("p (h b q) -> p h b q", b=TB_N, q=64)
            naRMv = naRM[:].rearrange("p (c j r) -> p c j r", j=8, r=8)
            psS = Pool(nc, es2, "ps", 4, [128, 512], F32, "ap_S")
            psO = Pool(nc, es2, "ps", 2, [128, 512], F32, "ap_O")
            psD = Pool(nc, es2, "ps", 2, [128, 512], F32, "ap_D")
            pT = Pool(nc, es2, "sb", 8, [128, 512], BF16, "a_pT")
            sT = Pool(nc, es2, "sb", 4, [128, 512], F32, "a_sT")
            LOOK = 3
            rd = Pool(nc, es2, "sb", 2, [128, 512], F32, "a_rd")
            ao = Pool(nc, es2, "sb", 2, [128, 512], BF16, "a_ao")

            def finish(po, pot, pd, pdt, row0, t0):
                r_, r_t = rd.get()
                P.op("dve", lambda e: e.reciprocal(out=r_[:], in_=pd[:, :]), r=[pdt], w=[r_t])
                a_, a_t = ao.get()
                P.op("dve", lambda e: e.tensor_tensor(out=a_[:], in0=po[:, :], in1=r_[:], op=ALU.mult), r=[pot, r_t], w=[a_t])
                P.dma("sp", aoT[row0:row0 + 128, t0:t0 + 512], a_[:], r=[a_t], w=[t_out])

            SC_MLA = 192 ** -0.5
            SC_NA = 128 ** -0.5
            for h in range(4):
                q_, q_t = qb.get()
                P.dma("sp", q_[:], qn_d[h], r=[t_d["qn"]], w=[q_t])
                if h % 2 == 0:
                    qp_, qp_t = qpb.get()
                    P.dma("sp", qp_[:], qp_d[h // 2], r=[t_d["qp"]], w=[qp_t])
                k_, k_t = kb.get()
                P.dma("sp", k_[:], kn_d[h], r=[t_d["kn"]], w=[k_t])
                v_, v_t = vb.get()
                P.dma("sp", v_[:].rearrange("p (t d) -> p t d", d=128),
                      vm_d[:, h * 128:(h + 1) * 128].rearrange("(t p) d -> p t d", p=128), r=[t_d["vm"]], w=[v_t])
                pb = 64 * (h % 2)
                for t0 in range(0, S, 512):
                    po, pot = psO.get()
                    pd, pdt = psD.get()

                    def issue_s(kt, t0=t0):
                        ps, pst = psS.get()
                        P.op("pe", lambda e, ps=ps, kt=kt: e.matmul(ps[:, :], k_[:, kt * 128:(kt + 1) * 128], q_[:, t0:t0 + 512],
                                                                    start=True, stop=False), r=[k_t, q_t], w=[pst])
                        P.op("pe", lambda e, ps=ps, kt=kt: e.matmul(ps[:, :], kpb[pb:pb + 64, kt * 128:(kt + 1) * 128],
                                                                    qp_[pb:pb + 64, t0:t0 + 512], start=False, stop=True),
                             r=[t_kpb, qp_t], w=[pst])
                        return ps, pst
                    pend = [issue_s(kt) for kt in range(min(LOOK, NKT))]
                    for kt in range(NKT):
                        ps, pst = pend.pop(0)
                        p_, p_t = pT.get()
                        P.op("act", lambda e, ps=ps, p_=p_: e.activation(out=p_[:], in_=ps[:, :], func=AF.Exp, scale=SC_MLA),
                             r=[pst], w=[p_t])
                        if kt + LOOK < NKT:
                            pend.append(issue_s(kt + LOOK))
                        P.op("pe", lambda e, p_=p_, kt=kt: e.matmul(po[:, :], v_[:, kt * 128:(kt + 1) * 128], p_[:],
                                                                    start=(kt == 0), stop=(kt == NKT - 1)), r=[v_t, p_t], w=[pot])
                        P.op("pe", lambda e, p_=p_, kt=kt: e.matmul(pd[:, :], ones_b[:, :], p_[:],
                                                                    start=(kt == 0), stop=(kt == NKT - 1)), r=[t_c, p_t], w=[pdt])
                    finish(po, pot, pd, pdt, h * 128, t0)
            for h in range(4):
                q_, q_t = qb.get()
                P.dma("sp", q_[:], qa_d[h], r=[t_d["qa"]], w=[q_t])
                k_, k_t = kb.get()
                P.dma("sp", k_[:], ka_d[h], r=[t_d["ka"]], w=[k_t])
                v_, v_t = vb.get()
                P.dma("sp", v_[:].rearrange("p (t d) -> p t d", d=128),
                      va_d[:, h * 128:(h + 1) * 128].rearrange("(t p) d -> p t d", p=128), r=[t_d["va"]], w=[v_t])
                for qc in range(4):
                    t0 = qc * 512
                    jlo, jhi = NA_J[qc]
                    kts = list(range(jlo, jhi)) + [16, 17]
                    po, pot = psO.get()
                    pd, pdt = psD.get()

                    def issue_s(kt, t0=t0):
                        ps, pst = psS.get()
                        P.op("pe", lambda e, ps=ps, kt=kt: e.matmul(ps[:, :], k_[:, kt * 128:(kt + 1) * 128], q_[:, t0:t0 + 512],
                                                                    start=True, stop=True), r=[k_t, q_t], w=[pst])
                        return ps, pst
                    pend = [issue_s(kt) for kt in kts[:LOOK]]
                    for idx, kt in enumerate(kts):
                        ps, pst = pend.pop(0)
                        p_, p_t = pT.get()
                        if kt < 16:
                            b0 = 8 * qc - 2 * kt + 7 + TB_OFF
                            s_, s_t = sT.get()
                            P.op("dve", lambda e, ps=ps, s_=s_, b0=b0: e.scalar_tensor_tensor(
                                out=s_[:].rearrange("p (r q) -> p r q", q=64), in0=ps[:, :].rearrange("p (r q) -> p r q", q=64),
                                scalar=SC_NA, in1=naTv[:, h, b0:b0 + 8, :], op0=ALU.mult, op1=ALU.add), r=[pst, t_c], w=[s_t])
                            jj = kt - jlo
                            P.op("pool" if idx % 3 == 2 else "dve", lambda e, s_=s_, jj=jj: e.tensor_tensor(
                                out=s_[:].rearrange("p (r q) -> p r q", q=64), in0=s_[:].rearrange("p (r q) -> p r q", q=64),
                                in1=naRMv[:, qc, jj, :].unsqueeze(2).to_broadcast([128, 8, 64]), op=ALU.add), r=[s_t, t_c], w=[s_t])
                            P.op("act", lambda e, s_=s_, p_=p_: e.activation(out=p_[:], in_=s_[:], func=AF.Exp), r=[s_t], w=[p_t])
                        else:
                            P.op("act", lambda e, ps=ps, p_=p_: e.activation(out=p_[:], in_=ps[:, :], func=AF.Exp, scale=SC_NA),
                                 r=[pst], w=[p_t])
                        if idx + LOOK < len(kts):
                            pend.append(issue_s(kts[idx + LOOK]))
                        last = (idx == len(kts) - 1)
                        P.op("pe", lambda e, p_=p_, kt=kt, idx=idx, last=last: e.matmul(
                            po[:, :], v_[:, kt * 128:(kt + 1) * 128], p_[:], start=(idx == 0), stop=last), r=[v_t, p_t], w=[pot])
                        P.op("pe", lambda e, p_=p_, idx=idx, last=last: e.matmul(pd[:, :], ones_b[:, :], p_[:], start=(idx == 0), stop=last),
                             r=[t_c, p_t], w=[pdt])
                    finish(po, pot, pd, pdt, 512 + h * 128, t0)
            P.barrier()


def na_tables_host(rpb4):
    NEG = np.float32(-1e30)
    qcol = np.arange(64)
    kc = np.arange(64)
    cs = np.clip(qcol - 8, 0, 48)
    col_ok = (kc[None, :] >= cs[:, None]) & (kc[None, :] < cs[:, None] + 16)
    dc = np.clip(kc[None, :] - qcol[:, None], -15, 15) + 15
    T = np.full((128, 4, TB_N, 64), NEG, dtype=np.float32)
    for rk in range(2):
        for bi in range(TB_N):
            b = bi - TB_OFF
            a = 14 - b + rk
            if 0 <= a <= 14:
                for h in range(4):
                    vals = rpb4[h, a][dc]
                    blk = np.where(col_ok, vals, NEG)
                    T[rk * 64:(rk + 1) * 64, h, bi, :] = blk.T
    RM = np.full((128, 4, 8, 8), NEG, dtype=np.float32)
    for qc in range(4):
        jlo, jhi = NA_J[qc]
        for jj in range(jhi - jlo):
            j = jlo + jj
            for rq in range(8):
                r = 8 * qc + rq
                rs = min(max(r - 4, 0), 24)
                for rk in range(2):
                    kr = 2 * j + rk
                    if rs <= kr <= rs + 7:
                        RM[rk * 64:(rk + 1) * 64, qc, jj, rq] = 0.0
    return T.reshape(128, -1), RM.reshape(128, -1)


def rope_tables_host():
    t = np.arange(S)
    row = (t // 64).astype(np.float32)
    col = (t % 64).astype(np.float32)
    n_freq = 16
    inv = (10000.0 ** (-np.arange(n_freq, dtype=np.float32) / n_freq)).astype(np.float32)
    ang = np.concatenate([row[:, None] * inv, col[:, None] * inv], axis=1)
    cos = np.cos(ang).astype(np.float32)
    sin = np.sin(ang).astype(np.float32)
    i = np.arange(128)
    f = (i % 64) % 32
    sign = np.where((i % 64) < 32, -1.0, 1.0).astype(np.float32)
    cosT = np.ascontiguousarray(cos[:, f].T)
    sinT = np.ascontiguousarray((sin[:, f] * sign[None, :]).T)
    return cosT, sinT


def attn_host_inputs(inp, m0, b, hf):
    def fm(v):
        return np.ascontiguousarray(v.reshape(16, 128).T)
    w = inp["ab_w_in"][0]
    hs = [4 * hf + i for i in range(4)]
    cols = []
    for h in hs:
        cols += list(range(h * 192, h * 192 + 128))
    for h in hs:
        cols += list(range(h * 192 + 128, h * 192 + 192))
    for h in hs:
        cols += list(range(h * 192 + 160, h * 192 + 192)) + list(range(h * 192 + 128, h * 192 + 160))
    cols += list(range(1536, 2048))
    kp = list(range(2048, 2112))
    kps = list(range(2080, 2112)) + list(range(2048, 2080))
    cols += kp + kp + kps + kps
    for off in (2112, 3136, 4160):
        for h in hs:
            cols += list(range(off + h * 128, off + (h + 1) * 128))
    w_in_r = np.ascontiguousarray(w[:, cols])
    wu = inp["ab_w_ukv"][0]
    ucols = []
    for h in hs:
        ucols += list(range(h * 256, h * 256 + 128))
    for h in hs:
        ucols += list(range(h * 256 + 128, h * 256 + 256))
    w_ukv_r = np.ascontiguousarray(wu[:, ucols])
    naT, naRM = na_tables_host(inp["ab_rpb"][0][4 * hf:4 * hf + 4])
    cosT, sinT = rope_tables_host()
    mb = m0[b]
    mc = m0[4]
    modv = np.concatenate([fm(mb[0:2048]), fm(mb[2048:4096]), fm(mc[0:2048]), fm(mc[2048:4096])], axis=1)
    return {"xT": np.ascontiguousarray(inp["x"][b].T), "ctxT": np.ascontiguousarray(inp["ctx"][b].T), "modv": modv,
            "w_in_r": w_in_r, "kvn": np.ascontiguousarray(inp["ab_kv_norm"][0].reshape(4, 128).T), "w_ukv_r": w_ukv_r,
            "cosT": cosT, "sinT": sinT, "na_T": naT, "na_RM": naRM}


def build_attn_prog():
    nc = bass.Bass("TRN2", target_bir_lowering=False)
    consts = make_consts()
    with ExitStack() as es:
        P = Prog(nc, es)
        cst = declare_consts(nc, consts)
        xT = dram_in(nc, "xT", [D, S])
        ctxT = dram_in(nc, "ctxT", [D, LC])
        modv = dram_in(nc, "modv", [128, 64])
        w_in_r = dram_in(nc, "w_in_r", [D, 3328])
        kvn = dram_in(nc, "kvn", [128, 4])
        w_ukv_r = dram_in(nc, "w_ukv_r", [512, 1024])
        cosT = dram_in(nc, "cosT", [128, S])
        sinT = dram_in(nc, "sinT", [128, S])
        na_T = dram_in(nc, "na_T", [128, 4 * TB_N * 64])
        na_RM = dram_in(nc, "na_RM", [128, 256])
        aoT = dram_out(nc, "aoT", [1024, S], BF16)
        stage_attn(P, cst, xT, ctxT, modv, w_in_r, kvn, w_ukv_r, cosT, sinT, na_T, na_RM, aoT, Tr())
        P.barrier()
    return nc, consts


def stage_moe_ffn(P, xe_in, w1, w3, w2, ye_out, t_in, t_out, load_xe=None, nb=4, n_exp=2):
    nc = P.nc
    if load_xe is None:
        def load_xe(dst, ex, b, w):
            P.dma("sp", dst, xe_in[ex, b].rearrange("p (c s) -> p c s", s=CAP), r=[t_in], w=w)
    NFT = FF // 128
    NT = nb * CAP
    TCH = min(512, NT)
    with ExitStack() as es:
        xe_t = [es.enter_context(SB(nc, "f_xe%d" % i, [128, 16 * NT], BF16)) for i in range(2)]
        t_xe = [Tr(), Tr()]
        wbuf = Pool(nc, es, "sb", 3 if nb == 1 else 2, [128, 16 * FF], BF16, "f_w")
        w2buf = Pool(nc, es, "sb", 3 if nb == 1 else 2, [128, NFT * 512], BF16, "f_w2")
        h1_t = es.enter_context(SB(nc, "f_h1", [128, NFT * NT], BF16))
        h1 = h1_t[:].rearrange("p (f t) -> p f t", t=NT)
        t_h1 = Tr()
        stg = Pool(nc, es, "sb", 3, [128, 512], BF16, "f_stg")
        pspool = Pool(nc, es, "ps", 6, [128, 512], F32, "fp_p")
        def fetch(ex):
            xv = xe_t[ex % 2][:].rearrange("p (c b s) -> p c b s", b=nb, s=CAP)
            for b in range(nb):
                load_xe(xv[:, :, b, :], ex, b, [t_xe[ex % 2]])
        fetch(0)
        for ex in range(n_exp):
            if ex + 1 < n_exp:
                fetch(ex + 1)
            xf = xe_t[ex % 2][:].rearrange("p (c t) -> p c t", t=NT)
            for which, wsrc in ((0, w1), (1, w3)):
                wt, wtr = wbuf.get()
                wv = wt[:].rearrange("p (k c) -> p k c", c=FF)
                for half in range(2):
                    P.dma("pool", wv[:, half * 8:(half + 1) * 8, :],
                          wsrc[ex, half * 1024:(half + 1) * 1024, :].rearrange("(k p) c -> p k c", p=128), w=[wtr])
                for f in range(NFT):
                    for t0 in range(0, NT, TCH):
                        ps, ptr = pspool.get()
                        for k in range(16):
                            P.op("pe", lambda e, ps=ps, k=k, f=f, t0=t0: e.matmul(ps[:, 0:TCH], wv[:, k, f * 128:(f + 1) * 128],
                                                                                  xf[:, k, t0:t0 + TCH], start=(k == 0), stop=(k == 15)),
                                 r=[wtr, t_xe[ex % 2]], w=[ptr])
                        if which == 0:
                            P.op("act", lambda e, ps=ps, f=f, t0=t0: e.activation(out=h1[:, f, t0:t0 + TCH], in_=ps[:, 0:TCH], func=AF.Silu),
                                 r=[ptr], w=[t_h1])
                        else:
                            P.op("dve", lambda e, ps=ps, f=f, t0=t0: e.tensor_tensor(out=h1[:, f, t0:t0 + TCH], in0=h1[:, f, t0:t0 + TCH],
                                                                                     in1=ps[:, 0:TCH], op=ALU.mult), r=[ptr, t_h1], w=[t_h1])
            for dq in range(4):
                w2t, w2tr = w2buf.get()
                w2v = w2t[:].rearrange("p (f d) -> p f d", d=512)
                P.dma("pool", w2v, w2[ex, :, dq * 512:(dq + 1) * 512].rearrange("(f p) d -> p f d", p=128), w=[w2tr])
                for g in range(2 * nb):
                    ps, ptr = pspool.get()
                    for f in range(NFT):
                        P.op("pe", lambda e, ps=ps, f=f, g=g: e.matmul(ps[:, :], h1[:, f, g * 128:(g + 1) * 128], w2v[:, f, :],
                                                                       start=(f == 0), stop=(f == NFT - 1)), r=[t_h1, w2tr], w=[ptr])
                    s_, s_t = stg.get()
                    evac_copy(P, s_[:], ps[:, :], [ptr], [s_t])
                    P.dma("sp", ye_out[ex, g, :, dq * 512:(dq + 1) * 512], s_[:], r=[s_t], w=[t_out])
        P.barrier()


def stage_scatter_ln(P, cst, ye_in, pos_in, gate_in, hT, vecs, outT, t_in, t_out, load_ye=None, load_pg=None):
    nc = P.nc
    if load_ye is None:
        def load_ye(dst, ex, dh, w):
            P.dma("sp", dst, ye_in[ex, :, :, dh * 1024:(dh + 1) * 1024].rearrange("s p d -> p s d"), r=[t_in], w=w)
    if load_pg is None:
        def load_pg(pos, gate, w):
            P.dma("sp", pos, pos_in[:, :], r=[t_in], w=w)
            P.dma("sp", gate, gate_in[:, :], r=[t_in], w=w)
    T = 1024
    z_d = dram_scratch(nc, "s_z%d" % id(outT), [D, T], F32)
    t_z = Tr()
    with ExitStack() as es:
        def sb(name, shape, dt):
            return es.enter_context(SB(nc, "s_" + name, shape, dt))
        vec_sb = sb("vec", [128, 48], F32)
        ones_f = sb("ones_f", [128, 128], F32)
        t_c = Tr()
        load_pieces(P, vec_sb, vecs, [t_c])
        P.dma("sp", ones_f[:], cst["ones_f"][:, :], w=[t_c])
        with ExitStack() as es1:
            pidx = es1.enter_context(SB(nc, "s_pidx", [128, 2], F32))
            selrow = es1.enter_context(SB(nc, "s_selrow", [16, 16 * 128], F32))
            pos = es1.enter_context(SB(nc, "s_pos", [16, T], F32))
            gate = es1.enter_context(SB(nc, "s_gate", [16, T], F32))
            P.dma("sp", pidx[:], cst["pidx"][:, :], w=[t_c])
            P.dma("sp", selrow[:].rearrange("p (e m) -> p e m", m=128), cst["selrow16"][:, :, :], w=[t_c])
            load_pg(pos[:], gate[:], [t_c])
            selrow_b = es1.enter_context(SB(nc, "s_selrowb", [16, 16 * 128], BF16))
            pos_b = es1.enter_context(SB(nc, "s_posb", [16, T], BF16))
            gate_b = es1.enter_context(SB(nc, "s_gateb", [16, T], BF16))
            P.op("dve", lambda e: e.tensor_copy(out=selrow_b[:], in_=selrow[:]), r=[t_c], w=[t_c])
            P.op("dve", lambda e: e.tensor_copy(out=pos_b[:], in_=pos[:]), r=[t_c], w=[t_c])
            P.op("dve", lambda e: e.tensor_copy(out=gate_b[:], in_=gate[:]), r=[t_c], w=[t_c])
            selg_t = es1.enter_context(SB(nc, "s_selg", [128, 32 * T], BF16))
            selg = selg_t[:].rearrange("p (g t) -> p g t", t=T)
            t_selg = Tr()
            psB = Pool(nc, es1, "ps", 4, [128, 512], F32, "sp_B")
            psO = Pool(nc, es1, "ps", 2, [128, 512], F32, "sp_O")
            gb = Pool(nc, es1, "sb", 2, [128, 512], F32, "s_gb")
            for ex in range(16):
                for tq in range(T // 512):
                    pb, pbt = psB.get()
                    pg, pgt = psB.get()
                    P.op("pe", lambda e, pb=pb, ex=ex, tq=tq: e.matmul(pb[:, :], selrow_b[:, ex * 128:(ex + 1) * 128],
                                                                       pos_b[:, tq * 512:(tq + 1) * 512], start=True, stop=True),
                         r=[t_c], w=[pbt])
                    P.op("pe", lambda e, pg=pg, ex=ex, tq=tq: e.matmul(pg[:, :], selrow_b[:, ex * 128:(ex + 1) * 128],
                                                                       gate_b[:, tq * 512:(tq + 1) * 512], start=True, stop=True),
                         r=[t_c], w=[pgt])
                    g_, g_t = gb.get()
                    P.op("act", lambda e, g_=g_, pg=pg: e.copy(out=g_[:], in_=pg[:, :]), r=[pgt], w=[g_t])
                    for st in range(2):
                        P.op("dve", lambda e, pb=pb, g_=g_, st=st, ex=ex, tq=tq: e.scalar_tensor_tensor(
                            out=selg[:, ex * 2 + st, tq * 512:(tq + 1) * 512], in0=pb[:, :], scalar=pidx[:, st:st + 1],
                            in1=g_[:], op0=ALU.is_equal, op1=ALU.mult), r=[pbt, g_t, t_c], w=[t_selg])
            ye_t = es1.enter_context(SB(nc, "s_ye", [128, 32 * 1024], BF16))
            ye = ye_t[:].rearrange("p (g d) -> p g d", d=1024)
            t_ye = Tr()
            hin = Pool(nc, es1, "sb", 2, [128, 512], F32, "s_hin")
            yg = Pool(nc, es1, "sb", 2, [128, 512], F32, "s_yg")
            for dh in range(2):
                for ex in range(16):
                    load_ye(ye[:, ex * 2:ex * 2 + 2, :], ex, dh, [t_ye])
                for dt in range(8):
                    c = dh * 8 + dt
                    for tq in range(T // 512):
                        po, pot = psO.get()
                        for g in range(32):
                            P.op("pe", lambda e, po=po, g=g, dt=dt, tq=tq: e.matmul(po[:, :], ye[:, g, dt * 128:(dt + 1) * 128],
                                                                                    selg[:, g, tq * 512:(tq + 1) * 512],
                                                                                    start=(g == 0), stop=(g == 31)),
                                 r=[t_ye, t_selg], w=[pot])
                        h_, h_t = hin.get()
                        P.dma("sp", h_[:], hT[c * 128:(c + 1) * 128, tq * 512:(tq + 1) * 512], r=[t_in], w=[h_t])
                        y_, y_t = yg.get()
                        P.op("act", lambda e, y_=y_, po=po, c=c: e.activation(out=y_[:], in_=po[:, :], func=AF.Identity,
                                                                              scale=vec_sb[:, c:c + 1]), r=[pot, t_c], w=[y_t])
                        P.op("dve", lambda e, y_=y_, h_=h_: e.scalar_tensor_tensor(out=y_[:], in0=h_[:], scalar=ALPHA, in1=y_[:],
                                                                                   op0=ALU.mult, op1=ALU.add), r=[h_t, y_t], w=[y_t])
                        P.dma("sp", z_d[c * 128:(c + 1) * 128, tq * 512:(tq + 1) * 512], y_[:], r=[y_t], w=[t_z])
            P.barrier()
        z_t = sb("z", [128, 16 * T], F32)
        z = z_t[:].rearrange("p (c t) -> p c t", t=T)
        t_zs = Tr()
        P.dma("sp", z, z_d.rearrange("(c p) t -> p c t", p=128), r=[t_z], w=[t_zs])
        pspool = Pool(nc, es, "ps", 4, [128, 512], F32, "sp_L")
        ln_apply(P, es, z, t_zs, T, vec_sb[:, 16:32], vec_sb[:, 32:48], t_c, ones_f, t_c, outT, t_out, pspool, "s_ln")
        P.barrier()


def stage_oproj_ln(P, cst, ao_in, w_out, hT, vecs, outT, t_in, t_out, load_ao=None):
    nc = P.nc
    if load_ao is None:
        def load_ao(dst, w):
            P.dma("sp", dst, ao_in.rearrange("(c p) t -> p c t", p=128), r=[t_in], w=w)
    T = 1024
    with ExitStack() as es:
        def sb(name, shape, dt):
            return es.enter_context(SB(nc, "o_" + name, shape, dt))
        vec_sb = sb("vec", [128, 48], F32)
        ones_f = sb("ones_f", [128, 128], F32)
        t_c = Tr()
        load_pieces(P, vec_sb, vecs, [t_c])
        P.dma("sp", ones_f[:], cst["ones_f"][:, :], w=[t_c])
        z_t = sb("z", [128, 16 * T], F32)
        z = z_t[:].rearrange("p (c t) -> p c t", t=T)
        t_z = Tr()
        with ExitStack() as es1:
            ao_t = es1.enter_context(SB(nc, "o_ao", [128, 16 * T], BF16))
            ao = ao_t[:].rearrange("p (c t) -> p c t", t=T)
            t_ao = Tr()
            load_ao(ao, [t_ao])
            wpool = Pool(nc, es1, "sb", 2, [128, 16 * 512], BF16, "o_w")
            pspool = Pool(nc, es1, "ps", 4, [128, 512], F32, "op_p")
            hin = Pool(nc, es1, "sb", 2, [128, 512], F32, "o_hin")
            yg = Pool(nc, es1, "sb", 2, [128, 512], F32, "o_yg")
            for grp in range(4):
                wv, wtr = load_wgroup(P, wpool, w_out, grp * 512, 512)
                for ft in range(4):
                    c = grp * 4 + ft
                    for t0 in range(0, T, 512):
                        ps, ptr = proj_fm(P, pspool, wv, wtr, ft, ao, t_ao, t0, 512)
                        h_, h_t = hin.get()
                        P.dma("sp", h_[:], hT[c * 128:(c + 1) * 128, t0:t0 + 512], r=[t_in], w=[h_t])
                        y_, y_t = yg.get()
                        P.op("act", lambda e, y_=y_, ps=ps, c=c: e.activation(out=y_[:], in_=ps[:, :], func=AF.Identity,
                                                                              scale=vec_sb[:, c:c + 1]), r=[ptr, t_c], w=[y_t])
                        P.op("dve", lambda e, y_=y_, h_=h_, c=c, t0=t0: e.scalar_tensor_tensor(
                            out=z[:, c, t0:t0 + 512], in0=h_[:], scalar=ALPHA, in1=y_[:], op0=ALU.mult, op1=ALU.add),
                            r=[h_t, y_t], w=[t_z])
            P.barrier()
        pspool2 = Pool(nc, es, "ps", 4, [128, 512], F32, "op_L")
        ln_apply(P, es, z, t_z, T, vec_sb[:, 16:32], vec_sb[:, 32:48], t_c, ones_f, t_c, outT, t_out, pspool2, "o_ln")
        P.barrier()


NF = 17


def stage_cd(P, cst, hT, modv, w_in_c, cvec, fw1, fw2, fvec, fw3_c, delta_b, cdT, t_in, t_out, load_h=None):
    nc = P.nc
    if load_h is None:
        def load_h(dst, c, w):
            P.dma("sp", dst, hT[c * 128:(c + 1) * 128, :], r=[t_in], w=w)
    with ExitStack() as es:
        def sb(name, shape, dt):
            return es.enter_context(SB(nc, "h_" + name, shape, dt))
        modv_sb = sb("modv", [128, 32], F32)
        sc1p = sb("sc1p", [128, 16], F32)
        cvec_sb = sb("cvec", [128, 56], F32)
        ident_b = sb("ident_b", [128, 128], BF16)
        wN = sb("wN", [128, NF], F32)
        t01n = sb("t01n", [128, 16], F32)
        mask0 = sb("mask0", [128, 1], F32)
        dlt = sb("dlt", [128, 512], F32)
        t_c = Tr()
        load_pieces(P, modv_sb, modv, [t_c])
        P.dma("sp", cvec_sb[:], cvec[:, :], w=[t_c])
        P.dma("sp", ident_b[:], cst["ident_b"][:, :], w=[t_c])
        P.dma("sp", wN[:], cst["wN"][:, :], w=[t_c])
        P.dma("sp", t01n[:], cst["t01n"][:, :], w=[t_c])
        P.dma("sp", mask0[:], cst["mask0"][:, :], w=[t_c])
        P.dma("sp", dlt[:], delta_b[:, :], w=[t_c])
        P.op("dve", lambda e: e.tensor_scalar(out=sc1p[:], in0=modv_sb[:, 16:32], scalar1=1.0, scalar2=None, op0=ALU.add),
             r=[t_c], w=[t_c])
        xs_t = [sb("xs%d" % o, [128, 4 * S], BF16) for o in range(3)]
        xs = [t[:].rearrange("p (c t) -> p c t", t=S) for t in xs_t]
        t_xs = [Tr(), Tr(), Tr()]
        es_g = ExitStack()
        g_t = es_g.enter_context(SB(nc, "h_g", [128, 4 * S], BF16))
        gT = g_t[:].rearrange("p (c t) -> p c t", t=S)
        t_g = Tr()
        with ExitStack() as es1:
            u_t = es1.enter_context(SB(nc, "h_u", [128, 16 * S], BF16))
            u = u_t[:].rearrange("p (k t) -> p k t", t=S)
            t_u = Tr()
            es_x = ExitStack()
            xin = Pool(nc, es_x, "sb", 2, [128, S], F32, "h_xin")
            for c in range(16):
                xt, xtr = xin.get()
                load_h(xt[:], c, [xtr])
                P.op("act", lambda e, c=c, xt=xt: e.activation(out=u[:, c, :], in_=xt[:], func=AF.Identity,
                                                               bias=modv_sb[:, c:c + 1], scale=sc1p[:, c:c + 1]),
                     r=[xtr, t_c], w=[t_u])
            P.barrier()
            es_x.close()
            wpool = Pool(nc, es1, "sb", 2, [128, 16 * 512], BF16, "h_w")
            pspool = Pool(nc, es1, "ps", 4, [128, 512], F32, "hp_p")
            prow = Pool(nc, es1, "sb", 2, [128, S + 2], F32, "h_prow")
            acc = Pool(nc, es1, "sb", 2, [128, S], F32, "h_acc")
            for o in range(3):
                wv, wtr = load_wgroup(P, wpool, w_in_c, o * 512, 512)
                for ct in range(4):
                    pr, prt = prow.get()
                    P.op("pool", lambda e, pr=pr: e.memset(pr[:, 0:1], 0.0), w=[prt])
                    P.op("pool", lambda e, pr=pr: e.memset(pr[:, S + 1:S + 2], 0.0), w=[prt])
                    for t0 in range(0, S, 512):
                        ps, ptr = proj_fm(P, pspool, wv, wtr, ct, u, t_u, t0, 512)
                        evac_copy(P, pr[:, 1 + t0:1 + t0 + 512], ps[:, :], [ptr], [prt])
                    a_, a_t = acc.get()
                    j = (o * 4 + ct) * 4
                    P.op("act", lambda e, a_=a_, pr=pr, j=j: e.activation(out=a_[:], in_=pr[:, 1:S + 1], func=AF.Identity,
                                                                          bias=cvec_sb[:, j + 3:j + 4], scale=cvec_sb[:, j + 1:j + 2]),
                         r=[prt, t_c], w=[a_t])
                    P.op("dve", lambda e, a_=a_, pr=pr, j=j: e.scalar_tensor_tensor(out=a_[:], in0=pr[:, 0:S], scalar=cvec_sb[:, j:j + 1],
                                                                                    in1=a_[:], op0=ALU.mult, op1=ALU.add),
                         r=[prt, a_t, t_c], w=[a_t])
                    P.op("dve", lambda e, a_=a_, pr=pr, j=j, o=o, ct=ct: e.scalar_tensor_tensor(
                        out=xs[o][:, ct, :], in0=pr[:, 2:S + 2], scalar=cvec_sb[:, j + 2:j + 3], in1=a_[:], op0=ALU.mult, op1=ALU.add),
                        r=[prt, a_t, t_c], w=[t_xs[o]])
            wv, wtr = load_wgroup(P, wpool, w_in_c, 1536, 512)
            for ct in range(4):
                for t0 in range(0, S, 512):
                    ps, ptr = proj_fm(P, pspool, wv, wtr, ct, u, t_u, t0, 512)
                    evac_copy(P, gT[:, ct, t0:t0 + 512], ps[:, :], [ptr], [t_g])
            P.barrier()
        with ExitStack() as es2:
            csw = es2.enter_context(SB(nc, "h_csw", [128, 2 * 512], BF16))
            P.dma("sp", csw[:].rearrange("p (k c) -> p k c", c=512), cst["CSW"].rearrange("(k p) c -> p k c", p=128), w=[t_c])
            cswv = csw[:].rearrange("p (k c) -> p k c", c=512)
            ab_t = es2.enter_context(SB(nc, "h_ab", [128, 16 * 2 * 512], BF16))
            ab = ab_t[:].rearrange("p (t g c) -> p t g c", g=2, c=512)
            t_ab = Tr()
            pspool = Pool(nc, es2, "ps", 8, [128, 512], F32, "hp_f")
            for tt in range(16):
                for grp in range(2):
                    ps, ptr = pspool.get()
                    for k in range(2):
                        P.op("pe", lambda e, ps=ps, k=k, grp=grp, tt=tt: e.matmul(ps[:, :], gT[:, grp * 2 + k, tt * 128:(tt + 1) * 128],
                                                                                  cswv[:, k, :], start=(k == 0), stop=(k == 1)),
                             r=[t_g, t_c], w=[ptr])
                    evac_copy(P, ab[:, tt, grp, :], ps[:, :], [ptr], [t_ab])
            rblk = Pool(nc, es2, "sb", 4, [128, 1024], BF16, "h_frb")
            ost = Pool(nc, es2, "sb", 3, [128, 512], BF16, "h_fo")
            for th in range(2):
                accs = [pspool.get() for _ in range(8)]
                for tc_ in range(16):
                    rc, rct = rblk.get()
                    rs_, rst = rblk.get()
                    P.dma("sp", rc[:], cst["FC"][tc_ * 128:(tc_ + 1) * 128, th * 1024:(th + 1) * 1024], w=[rct])
                    P.dma("sp", rs_[:], cst["FS"][tc_ * 128:(tc_ + 1) * 128, th * 1024:(th + 1) * 1024], w=[rst])
                    for ct in range(4):
                        grp, half = ct // 2, ct % 2
                        for tq in range(2):
                            ps, ptr = accs[ct * 2 + tq]
                            P.op("pe", lambda e, ps=ps, rc=rc, tc_=tc_, grp=grp, half=half, tq=tq: e.matmul(
                                ps[:, :], ab[:, tc_, grp, half * 128:(half + 1) * 128], rc[:, tq * 512:(tq + 1) * 512],
                                start=(tc_ == 0), stop=False), r=[t_ab, rct], w=[ptr])
                            P.op("pe", lambda e, ps=ps, rs_=rs_, tc_=tc_, grp=grp, half=half, tq=tq: e.matmul(
                                ps[:, :], ab[:, tc_, grp, 256 + half * 128:256 + (half + 1) * 128], rs_[:, tq * 512:(tq + 1) * 512],
                                start=False, stop=(tc_ == 15)), r=[t_ab, rst], w=[ptr])
                for ct in range(4):
                    for tq in range(2):
                        ps, ptr = accs[ct * 2 + tq]
                        o_, o_t = ost.get()
                        evac_copy(P, o_[:], ps[:, :], [ptr], [o_t])
                        P.dma("sp", cdT[512 + ct * 128:512 + (ct + 1) * 128, th * 1024 + tq * 512:th * 1024 + (tq + 1) * 512], o_[:],
                              r=[o_t], w=[t_out])
            P.barrier()
        es_g.close()
        hd2 = sb("hd2", [64, S], F32)
        t_hd2 = Tr()
        with ExitStack() as es3:
            zf = es3.enter_context(SB(nc, "h_zf", [33, S], F32))
            fw1_sb = es3.enter_context(SB(nc, "h_fw1", [33, 64], F32))
            fw2_sb = es3.enter_context(SB(nc, "h_fw2", [64, 64], F32))
            fv = es3.enter_context(SB(nc, "h_fv", [64, 8], F32))
            hd1 = es3.enter_context(SB(nc, "h_hd1", [64, S], F32))
            tmp = es3.enter_context(SB(nc, "h_ftmp", [64, S], F32))
            t_f = Tr()
            t_hd1 = Tr()
            t_tmp = Tr()
            P.dma("sp", zf[:], cst["zfeatT"][:, :], w=[t_f])
            P.dma("sp", fw1_sb[:], fw1[:, :], w=[t_f])
            P.dma("sp", fw2_sb[:], fw2[:, :], w=[t_f])
            P.dma("sp", fv[:, 0:4], fvec[:, :], w=[t_f])
            for (a, bcol, so, bo) in ((1, 0, 4, 5), (3, 2, 6, 7)):
                P.op("dve", lambda e, a=a, so=so: e.tensor_scalar(out=fv[:, so:so + 1], in0=fv[:, a:a + 1], scalar1=1.0 / 3, scalar2=None,
                                                                  op0=ALU.mult), r=[t_f], w=[t_f])
                P.op("dve", lambda e, so=so, bcol=bcol, bo=bo: e.tensor_tensor(out=fv[:, bo:bo + 1], in0=fv[:, so:so + 1],
                                                                               in1=fv[:, bcol:bcol + 1], op=ALU.mult), r=[t_f], w=[t_f])
            pspool = Pool(nc, es3, "ps", 4, [128, 512], F32, "hp_m")
            for (wsb, src, t_src, dst, t_dst, so, bo, kk) in ((fw1_sb, zf, t_f, hd1, t_hd1, 4, 5, 33), (fw2_sb, hd1, t_hd1, hd2, t_hd2, 6, 7, 64)):
                for t0 in range(0, S, 512):
                    ps, ptr = pspool.get()
                    P.op("pe", lambda e, ps=ps, wsb=wsb, src=src, t0=t0, kk=kk: e.matmul(ps[0:64, :], wsb[0:kk, :], src[0:kk, t0:t0 + 512],
                                                                                        start=True, stop=True), r=[t_f, t_src], w=[ptr])
                    P.op("act", lambda e, ps=ps, t0=t0, so=so, bo=bo: e.activation(out=tmp[:, t0:t0 + 512], in_=ps[0:64, :], func=AF.Sin,
                                                                                   bias=fv[:, bo:bo + 1], scale=fv[:, so:so + 1]),
                         r=[ptr, t_f], w=[t_tmp])
                    P.op("dve", lambda e, dst=dst, t0=t0: e.tensor_tensor(out=dst[:, t0:t0 + 512], in0=tmp[:, t0:t0 + 512],
                                                                          in1=tmp[:, t0:t0 + 512], op=ALU.mult), r=[t_tmp], w=[t_dst])
                    P.op("dve", lambda e, dst=dst, t0=t0: e.tensor_scalar(out=dst[:, t0:t0 + 512], in0=dst[:, t0:t0 + 512], scalar1=-4.0,
                                                                          scalar2=3.0, op0=ALU.mult, op1=ALU.add), r=[t_dst], w=[t_dst])
                    P.op("dve", lambda e, dst=dst, t0=t0: e.tensor_tensor(out=dst[:, t0:t0 + 512], in0=dst[:, t0:t0 + 512],
                                                                          in1=tmp[:, t0:t0 + 512], op=ALU.mult), r=[t_dst, t_tmp], w=[t_dst])
            P.barrier()
        fw3_sb = sb("fw3", [64, 2048], F32)
        P.dma("sp", fw3_sb[:], fw3_c[:, :], w=[t_c])
        dec_t = sb("dec", [128, 16 * 512], BF16)
        dec = dec_t[:].rearrange("p (t c) -> p t c", c=512)
        t_dec = Tr()
        for tt in range(16):
            P.op("act", lambda e, tt=tt: e.activation(out=dec[:, tt, :], in_=dlt[:], func=AF.Exp, scale=t01n[:, tt:tt + 1]),
                 r=[t_c], w=[t_dec])
        zcur = xs[0]
        t_zcur = t_xs[0]
        for o in range(2):
            with ExitStack() as es4:
                hc_t = es4.enter_context(SB(nc, "h_hc%d" % o, [128, 16 * 2 * 512], BF16))
                hc = hc_t[:].rearrange("p (t k c) -> p t k c", k=2, c=512)
                t_hc = Tr()
                zt_t = es4.enter_context(SB(nc, "h_zt%d" % o, [128, 16 * 512], BF16))
                zt = zt_t[:].rearrange("p (t c) -> p t c", c=512)
                t_zt = Tr()
                yy_t = es4.enter_context(SB(nc, "h_yy%d" % o, [128, NF * 2 * 512], BF16))
                yy = yy_t[:].rearrange("p (f k c) -> p f k c", k=2, c=512)
                t_yy = Tr()
                with ExitStack() as es5:
                    pspool = Pool(nc, es5, "ps", 8, [128, 512], F32, "hp_h")
                    ftmp = Pool(nc, es5, "sb", 4, [128, 512], F32, "h_hft")
                    for tt in range(16):
                        pf, pft = pspool.get()
                        pb, pbt = pspool.get()
                        P.op("pe", lambda e, pf=pf, tt=tt: e.matmul(pf[:, :], hd2[:, tt * 128:(tt + 1) * 128],
                                                                    fw3_sb[:, (o * 2) * 512:(o * 2 + 1) * 512], start=True, stop=True),
                             r=[t_hd2, t_c], w=[pft])
                        P.op("pe", lambda e, pb=pb, tt=tt: e.matmul(pb[:, :], hd2[:, tt * 128:(tt + 1) * 128],
                                                                    fw3_sb[:, (o * 2 + 1) * 512:(o * 2 + 2) * 512], start=True, stop=True),
                             r=[t_hd2, t_c], w=[pbt])
                        f1, f1t = ftmp.get()
                        f2, f2t = ftmp.get()
                        P.op("dve", lambda e, f1=f1, pf=pf, tt=tt: e.tensor_tensor(out=f1[:], in0=pf[:, :], in1=dec[:, tt, :], op=ALU.mult),
                             r=[pft, t_dec], w=[f1t])
                        if tt == 0:
                            P.op("dve", lambda e, f2=f2, pb=pb, tt=tt: e.scalar_tensor_tensor(out=f2[:], in0=pb[:, :], scalar=mask0[:, 0:1],
                                                                                              in1=dec[:, tt, :], op0=ALU.mult, op1=ALU.mult),
                                 r=[pbt, t_dec, t_c], w=[f2t])
                        else:
                            P.op("dve", lambda e, f2=f2, pb=pb, tt=tt: e.tensor_tensor(out=f2[:], in0=pb[:, :], in1=dec[:, tt, :], op=ALU.mult),
                                 r=[pbt, t_dec], w=[f2t])
                        P.op("pool", lambda e, f1=f1, f2=f2, tt=tt: e.tensor_tensor(out=hc[:, tt, 0, :], in0=f1[:], in1=f2[:], op=ALU.add),
                             r=[f1t, f2t], w=[t_hc])
                        P.op("pool", lambda e, f1=f1, f2=f2, tt=tt: e.tensor_tensor(out=hc[:, tt, 1, :], in0=f2[:], in1=f1[:], op=ALU.subtract),
                             r=[f1t, f2t], w=[t_hc])
                    P.barrier()
                with ExitStack() as es5:
                    pstr = Pool(nc, es5, "ps", 2, [128, 1024], BF16, "hp_t")
                    for tt in range(16):
                        pt, ptt = pstr.get()
                        for ct in range(4):
                            P.op("pe", lambda e, pt=pt, ct=ct, tt=tt: e.transpose(pt[:, ct * 128:(ct + 1) * 128],
                                                                                  zcur[:, ct, tt * 128:(tt + 1) * 128], ident_b[:]),
                                 r=[t_zcur, t_c], w=[ptt])
                        evac_copy(P, zt[:, tt, :], pt[:, 0:512], [ptt], [t_zt])
                    P.barrier()
                with ExitStack() as es5:
                    pspool = Pool(nc, es5, "ps", 8, [128, 512], F32, "hp_w")
                    cblk = Pool(nc, es5, "sb", 4, [128, 16 * 128], BF16, "h_cb")
                    hsb = Pool(nc, es5, "sb", 4, [128, 512], F32, "h_hsb")
                    ttmp = Pool(nc, es5, "sb", 4, [128, 512], F32, "h_tt")
                    for fc in range(NF):
                        cb, cbt = cblk.get()
                        sbk, sbt = cblk.get()
                        cbv = cb[:].rearrange("p (t f) -> p t f", f=128)
                        sbv = sbk[:].rearrange("p (t f) -> p t f", f=128)
                        P.dma("sp", cbv, cst["CC"][0:S, fc * 128:(fc + 1) * 128].rearrange("(t p) f -> p t f", p=128), w=[cbt])
                        P.dma("sp", sbv, cst["SS"][0:S, fc * 128:(fc + 1) * 128].rearrange("(t p) f -> p t f", p=128), w=[sbt])
                        pHr, pHrt = pspool.get()
                        pHi, pHit = pspool.get()
                        pZr, pZrt = pspool.get()
                        pZs, pZst = pspool.get()
                        for tc_ in range(16):
                            st_, sp_ = (tc_ == 0), (tc_ == 15)
                            P.op("pe", lambda e, pHr=pHr, cbv=cbv, tc_=tc_, st_=st_, sp_=sp_: e.matmul(pHr[:, :], cbv[:, tc_, :], hc[:, tc_, 0, :],
                                                                                                      start=st_, stop=sp_), r=[cbt, t_hc], w=[pHrt])
                            P.op("pe", lambda e, pHi=pHi, sbv=sbv, tc_=tc_, st_=st_, sp_=sp_: e.matmul(pHi[:, :], sbv[:, tc_, :], hc[:, tc_, 1, :],
                                                                                                      start=st_, stop=sp_), r=[sbt, t_hc], w=[pHit])
                            P.op("pe", lambda e, pZr=pZr, cbv=cbv, tc_=tc_, st_=st_, sp_=sp_: e.matmul(pZr[:, :], cbv[:, tc_, :], zt[:, tc_, :],
                                                                                                      start=st_, stop=sp_), r=[cbt, t_zt], w=[pZrt])
                            P.op("pe", lambda e, pZs=pZs, sbv=sbv, tc_=tc_, st_=st_, sp_=sp_: e.matmul(pZs[:, :], sbv[:, tc_, :], zt[:, tc_, :],
                                                                                                      start=st_, stop=sp_), r=[sbt, t_zt], w=[pZst])
                        hr, hrt = hsb.get()
                        hi, hit = hsb.get()
                        P.op("act", lambda e, hr=hr, pHr=pHr, fc=fc: e.activation(out=hr[:], in_=pHr[:, :], func=AF.Identity, scale=wN[:, fc:fc + 1]),
                             r=[pHrt, t_c], w=[hrt])
                        P.op("act", lambda e, hi=hi, pHi=pHi, fc=fc: e.activation(out=hi[:], in_=pHi[:, :], func=AF.Identity, scale=wN[:, fc:fc + 1]),
                             r=[pHit, t_c], w=[hit])
                        a1, a1t = ttmp.get()
                        a2, a2t = ttmp.get()
                        P.op("dve", lambda e, a1=a1, pZr=pZr, hr=hr: e.tensor_tensor(out=a1[:], in0=pZr[:, :], in1=hr[:], op=ALU.mult),
                             r=[pZrt, hrt], w=[a1t])
                        P.op("dve", lambda e, a2=a2, pZs=pZs, hi=hi: e.tensor_tensor(out=a2[:], in0=pZs[:, :], in1=hi[:], op=ALU.mult),
                             r=[pZst, hit], w=[a2t])
                        P.op("pool", lambda e, a1=a1, a2=a2, fc=fc: e.tensor_tensor(out=yy[:, fc, 0, :], in0=a1[:], in1=a2[:], op=ALU.add),
                             r=[a1t, a2t], w=[t_yy])
                        a3, a3t = ttmp.get()
                        a4, a4t = ttmp.get()
                        P.op("dve", lambda e, a3=a3, pZs=pZs, hr=hr: e.tensor_tensor(out=a3[:], in0=pZs[:, :], in1=hr[:], op=ALU.mult),
                             r=[pZst, hrt], w=[a3t])
                        P.op("dve", lambda e, a4=a4, pZr=pZr, hi=hi: e.tensor_tensor(out=a4[:], in0=pZr[:, :], in1=hi[:], op=ALU.mult),
                             r=[pZrt, hit], w=[a4t])
                        P.op("pool", lambda e, a3=a3, a4=a4, fc=fc: e.tensor_tensor(out=yy[:, fc, 1, :], in0=a3[:], in1=a4[:], op=ALU.subtract),
                             r=[a3t, a4t], w=[t_yy])
                    P.barrier()
                with ExitStack() as es5:
                    pspool = Pool(nc, es5, "ps", 8, [128, 512], F32, "hp_i")
                    rblk = Pool(nc, es5, "sb", 4, [128, 1024], BF16, "h_rb")
                    ev = Pool(nc, es5, "sb", 3, [128, 512], F32, "h_ev")
                    znew = xs[o + 1]
                    t_zn = t_xs[o + 1]
                    ost = Pool(nc, es5, "sb", 3, [128, 512], BF16, "h_ho")
                    for th in range(2):
                        accs = [pspool.get() for _ in range(8)]
                        for fc in range(NF):
                            rc, rct = rblk.get()
                            rs_, rst = rblk.get()
                            P.dma("sp", rc[:], cst["CC"][fc * 128:(fc + 1) * 128, th * 1024:(th + 1) * 1024], w=[rct])
                            P.dma("sp", rs_[:], cst["SS"][fc * 128:(fc + 1) * 128, th * 1024:(th + 1) * 1024], w=[rst])
                            for ct in range(4):
                                for tq in range(2):
                                    ps, ptr = accs[ct * 2 + tq]
                                    P.op("pe", lambda e, ps=ps, rc=rc, fc=fc, ct=ct, tq=tq: e.matmul(
                                        ps[:, :], yy[:, fc, 0, ct * 128:(ct + 1) * 128], rc[:, tq * 512:(tq + 1) * 512],
                                        start=(fc == 0), stop=False), r=[t_yy, rct], w=[ptr])
                                    P.op("pe", lambda e, ps=ps, rs_=rs_, fc=fc, ct=ct, tq=tq: e.matmul(
                                        ps[:, :], yy[:, fc, 1, ct * 128:(ct + 1) * 128], rs_[:, tq * 512:(tq + 1) * 512],
                                        start=False, stop=(fc == NF - 1)), r=[t_yy, rst], w=[ptr])
                        for ct in range(4):
                            for tq in range(2):
                                ps, ptr = accs[ct * 2 + tq]
                                t0 = th * 1024 + tq * 512
                                e_, e_t = ev.get()
                                j = 48 + o * 4 + ct
                                P.op("dve", lambda e, e_=e_, ps=ps, ct=ct, t0=t0, j=j: e.scalar_tensor_tensor(
                                    out=e_[:], in0=zcur[:, ct, t0:t0 + 512], scalar=cvec_sb[:, j:j + 1], in1=ps[:, :],
                                    op0=ALU.mult, op1=ALU.add), r=[ptr, t_zcur, t_c], w=[e_t])
                                if o == 0:
                                    P.op("pool", lambda e, e_=e_, ct=ct, t0=t0: e.tensor_tensor(
                                        out=znew[:, ct, t0:t0 + 512], in0=znew[:, ct, t0:t0 + 512], in1=e_[:], op=ALU.mult),
                                        r=[e_t, t_zn], w=[t_zn])
                                else:
                                    o_, o_t = ost.get()
                                    P.op("pool", lambda e, e_=e_, o_=o_, ct=ct, t0=t0: e.tensor_tensor(
                                        out=o_[:], in0=znew[:, ct, t0:t0 + 512], in1=e_[:], op=ALU.mult), r=[e_t, t_zn], w=[o_t])
                                    P.dma("sp", cdT[ct * 128:(ct + 1) * 128, t0:t0 + 512], o_[:], r=[o_t], w=[t_out])
                    P.barrier()
            zcur = xs[o + 1]
            t_zcur = t_xs[o + 1]
        P.barrier()


def cd_consts():
    c = {}
    L = S
    t01 = np.linspace(0.0, 1.0, L, dtype=np.float32)
    w = (2.0 * np.pi * np.arange(L, dtype=np.float32) / L).astype(np.float32)
    bands = np.linspace(1e-4, 15, 16, dtype=np.float32)
    z = np.concatenate([t01[:, None], np.cos(w[:, None] * bands), -np.sin(w[:, None] * bands)], -1).astype(np.float32)
    c["zfeatT"] = np.ascontiguousarray(z.T)
    n = NF * 128
    a = np.arange(n, dtype=np.int64)
    prod = (a[:, None] * a[None, :]) % 4096
    ang = prod.astype(np.float64) * (2.0 * np.pi / 4096)
    valid = (a[:, None] <= 2048) & (a[None, :] <= 2048)
    c["CC"] = np.where(valid, np.cos(ang), 0.0).astype(np.float32).astype(ml_dtypes.bfloat16)
    c["SS"] = np.where(valid, np.sin(ang), 0.0).astype(np.float32).astype(ml_dtypes.bfloat16)
    wf = np.where((a == 0) | (a == 2048), 1.0, 2.0) / 4096.0
    wf = np.where(a <= 2048, wf, 0.0)
    c["wN"] = np.ascontiguousarray(wf.reshape(NF, 128).T).astype(np.float32)
    c["t01n"] = np.ascontiguousarray((-t01).reshape(16, 128).T).astype(np.float32)
    m0 = np.ones((128, 1), dtype=np.float32)
    m0[0, 0] = 0.0
    c["mask0"] = m0
    t = np.arange(L, dtype=np.int64)
    pf = ((t[:, None] * t[None, :]) % L).astype(np.float64) * (2.0 * np.pi / L)
    c["FC"] = np.cos(pf).astype(np.float32).astype(ml_dtypes.bfloat16)
    c["FS"] = np.sin(pf).astype(np.float32).astype(ml_dtypes.bfloat16)
    k = np.arange(256, dtype=np.int64)
    pw = ((k[:, None] * k[None, :]) % 256).astype(np.float64) * (2.0 * np.pi / 256)
    sc = 1.0 / np.sqrt(float(L) * 256.0)
    c["CSW"] = np.concatenate([np.cos(pw) * sc, -np.sin(pw) * sc], axis=1).astype(np.float32).astype(ml_dtypes.bfloat16)
    return c


def cd_host_inputs(inp, m1, b, hf):
    def fm(v, n=16):
        return np.ascontiguousarray(v.reshape(n, 128).T)
    w = inp["cd_w_in"][0]
    cols = []
    for o in range(3):
        cols += list(range(o * 1024 + 512 * hf, o * 1024 + 512 * hf + 512))
    cols += list(range(3072 + 512 * hf, 3072 + 512 * hf + 512))
    cw = inp["cd_conv_w"][0]
    cb = inp["cd_conv_b"][0]
    cvec = np.zeros((128, 56), dtype=np.float32)
    for o in range(3):
        for ct in range(4):
            ch = o * 1024 + 512 * hf + ct * 128 + np.arange(128)
            j = (o * 4 + ct) * 4
            cvec[:, j] = cw[0, ch]
            cvec[:, j + 1] = cw[1, ch]
            cvec[:, j + 2] = cw[2, ch]
            cvec[:, j + 3] = cb[ch]
    sk = inp["cd_skip"][0]
    for o in range(2):
        for ct in range(4):
            cvec[:, 48 + o * 4 + ct] = sk[o, 512 * hf + ct * 128 + np.arange(128)]
    fw3 = inp["cd_filt_w3"][0]
    f3c = []
    for o in range(2):
        for dr in range(2):
            f3c += list(range(o * 2048 + dr * 1024 + 512 * hf, o * 2048 + dr * 1024 + 512 * hf + 512))
    fvec = np.stack([inp["cd_filt_b1"][0], inp["cd_filt_freq1"][0], inp["cd_filt_b2"][0], inp["cd_filt_freq2"][0]], axis=1)
    import math
    mn = math.log(1e-2) / 1.5
    mx = math.log(1e-2) / 0.3
    deltas = np.abs(np.linspace(mn, mx, 1024, dtype=np.float32))[512 * hf:512 * hf + 512]
    mb = m1[b]
    return {"modv": np.concatenate([fm(mb[0:2048]), fm(mb[2048:4096])], axis=1), "w_in_c": np.ascontiguousarray(w[:, cols]),
            "cvec": cvec, "fw1": np.ascontiguousarray(inp["cd_filt_w1"][0]), "fw2": np.ascontiguousarray(inp["cd_filt_w2"][0]),
            "fvec": np.ascontiguousarray(fvec.astype(np.float32)), "fw3_c": np.ascontiguousarray(fw3[:, f3c]),
            "delta_b": np.tile(deltas[None, :], (128, 1)).astype(np.float32)}


def build_cd_prog():
    nc = bass.Bass("TRN2", target_bir_lowering=False)
    consts = make_consts()
    consts.update(cd_consts())
    with ExitStack() as es:
        P = Prog(nc, es)
        cst = declare_consts(nc, consts)
        hT = dram_in(nc, "hT", [D, S])
        modv = dram_in(nc, "modv", [128, 32])
        w_in_c = dram_in(nc, "w_in_c", [D, 2048])
        cvec = dram_in(nc, "cvec", [128, 56])
        fw1 = dram_in(nc, "fw1", [33, 64])
        fw2 = dram_in(nc, "fw2", [64, 64])
        fvec = dram_in(nc, "fvec", [64, 4])
        fw3_c = dram_in(nc, "fw3_c", [64, 2048])
        delta_b = dram_in(nc, "delta_b", [128, 512])
        cdT = dram_out(nc, "cdT", [1024, S], BF16)
        stage_cd(P, cst, hT, modv, w_in_c, cvec, fw1, fw2, fvec, fw3_c, delta_b, cdT, Tr(), Tr())
        P.barrier()
    return nc, consts


ADA_BF16 = True


def stage_ada(P, csT, adaw, adab, mT, t_out):
    nc = P.nc
    with ExitStack() as es:
        cs = es.enter_context(SB(nc, "d_cs", [128, 80], F32))
        sT = es.enter_context(SB(nc, "d_sT", [128, 80], F32))
        bia = es.enter_context(SB(nc, "d_b", [128, 24], F32))
        res = es.enter_context(SB(nc, "d_res", [128, 120], F32))
        t_c = Tr()
        t_res = Tr()
        P.dma("sp", cs[:], csT[:, :], w=[t_c])
        P.dma("sp", bia[:], adab[:, :], w=[t_c])
        sTb = es.enter_context(SB(nc, "d_sTb", [128, 80], BF16))
        P.op("act", lambda e: e.activation(out=sT[:], in_=cs[:], func=AF.Silu), r=[t_c], w=[t_c])
        P.op("dve", lambda e: e.tensor_copy(out=sTb[:], in_=sT[:]), r=[t_c], w=[t_c])
        wb = Pool(nc, es, "sb", 3, [128, 1536], BF16 if ADA_BF16 else F32, "d_w")
        ps = [es.enter_context(PSM(nc, "dp_%d" % i, [128, 512], F32)) for i in range(2)]
        t_ps = [Tr(), Tr()]
        for l in range(2):
            for k in range(16):
                w_, w_t = wb.get()
                P.dma("pool" if ADA_BF16 else "sp", w_[:], adaw[l, k * 128:(k + 1) * 128, :], w=[w_t])
                for tl in range(12):
                    P.op("pe", lambda e, w_=w_, l=l, k=k, tl=tl: e.matmul(ps[l][:, tl * 5:(tl + 1) * 5], w_[:, tl * 128:(tl + 1) * 128],
                                                                          (sTb if ADA_BF16 else sT)[:, k * 5:(k + 1) * 5], start=(k == 0 and tl == 0),
                                                                          stop=(k == 15), skip_group_check=True),
                         r=[w_t, t_c], w=[t_ps[l]])
            P.op("dve", lambda e, l=l: e.tensor_tensor(out=res[:, l * 60:(l + 1) * 60].rearrange("p (t j) -> p t j", j=5),
                                                       in0=ps[l][:, 0:60].rearrange("p (t j) -> p t j", j=5),
                                                       in1=bia[:, l * 12:(l + 1) * 12].unsqueeze(2).to_broadcast([128, 12, 5]), op=ALU.add),
                 r=[t_ps[l], t_c], w=[t_res])
        P.dma("sp", mT[:, :], res[:], r=[t_res], w=[t_out])
        P.barrier()


def _new():
    return bass.Bass("TRN2", target_bir_lowering=False)


def build_ada_prog():
    nc = _new()
    with ExitStack() as es:
        P = Prog(nc, es)
        csT = dram_in(nc, "csT", [128, 80])
        adaw = dram_in(nc, "adaw", [2, D, 1536])
        adab = dram_in(nc, "adab", [128, 24])
        mT = dram_out(nc, "mT", [128, 120])
        stage_ada(P, csT, adaw, adab, mT, Tr())
    return nc, {}


def build_route_prog():
    nc = _new()
    consts = make_consts()
    with ExitStack() as es:
        P = Prog(nc, es)
        cst = declare_consts(nc, consts)
        hT = dram_in(nc, "hT", [D, S])
        modv = dram_in(nc, "modv", [128, 32])
        router = dram_in(nc, "router", [D, 16])
        xe_out = dram_out(nc, "xe_out", [8, 128, 16 * CAP], BF16)
        pos_out = dram_out(nc, "pos_out", [16, S])
        gate_out = dram_out(nc, "gate_out", [16, S])
        stage_moe_route(P, cst, hT, modv, router, xe_out, pos_out, gate_out, Tr(), Tr())
    return nc, consts


def build_ffn_prog():
    nc = _new()
    with ExitStack() as es:
        P = Prog(nc, es)
        xe_in = dram_in(nc, "xe_in", [2, 4, 128, 16 * CAP], BF16)
        w1 = dram_in(nc, "w1", [2, D, FF])
        w3 = dram_in(nc, "w3", [2, D, FF])
        w2 = dram_in(nc, "w2", [2, FF, D])
        ye_out = dram_out(nc, "ye_out", [2, 8, 128, D], BF16)
        stage_moe_ffn(P, xe_in, w1, w3, w2, ye_out, Tr(), Tr())
    return nc, {}


def build_scat_prog():
    nc = _new()
    consts = make_consts()
    with ExitStack() as es:
        P = Prog(nc, es)
        cst = declare_consts(nc, consts)
        ye_in = dram_in(nc, "ye_in", [16, 2, 128, D], BF16)
        pos_in = dram_in(nc, "pos_in", [16, 1024])
        gate_in = dram_in(nc, "gate_in", [16, 1024])
        hT = dram_in(nc, "hT", [D, 1024])
        vecs = dram_in(nc, "vecs", [128, 48])
        outT = dram_out(nc, "outT", [D, 1024])
        stage_scatter_ln(P, cst, ye_in, pos_in, gate_in, hT, vecs, outT, Tr(), Tr())
    return nc, consts


def build_oproj_prog():
    nc = _new()
    consts = make_consts()
    with ExitStack() as es:
        P = Prog(nc, es)
        cst = declare_consts(nc, consts)
        ao_in = dram_in(nc, "ao_in", [2048, 1024], BF16)
        w_out = dram_in(nc, "w_out", [2048, D])
        hT = dram_in(nc, "hT", [D, 1024])
        vecs = dram_in(nc, "vecs", [128, 48])
        outT = dram_out(nc, "outT", [D, 1024])
        stage_oproj_ln(P, cst, ao_in, w_out, hT, vecs, outT, Tr(), Tr())
    return nc, consts


def _fm(v, n=16):
    return np.ascontiguousarray(np.asarray(v, dtype=np.float32).reshape(n, 128).T)


def _run(nc, consts, in_maps):
    for im in in_maps:
        for k, v in consts.items():
            im["c_" + k] = v
    res = run_bass_kernel_spmd(nc, in_maps, core_ids=list(range(len(in_maps))))
    return res.results


DEBUG = {}


def _dbg(name, arr):
    if "ref" in DEBUG:
        ref = DEBUG["ref"][name]
        err = float(np.sqrt(((arr - ref) ** 2).mean() / (ref ** 2).mean()))
        print("DBG %s relerr %.5f" % (name, err), flush=True)


def kernel_unfused(**inp):
    inp = {k: np.asarray(v) for k, v in inp.items()}
    progs = {}

    def prog(name, fn):
        if name not in progs:
            progs[name] = fn()
        return progs[name]

    nc, consts = prog("ada", build_ada_prog)
    cvs = np.concatenate([inp["c"], inp["c_ctx"][None, :]], axis=0)
    csT = np.ascontiguousarray(cvs.reshape(5, 16, 128).transpose(2, 1, 0).reshape(128, 80))
    ims = []
    for c in range(8):
        ab = inp["ada_b"][:, c * 1536:(c + 1) * 1536].reshape(2, 12, 128)
        ims.append({"csT": csT, "adaw": np.ascontiguousarray(inp["ada_w"][:, :, c * 1536:(c + 1) * 1536]),
                    "adab": np.ascontiguousarray(ab.transpose(2, 0, 1).reshape(128, 24))})
    r = _run(nc, consts, ims)
    m = np.zeros((2, 5, 12288), dtype=np.float32)
    for c in range(8):
        o = np.asarray(r[c]["mT"]).reshape(128, 2, 12, 5)
        m[:, :, c * 1536:(c + 1) * 1536] = o.transpose(1, 3, 2, 0).reshape(2, 5, 1536)
    if "ref" in DEBUG:
        _dbg("m0", m[0, 0:4])
        _dbg("m1", m[1, 0:4])

    def vecs(l, b, which):
        mb = m[l, b]
        g = mb[2 * D:3 * D] if which == 1 else mb[5 * D:6 * D]
        lg = inp["ln1_g" if which == 1 else "ln2_g"][l]
        lb = inp["ln1_b" if which == 1 else "ln2_b"][l]
        return np.concatenate([_fm(g), _fm(lg), _fm(lb)], axis=1)

    def oproj(ao_full, w_out, hT_halves, l):
        nc, consts = prog("oproj", build_oproj_prog)
        ims = []
        for core in range(8):
            b, th = core // 2, core % 2
            ims.append({"ao_in": np.ascontiguousarray(ao_full[b][:, th * 1024:(th + 1) * 1024]), "w_out": w_out,
                        "hT": hT_halves[core], "vecs": vecs(l, b, 1)})
        r = _run(nc, consts, ims)
        return [np.asarray(r[c]["outT"]) for c in range(8)]

    def moe(hT_halves, l):
        hfull = [np.ascontiguousarray(np.concatenate([hT_halves[2 * b], hT_halves[2 * b + 1]], axis=1)) for b in range(4)]
        nc, consts = prog("route", build_route_prog)
        ims = []
        perms = []
        for core in range(8):
            b, hf = core // 2, core % 2
            perm = np.r_[np.arange(8 * hf, 8 * hf + 8), np.arange(8 * (1 - hf), 8 * (1 - hf) + 8)]
            perms.append(perm)
            mb = m[l, b]
            ims.append({"hT": hfull[b], "modv": np.concatenate([_fm(mb[3 * D:4 * D]), _fm(mb[4 * D:5 * D])], axis=1),
                        "router": np.ascontiguousarray(inp["router"][l][:, perm])})
        rr = _run(nc, consts, ims)
        nc, consts = prog("ffn", build_ffn_prog)
        ims = []
        for c in range(8):
            xe = np.stack([np.stack([np.asarray(rr[2 * b + (2 * c + i) // 8]["xe_out"])[(2 * c + i) % 8] for b in range(4)]) for i in range(2)])
            ims.append({"xe_in": xe, "w1": inp["exp_w1"][l, 2 * c:2 * c + 2], "w3": inp["exp_w3"][l, 2 * c:2 * c + 2],
                        "w2": inp["exp_w2"][l, 2 * c:2 * c + 2]})
        rf = _run(nc, consts, ims)
        nc, consts = prog("scat", build_scat_prog)
        ims = []
        for core in range(8):
            b, th = core // 2, core % 2
            perm = perms[core]
            ye = np.stack([np.asarray(rf[e // 2]["ye_out"])[e % 2, 2 * b:2 * b + 2] for e in perm])
            ims.append({"ye_in": ye, "pos_in": np.ascontiguousarray(np.asarray(rr[core]["pos_out"])[:, th * 1024:(th + 1) * 1024]),
                        "gate_in": np.ascontiguousarray(np.asarray(rr[core]["gate_out"])[:, th * 1024:(th + 1) * 1024]),
                        "hT": hT_halves[core], "vecs": vecs(l, b, 2)})
        rs = _run(nc, consts, ims)
        return [np.asarray(rs[c]["outT"]) for c in range(8)]

    def dbg_h(name, halves):
        if "ref" in DEBUG:
            full = np.stack([np.concatenate([halves[2 * b], halves[2 * b + 1]], axis=1).T for b in range(4)])
            _dbg(name, full)

    nc, consts = prog("attn", build_attn_prog)
    ims = [attn_host_inputs(inp, m[0], core // 2, core % 2) for core in range(8)]
    ra = _run(nc, consts, ims)
    ao_full = []
    for b in range(4):
        a0 = np.asarray(ra[2 * b]["aoT"])
        a1 = np.asarray(ra[2 * b + 1]["aoT"])
        ao_full.append(np.concatenate([a0[0:512], a1[0:512], a0[512:1024], a1[512:1024]], axis=0))
    xT_halves = [np.ascontiguousarray(inp["x"][core // 2][(core % 2) * 1024:(core % 2 + 1) * 1024].T) for core in range(8)]
    h = oproj(ao_full, np.ascontiguousarray(inp["ab_w_out"][0]), xT_halves, 0)
    dbg_h("h0a", h)
    h = moe(h, 0)
    dbg_h("h0b", h)
    nc, consts = prog("cd", build_cd_prog)
    ims = []
    for core in range(8):
        b, hf = core // 2, core % 2
        im = cd_host_inputs(inp, m[1], b, hf)
        im["hT"] = np.ascontiguousarray(np.concatenate([h[2 * b], h[2 * b + 1]], axis=1))
        ims.append(im)
    rc = _run(nc, consts, ims)
    cd_full = []
    for b in range(4):
        a0 = np.asarray(rc[2 * b]["cdT"])
        a1 = np.asarray(rc[2 * b + 1]["cdT"])
        cd_full.append(np.concatenate([a0[0:512], a1[0:512], a0[512:1024], a1[512:1024]], axis=0))
    h = oproj(cd_full, np.ascontiguousarray(inp["cd_w_out"][0]), h, 1)
    dbg_h("h1a", h)
    h = moe(h, 1)
    dbg_h("h1b", h)
    out = np.zeros((NB, S, D), dtype=np.float32)
    for core in range(8):
        b, th = core // 2, core % 2
        out[b, th * 1024:(th + 1) * 1024, :] = h[core].T
    return out


U32 = mybir.dt.uint32


def build_fused_prog():
    nc = _new()
    consts = make_consts()
    consts.update(cd_consts())
    with ExitStack() as es:
        P = Prog(nc, es)
        cst = declare_consts(nc, consts)
        I = {}

        def din(name, shape, dt=F32):
            I[name] = dram_in(nc, name, shape, dt)
            return I[name]
        csT = din("csT", [128, 80]); adaw = din("adaw", [2, D, 1536]); adab = din("adab", [128, 24]); oh5 = din("oh5", [128, 5])
        xT = din("xT", [D, S]); ctxT = din("ctxT", [D, LC]); w_in_r = din("w_in_r", [D, 3328]); kvn = din("kvn", [128, 4])
        w_ukv_r = din("w_ukv_r", [512, 1024]); cosT = din("cosT", [128, S]); sinT = din("sinT", [128, S])
        na_T = din("na_T", [128, 4 * TB_N * 64]); na_RM = din("na_RM", [128, 256])
        xown = din("xown", [D, 1024]); w_out0 = din("w_out0", [2048, D]); w_out1 = din("w_out1", [2048, D])
        lnv = din("lnv", [128, 128])
        router = [din("router0", [D, 16]), din("router1", [D, 16])]
        w1 = [din("w1_0", [2, D, FF]), din("w1_1", [2, D, FF])]
        w3 = [din("w3_0", [2, D, FF]), din("w3_1", [2, D, FF])]
        w2 = [din("w2_0", [2, FF, D]), din("w2_1", [2, FF, D])]
        w_in_c = din("w_in_c", [D, 2048]); cvec = din("cvec", [128, 56]); fw1 = din("fw1", [33, 64]); fw2 = din("fw2", [64, 64])
        fvec = din("fvec", [64, 4]); fw3_c = din("fw3_c", [64, 2048]); delta_b = din("delta_b", [128, 512])
        idx_ao = din("idx_ao", [128, 16], U32); idx_h = din("idx_h", [128, 32], U32); idx_xe = din("idx_xe", [128, 8], U32)
        idx_ye = din("idx_ye", [128, 64], U32); oh2 = din("oh2", [128, 2])
        outT = dram_out(nc, "outT", [D, 1024])
        idx_sb = es.enter_context(SB(nc, "x_idx", [128, 120], U32))
        oh2_sb = es.enter_context(SB(nc, "x_oh2", [128, 2], F32))
        t_pg = Tr()
        t_i = Tr()
        P.dma("sp", idx_sb[:, 0:16], idx_ao[:, :], w=[t_i])
        P.dma("sp", idx_sb[:, 16:48], idx_h[:, :], w=[t_i])
        P.dma("sp", idx_sb[:, 48:56], idx_xe[:, :], w=[t_i])
        P.dma("sp", idx_sb[:, 56:120], idx_ye[:, :], w=[t_i])
        P.dma("sp", oh2_sb[:], oh2[:, :], w=[t_i])
        IA, IH, IX, IY = 0, 16, 48, 56

        def ds(name, shape, dt=F32):
            return dram_scratch(nc, name, shape, dt)
        mT_d = ds("x_mT", [128, 120]); gat_m = ds("x_gm", [8 * 128, 120])
        mod_d = ds("x_mod", [128, 2 * 96]); ctx_d = ds("x_ctx", [128, 2 * 96])
        stage_ada(P, csT, adaw, adab, mT_d, Tr())
        P.barrier()
        P.allgather(mT_d[:, :], gat_m[:, :])
        P.barrier()
        with ExitStack() as es0:
            mall = es0.enter_context(SB(nc, "x_mall", [128, 8 * 120], F32))
            tmp = es0.enter_context(SB(nc, "x_mtmp", [128, 8 * 120], F32))
            msel = es0.enter_context(SB(nc, "x_msel", [128, 2 * 192], F32))
            oh5_sb = es0.enter_context(SB(nc, "x_oh5", [128, 5], F32))
            t_m = Tr()
            P.dma("sp", mall[:].rearrange("p (c x) -> p c x", x=120), gat_m.rearrange("(c p) x -> p c x", p=128), w=[t_m])
            P.dma("sp", oh5_sb[:], oh5[:, :], w=[t_m])
            P.op("dve", lambda e: e.tensor_tensor(out=tmp[:].rearrange("p (q j) -> p q j", j=5), in0=mall[:].rearrange("p (q j) -> p q j", j=5),
                                                  in1=oh5_sb[:].unsqueeze(1).to_broadcast([128, 192, 5]), op=ALU.mult), r=[t_m], w=[t_m])
            P.op("dve", lambda e: e.tensor_reduce(out=msel[:, 0:192], in_=tmp[:].rearrange("p (q j) -> p q j", j=5), axis=AX.X, op=ALU.add),
                 r=[t_m], w=[t_m])
            P.op("dve", lambda e: e.tensor_copy(out=msel[:, 192:384], in_=mall[:].rearrange("p (q j) -> p q j", j=5)[:, :, 4]), r=[t_m], w=[t_m])
            for l in range(2):
                for (src0, dst) in ((0, mod_d), (192, ctx_d)):
                    P.dma("sp", dst[:, l * 96:(l + 1) * 96].rearrange("p (c t) -> p c t", t=12),
                          msel[:, src0:src0 + 192].rearrange("p (c l t) -> p c l t", l=2, t=12)[:, :, l, :], r=[t_m], w=[t_m])
            P.barrier()

        def modp(l, a, b_):
            return mod_d[:, l * 96 + a:l * 96 + b_]

        def lnp(l, k):
            return lnv[:, (l * 4 + k) * 16:(l * 4 + k + 1) * 16]
        ao_d = ds("x_ao", [1024, S], BF16); gat_ao = ds("x_gao", [8 * 1024, S], BF16)
        stage_attn(P, cst, xT, ctxT, [modp(0, 0, 32), ctx_d[:, 0:32]], w_in_r, kvn, w_ukv_r, cosT, sinT, na_T, na_RM, ao_d, Tr())
        P.barrier()
        P.allgather(ao_d[:, :], gat_ao[:, :])
        P.barrier()

        def mk_load_ao(gat):
            rows = gat.rearrange("r (h t) -> (r h) t", h=2)

            def load_ao(dst, w):
                for c in range(16):
                    P.idma(dst[:, c, :], rows, idx_sb[:, IA + c:IA + c + 1], r=[t_i], w=w)
            return load_ao

        def mk_load_h(gat):
            def load_h(dst, c, w):
                for half in range(2):
                    P.idma(dst[:, half * 1024:(half + 1) * 1024], gat[:, :], idx_sb[:, IH + c * 2 + half:IH + c * 2 + half + 1], r=[t_i], w=w)
            return load_h

        def layer_moe(l, hA_d, h_out):
            gat_h = ds("x_gh%d" % l, [8 * D, 1024])
            P.allgather(hA_d[:, :], gat_h[:, :])
            P.barrier()
            xe_d = ds("x_xe%d" % l, [8, 128, 16 * CAP], BF16); gat_xe = ds("x_gxe%d" % l, [8 * 8 * 128, 16 * CAP], BF16)
            pos_d = ds("x_pos%d" % l, [16, S]); gate_d = ds("x_gate%d" % l, [16, S])
            stage_moe_route(P, cst, None, [modp(l, 48, 80)], router[l], xe_d, pos_d, gate_d, Tr(), Tr(), load_h=mk_load_h(gat_h))
            P.barrier()
            P.allgather(xe_d.rearrange("e p x -> (e p) x"), gat_xe[:, :])
            P.barrier()
            ye_d = ds("x_ye%d" % l, [2, 8, 128, D], BF16); gat_ye = ds("x_gye%d" % l, [8 * 16 * 128, D], BF16)
            gxe3 = gat_xe.rearrange("r (c s) -> r c s", s=CAP)

            def load_xe(dst, ex, b, w):
                P.idma(dst, gxe3, idx_sb[:, IX + ex * 4 + b:IX + ex * 4 + b + 1], r=[t_i], w=w)
            stage_moe_ffn(P, None, w1[l], w3[l], w2[l], ye_d, Tr(), Tr(), load_xe=load_xe)
            P.barrier()
            P.allgather(ye_d.rearrange("e g p d -> (e g p) d"), gat_ye[:, :])
            P.barrier()
            gye_rows = gat_ye.rearrange("r (h d) -> (r h) d", h=2)

            def load_ye(dst, q, dh, w):
                for st in range(2):
                    col = IY + (q * 2 + st) * 2 + dh
                    P.idma(dst[:, st, :], gye_rows, idx_sb[:, col:col + 1], r=[t_i], w=w)

            pgt = []

            def load_pg(pos, gate, w):
                for (src, dstp) in ((pos_d, pos), (gate_d, gate)):
                    P.dma("sp", pgt[0][:], src[:, :], w=[t_pg])
                    P.op("dve", lambda e, dstp=dstp: e.tensor_scalar(out=dstp, in0=pgt[0][:, 0:1024], scalar1=oh2_sb[0:16, 0:1], scalar2=None,
                                                                     op0=ALU.mult), r=[t_pg, t_i], w=w)
                    P.op("dve", lambda e, dstp=dstp: e.scalar_tensor_tensor(out=dstp, in0=pgt[0][:, 1024:2048], scalar=oh2_sb[0:16, 1:2],
                                                                            in1=dstp, op0=ALU.mult, op1=ALU.add), r=[t_pg, t_i], w=w)
            with ExitStack() as es_pg:
                pgt.append(es_pg.enter_context(SB(nc, "x_pgt", [16, S], F32)))
                stage_scatter_ln(P, cst, None, None, None, hA_d, [modp(l, 80, 96), lnp(l, 2), lnp(l, 3)], h_out, Tr(), Tr(),
                                 load_ye=load_ye, load_pg=load_pg)
                P.barrier()
                pgt.pop()

        hA0 = ds("x_hA0", [D, 1024])
        stage_oproj_ln(P, cst, None, w_out0, xown, [modp(0, 32, 48), lnp(0, 0), lnp(0, 1)], hA0, Tr(), Tr(), load_ao=mk_load_ao(gat_ao))
        P.barrier()
        hB0 = ds("x_hB0", [D, 1024])
        layer_moe(0, hA0, hB0)
        gat_hb = ds("x_ghb", [8 * D, 1024])
        P.allgather(hB0[:, :], gat_hb[:, :])
        P.barrier()
        cd_d = ds("x_cd", [1024, S], BF16); gat_cd = ds("x_gcd", [8 * 1024, S], BF16)
        stage_cd(P, cst, None, [modp(1, 0, 32)], w_in_c, cvec, fw1, fw2, fvec, fw3_c, delta_b, cd_d, Tr(), Tr(), load_h=mk_load_h(gat_hb))
        P.barrier()
        P.allgather(cd_d[:, :], gat_cd[:, :])
        P.barrier()
        hA1 = ds("x_hA1", [D, 1024])
        stage_oproj_ln(P, cst, None, w_out1, hB0, [modp(1, 32, 48), lnp(1, 0), lnp(1, 1)], hA1, Tr(), Tr(), load_ao=mk_load_ao(gat_cd))
        P.barrier()
        layer_moe(1, hA1, outT)
        P.barrier()
    return nc, consts


def fused_host_inputs(inp, consts):
    ims = []
    cvs = np.concatenate([inp["c"], inp["c_ctx"][None, :]], axis=0)
    csT = np.ascontiguousarray(cvs.reshape(5, 16, 128).transpose(2, 1, 0).reshape(128, 80))
    cd_in = {k: inp[k] for k in inp if k.startswith("cd_")}
    m_dummy = np.zeros((5, 12288), dtype=np.float32)
    p = np.arange(128)
    for core in range(8):
        b, hf = core // 2, core % 2
        im = {}
        ab = inp["ada_b"][:, core * 1536:(core + 1) * 1536].reshape(2, 12, 128)
        im["csT"] = csT
        im["adaw"] = np.ascontiguousarray(inp["ada_w"][:, :, core * 1536:(core + 1) * 1536])
        im["adab"] = np.ascontiguousarray(ab.transpose(2, 0, 1).reshape(128, 24))
        oh5 = np.zeros((128, 5), dtype=np.float32); oh5[:, b] = 1.0
        im["oh5"] = oh5
        a = attn_host_inputs(inp, m_dummy, b, hf)
        a.pop("modv")
        im.update(a)
        im["xown"] = np.ascontiguousarray(inp["x"][b][hf * 1024:(hf + 1) * 1024].T)
        im["w_out0"] = np.ascontiguousarray(inp["ab_w_out"][0])
        im["w_out1"] = np.ascontiguousarray(inp["cd_w_out"][0])
        lnv = np.zeros((128, 128), dtype=np.float32)
        for l in range(2):
            for k, nm in enumerate(("ln1_g", "ln1_b", "ln2_g", "ln2_b")):
                lnv[:, (l * 4 + k) * 16:(l * 4 + k + 1) * 16] = _fm(inp[nm][l])
        im["lnv"] = lnv
        perm = np.r_[np.arange(8 * hf, 8 * hf + 8), np.arange(8 * (1 - hf), 8 * (1 - hf) + 8)]
        for l in range(2):
            im["router%d" % l] = np.ascontiguousarray(inp["router"][l][:, perm])
            im["w1_%d" % l] = inp["exp_w1"][l, 2 * core:2 * core + 2]
            im["w3_%d" % l] = inp["exp_w3"][l, 2 * core:2 * core + 2]
            im["w2_%d" % l] = inp["exp_w2"][l, 2 * core:2 * core + 2]
        cdh = cd_host_inputs(cd_in, np.zeros((4, 12288), dtype=np.float32), b, hf)
        cdh.pop("modv")
        im.update(cdh)
        idx_ao = np.zeros((128, 16), dtype=np.uint32)
        for c in range(16):
            part = c // 4
            rank = 2 * b + (part % 2)
            row = (part // 2) * 512 + (c % 4) * 128 + p
            idx_ao[:, c] = (rank * 1024 + row) * 2 + hf
        idx_h = np.zeros((128, 32), dtype=np.uint32)
        for c in range(16):
            for half in range(2):
                idx_h[:, c * 2 + half] = (2 * b + half) * D + c * 128 + p
        idx_xe = np.zeros((128, 8), dtype=np.uint32)
        for ex in range(2):
            e = 2 * core + ex
            for bb in range(4):
                rank = 2 * bb + e // 8
                idx_xe[:, ex * 4 + bb] = (rank * 8 + e % 8) * 128 + p
        idx_ye = np.zeros((128, 64), dtype=np.uint32)
        for q in range(16):
            e = perm[q]
            for st in range(2):
                row = ((e // 2) * 2 + e % 2) * 8 + (2 * b + st)
                for dh in range(2):
                    idx_ye[:, (q * 2 + st) * 2 + dh] = (row * 128 + p) * 2 + dh
        oh2 = np.zeros((128, 2), dtype=np.float32); oh2[:, hf] = 1.0
        im.update({"idx_ao": idx_ao, "idx_h": idx_h, "idx_xe": idx_xe, "idx_ye": idx_ye, "oh2": oh2})
        ims.append(im)
    return ims


def kernel_fused(**inp):
    inp = {k: np.asarray(v) for k, v in inp.items()}
    nc, consts = build_fused_prog()
    ims = fused_host_inputs(inp, consts)
    r = _run(nc, consts, ims)
    out = np.zeros((NB, S, D), dtype=np.float32)
    for core in range(8):
        b, th = core // 2, core % 2
        out[b, th * 1024:(th + 1) * 1024, :] = np.asarray(r[core]["outT"]).T
    return out


def build_solo_prog(l0_only=False):
    nc = _new()
    consts = make_consts()
    consts.update(cd_consts())
    with ExitStack() as es:
        P = Prog(nc, es)
        cst = declare_consts(nc, consts)

        def din(name, shape, dt=F32):
            return dram_in(nc, name, shape, dt)

        def ds(name, shape, dt=F32):
            return dram_scratch(nc, name, shape, dt)
        csT = din("csT", [128, 80]); adaw = din("adaw", [2, D, 12288]); adab = din("adab", [128, 8 * 24]); oh5 = din("oh5", [128, 5])
        xT = din("xT", [D, S]); ctxT = din("ctxT", [D, LC]); kvn = din("kvn", [128, 4])
        w_in_r = [din("w_in_r%d" % h, [D, 3328]) for h in range(2)]
        w_ukv_r = [din("w_ukv_r%d" % h, [512, 1024]) for h in range(2)]
        na_T = [din("na_T%d" % h, [128, 4 * TB_N * 64]) for h in range(2)]
        cosT = din("cosT", [128, S]); sinT = din("sinT", [128, S]); na_RM = din("na_RM", [128, 256])
        w_out = [din("w_out0", [2048, D]), din("w_out1", [2048, D])]
        lnv = din("lnv", [128, 128])
        router = [din("router%d" % l, [D, 16]) for l in range(2)]
        nl = 1 if l0_only else 2
        w1 = [din("w1_%d" % l, [16, D, FF]) for l in range(nl)]
        w3 = [din("w3_%d" % l, [16, D, FF]) for l in range(nl)]
        w2 = [din("w2_%d" % l, [16, FF, D]) for l in range(nl)]
        w_in_c = [din("w_in_c%d" % h, [D, 2048]) for h in range(2)]
        cvec = [din("cvec%d" % h, [128, 56]) for h in range(2)]
        fw3_c = [din("fw3_c%d" % h, [64, 2048]) for h in range(2)]
        delta_b = [din("delta_b%d" % h, [128, 512]) for h in range(2)]
        fw1 = din("fw1", [33, 64]); fw2 = din("fw2", [64, 64]); fvec = din("fvec", [64, 4])
        outT = dram_out(nc, "outT", [D, S])
        m_d = ds("y_m", [8 * 128, 120])
        for sl in range(8):
            stage_ada(P, csT, adaw[:, :, sl * 1536:(sl + 1) * 1536], adab[:, sl * 24:(sl + 1) * 24], m_d[sl * 128:(sl + 1) * 128, :], Tr())
            P.barrier()
        mod_d = ds("y_mod", [128, 2 * 96]); ctx_d = ds("y_ctx", [128, 2 * 96])
        with ExitStack() as es0:
            mall = es0.enter_context(SB(nc, "y_mall", [128, 8 * 120], F32))
            tmp = es0.enter_context(SB(nc, "y_mtmp", [128, 8 * 120], F32))
            msel = es0.enter_context(SB(nc, "y_msel", [128, 2 * 192], F32))
            oh5_sb = es0.enter_context(SB(nc, "y_oh5", [128, 5], F32))
            t_m = Tr()
            P.dma("sp", mall[:].rearrange("p (c x) -> p c x", x=120), m_d.rearrange("(c p) x -> p c x", p=128), w=[t_m])
            P.dma("sp", oh5_sb[:], oh5[:, :], w=[t_m])
            P.op("dve", lambda e: e.tensor_tensor(out=tmp[:].rearrange("p (q j) -> p q j", j=5), in0=mall[:].rearrange("p (q j) -> p q j", j=5),
                                                  in1=oh5_sb[:].unsqueeze(1).to_broadcast([128, 192, 5]), op=ALU.mult), r=[t_m], w=[t_m])
            P.op("dve", lambda e: e.tensor_reduce(out=msel[:, 0:192], in_=tmp[:].rearrange("p (q j) -> p q j", j=5), axis=AX.X, op=ALU.add),
                 r=[t_m], w=[t_m])
            P.op("dve", lambda e: e.tensor_copy(out=msel[:, 192:384], in_=mall[:].rearrange("p (q j) -> p q j", j=5)[:, :, 4]), r=[t_m], w=[t_m])
            for l in range(2):
                for (src0, dst) in ((0, mod_d), (192, ctx_d)):
                    P.dma("sp", dst[:, l * 96:(l + 1) * 96].rearrange("p (c t) -> p c t", t=12),
                          msel[:, src0:src0 + 192].rearrange("p (c l t) -> p c l t", l=2, t=12)[:, :, l, :], r=[t_m], w=[t_m])
            P.barrier()

        def modp(l, a, b_):
            return mod_d[:, l * 96 + a:l * 96 + b_]

        def lnp(l, k):
            return lnv[:, (l * 4 + k) * 16:(l * 4 + k + 1) * 16]

        def mk_load_ao(parts, th):
            def load_ao(dst, w):
                for c in range(16):
                    part = c // 4
                    src = parts[part % 2]
                    r0 = (part // 2) * 512 + (c % 4) * 128
                    P.dma("sp", dst[:, c, :], src[r0:r0 + 128, th * 1024:(th + 1) * 1024], w=w)
            return load_ao

        def mk_load_h(halves):
            def load_h(dst, c, w):
                for half in range(2):
                    P.dma("sp", dst[:, half * 1024:(half + 1) * 1024], halves[half][c * 128:(c + 1) * 128, :], w=w)
            return load_h

        def oproj_both(l, parts, resid_halves, name):
            outs = []
            for th in range(2):
                o = ds("y_%s%d" % (name, th), [D, 1024])
                stage_oproj_ln(P, cst, None, w_out[l], resid_halves[th], [modp(l, 32, 48), lnp(l, 0), lnp(l, 1)], o, Tr(), Tr(),
                               load_ao=mk_load_ao(parts, th))
                P.barrier()
                outs.append(o)
            return outs

        def moe_both(l, hA, out_halves):
            xe_d = ds("y_xe", [16, 128, 16 * CAP], BF16); pos_d = ds("y_pos", [16, S]); gate_d = ds("y_gate", [16, S])
            stage_moe_route16(P, cst, None, [modp(l, 48, 80)], router[l], xe_d, pos_d, gate_d, Tr(), Tr(), load_h=mk_load_h(hA))
            P.barrier()
            ye_d = ds("y_ye", [16, 2, 128, D], BF16)

            def load_xe(dst, ex, b, w):
                P.dma("sp", dst, xe_d[ex].rearrange("p (c s) -> p c s", s=CAP), w=w)
            stage_moe_ffn(P, None, w1[l], w3[l], w2[l], ye_d, Tr(), Tr(), load_xe=load_xe, nb=1, n_exp=16)
            P.barrier()
            for th in range(2):
                def load_ye(dst, q, dh, w):
                    P.dma("sp", dst, ye_d[q, :, :, dh * 1024:(dh + 1) * 1024].rearrange("s p d -> p s d"), w=w)

                def load_pg(pos_sb, gate_sb, w, th=th):
                    P.dma("sp", pos_sb, pos_d[:, th * 1024:(th + 1) * 1024], w=w)
                    P.dma("sp", gate_sb, gate_d[:, th * 1024:(th + 1) * 1024], w=w)
                stage_scatter_ln(P, cst, None, None, None, hA[th], [modp(l, 80, 96), lnp(l, 2), lnp(l, 3)], out_halves[th], Tr(), Tr(),
                                 load_ye=load_ye, load_pg=load_pg)
                P.barrier()

        ao = []
        for hf in range(2):
            a_d = ds("y_ao", [1024, S], BF16)
            stage_attn(P, cst, xT, ctxT, [modp(0, 0, 32), ctx_d[:, 0:32]], w_in_r[hf], kvn, w_ukv_r[hf], cosT, sinT, na_T[hf], na_RM, a_d, Tr())
            P.barrier()
            ao.append(a_d)
        hA0 = oproj_both(0, ao, [xT[:, 0:1024], xT[:, 1024:2048]], "hA0")
        if l0_only:
            moe_both(0, hA0, [outT[:, 0:1024], outT[:, 1024:2048]])
            P.barrier()
            return nc, consts
        hB0 = [ds("y_hB0_%d" % th, [D, 1024]) for th in range(2)]
        moe_both(0, hA0, hB0)
        cd = []
        for hf in range(2):
            c_d = ds("y_cd", [1024, S], BF16)
            stage_cd(P, cst, None, [modp(1, 0, 32)], w_in_c[hf], cvec[hf], fw1, fw2, fvec, fw3_c[hf], delta_b[hf], c_d, Tr(), Tr(),
                     load_h=mk_load_h(hB0))
            P.barrier()
            cd.append(c_d)
        hA1 = oproj_both(1, cd, hB0, "hA1")
        moe_both(1, hA1, [outT[:, 0:1024], outT[:, 1024:2048]])
        P.barrier()
    return nc, consts


def solo_host_inputs(inp):
    ims = []
    cvs = np.concatenate([inp["c"], inp["c_ctx"][None, :]], axis=0)
    csT = np.ascontiguousarray(cvs.reshape(5, 16, 128).transpose(2, 1, 0).reshape(128, 80))
    adab = np.concatenate([np.ascontiguousarray(inp["ada_b"][:, c * 1536:(c + 1) * 1536].reshape(2, 12, 128).transpose(2, 0, 1).reshape(128, 24))
                           for c in range(8)], axis=1)
    cd_in = {k: inp[k] for k in inp if k.startswith("cd_")}
    lnv = np.zeros((128, 128), dtype=np.float32)
    for l in range(2):
        for k, nm in enumerate(("ln1_g", "ln1_b", "ln2_g", "ln2_b")):
            lnv[:, (l * 4 + k) * 16:(l * 4 + k + 1) * 16] = _fm(inp[nm][l])
    zero5 = np.zeros((5, 12288), dtype=np.float32)
    per_b = {}
    for b in range(4):
        im = {"csT": csT, "adaw": inp["ada_w"], "adab": np.ascontiguousarray(adab), "lnv": lnv}
        oh5 = np.zeros((128, 5), dtype=np.float32); oh5[:, b] = 1.0
        im["oh5"] = oh5
        for hf in range(2):
            a = attn_host_inputs(inp, zero5, b, hf)
            for k in ("w_in_r", "w_ukv_r", "na_T"):
                im["%s%d" % (k, hf)] = a[k]
            if hf == 0:
                for k in ("xT", "ctxT", "kvn", "cosT", "sinT", "na_RM"):
                    im[k] = a[k]
            cdh = cd_host_inputs(cd_in, zero5[:4], b, hf)
            for k in ("w_in_c", "cvec", "fw3_c", "delta_b"):
                im["%s%d" % (k, hf)] = cdh[k]
            if hf == 0:
                for k in ("fw1", "fw2", "fvec"):
                    im[k] = cdh[k]
        for l in range(2):
            im["router%d" % l] = np.ascontiguousarray(inp["router"][l])
        im["w_out0"] = np.ascontiguousarray(inp["ab_w_out"][0])
        im["w_out1"] = np.ascontiguousarray(inp["cd_w_out"][0])
        for l in range(2):
            im["w1_%d" % l] = inp["exp_w1"][l]
            im["w3_%d" % l] = inp["exp_w3"][l]
            im["w2_%d" % l] = inp["exp_w2"][l]
        per_b[b] = im
    for b in range(4):
        ims.append(per_b[b])
    return ims


def kernel(**inp):
    inp = {k: np.asarray(v) for k, v in inp.items()}
    nc, consts = build_solo_prog()
    ims = solo_host_inputs(inp)
    r = _run(nc, consts, ims)
    out = np.zeros((NB, S, D), dtype=np.float32)
    for b in range(4):
        out[b] = np.asarray(r[b]["outT"]).T
    return out
```

```python
import numpy as np
from contextlib import ExitStack
import ml_dtypes
import concourse.bass as bass
import concourse.mybir as mybir
from concourse.bass_utils import run_bass_kernel_spmd

F32 = mybir.dt.float32
BF16 = mybir.dt.bfloat16
AF = mybir.ActivationFunctionType
ALU = mybir.AluOpType
AX = mybir.AxisListType

D = 2048
S = 2048
NB = 4
LC = 256
NE = 16
FF = 1408
CAP = 256
ALPHA = (2.0 * 2) ** 0.25
LN_EPS = 1e-6


_cnt = [0]


def SB(nc, name, shape, dt):
    _cnt[0] += 1
    return nc.sbuf_tensor("%s_u%d" % (name, _cnt[0]), shape, dt)


def PSM(nc, name, shape, dt):
    _cnt[0] += 1
    return nc.psum_tensor("%s_u%d" % (name, _cnt[0]), shape, dt)


def load_pieces(P, dst, pieces, w):
    if not isinstance(pieces, (list, tuple)):
        pieces = [pieces]
    off = 0
    for ap in pieces:
        n = ap.shape[-1]
        P.dma("sp", dst[:, off:off + n], ap, w=w)
        off += n


class Tr:
    __slots__ = ("w", "r")

    def __init__(self):
        self.w = None
        self.r = {}


class Prog:
    ENG = ("pe", "act", "dve", "pool", "sp")

    def __init__(self, nc, es):
        self.nc = nc
        self.eobj = {"pe": nc.tensor, "act": nc.scalar, "dve": nc.vector, "pool": nc.gpsimd, "sp": nc.sync}
        self.es = es
        self.gen = {e: 0 for e in ("pe", "act", "dve", "pool")}
        self.esem = {(e, 0): es.enter_context(nc.semaphore("s_" + e)) for e in ("pe", "act", "dve", "pool")}
        self.cnt = {e: 0 for e in self.gen}
        nds = {"sp": 40, "pool": 32, "act": 1}
        self.dsem = {e: [es.enter_context(nc.semaphore("d_%s%d" % (e, i))) for i in range(n)] for e, n in nds.items()}
        self.dcnt = {e: [0] * n for e, n in nds.items()}
        self.dnext = {e: 0 for e in nds}
        self.seen = {e: {} for e in self.ENG}

    def _sem(self, k):
        return self.esem[(k[1], k[2])] if k[0] == "e" else self.dsem[k[1]][k[2]]

    def _sync(self, eng, r, w, mykey, myval):
        need = {}
        for t in r:
            if t.w is not None and need.get(t.w[0], 0) < t.w[1]:
                need[t.w[0]] = t.w[1]
        for t in w:
            if t.w is not None and need.get(t.w[0], 0) < t.w[1]:
                need[t.w[0]] = t.w[1]
            for k, v in t.r.items():
                if need.get(k, 0) < v:
                    need[k] = v
        seen = self.seen[eng]
        e = self.eobj[eng]
        for k, v in need.items():
            if eng == "pe" and k[0] == "e" and k[1] == "pe":
                continue
            if seen.get(k, 0) < v:
                seen[k] = v
                e.wait_ge(self._sem(k), v)
        for t in r:
            if t.r.get(mykey, 0) < myval:
                t.r[mykey] = myval
        for t in w:
            t.w = (mykey, myval)
            t.r = {}

    def op(self, eng, fn, r=(), w=()):
        self.cnt[eng] += 1
        g = self.gen[eng]
        self._sync(eng, r, w, ("e", eng, g), self.cnt[eng])
        fn(self.eobj[eng]).then_inc(self.esem[(eng, g)], 1)

    def dma(self, eng, out, in_, r=(), w=()):
        i = self.dnext[eng]
        self.dnext[eng] = (i + 1) % len(self.dsem[eng])
        key = ("d", eng, i)
        prev = self.dcnt[eng][i]
        self.dcnt[eng][i] = prev + 16
        if prev > 0 and self.seen[eng].get(key, 0) < prev:
            self.seen[eng][key] = prev
            self.eobj[eng].wait_ge(self.dsem[eng][i], prev)
        self._sync(eng, r, w, key, prev + 16)
        self.eobj[eng].dma_start(out=out, in_=in_).then_inc(self.dsem[eng][i], 16)

    def idma(self, out, in_, idx, r=(), w=()):
        eng = "pool"
        i = self.dnext[eng]
        self.dnext[eng] = (i + 1) % len(self.dsem[eng])
        key = ("d", eng, i)
        prev = self.dcnt[eng][i]
        self.dcnt[eng][i] = prev + 16
        if prev > 0 and self.seen[eng].get(key, 0) < prev:
            self.seen[eng][key] = prev
            self.eobj[eng].wait_ge(self.dsem[eng][i], prev)
        self._sync(eng, r, w, key, prev + 16)
        self.nc.gpsimd.indirect_dma_start(out=out, out_offset=None, in_=in_,
                                          in_offset=bass.IndirectOffsetOnAxis(ap=idx, axis=0)).then_inc(self.dsem[eng][i], 16)

    def allgather(self, src, dst, r=(), w=()):
        eng = "pool"
        i = self.dnext[eng]
        self.dnext[eng] = (i + 1) % len(self.dsem[eng])
        key = ("d", eng, i)
        prev = self.dcnt[eng][i]
        self.dcnt[eng][i] = prev + 16
        if prev > 0 and self.seen[eng].get(key, 0) < prev:
            self.seen[eng][key] = prev
            self.eobj[eng].wait_ge(self.dsem[eng][i], prev)
        self._sync(eng, r, w, key, prev + 16)
        self.nc.gpsimd.collective_compute("AllGather", ALU.bypass, replica_groups=[list(range(8))], ins=[src],
                                          outs=[dst]).then_inc(self.dsem[eng][i], 16)

    def barrier(self):
        for eng in self.ENG:
            seen = self.seen[eng]
            e = self.eobj[eng]
            for k2, c in self.cnt.items():
                k = ("e", k2, self.gen[k2])
                if c > 0 and seen.get(k, 0) < c:
                    seen[k] = c
                    e.wait_ge(self.esem[(k2, self.gen[k2])], c)
            for q, lst in self.dcnt.items():
                for i, c in enumerate(lst):
                    k = ("d", q, i)
                    if c > 0 and seen.get(k, 0) < c:
                        seen[k] = c
                        e.wait_ge(self.dsem[q][i], c)
        for k2 in self.cnt:
            if self.cnt[k2] > 24000:
                self.gen[k2] += 1
                self.esem[(k2, self.gen[k2])] = self.es.enter_context(self.nc.semaphore("s_%s_g%d" % (k2, self.gen[k2])))
                self.cnt[k2] = 0


def make_consts():
    c = {}
    c["iota_row"] = np.tile(np.arange(256, dtype=np.float32)[None, :], (128, 1))
    pid = np.arange(128, dtype=np.float32)
    c["pidx"] = np.stack([pid, pid + 128], axis=1).astype(np.float32)
    c["ident_f"] = np.eye(128, dtype=np.float32)
    c["ident_b"] = np.eye(128, dtype=np.float32).astype(ml_dtypes.bfloat16)
    c["ones_f"] = np.ones((128, 128), dtype=np.float32)
    sel = np.zeros((16, 8, 128), dtype=np.float32)
    for e in range(8):
        sel[e, e, :] = 1.0
    c["selrow"] = sel
    thl = np.zeros((128, 16, 2), dtype=np.float32)
    thl[:, :, 0] = np.arange(16)[None, :]
    thl[:, :, 1] = np.arange(128)[:, None]
    c["tokhl"] = thl.reshape(128, 32).astype(ml_dtypes.bfloat16)
    sel16 = np.zeros((16, 16, 128), dtype=np.float32)
    for e in range(16):
        sel16[e, e, :] = 1.0
    c["selrow16"] = sel16
    return c


def stage_moe_route(P, cst, hT, modv, router, xe_out, pos_out, gate_out, t_in, t_out, load_h=None):
    nc = P.nc
    if load_h is None:
        def load_h(dst, c, w):
            P.dma("sp", dst, hT[c * 128:(c + 1) * 128, :], r=[t_in], w=w)
    NDC = D // 128
    NTT = S // 128
    NFT = FF // 128
    with ExitStack() as es:
        def sb(name, shape, dt):
            return es.enter_context(SB(nc, "m_" + name, shape, dt))

        def pst(name, shape, dt):
            return es.enter_context(PSM(nc, "mp_" + name, shape, dt))

        regA = sb("regA", [128, NTT * D], BF16)
        modv_sb = sb("modv", [128, 32], F32)
        sc1p = sb("sc1p", [128, 16], F32)
        router_sb = sb("router", [128, NDC * 16], F32)
        iota_row = sb("iota_row", [128, 256], F32)
        pidx = sb("pidx", [128, 2], F32)
        ident_f = sb("ident_f", [128, 128], F32)
        ident_b = sb("ident_b", [128, 128], BF16)
        ones_f = sb("ones_f", [128, 128], F32)
        selrow = sb("selrow", [16, 8 * 128], F32)
        t_c = Tr()
        load_pieces(P, modv_sb, modv, [t_c])
        P.dma("sp", router_sb[:].rearrange("p (k e) -> p k e", e=16), router.rearrange("(k p) e -> p k e", p=128), w=[t_c])
        P.dma("sp", iota_row[:], cst["iota_row"][:, :], w=[t_c])
        P.dma("sp", pidx[:], cst["pidx"][:, :], w=[t_c])
        P.dma("sp", ident_f[:], cst["ident_f"][:, :], w=[t_c])
        P.dma("sp", ident_b[:], cst["ident_b"][:, :], w=[t_c])
        P.dma("sp", ones_f[:], cst["ones_f"][:, :], w=[t_c])
        P.dma("sp", selrow[:].rearrange("p (e m) -> p e m", m=128), cst["selrow"][:, :, :], w=[t_c])
        P.op("dve", lambda e: e.tensor_scalar(out=sc1p[:], in0=modv_sb[:, 16:32], scalar1=1.0, scalar2=None, op0=ALU.add),
             r=[t_c], w=[t_c])

        u2tok = regA[:].rearrange("p (t d) -> p t d", d=D)
        pos = sb("pos", [16, S], F32)
        gate = sb("gate", [16, S], F32)
        posT = sb("posT", [128, NTT * 16], F32)
        with ExitStack() as es1:
            hbuf = [es1.enter_context(SB(nc, "m_hbuf%d" % i, [128, S], F32)) for i in range(2)]
            u2f = [es1.enter_context(SB(nc, "m_u2f%d" % i, [128, S], F32)) for i in range(2)]
            u2b = [es1.enter_context(SB(nc, "m_u2b%d" % i, [128, S], BF16)) for i in range(2)]
            psL = [es1.enter_context(PSM(nc, "mp_L%d" % i, [128, 512], F32)) for i in range(4)]
            psT = [es1.enter_context(PSM(nc, "mp_T%d" % i, [128, 1024], BF16)) for i in range(2)]
            max8 = es1.enter_context(SB(nc, "m_max8", [16, 8], F32))
            t_h = [Tr(), Tr()]
            t_uf = [Tr(), Tr()]
            t_ub = [Tr(), Tr()]
            t_L = Tr()
            t_T = [Tr(), Tr()]
            t_u2tok = Tr()
            nT = 0
            for c in range(NDC):
                i = c % 2
                load_h(hbuf[i][:], c, [t_h[i]])
                P.op("act", lambda e, i=i, c=c: e.activation(out=u2f[i][:], in_=hbuf[i][:], func=AF.Identity,
                                                             bias=modv_sb[:, c:c + 1], scale=sc1p[:, c:c + 1]),
                     r=[t_h[i], t_c], w=[t_uf[i]])
                for tq in range(4):
                    P.op("pe", lambda e, i=i, c=c, tq=tq: e.matmul(psL[tq][0:16, :], router_sb[:, c * 16:(c + 1) * 16],
                                                                   u2f[i][:, tq * 512:(tq + 1) * 512],
                                                                   start=(c == 0), stop=(c == NDC - 1)),
                         r=[t_uf[i], t_c], w=[t_L])
                P.op("dve", lambda e, i=i: e.tensor_copy(out=u2b[i][:], in_=u2f[i][:]), r=[t_uf[i]], w=[t_ub[i]])
                for g in range(4):
                    j = nT % 2
                    nT += 1
                    for k in range(4):
                        t = g * 4 + k
                        P.op("pe", lambda e, i=i, j=j, k=k, t=t: e.transpose(psT[j][:, k * 128:(k + 1) * 128],
                                                                             u2b[i][:, t * 128:(t + 1) * 128], ident_b[:]),
                             r=[t_ub[i], t_c], w=[t_T[j]])
                    P.op("dve" if g % 2 == 0 else "act",
                         (lambda e, j=j, g=g, c=c: e.tensor_copy(out=u2tok[:, g * 4:(g + 1) * 4, c * 128:(c + 1) * 128],
                                                                 in_=psT[j][:, 0:512].rearrange("p (k d) -> p k d", d=128)))
                         if g % 2 == 0 else
                         (lambda e, j=j, g=g, c=c: e.copy(out=u2tok[:, g * 4:(g + 1) * 4, c * 128:(c + 1) * 128],
                                                          in_=psT[j][:, 0:512].rearrange("p (k d) -> p k d", d=128))),
                         r=[t_T[j]], w=[t_u2tok])
            ex = hbuf[0]
            work = hbuf[1]
            t_ex = t_h[0]
            t_work = t_h[1]
            aff = u2f[0][0:16, :]
            mask = u2f[1][0:16, :]
            onesr = ex[0:16, :]
            t_aff = t_uf[0]
            t_mask = t_uf[1]
            t_ones = t_ex
            for tq in range(4):
                P.op("act", lambda e, tq=tq: e.activation(out=ex[0:16, tq * 512:(tq + 1) * 512], in_=psL[tq][0:16, :], func=AF.Exp),
                     r=[t_L], w=[t_ex])
            t_L2 = Tr()
            for tq in range(4):
                P.op("pe", lambda e, tq=tq: e.matmul(psL[tq][0:16, :], ones_f[0:16, 0:16], ex[0:16, tq * 512:(tq + 1) * 512],
                                                     start=True, stop=True), r=[t_ex, t_c, t_L], w=[t_L])
            for tq in range(4):
                P.op("dve", lambda e, tq=tq: e.reciprocal(out=work[0:16, tq * 512:(tq + 1) * 512], in_=psL[tq][0:16, :]),
                     r=[t_L], w=[t_work])
            P.op("dve", lambda e: e.tensor_tensor(out=aff, in0=ex[0:16, :], in1=work[0:16, :], op=ALU.mult),
                 r=[t_ex, t_work], w=[t_aff])
            P.op("dve", lambda e: e.tensor_copy(out=work[0:16, :], in_=aff), r=[t_aff], w=[t_work])
            t_m8 = Tr()
            for it in range(CAP // 8):
                P.op("dve", lambda e: e.max(out=max8[:], in_=work[0:16, :]), r=[t_work], w=[t_m8])
                P.op("dve", lambda e: e.match_replace(out=work[0:16, :], in_to_replace=max8[:], in_values=work[0:16, :],
                                                      imm_value=-1.0), r=[t_m8, t_work], w=[t_work])
            t_pos = Tr(); t_gate = Tr()
            P.op("pool", lambda e: e.memset(onesr, 1.0), w=[t_ones])
            P.op("dve", lambda e: e.tensor_tensor(out=mask, in0=work[0:16, :], in1=aff, op=ALU.not_equal),
                 r=[t_work, t_aff], w=[t_mask])
            P.op("dve", lambda e: e.tensor_tensor_scan(out=pos[:], data0=onesr, data1=mask, initial=0.0,
                                                       op0=ALU.mult, op1=ALU.add), r=[t_ones, t_mask], w=[t_pos])
            P.op("dve", lambda e: e.tensor_tensor(out=pos[:], in0=pos[:], in1=mask, op=ALU.mult), r=[t_mask, t_pos], w=[t_pos])
            P.op("dve", lambda e: e.tensor_scalar(out=pos[:], in0=pos[:], scalar1=-1.0, scalar2=None, op0=ALU.add),
                 r=[t_pos], w=[t_pos])
            P.op("dve", lambda e: e.tensor_tensor(out=gate[:], in0=aff, in1=mask, op=ALU.mult), r=[t_aff, t_mask], w=[t_gate])
            t_posT = Tr()
            psP = psL[0]
            for t in range(NTT):
                P.op("pe", lambda e, t=t: e.transpose(psP[:, t * 16:(t + 1) * 16], pos[:, t * 128:(t + 1) * 128], ident_f[0:16, 0:16]),
                     r=[t_pos, t_c, t_L], w=[t_L])
            P.op("dve", lambda e: e.tensor_copy(out=posT[:], in_=psP[:, 0:NTT * 16]), r=[t_L], w=[t_posT])
            P.barrier()
        regB = sb("regB", [128, 8 * NDC * CAP], BF16)
        xeT = regB[:].rearrange("p (e c s) -> p e c s", c=NDC, s=CAP)
        t_xeT = [Tr() for _ in range(8)]
        with ExitStack() as es2:
            selb = [es2.enter_context(SB(nc, "m_sel%d" % i, [128, NTT * CAP], BF16)) for i in range(2)]
            psG = [es2.enter_context(PSM(nc, "mp_G%d" % i, [128, 512], F32)) for i in range(4)]
            t_sel = [Tr(), Tr()]
            t_G = [Tr() for _ in range(4)]
            nG = 0
            for ex_i in range(8):
                i = ex_i % 2
                for t in range(NTT):
                    P.op("dve" if t % 2 == 0 else "pool",
                         lambda e, i=i, t=t, ex_i=ex_i: e.tensor_scalar(out=selb[i][:, t * CAP:(t + 1) * CAP], in0=iota_row[:],
                                                                        scalar1=posT[:, t * 16 + ex_i:t * 16 + ex_i + 1],
                                                                        scalar2=None, op0=ALU.is_equal),
                         r=[t_posT, t_c], w=[t_sel[i]])
                for dc in range(NDC):
                    g = nG % 4
                    nG += 1
                    for t in range(NTT):
                        P.op("pe", lambda e, g=g, t=t, dc=dc, i=i: e.matmul(psG[g][:, 0:CAP], u2tok[:, t, dc * 128:(dc + 1) * 128],
                                                                            selb[i][:, t * CAP:(t + 1) * CAP],
                                                                            start=(t == 0), stop=(t == NTT - 1)),
                             r=[t_u2tok, t_sel[i]], w=[t_G[g]])
                    if dc % 2 == 0:
                        P.op("act", lambda e, g=g, dc=dc, ex_i=ex_i: e.copy(out=xeT[:, ex_i, dc, :], in_=psG[g][:, 0:CAP]),
                             r=[t_G[g]], w=[t_xeT[ex_i]])
                    else:
                        P.op("dve", lambda e, g=g, dc=dc, ex_i=ex_i: e.tensor_copy(out=xeT[:, ex_i, dc, :], in_=psG[g][:, 0:CAP]),
                             r=[t_G[g]], w=[t_xeT[ex_i]])
            P.barrier()
        for ex_i in range(8):
            P.dma("sp", xe_out[ex_i], regB[:, ex_i * NDC * CAP:(ex_i + 1) * NDC * CAP], r=[t_xeT[ex_i]], w=[t_out])
        P.dma("sp", pos_out[:, :], pos[:], r=[t_pos], w=[t_out])
        P.dma("sp", gate_out[:, :], gate[:], r=[t_gate], w=[t_out])
        P.barrier()


def stage_moe_route16(P, cst, hT, modv, router, xe_out, pos_out, gate_out, t_in, t_out, load_h=None):
    nc = P.nc
    if load_h is None:
        def load_h(dst, c, w):
            P.dma("sp", dst, hT[c * 128:(c + 1) * 128, :], r=[t_in], w=w)
    NDC = D // 128
    NTT = S // 128
    NFT = FF // 128
    with ExitStack() as es:
        def sb(name, shape, dt):
            return es.enter_context(SB(nc, "m_" + name, shape, dt))

        def pst(name, shape, dt):
            return es.enter_context(PSM(nc, "mp_" + name, shape, dt))

        regA = sb("regA", [128, NTT * D], BF16)
        modv_sb = sb("modv", [128, 32], F32)
        sc1p = sb("sc1p", [128, 16], F32)
        router_sb = sb("router", [128, NDC * 16], F32)
        iota_row = sb("iota_row", [128, 256], F32)
        pidx = sb("pidx", [128, 2], F32)
        ident_f = sb("ident_f", [128, 128], F32)
        ident_b = sb("ident_b", [128, 128], BF16)
        ones_f = sb("ones_f", [128, 128], F32)
        selrow = sb("selrow", [16, 8 * 128], F32)
        t_c = Tr()
        load_pieces(P, modv_sb, modv, [t_c])
        P.dma("sp", router_sb[:].rearrange("p (k e) -> p k e", e=16), router.rearrange("(k p) e -> p k e", p=128), w=[t_c])
        P.dma("sp", iota_row[:], cst["iota_row"][:, :], w=[t_c])
        P.dma("sp", pidx[:], cst["pidx"][:, :], w=[t_c])
        P.dma("sp", ident_f[:], cst["ident_f"][:, :], w=[t_c])
        P.dma("sp", ident_b[:], cst["ident_b"][:, :], w=[t_c])
        P.dma("sp", ones_f[:], cst["ones_f"][:, :], w=[t_c])
        P.dma("sp", selrow[:].rearrange("p (e m) -> p e m", m=128), cst["selrow"][:, :, :], w=[t_c])
        P.op("dve", lambda e: e.tensor_scalar(out=sc1p[:], in0=modv_sb[:, 16:32], scalar1=1.0, scalar2=None, op0=ALU.add),
             r=[t_c], w=[t_c])

        u2tok = regA[:].rearrange("p (t d) -> p t d", d=D)
        pos = sb("pos", [16, S], F32)
        gate = sb("gate", [16, S], F32)
        posT = sb("posT", [128, NTT * 16], F32)
        with ExitStack() as es1:
            hbuf = [es1.enter_context(SB(nc, "m_hbuf%d" % i, [128, S], F32)) for i in range(2)]
            u2f = [es1.enter_context(SB(nc, "m_u2f%d" % i, [128, S], F32)) for i in range(2)]
            u2b = [es1.enter_context(SB(nc, "m_u2b%d" % i, [128, S], BF16)) for i in range(2)]
            psL = [es1.enter_context(PSM(nc, "mp_L%d" % i, [128, 512], F32)) for i in range(4)]
            psT = [es1.enter_context(PSM(nc, "mp_T%d" % i, [128, 1024], BF16)) for i in range(2)]
            max8 = es1.enter_context(SB(nc, "m_max8", [16, 8], F32))
            t_h = [Tr(), Tr()]
            t_uf = [Tr(), Tr()]
            t_ub = [Tr(), Tr()]
            t_L = Tr()
            t_T = [Tr(), Tr()]
            t_u2tok = Tr()
            nT = 0
            for c in range(NDC):
                i = c % 2
                load_h(hbuf[i][:], c, [t_h[i]])
                P.op("act", lambda e, i=i, c=c: e.activation(out=u2f[i][:], in_=hbuf[i][:], func=AF.Identity,
                                                             bias=modv_sb[:, c:c + 1], scale=sc1p[:, c:c + 1]),
                     r=[t_h[i], t_c], w=[t_uf[i]])
                for tq in range(4):
                    P.op("pe", lambda e, i=i, c=c, tq=tq: e.matmul(psL[tq][0:16, :], router_sb[:, c * 16:(c + 1) * 16],
                                                                   u2f[i][:, tq * 512:(tq + 1) * 512],
                                                                   start=(c == 0), stop=(c == NDC - 1)),
                         r=[t_uf[i], t_c], w=[t_L])
                P.op("dve", lambda e, i=i: e.tensor_copy(out=u2b[i][:], in_=u2f[i][:]), r=[t_uf[i]], w=[t_ub[i]])
                for g in range(4):
                    j = nT % 2
                    nT += 1
                    for k in range(4):
                        t = g * 4 + k
                        P.op("pe", lambda e, i=i, j=j, k=k, t=t: e.transpose(psT[j][:, k * 128:(k + 1) * 128],
                                                                             u2b[i][:, t * 128:(t + 1) * 128], ident_b[:]),
                             r=[t_ub[i], t_c], w=[t_T[j]])
                    P.op("dve" if g % 2 == 0 else "act",
                         (lambda e, j=j, g=g, c=c: e.tensor_copy(out=u2tok[:, g * 4:(g + 1) * 4, c * 128:(c + 1) * 128],
                                                                 in_=psT[j][:, 0:512].rearrange("p (k d) -> p k d", d=128)))
                         if g % 2 == 0 else
                         (lambda e, j=j, g=g, c=c: e.copy(out=u2tok[:, g * 4:(g + 1) * 4, c * 128:(c + 1) * 128],
                                                          in_=psT[j][:, 0:512].rearrange("p (k d) -> p k d", d=128))),
                         r=[t_T[j]], w=[t_u2tok])
            ex = hbuf[0]
            work = hbuf[1]
            t_ex = t_h[0]
            t_work = t_h[1]
            aff = u2f[0][0:16, :]
            mask = u2f[1][0:16, :]
            onesr = ex[0:16, :]
            t_aff = t_uf[0]
            t_mask = t_uf[1]
            t_ones = t_ex
            for tq in range(4):
                P.op("act", lambda e, tq=tq: e.activation(out=ex[0:16, tq * 512:(tq + 1) * 512], in_=psL[tq][0:16, :], func=AF.Exp),
                     r=[t_L], w=[t_ex])
            t_L2 = Tr()
            for tq in range(4):
                P.op("pe", lambda e, tq=tq: e.matmul(psL[tq][0:16, :], ones_f[0:16, 0:16], ex[0:16, tq * 512:(tq + 1) * 512],
                                                     start=True, stop=True), r=[t_ex, t_c, t_L], w=[t_L])
            for tq in range(4):
                P.op("dve", lambda e, tq=tq: e.reciprocal(out=work[0:16, tq * 512:(tq + 1) * 512], in_=psL[tq][0:16, :]),
                     r=[t_L], w=[t_work])
            P.op("dve", lambda e: e.tensor_tensor(out=aff, in0=ex[0:16, :], in1=work[0:16, :], op=ALU.mult),
                 r=[t_ex, t_work], w=[t_aff])
            P.op("dve", lambda e: e.tensor_copy(out=work[0:16, :], in_=aff), r=[t_aff], w=[t_work])
            t_m8 = Tr()
            for it in range(CAP // 8):
                P.op("dve", lambda e: e.max(out=max8[:], in_=work[0:16, :]), r=[t_work], w=[t_m8])
                P.op("dve", lambda e: e.match_replace(out=work[0:16, :], in_to_replace=max8[:], in_values=work[0:16, :],
                                                      imm_value=-1.0), r=[t_m8, t_work], w=[t_work])
            t_pos = Tr(); t_gate = Tr()
            P.op("pool", lambda e: e.memset(onesr, 1.0), w=[t_ones])
            P.op("dve", lambda e: e.tensor_tensor(out=mask, in0=work[0:16, :], in1=aff, op=ALU.not_equal),
                 r=[t_work, t_aff], w=[t_mask])
            P.op("dve", lambda e: e.tensor_tensor_scan(out=pos[:], data0=onesr, data1=mask, initial=0.0,
                                                       op0=ALU.mult, op1=ALU.add), r=[t_ones, t_mask], w=[t_pos])
            P.op("dve", lambda e: e.tensor_tensor(out=pos[:], in0=pos[:], in1=mask, op=ALU.mult), r=[t_mask, t_pos], w=[t_pos])
            P.op("dve", lambda e: e.tensor_scalar(out=pos[:], in0=pos[:], scalar1=-1.0, scalar2=None, op0=ALU.add),
                 r=[t_pos], w=[t_pos])
            P.op("dve", lambda e: e.tensor_tensor(out=gate[:], in0=aff, in1=mask, op=ALU.mult), r=[t_aff, t_mask], w=[t_gate])
            t_posT = Tr()
            psP = psL[0]
            for t in range(NTT):
                P.op("pe", lambda e, t=t: e.transpose(psP[:, t * 16:(t + 1) * 16], pos[:, t * 128:(t + 1) * 128], ident_f[0:16, 0:16]),
                     r=[t_pos, t_c, t_L], w=[t_L])
            P.op("dve", lambda e: e.tensor_copy(out=posT[:], in_=psP[:, 0:NTT * 16]), r=[t_L], w=[t_posT])
            P.barrier()
        u2_d = dram_scratch(nc, "m_u2d", [S, D], BF16)
        t_u2d = Tr()
        for t in range(NTT):
            P.dma("sp", u2_d[t * 128:(t + 1) * 128, :], u2tok[:, t, :], r=[t_u2tok], w=[t_u2d])
        with ExitStack() as es2:
            selb = [es2.enter_context(SB(nc, "m_sel%d" % i, [128, NTT * CAP], BF16)) for i in range(2)]
            tokhl = es2.enter_context(SB(nc, "m_tokhl", [128, 32], BF16))
            P.dma("sp", tokhl[:], cst["tokhl"][:, :], w=[t_c])
            psI = Pool(nc, es2, "ps", 2, [128, 512], F32, "mp_I")
            psX = Pool(nc, es2, "ps", 4, [128, 1024], BF16, "mp_X")
            xst = Pool(nc, es2, "sb", 2, [128, NDC * CAP], BF16, "m_xst")
            xtm = Pool(nc, es2, "sb", 4, [128, D], BF16, "m_xtm")
            idxf = Pool(nc, es2, "sb", 4, [128, 1], F32, "m_idxf")
            hlp = Pool(nc, es2, "sb", 4, [128, 2], F32, "m_hl")
            idxu = Pool(nc, es2, "sb", 4, [128, 1], mybir.dt.uint32, "m_idxu")
            t_sel = [Tr(), Tr()]
            posTv = posT[:].rearrange("p (t e) -> p t e", e=16)
            for ex_i in range(16):
                i = ex_i % 2
                P.op("dve", lambda e, i=i, ex_i=ex_i: e.tensor_tensor(
                    out=selb[i][:].rearrange("p (t s) -> p t s", s=CAP), in0=iota_row[:].unsqueeze(1).to_broadcast([128, NTT, CAP]),
                    in1=posTv[:, :, ex_i:ex_i + 1].to_broadcast([128, NTT, CAP]), op=ALU.is_equal), r=[t_posT, t_c], w=[t_sel[i]])
                xs_, xs_t = xst.get()
                xsv = xs_[:].rearrange("p (c s) -> p c s", s=CAP)
                for half in range(2):
                    pi, pit = psI.get()
                    for t in range(NTT):
                        P.op("pe", lambda e, pi=pi, t=t, half=half, i=i: e.matmul(
                            pi[:, 0:2], selb[i][:, t * CAP + half * 128:t * CAP + (half + 1) * 128], tokhl[:, t * 2:t * 2 + 2],
                            start=(t == 0), stop=(t == NTT - 1)), r=[t_sel[i], t_c], w=[pit])
                    f_, f_t = idxf.get()
                    u_, u_t = idxu.get()
                    hl_, hl_t = hlp.get()
                    P.op("act", lambda e, hl_=hl_, pi=pi: e.copy(out=hl_[:], in_=pi[:, 0:2]), r=[pit], w=[hl_t])
                    P.op("dve", lambda e, f_=f_, hl_=hl_: e.tensor_scalar(out=f_[:], in0=hl_[:, 0:1], scalar1=128.0, scalar2=hl_[:, 1:2],
                                                                          op0=ALU.mult, op1=ALU.add), r=[hl_t], w=[f_t])
                    P.op("dve", lambda e, f_=f_, u_=u_: e.tensor_copy(out=u_[:], in_=f_[:]), r=[f_t], w=[u_t])
                    x_, x_t = xtm.get()
                    P.idma(x_[:], u2_d[:, :], u_[:, 0:1], r=[u_t, t_u2d], w=[x_t])
                    for c0 in range(0, NDC, 8):
                        px, pxt = psX.get()
                        for k in range(8):
                            P.op("pe", lambda e, px=px, x_=x_, c0=c0, k=k: e.transpose(px[:, k * 128:(k + 1) * 128],
                                                                                      x_[:, (c0 + k) * 128:(c0 + k + 1) * 128], ident_b[:]),
                                 r=[x_t, t_c], w=[pxt])
                        evac_copy(P, xsv[:, c0:c0 + 8, half * 128:(half + 1) * 128], px[:, :].rearrange("p (k s) -> p k s", s=128),
                                  [pxt], [xs_t])
                P.dma("sp", xe_out[ex_i], xs_[:], r=[xs_t], w=[t_out])
            P.barrier()
        P.dma("sp", pos_out[:, :], pos[:], r=[t_pos], w=[t_out])
        P.dma("sp", gate_out[:, :], gate[:], r=[t_gate], w=[t_out])
        P.barrier()


def dram_in(nc, name, shape, dt=F32):
    return nc.dram_tensor(name, list(shape), dt, kind="ExternalInput").ap()


def dram_out(nc, name, shape, dt=F32):
    return nc.dram_tensor(name, list(shape), dt, kind="ExternalOutput").ap()


def declare_consts(nc, consts):
    out = {}
    for k, v in consts.items():
        dt = BF16 if v.dtype == ml_dtypes.bfloat16 else F32
        out[k] = dram_in(nc, "c_" + k, v.shape, dt)
    return out


_uid = [0]


class Pool:
    def __init__(self, nc, es, kind, n, shape, dt, name):
        mk = (lambda *a: SB(nc, *a)) if kind == "sb" else (lambda *a: PSM(nc, *a))
        _uid[0] += 1
        self.t = [es.enter_context(mk("%s_%d_%d" % (name, _uid[0], i), shape, dt)) for i in range(n)]
        self.tr = [Tr() for _ in range(n)]
        self.i = 0

    def get(self):
        i = self.i
        self.i = (i + 1) % len(self.t)
        return self.t[i], self.tr[i]


def dram_scratch(nc, name, shape, dt):
    _cnt[0] += 1
    return nc.dram_tensor("%s_u%d" % (name, _cnt[0]), list(shape), dt, kind="Internal").ap()


_alt = [0]


def evac_copy(P, out, in_, r, w):
    _alt[0] ^= 1
    if _alt[0]:
        P.op("act", lambda e: e.copy(out=out, in_=in_), r=r, w=w)
    else:
        P.op("dve", lambda e: e.tensor_copy(out=out, in_=in_), r=r, w=w)


def load_wgroup(P, wpool, wsrc, c0, ncols, nk=16):
    wt, wtr = wpool.get()
    view = wt[:, 0:nk * ncols].rearrange("p (k c) -> p k c", c=ncols)
    P.dma("pool", view, wsrc[:, c0:c0 + ncols].rearrange("(k p) c -> p k c", p=128), w=[wtr])
    return view, wtr


def proj_fm(P, pspool, wv, wtr, ft, xT, t_x, t0, n, nk=16):
    ps, ptr = pspool.get()
    for k in range(nk):
        P.op("pe", lambda e, k=k: e.matmul(ps[:, 0:n], wv[:, k, ft * 128:(ft + 1) * 128], xT[:, k, t0:t0 + n],
                                           start=(k == 0), stop=(k == nk - 1)), r=[wtr, t_x], w=[ptr])
    return ps, ptr


def proj_tm(P, pspool, wv, wtr, c0, ncols, xT, t_x, tt, nk=16):
    ps, ptr = pspool.get()
    for k in range(nk):
        P.op("pe", lambda e, k=k: e.matmul(ps[:, 0:ncols], xT[:, k, tt * 128:(tt + 1) * 128], wv[:, k, c0:c0 + ncols],
                                           start=(k == 0), stop=(k == nk - 1)), r=[wtr, t_x], w=[ptr])
    return ps, ptr


def ln_apply(P, es, z, t_z, T, g_sb, b_sb, t_gb, ones_f, t_c, outT, t_out, pspool, name):
    nc = P.nc
    sq = Pool(nc, es, "sb", 3, [128, 512], BF16, name + "_sq")
    zb = Pool(nc, es, "sb", 3, [128, 512], BF16, name + "_zb")
    ones_b = es.enter_context(SB(nc, name + "_1b", [128, 128], BF16))
    P.op("dve", lambda e: e.tensor_copy(out=ones_b[:], in_=ones_f[:]), r=[t_c], w=[t_c])
    st = Pool(nc, es, "sb", 2, [128, 4 * 512], F32, name + "_st")
    ot = Pool(nc, es, "sb", 3, [128, 512], F32, name + "_ot")
    for t0 in range(0, T, 512):
        p1, p1t = pspool.get()
        p2, p2t = pspool.get()
        for c in range(16):
            s, s_t = sq.get()
            P.op("act", lambda e, c=c, s=s: e.activation(out=s[:], in_=z[:, c, t0:t0 + 512], func=AF.Square), r=[t_z], w=[s_t])
            b_, b_t = zb.get()
            P.op("pool" if c % 2 else "dve", lambda e, c=c, b_=b_: e.tensor_copy(out=b_[:], in_=z[:, c, t0:t0 + 512]), r=[t_z], w=[b_t])
            P.op("pe", lambda e, c=c, b_=b_: e.matmul(p1[:, :], ones_b[:, :], b_[:], start=(c == 0), stop=(c == 15)),
                 r=[b_t, t_c], w=[p1t])
            P.op("pe", lambda e, c=c, s=s: e.matmul(p2[:, :], ones_b[:, :], s[:], start=(c == 0), stop=(c == 15)),
                 r=[s_t, t_c], w=[p2t])
        stt, st_t = st.get()
        mean = stt[:, 0:512]
        rstd = stt[:, 512:1024]
        tmp = stt[:, 1024:1536]
        P.op("dve", lambda e: e.tensor_scalar(out=mean, in0=p1[:, :], scalar1=1.0 / D, scalar2=None, op0=ALU.mult), r=[p1t], w=[st_t])
        P.op("dve", lambda e: e.tensor_tensor(out=tmp, in0=mean, in1=mean, op=ALU.mult), r=[st_t], w=[st_t])
        P.op("dve", lambda e: e.scalar_tensor_tensor(out=tmp, in0=p2[:, :], scalar=1.0 / D, in1=tmp, op0=ALU.mult, op1=ALU.subtract),
             r=[p2t, st_t], w=[st_t])
        P.op("dve", lambda e: e.tensor_scalar(out=tmp, in0=tmp, scalar1=LN_EPS, scalar2=None, op0=ALU.add), r=[st_t], w=[st_t])
        P.op("act", lambda e: e.activation(out=tmp, in_=tmp, func=AF.Sqrt), r=[st_t], w=[st_t])
        P.op("dve", lambda e: e.reciprocal(out=rstd, in_=tmp), r=[st_t], w=[st_t])
        for c in range(16):
            o, o_t = ot.get()
            P.op("dve", lambda e, c=c, o=o: e.tensor_tensor(out=o[:], in0=z[:, c, t0:t0 + 512], in1=mean, op=ALU.subtract),
                 r=[t_z, st_t], w=[o_t])
            P.op("dve", lambda e, o=o: e.tensor_tensor(out=o[:], in0=o[:], in1=rstd, op=ALU.mult), r=[st_t, o_t], w=[o_t])
            P.op("act", lambda e, c=c, o=o: e.activation(out=o[:], in_=o[:], func=AF.Identity, bias=b_sb[:, c:c + 1],
                                                         scale=g_sb[:, c:c + 1]), r=[o_t, t_gb], w=[o_t])
            P.dma("sp", outT[c * 128:(c + 1) * 128, t0:t0 + 512], o[:], r=[o_t], w=[t_out])


NA_J = [(0, 6), (2, 10), (6, 14), (10, 16)]
TB_OFF = 3
TB_N = 22
LT = S + LC


def stage_attn(P, cst, xT, ctxT, modv, w_in_r, kvn, w_ukv_r, cosT, sinT, na_T, na_RM, aoT, t_out):
    nc = P.nc
    C_QN, C_QP, C_QPS, C_CKV, C_KP, C_KPS, C_QA, C_KA, C_VA = 0, 512, 768, 1024, 1536, 1664, 1792, 2304, 2816
    qn_d = dram_scratch(nc, "a_qn", [4, 128, S], BF16)
    qp_d = dram_scratch(nc, "a_qp", [2, 128, S], BF16)
    kn_d = dram_scratch(nc, "a_kn", [4, 128, LT], BF16)
    kp_d = dram_scratch(nc, "a_kp", [128, LT], BF16)
    qa_d = dram_scratch(nc, "a_qa", [4, 128, S], BF16)
    ka_d = dram_scratch(nc, "a_ka", [4, 128, LT], BF16)
    vm_d = dram_scratch(nc, "a_vm", [LT, 512], BF16)
    va_d = dram_scratch(nc, "a_va", [LT, 512], BF16)
    t_d = {k: Tr() for k in ("qn", "qp", "kn", "kp", "qa", "ka", "vm", "va")}
    with ExitStack() as es:
        def sb(name, shape, dt):
            return es.enter_context(SB(nc, "a_" + name, shape, dt))
        modv_sb = sb("modv", [128, 64], F32)
        sc1p = sb("sc1p", [128, 32], F32)
        kvn_sb = sb("kvn", [128, 4], F32)
        ones_f = sb("ones_f", [128, 128], F32)
        ones_b = sb("ones_b", [128, 128], BF16)
        t_c = Tr()
        load_pieces(P, modv_sb, modv, [t_c])
        P.dma("sp", kvn_sb[:], kvn[:, :], w=[t_c])
        P.dma("sp", ones_f[:], cst["ones_f"][:, :], w=[t_c])
        P.op("dve", lambda e: e.tensor_scalar(out=sc1p[:, 0:16], in0=modv_sb[:, 16:32], scalar1=1.0, scalar2=None, op0=ALU.add),
             r=[t_c], w=[t_c])
        P.op("dve", lambda e: e.tensor_scalar(out=sc1p[:, 16:32], in0=modv_sb[:, 48:64], scalar1=1.0, scalar2=None, op0=ALU.add),
             r=[t_c], w=[t_c])
        P.op("dve", lambda e: e.tensor_copy(out=ones_b[:], in_=ones_f[:]), r=[t_c], w=[t_c])
        with ExitStack() as es1:
            uall_t = es1.enter_context(SB(nc, "a_uall", [128, 16 * LT], BF16))
            uall = uall_t[:].rearrange("p (k t) -> p k t", t=LT)
            t_u = Tr()
            es_x = ExitStack()
            xin = Pool(nc, es_x, "sb", 2, [128, S], F32, "a_xin")
            for c in range(16):
                xt, xtr = xin.get()
                P.dma("sp", xt[:], xT[c * 128:(c + 1) * 128, :], w=[xtr])
                P.op("act", lambda e, c=c, xt=xt: e.activation(out=uall[:, c, 0:S], in_=xt[:], func=AF.Identity,
                                                               bias=modv_sb[:, c:c + 1], scale=sc1p[:, c:c + 1]),
                     r=[xtr, t_c], w=[t_u])
                xt2, xtr2 = xin.get()
                P.dma("sp", xt2[:, 0:LC], ctxT[c * 128:(c + 1) * 128, :], w=[xtr2])
                P.op("act", lambda e, c=c, xt2=xt2: e.activation(out=uall[:, c, S:LT], in_=xt2[:, 0:LC], func=AF.Identity,
                                                                 bias=modv_sb[:, 32 + c:33 + c], scale=sc1p[:, 16 + c:17 + c]),
                     r=[xtr2, t_c], w=[t_u])
            P.barrier()
            es_x.close()
            wpool = Pool(nc, es1, "sb", 2, [128, 16 * 512], BF16, "a_w")
            pspool = Pool(nc, es1, "ps", 4, [128, 512], F32, "ap_p")
            stg = Pool(nc, es1, "sb", 4, [128, 512], BF16, "a_stg")
            cs = es1.enter_context(SB(nc, "a_cos", [128, S], F32))
            sn = es1.enter_context(SB(nc, "a_sin", [128, S], F32))
            P.dma("sp", cs[:], cosT[:, :], w=[t_c])
            P.dma("sp", sn[:], sinT[:, :], w=[t_c])

            def plain(c0, ntile, ntok, dst, t_dst):
                wv, wtr = load_wgroup(P, wpool, w_in_r, c0, ntile * 128)
                for ft in range(ntile):
                    for t0 in range(0, ntok, 512):
                        ps, ptr = proj_fm(P, pspool, wv, wtr, ft, uall, t_u, t0, 512)
                        s_, s_t = stg.get()
                        evac_copy(P, s_[:], ps[:, :], [ptr], [s_t])
                        P.dma("sp", dst[ft, :, t0:t0 + 512], s_[:], r=[s_t], w=[t_dst])
            plain(C_QN, 4, S, qn_d, t_d["qn"])
            plain(C_QA, 4, S, qa_d, t_d["qa"])
            plain(C_KA, 4, LT // 512 * 512, ka_d, t_d["ka"])
            wv, wtr = load_wgroup(P, wpool, w_in_r, C_KA, 512)
            for ft in range(4):
                ps, ptr = proj_fm(P, pspool, wv, wtr, ft, uall, t_u, S, LC)
                s_, s_t = stg.get()
                evac_copy(P, s_[:, 0:LC], ps[:, 0:LC], [ptr], [s_t])
                P.dma("sp", ka_d[ft, :, S:LT], s_[:, 0:LC], r=[s_t], w=[t_d["ka"]])
            rtmp = Pool(nc, es1, "sb", 2, [128, 512], F32, "a_rtmp")

            def roped(c_p, c_s, ntile, dst_of, t_dst, with_ctx):
                wv, wtr = load_wgroup(P, wpool, w_in_r, c_p, ntile * 128)
                wv2, wtr2 = load_wgroup(P, wpool, w_in_r, c_s, ntile * 128)
                for ft in range(ntile):
                    for t0 in range(0, S, 512):
                        ps, ptr = proj_fm(P, pspool, wv, wtr, ft, uall, t_u, t0, 512)
                        ps2, ptr2 = proj_fm(P, pspool, wv2, wtr2, ft, uall, t_u, t0, 512)
                        r1, r1t = rtmp.get()
                        r2, r2t = rtmp.get()
                        P.op("dve", lambda e, r1=r1, ps=ps, t0=t0: e.tensor_tensor(out=r1[:], in0=ps[:, :], in1=cs[:, t0:t0 + 512], op=ALU.mult),
                             r=[ptr, t_c], w=[r1t])
                        P.op("dve", lambda e, r2=r2, ps2=ps2, t0=t0: e.tensor_tensor(out=r2[:], in0=ps2[:, :], in1=sn[:, t0:t0 + 512], op=ALU.mult),
                             r=[ptr2, t_c], w=[r2t])
                        s_, s_t = stg.get()
                        P.op("pool", lambda e, r1=r1, r2=r2, s_=s_: e.tensor_tensor(out=s_[:], in0=r1[:], in1=r2[:], op=ALU.add),
                             r=[r1t, r2t], w=[s_t])
                        P.dma("sp", dst_of(ft)[:, t0:t0 + 512], s_[:], r=[s_t], w=[t_dst])
                    if with_ctx:
                        ps, ptr = proj_fm(P, pspool, wv, wtr, ft, uall, t_u, S, LC)
                        s_, s_t = stg.get()
                        evac_copy(P, s_[:, 0:LC], ps[:, 0:LC], [ptr], [s_t])
                        P.dma("sp", dst_of(ft)[:, S:LT], s_[:, 0:LC], r=[s_t], w=[t_dst])
            roped(C_QP, C_QPS, 2, lambda ft: qp_d[ft], t_d["qp"], False)
            roped(C_KP, C_KPS, 1, lambda ft: kp_d, t_d["kp"], True)
            wv, wtr = load_wgroup(P, wpool, w_in_r, C_VA, 512)
            for tt in range(LT // 128):
                ps, ptr = proj_tm(P, pspool, wv, wtr, 0, 512, uall, t_u, tt)
                s_, s_t = stg.get()
                evac_copy(P, s_[:], ps[:, :], [ptr], [s_t])
                P.dma("sp", va_d[tt * 128:(tt + 1) * 128, :], s_[:], r=[s_t], w=[t_d["va"]])
            ckf_t = es1.enter_context(SB(nc, "a_ckf", [128, 4 * LT], F32))
            ckf = ckf_t[:].rearrange("p (k t) -> p k t", t=LT)
            ckn_t = es1.enter_context(SB(nc, "a_ckn", [128, 4 * LT], BF16))
            ckn = ckn_t[:].rearrange("p (k t) -> p k t", t=LT)
            t_ckf = Tr()
            t_ckn = Tr()
            wv, wtr = load_wgroup(P, wpool, w_in_r, C_CKV, 512)
            chunks = [(t0, 512) for t0 in range(0, S, 512)] + [(S, LC)]
            for ft in range(4):
                for (t0, n) in chunks:
                    ps, ptr = proj_fm(P, pspool, wv, wtr, ft, uall, t_u, t0, n)
                    evac_copy(P, ckf[:, ft, t0:t0 + n], ps[:, 0:n], [ptr], [t_ckf])
            rs = Pool(nc, es1, "sb", 2, [128, 512], F32, "a_rs")
            for (t0, n) in chunks:
                pss, psst = pspool.get()
                for k in range(4):
                    q_, q_t = rtmp.get()
                    P.op("act", lambda e, q_=q_, k=k, t0=t0, n=n: e.activation(out=q_[:, 0:n], in_=ckf[:, k, t0:t0 + n], func=AF.Square),
                         r=[t_ckf], w=[q_t])
                    P.op("pe", lambda e, q_=q_, k=k, n=n: e.matmul(pss[:, 0:n], ones_f[:, :], q_[:, 0:n], start=(k == 0), stop=(k == 3)),
                         r=[q_t, t_c], w=[psst])
                r_, r_t = rs.get()
                P.op("dve", lambda e, r_=r_, n=n: e.tensor_scalar(out=r_[:, 0:n], in0=pss[:, 0:n], scalar1=1.0 / 512, scalar2=LN_EPS,
                                                                  op0=ALU.mult, op1=ALU.add), r=[psst], w=[r_t])
                P.op("act", lambda e, r_=r_, n=n: e.activation(out=r_[:, 0:n], in_=r_[:, 0:n], func=AF.Sqrt), r=[r_t], w=[r_t])
                P.op("dve", lambda e, r_=r_, n=n: e.reciprocal(out=r_[:, 0:n], in_=r_[:, 0:n]), r=[r_t], w=[r_t])
                for k in range(4):
                    P.op("dve", lambda e, r_=r_, k=k, t0=t0, n=n: e.scalar_tensor_tensor(
                        out=ckn[:, k, t0:t0 + n], in0=ckf[:, k, t0:t0 + n], scalar=kvn_sb[:, k:k + 1], in1=r_[:, 0:n],
                        op0=ALU.mult, op1=ALU.mult), r=[t_ckf, r_t, t_c], w=[t_ckn])
            wv, wtr = load_wgroup(P, wpool, w_ukv_r, 0, 512, nk=4)
            for ft in range(4):
                for (t0, n) in chunks:
                    ps, ptr = proj_fm(P, pspool, wv, wtr, ft, ckn, t_ckn, t0, n, nk=4)
                    s_, s_t = stg.get()
                    evac_copy(P, s_[:, 0:n], ps[:, 0:n], [ptr], [s_t])
                    P.dma("sp", kn_d[ft, :, t0:t0 + n], s_[:, 0:n], r=[s_t], w=[t_d["kn"]])
            wv, wtr = load_wgroup(P, wpool, w_ukv_r, 512, 512, nk=4)
            for tt in range(LT // 128):
                ps, ptr = proj_tm(P, pspool, wv, wtr, 0, 512, ckn, t_ckn, tt, nk=4)
                s_, s_t = stg.get()
                evac_copy(P, s_[:], ps[:, :], [ptr], [s_t])
                P.dma("sp", vm_d[tt * 128:(tt + 1) * 128, :], s_[:], r=[s_t], w=[t_d["vm"]])
            P.barrier()
        with ExitStack() as es2:
            NKT = LT // 128
            qb = Pool(nc, es2, "sb", 2, [128, S], BF16, "a_q")
            qpb = Pool(nc, es2, "sb", 2, [128, S], BF16, "a_qp")
            kb = Pool(nc, es2, "sb", 2, [128, LT], BF16, "a_k")
            vb = Pool(nc, es2, "sb", 2, [128, NKT * 128], BF16, "a_v")
            kpb = es2.enter_context(SB(nc, "a_kpb", [128, LT], BF16))
            t_kpb = Tr()
            P.dma("sp", kpb[:], kp_d[:, :], r=[t_d["kp"]], w=[t_kpb])
            naT = es2.enter_context(SB(nc, "a_naT", [128, 4 * TB_N * 64], F32))
            naRM = es2.enter_context(SB(nc, "a_naRM", [128, 4 * 8 * 8], F32))
            P.dma("sp", naT[:], na_T[:, :], w=[t_c])
            P.dma("sp", naRM[:], na_RM[:, :], w=[t_c])
            naTv = naT[:].rearrange("p (h b q) -> p h b q", b=TB_N, q=64)
            naRMv = naRM[:].rearrange("p (c j r) -> p c j r", j=8, r=8)
            psS = Pool(nc, es2, "ps", 4, [128, 512], F32, "ap_S")
            psO = Pool(nc, es2, "ps", 2, [128, 512], F32, "ap_O")
            psD = Pool(nc, es2, "ps", 2, [128, 512], F32, "ap_D")
            pT = Pool(nc, es2, "sb", 6, [128, 512], BF16, "a_pT")
            sT = Pool(nc, es2, "sb", 4, [128, 512], F32, "a_sT")
            LOOK = 2
            rd = Pool(nc, es2, "sb", 2, [128, 512], F32, "a_rd")
            ao = Pool(nc, es2, "sb", 2, [128, 512], BF16, "a_ao")

            def finish(po, pot, pd, pdt, row0, t0):
                r_, r_t = rd.get()
                P.op("dve", lambda e: e.reciprocal(out=r_[:], in_=pd[:, :]), r=[pdt], w=[r_t])
                a_, a_t = ao.get()
                P.op("dve", lambda e: e.tensor_tensor(out=a_[:], in0=po[:, :], in1=r_[:], op=ALU.mult), r=[pot, r_t], w=[a_t])
                P.dma("sp", aoT[row0:row0 + 128, t0:t0 + 512], a_[:], r=[a_t], w=[t_out])

            SC_MLA = 192 ** -0.5
            SC_NA = 128 ** -0.5
            for h in range(4):
                q_, q_t = qb.get()
                P.dma("sp", q_[:], qn_d[h], r=[t_d["qn"]], w=[q_t])
                if h % 2 == 0:
                    qp_, qp_t = qpb.get()
                    P.dma("sp", qp_[:], qp_d[h // 2], r=[t_d["qp"]], w=[qp_t])
                k_, k_t = kb.get()
                P.dma("sp", k_[:], kn_d[h], r=[t_d["kn"]], w=[k_t])
                v_, v_t = vb.get()
                P.dma("sp", v_[:].rearrange("p (t d) -> p t d", d=128),
                      vm_d[:, h * 128:(h + 1) * 128].rearrange("(t p) d -> p t d", p=128), r=[t_d["vm"]], w=[v_t])
                pb = 64 * (h % 2)
                for t0 in range(0, S, 512):
                    po, pot = psO.get()
                    pd, pdt = psD.get()

                    def issue_s(kt, t0=t0):
                        ps, pst = psS.get()
                        P.op("pe", lambda e, ps=ps, kt=kt: e.matmul(ps[:, :], k_[:, kt * 128:(kt + 1) * 128], q_[:, t0:t0 + 512],
                                                                    start=True, stop=False), r=[k_t, q_t], w=[pst])
                        P.op("pe", lambda e, ps=ps, kt=kt: e.matmul(ps[:, :], kpb[pb:pb + 64, kt * 128:(kt + 1) * 128],
                                                                    qp_[pb:pb + 64, t0:t0 + 512], start=False, stop=True),
                             r=[t_kpb, qp_t], w=[pst])
                        return ps, pst
                    pend = [issue_s(kt) for kt in range(min(LOOK, NKT))]
                    for kt in range(NKT):
                        ps, pst = pend.pop(0)
                        p_, p_t = pT.get()
                        P.op("act", lambda e, ps=ps, p_=p_: e.activation(out=p_[:], in_=ps[:, :], func=AF.Exp, scale=SC_MLA),
                             r=[pst], w=[p_t])
                        if kt + LOOK < NKT:
                            pend.append(issue_s(kt + LOOK))
                        P.op("pe", lambda e, p_=p_, kt=kt: e.matmul(po[:, :], v_[:, kt * 128:(kt + 1) * 128], p_[:],
                                                                    start=(kt == 0), stop=(kt == NKT - 1)), r=[v_t, p_t], w=[pot])
                        P.op("pe", lambda e, p_=p_, kt=kt: e.matmul(pd[:, :], ones_b[:, :], p_[:],
                                                                    start=(kt == 0), stop=(kt == NKT - 1)), r=[t_c, p_t], w=[pdt])
                    finish(po, pot, pd, pdt, h * 128, t0)
            for h in range(4):
                q_, q_t = qb.get()
                P.dma("sp", q_[:], qa_d[h], r=[t_d["qa"]], w=[q_t])
                k_, k_t = kb.get()
                P.dma("sp", k_[:], ka_d[h], r=[t_d["ka"]], w=[k_t])
                v_, v_t = vb.get()
                P.dma("sp", v_[:].rearrange("p (t d) -> p t d", d=128),
                      va_d[:, h * 128:(h + 1) * 128].rearrange("(t p) d -> p t d", p=128), r=[t_d["va"]], w=[v_t])
                for qc in range(4):
                    t0 = qc * 512
                    jlo, jhi = NA_J[qc]
                    kts = list(range(jlo, jhi)) + [16, 17]
                    po, pot = psO.get()
                    pd, pdt = psD.get()

                    def issue_s(kt, t0=t0):
                        ps, pst = psS.get()
                        P.op("pe", lambda e, ps=ps, kt=kt: e.matmul(ps[:, :], k_[:, kt * 128:(kt + 1) * 128], q_[:, t0:t0 + 512],
                                                                    start=True, stop=True), r=[k_t, q_t], w=[pst])
                        return ps, pst
                    pend = [issue_s(kt) for kt in kts[:LOOK]]
                    for idx, kt in enumerate(kts):
                        ps, pst = pend.pop(0)
                        p_, p_t = pT.get()
                        if kt < 16:
                            b0 = 8 * qc - 2 * kt + 7 + TB_OFF
                            s_, s_t = sT.get()
                            P.op("dve", lambda e, ps=ps, s_=s_, b0=b0: e.scalar_tensor_tensor(
                                out=s_[:].rearrange("p (r q) -> p r q", q=64), in0=ps[:, :].rearrange("p (r q) -> p r q", q=64),
                                scalar=SC_NA, in1=naTv[:, h, b0:b0 + 8, :], op0=ALU.mult, op1=ALU.add), r=[pst, t_c], w=[s_t])
                            jj = kt - jlo
                            P.op("pool" if idx % 3 == 2 else "dve", lambda e, s_=s_, jj=jj: e.tensor_tensor(
                                out=s_[:].rearrange("p (r q) -> p r q", q=64), in0=s_[:].rearrange("p (r q) -> p r q", q=64),
                                in1=naRMv[:, qc, jj, :].unsqueeze(2).to_broadcast([128, 8, 64]), op=ALU.add), r=[s_t, t_c], w=[s_t])
                            P.op("act", lambda e, s_=s_, p_=p_: e.activation(out=p_[:], in_=s_[:], func=AF.Exp), r=[s_t], w=[p_t])
                        else:
                            P.op("act", lambda e, ps=ps, p_=p_: e.activation(out=p_[:], in_=ps[:, :], func=AF.Exp, scale=SC_NA),
                                 r=[pst], w=[p_t])
                        if idx + LOOK < len(kts):
                            pend.append(issue_s(kts[idx + LOOK]))
                        last = (idx == len(kts) - 1)
                        P.op("pe", lambda e, p_=p_, kt=kt, idx=idx, last=last: e.matmul(
                            po[:, :], v_[:, kt * 128:(kt + 1) * 128], p_[:], start=(idx == 0), stop=last), r=[v_t, p_t], w=[pot])
                        P.op("pe", lambda e, p_=p_, idx=idx, last=last: e.matmul(pd[:, :], ones_b[:, :], p_[:], start=(idx == 0), stop=last),
                             r=[t_c, p_t], w=[pdt])
                    finish(po, pot, pd, pdt, 512 + h * 128, t0)
            P.barrier()


def na_tables_host(rpb4):
    NEG = np.float32(-1e30)
    qcol = np.arange(64)
    kc = np.arange(64)
    cs = np.clip(qcol - 8, 0, 48)
    col_ok = (kc[None, :] >= cs[:, None]) & (kc[None, :] < cs[:, None] + 16)
    dc = np.clip(kc[None, :] - qcol[:, None], -15, 15) + 15
    T = np.full((128, 4, TB_N, 64), NEG, dtype=np.float32)
    for rk in range(2):
        for bi in range(TB_N):
            b = bi - TB_OFF
            a = 14 - b + rk
            if 0 <= a <= 14:
                for h in range(4):
                    vals = rpb4[h, a][dc]
                    blk = np.where(col_ok, vals, NEG)
                    T[rk * 64:(rk + 1) * 64, h, bi, :] = blk.T
    RM = np.full((128, 4, 8, 8), NEG, dtype=np.float32)
    for qc in range(4):
        jlo, jhi = NA_J[qc]
        for jj in range(jhi - jlo):
            j = jlo + jj
            for rq in range(8):
                r = 8 * qc + rq
                rs = min(max(r - 4, 0), 24)
                for rk in range(2):
                    kr = 2 * j + rk
                    if rs <= kr <= rs + 7:
                        RM[rk * 64:(rk + 1) * 64, qc, jj, rq] = 0.0
    return T.reshape(128, -1), RM.reshape(128, -1)


def rope_tables_host():
    t = np.arange(S)
    row = (t // 64).astype(np.float32)
    col = (t % 64).astype(np.float32)
    n_freq = 16
    inv = (10000.0 ** (-np.arange(n_freq, dtype=np.float32) / n_freq)).astype(np.float32)
    ang = np.concatenate([row[:, None] * inv, col[:, None] * inv], axis=1)
    cos = np.cos(ang).astype(np.float32)
    sin = np.sin(ang).astype(np.float32)
    i = np.arange(128)
    f = (i % 64) % 32
    sign = np.where((i % 64) < 32, -1.0, 1.0).astype(np.float32)
    cosT = np.ascontiguousarray(cos[:, f].T)
    sinT = np.ascontiguousarray((sin[:, f] * sign[None, :]).T)
    return cosT, sinT


def attn_host_inputs(inp, m0, b, hf):
    def fm(v):
        return np.ascontiguousarray(v.reshape(16, 128).T)
    w = inp["ab_w_in"][0]
    hs = [4 * hf + i for i in range(4)]
    cols = []
    for h in hs:
        cols += list(range(h * 192, h * 192 + 128))
    for h in hs:
        cols += list(range(h * 192 + 128, h * 192 + 192))
    for h in hs:
        cols += list(range(h * 192 + 160, h * 192 + 192)) + list(range(h * 192 + 128, h * 192 + 160))
    cols += list(range(1536, 2048))
    kp = list(range(2048, 2112))
    kps = list(range(2080, 2112)) + list(range(2048, 2080))
    cols += kp + kp + kps + kps
    for off in (2112, 3136, 4160):
        for h in hs:
            cols += list(range(off + h * 128, off + (h + 1) * 128))
    w_in_r = np.ascontiguousarray(w[:, cols])
    wu = inp["ab_w_ukv"][0]
    ucols = []
    for h in hs:
        ucols += list(range(h * 256, h * 256 + 128))
    for h in hs:
        ucols += list(range(h * 256 + 128, h * 256 + 256))
    w_ukv_r = np.ascontiguousarray(wu[:, ucols])
    naT, naRM = na_tables_host(inp["ab_rpb"][0][4 * hf:4 * hf + 4])
    cosT, sinT = rope_tables_host()
    mb = m0[b]
    mc = m0[4]
    modv = np.concatenate([fm(mb[0:2048]), fm(mb[2048:4096]), fm(mc[0:2048]), fm(mc[2048:4096])], axis=1)
    return {"xT": np.ascontiguousarray(inp["x"][b].T), "ctxT": np.ascontiguousarray(inp["ctx"][b].T), "modv": modv,
            "w_in_r": w_in_r, "kvn": np.ascontiguousarray(inp["ab_kv_norm"][0].reshape(4, 128).T), "w_ukv_r": w_ukv_r,
            "cosT": cosT, "sinT": sinT, "na_T": naT, "na_RM": naRM}


def build_attn_prog():
    nc = bass.Bass("TRN2", target_bir_lowering=False)
    consts = make_consts()
    with ExitStack() as es:
        P = Prog(nc, es)
        cst = declare_consts(nc, consts)
        xT = dram_in(nc, "xT", [D, S])
        ctxT = dram_in(nc, "ctxT", [D, LC])
        modv = dram_in(nc, "modv", [128, 64])
        w_in_r = dram_in(nc, "w_in_r", [D, 3328])
        kvn = dram_in(nc, "kvn", [128, 4])
        w_ukv_r = dram_in(nc, "w_ukv_r", [512, 1024])
        cosT = dram_in(nc, "cosT", [128, S])
        sinT = dram_in(nc, "sinT", [128, S])
        na_T = dram_in(nc, "na_T", [128, 4 * TB_N * 64])
        na_RM = dram_in(nc, "na_RM", [128, 256])
        aoT = dram_out(nc, "aoT", [1024, S], BF16)
        stage_attn(P, cst, xT, ctxT, modv, w_in_r, kvn, w_ukv_r, cosT, sinT, na_T, na_RM, aoT, Tr())
        P.barrier()
    return nc, consts


def stage_moe_ffn(P, xe_in, w1, w3, w2, ye_out, t_in, t_out, load_xe=None, nb=4, n_exp=2):
    nc = P.nc
    if load_xe is None:
        def load_xe(dst, ex, b, w):
            P.dma("sp", dst, xe_in[ex, b].rearrange("p (c s) -> p c s", s=CAP), r=[t_in], w=w)
    NFT = FF // 128
    NT = nb * CAP
    TCH = min(512, NT)
    with ExitStack() as es:
        xe_t = [es.enter_context(SB(nc, "f_xe%d" % i, [128, 16 * NT], BF16)) for i in range(2)]
        t_xe = [Tr(), Tr()]
        wbuf = Pool(nc, es, "sb", 2, [128, 16 * FF], BF16, "f_w")
        w2buf = Pool(nc, es, "sb", 2, [128, NFT * 512], BF16, "f_w2")
        h1_t = es.enter_context(SB(nc, "f_h1", [128, NFT * NT], BF16))
        h1 = h1_t[:].rearrange("p (f t) -> p f t", t=NT)
        t_h1 = Tr()
        stg = Pool(nc, es, "sb", 3, [128, 512], BF16, "f_stg")
        pspool = Pool(nc, es, "ps", 6, [128, 512], F32, "fp_p")
        def fetch(ex):
            xv = xe_t[ex % 2][:].rearrange("p (c b s) -> p c b s", b=nb, s=CAP)
            for b in range(nb):
                load_xe(xv[:, :, b, :], ex, b, [t_xe[ex % 2]])
        fetch(0)
        for ex in range(n_exp):
            if ex + 1 < n_exp:
                fetch(ex + 1)
            xf = xe_t[ex % 2][:].rearrange("p (c t) -> p c t", t=NT)
            for which, wsrc in ((0, w1), (1, w3)):
                wt, wtr = wbuf.get()
                wv = wt[:].rearrange("p (k c) -> p k c", c=FF)
                for half in range(2):
                    P.dma("pool", wv[:, half * 8:(half + 1) * 8, :],
                          wsrc[ex, half * 1024:(half + 1) * 1024, :].rearrange("(k p) c -> p k c", p=128), w=[wtr])
                for f in range(NFT):
                    for t0 in range(0, NT, TCH):
                        ps, ptr = pspool.get()
                        for k in range(16):
                            P.op("pe", lambda e, ps=ps, k=k, f=f, t0=t0: e.matmul(ps[:, 0:TCH], wv[:, k, f * 128:(f + 1) * 128],
                                                                                  xf[:, k, t0:t0 + TCH], start=(k == 0), stop=(k == 15)),
                                 r=[wtr, t_xe[ex % 2]], w=[ptr])
                        if which == 0:
                            P.op("act", lambda e, ps=ps, f=f, t0=t0: e.activation(out=h1[:, f, t0:t0 + TCH], in_=ps[:, 0:TCH], func=AF.Silu),
                                 r=[ptr], w=[t_h1])
                        else:
                            P.op("dve", lambda e, ps=ps, f=f, t0=t0: e.tensor_tensor(out=h1[:, f, t0:t0 + TCH], in0=h1[:, f, t0:t0 + TCH],
                                                                                     in1=ps[:, 0:TCH], op=ALU.mult), r=[ptr, t_h1], w=[t_h1])
            for dq in range(4):
                w2t, w2tr = w2buf.get()
                w2v = w2t[:].rearrange("p (f d) -> p f d", d=512)
                P.dma("pool", w2v, w2[ex, :, dq * 512:(dq + 1) * 512].rearrange("(f p) d -> p f d", p=128), w=[w2tr])
                for g in range(2 * nb):
                    ps, ptr = pspool.get()
                    for f in range(NFT):
                        P.op("pe", lambda e, ps=ps, f=f, g=g: e.matmul(ps[:, :], h1[:, f, g * 128:(g + 1) * 128], w2v[:, f, :],
                                                                       start=(f == 0), stop=(f == NFT - 1)), r=[t_h1, w2tr], w=[ptr])
                    s_, s_t = stg.get()
                    evac_copy(P, s_[:], ps[:, :], [ptr], [s_t])
                    P.dma("sp", ye_out[ex, g, :, dq * 512:(dq + 1) * 512], s_[:], r=[s_t], w=[t_out])
        P.barrier()


def stage_scatter_ln(P, cst, ye_in, pos_in, gate_in, hT, vecs, outT, t_in, t_out, load_ye=None, load_pg=None):
    nc = P.nc
    if load_ye is None:
        def load_ye(dst, ex, dh, w):
            P.dma("sp", dst, ye_in[ex, :, :, dh * 1024:(dh + 1) * 1024].rearrange("s p d -> p s d"), r=[t_in], w=w)
    if load_pg is None:
        def load_pg(pos, gate, w):
            P.dma("sp", pos, pos_in[:, :], r=[t_in], w=w)
            P.dma("sp", gate, gate_in[:, :], r=[t_in], w=w)
    T = 1024
    z_d = dram_scratch(nc, "s_z%d" % id(outT), [D, T], F32)
    t_z = Tr()
    with ExitStack() as es:
        def sb(name, shape, dt):
            return es.enter_context(SB(nc, "s_" + name, shape, dt))
        vec_sb = sb("vec", [128, 48], F32)
        ones_f = sb("ones_f", [128, 128], F32)
        t_c = Tr()
        load_pieces(P, vec_sb, vecs, [t_c])
        P.dma("sp", ones_f[:], cst["ones_f"][:, :], w=[t_c])
        with ExitStack() as es1:
            pidx = es1.enter_context(SB(nc, "s_pidx", [128, 2], F32))
            selrow = es1.enter_context(SB(nc, "s_selrow", [16, 16 * 128], F32))
            pos = es1.enter_context(SB(nc, "s_pos", [16, T], F32))
            gate = es1.enter_context(SB(nc, "s_gate", [16, T], F32))
            P.dma("sp", pidx[:], cst["pidx"][:, :], w=[t_c])
            P.dma("sp", selrow[:].rearrange("p (e m) -> p e m", m=128), cst["selrow16"][:, :, :], w=[t_c])
            load_pg(pos[:], gate[:], [t_c])
            selrow_b = es1.enter_context(SB(nc, "s_selrowb", [16, 16 * 128], BF16))
            pos_b = es1.enter_context(SB(nc, "s_posb", [16, T], BF16))
            gate_b = es1.enter_context(SB(nc, "s_gateb", [16, T], BF16))
            P.op("dve", lambda e: e.tensor_copy(out=selrow_b[:], in_=selrow[:]), r=[t_c], w=[t_c])
            P.op("dve", lambda e: e.tensor_copy(out=pos_b[:], in_=pos[:]), r=[t_c], w=[t_c])
            P.op("dve", lambda e: e.tensor_copy(out=gate_b[:], in_=gate[:]), r=[t_c], w=[t_c])
            selg_t = es1.enter_context(SB(nc, "s_selg", [128, 32 * T], BF16))
            selg = selg_t[:].rearrange("p (g t) -> p g t", t=T)
            t_selg = Tr()
            psB = Pool(nc, es1, "ps", 4, [128, 512], F32, "sp_B")
            psO = Pool(nc, es1, "ps", 2, [128, 512], F32, "sp_O")
            gb = Pool(nc, es1, "sb", 2, [128, 512], F32, "s_gb")
            for ex in range(16):
                for tq in range(T // 512):
                    pb, pbt = psB.get()
                    pg, pgt = psB.get()
                    P.op("pe", lambda e, pb=pb, ex=ex, tq=tq: e.matmul(pb[:, :], selrow_b[:, ex * 128:(ex + 1) * 128],
                                                                       pos_b[:, tq * 512:(tq + 1) * 512], start=True, stop=True),
                         r=[t_c], w=[pbt])
                    P.op("pe", lambda e, pg=pg, ex=ex, tq=tq: e.matmul(pg[:, :], selrow_b[:, ex * 128:(ex + 1) * 128],
                                                                       gate_b[:, tq * 512:(tq + 1) * 512], start=True, stop=True),
                         r=[t_c], w=[pgt])
                    g_, g_t = gb.get()
                    P.op("act", lambda e, g_=g_, pg=pg: e.copy(out=g_[:], in_=pg[:, :]), r=[pgt], w=[g_t])
                    for st in range(2):
                        P.op("dve", lambda e, pb=pb, g_=g_, st=st, ex=ex, tq=tq: e.scalar_tensor_tensor(
                            out=selg[:, ex * 2 + st, tq * 512:(tq + 1) * 512], in0=pb[:, :], scalar=pidx[:, st:st + 1],
                            in1=g_[:], op0=ALU.is_equal, op1=ALU.mult), r=[pbt, g_t, t_c], w=[t_selg])
            ye_t = es1.enter_context(SB(nc, "s_ye", [128, 32 * 1024], BF16))
            ye = ye_t[:].rearrange("p (g d) -> p g d", d=1024)
            t_ye = Tr()
            hin = Pool(nc, es1, "sb", 2, [128, 512], F32, "s_hin")
            yg = Pool(nc, es1, "sb", 2, [128, 512], F32, "s_yg")
            for dh in range(2):
                for ex in range(16):
                    load_ye(ye[:, ex * 2:ex * 2 + 2, :], ex, dh, [t_ye])
                for dt in range(8):
                    c = dh * 8 + dt
                    for tq in range(T // 512):
                        po, pot = psO.get()
                        for g in range(32):
                            P.op("pe", lambda e, po=po, g=g, dt=dt, tq=tq: e.matmul(po[:, :], ye[:, g, dt * 128:(dt + 1) * 128],
                                                                                    selg[:, g, tq * 512:(tq + 1) * 512],
                                                                                    start=(g == 0), stop=(g == 31)),
                                 r=[t_ye, t_selg], w=[pot])
                        h_, h_t = hin.get()
                        P.dma("sp", h_[:], hT[c * 128:(c + 1) * 128, tq * 512:(tq + 1) * 512], r=[t_in], w=[h_t])
                        y_, y_t = yg.get()
                        P.op("act", lambda e, y_=y_, po=po, c=c: e.activation(out=y_[:], in_=po[:, :], func=AF.Identity,
                                                                              scale=vec_sb[:, c:c + 1]), r=[pot, t_c], w=[y_t])
                        P.op("dve", lambda e, y_=y_, h_=h_: e.scalar_tensor_tensor(out=y_[:], in0=h_[:], scalar=ALPHA, in1=y_[:],
                                                                                   op0=ALU.mult, op1=ALU.add), r=[h_t, y_t], w=[y_t])
                        P.dma("sp", z_d[c * 128:(c + 1) * 128, tq * 512:(tq + 1) * 512], y_[:], r=[y_t], w=[t_z])
            P.barrier()
        z_t = sb("z", [128, 16 * T], F32)
        z = z_t[:].rearrange("p (c t) -> p c t", t=T)
        t_zs = Tr()
        P.dma("sp", z, z_d.rearrange("(c p) t -> p c t", p=128), r=[t_z], w=[t_zs])
        pspool = Pool(nc, es, "ps", 4, [128, 512], F32, "sp_L")
        ln_apply(P, es, z, t_zs, T, vec_sb[:, 16:32], vec_sb[:, 32:48], t_c, ones_f, t_c, outT, t_out, pspool, "s_ln")
        P.barrier()


def stage_oproj_ln(P, cst, ao_in, w_out, hT, vecs, outT, t_in, t_out, load_ao=None):
    nc = P.nc
    if load_ao is None:
        def load_ao(dst, w):
            P.dma("sp", dst, ao_in.rearrange("(c p) t -> p c t", p=128), r=[t_in], w=w)
    T = 1024
    with ExitStack() as es:
        def sb(name, shape, dt):
            return es.enter_context(SB(nc, "o_" + name, shape, dt))
        vec_sb = sb("vec", [128, 48], F32)
        ones_f = sb("ones_f", [128, 128], F32)
        t_c = Tr()
        load_pieces(P, vec_sb, vecs, [t_c])
        P.dma("sp", ones_f[:], cst["ones_f"][:, :], w=[t_c])
        z_t = sb("z", [128, 16 * T], F32)
        z = z_t[:].rearrange("p (c t) -> p c t", t=T)
        t_z = Tr()
        with ExitStack() as es1:
            ao_t = es1.enter_context(SB(nc, "o_ao", [128, 16 * T], BF16))
            ao = ao_t[:].rearrange("p (c t) -> p c t", t=T)
            t_ao = Tr()
            load_ao(ao, [t_ao])
            wpool = Pool(nc, es1, "sb", 2, [128, 16 * 512], BF16, "o_w")
            pspool = Pool(nc, es1, "ps", 4, [128, 512], F32, "op_p")
            hin = Pool(nc, es1, "sb", 2, [128, 512], F32, "o_hin")
            yg = Pool(nc, es1, "sb", 2, [128, 512], F32, "o_yg")
            for grp in range(4):
                wv, wtr = load_wgroup(P, wpool, w_out, grp * 512, 512)
                for ft in range(4):
                    c = grp * 4 + ft
                    for t0 in range(0, T, 512):
                        ps, ptr = proj_fm(P, pspool, wv, wtr, ft, ao, t_ao, t0, 512)
                        h_, h_t = hin.get()
                        P.dma("sp", h_[:], hT[c * 128:(c + 1) * 128, t0:t0 + 512], r=[t_in], w=[h_t])
                        y_, y_t = yg.get()
                        P.op("act", lambda e, y_=y_, ps=ps, c=c: e.activation(out=y_[:], in_=ps[:, :], func=AF.Identity,
                                                                              scale=vec_sb[:, c:c + 1]), r=[ptr, t_c], w=[y_t])
                        P.op("dve", lambda e, y_=y_, h_=h_, c=c, t0=t0: e.scalar_tensor_tensor(
                            out=z[:, c, t0:t0 + 512], in0=h_[:], scalar=ALPHA, in1=y_[:], op0=ALU.mult, op1=ALU.add),
                            r=[h_t, y_t], w=[t_z])
            P.barrier()
        pspool2 = Pool(nc, es, "ps", 4, [128, 512], F32, "op_L")
        ln_apply(P, es, z, t_z, T, vec_sb[:, 16:32], vec_sb[:, 32:48], t_c, ones_f, t_c, outT, t_out, pspool2, "o_ln")
        P.barrier()


NF = 17


def stage_cd(P, cst, hT, modv, w_in_c, cvec, fw1, fw2, fvec, fw3_c, delta_b, cdT, t_in, t_out, load_h=None):
    nc = P.nc
    if load_h is None:
        def load_h(dst, c, w):
            P.dma("sp", dst, hT[c * 128:(c + 1) * 128, :], r=[t_in], w=w)
    with ExitStack() as es:
        def sb(name, shape, dt):
            return es.enter_context(SB(nc, "h_" + name, shape, dt))
        modv_sb = sb("modv", [128, 32], F32)
        sc1p = sb("sc1p", [128, 16], F32)
        cvec_sb = sb("cvec", [128, 56], F32)
        ident_b = sb("ident_b", [128, 128], BF16)
        wN = sb("wN", [128, NF], F32)
        t01n = sb("t01n", [128, 16], F32)
        mask0 = sb("mask0", [128, 1], F32)
        dlt = sb("dlt", [128, 512], F32)
        t_c = Tr()
        load_pieces(P, modv_sb, modv, [t_c])
        P.dma("sp", cvec_sb[:], cvec[:, :], w=[t_c])
        P.dma("sp", ident_b[:], cst["ident_b"][:, :], w=[t_c])
        P.dma("sp", wN[:], cst["wN"][:, :], w=[t_c])
        P.dma("sp", t01n[:], cst["t01n"][:, :], w=[t_c])
        P.dma("sp", mask0[:], cst["mask0"][:, :], w=[t_c])
        P.dma("sp", dlt[:], delta_b[:, :], w=[t_c])
        P.op("dve", lambda e: e.tensor_scalar(out=sc1p[:], in0=modv_sb[:, 16:32], scalar1=1.0, scalar2=None, op0=ALU.add),
             r=[t_c], w=[t_c])
        xs_t = [sb("xs%d" % o, [128, 4 * S], BF16) for o in range(3)]
        xs = [t[:].rearrange("p (c t) -> p c t", t=S) for t in xs_t]
        t_xs = [Tr(), Tr(), Tr()]
        es_g = ExitStack()
        g_t = es_g.enter_context(SB(nc, "h_g", [128, 4 * S], BF16))
        gT = g_t[:].rearrange("p (c t) -> p c t", t=S)
        t_g = Tr()
        with ExitStack() as es1:
            u_t = es1.enter_context(SB(nc, "h_u", [128, 16 * S], BF16))
            u = u_t[:].rearrange("p (k t) -> p k t", t=S)
            t_u = Tr()
            es_x = ExitStack()
            xin = Pool(nc, es_x, "sb", 2, [128, S], F32, "h_xin")
            for c in range(16):
                xt, xtr = xin.get()
                load_h(xt[:], c, [xtr])
                P.op("act", lambda e, c=c, xt=xt: e.activation(out=u[:, c, :], in_=xt[:], func=AF.Identity,
                                                               bias=modv_sb[:, c:c + 1], scale=sc1p[:, c:c + 1]),
                     r=[xtr, t_c], w=[t_u])
            P.barrier()
            es_x.close()
            wpool = Pool(nc, es1, "sb", 2, [128, 16 * 512], BF16, "h_w")
            pspool = Pool(nc, es1, "ps", 4, [128, 512], F32, "hp_p")
            prow = Pool(nc, es1, "sb", 2, [128, S + 2], F32, "h_prow")
            acc = Pool(nc, es1, "sb", 2, [128, S], F32, "h_acc")
            for o in range(3):
                wv, wtr = load_wgroup(P, wpool, w_in_c, o * 512, 512)
                for ct in range(4):
                    pr, prt = prow.get()
                    P.op("pool", lambda e, pr=pr: e.memset(pr[:, 0:1], 0.0), w=[prt])
                    P.op("pool", lambda e, pr=pr: e.memset(pr[:, S + 1:S + 2], 0.0), w=[prt])
                    for t0 in range(0, S, 512):
                        ps, ptr = proj_fm(P, pspool, wv, wtr, ct, u, t_u, t0, 512)
                        evac_copy(P, pr[:, 1 + t0:1 + t0 + 512], ps[:, :], [ptr], [prt])
                    a_, a_t = acc.get()
                    j = (o * 4 + ct) * 4
                    P.op("act", lambda e, a_=a_, pr=pr, j=j: e.activation(out=a_[:], in_=pr[:, 1:S + 1], func=AF.Identity,
                                                                          bias=cvec_sb[:, j + 3:j + 4], scale=cvec_sb[:, j + 1:j + 2]),
                         r=[prt, t_c], w=[a_t])
                    P.op("dve", lambda e, a_=a_, pr=pr, j=j: e.scalar_tensor_tensor(out=a_[:], in0=pr[:, 0:S], scalar=cvec_sb[:, j:j + 1],
                                                                                    in1=a_[:], op0=ALU.mult, op1=ALU.add),
                         r=[prt, a_t, t_c], w=[a_t])
                    P.op("dve", lambda e, a_=a_, pr=pr, j=j, o=o, ct=ct: e.scalar_tensor_tensor(
                        out=xs[o][:, ct, :], in0=pr[:, 2:S + 2], scalar=cvec_sb[:, j + 2:j + 3], in1=a_[:], op0=ALU.mult, op1=ALU.add),
                        r=[prt, a_t, t_c], w=[t_xs[o]])
            wv, wtr = load_wgroup(P, wpool, w_in_c, 1536, 512)
            for ct in range(4):
                for t0 in range(0, S, 512):
                    ps, ptr = proj_fm(P, pspool, wv, wtr, ct, u, t_u, t0, 512)
                    evac_copy(P, gT[:, ct, t0:t0 + 512], ps[:, :], [ptr], [t_g])
            P.barrier()
        with ExitStack() as es2:
            csw = es2.enter_context(SB(nc, "h_csw", [128, 2 * 512], BF16))
            P.dma("sp", csw[:].rearrange("p (k c) -> p k c", c=512), cst["CSW"].rearrange("(k p) c -> p k c", p=128), w=[t_c])
            cswv = csw[:].rearrange("p (k c) -> p k c", c=512)
            ab_t = es2.enter_context(SB(nc, "h_ab", [128, 16 * 2 * 512], BF16))
            ab = ab_t[:].rearrange("p (t g c) -> p t g c", g=2, c=512)
            t_ab = Tr()
            pspool = Pool(nc, es2, "ps", 8, [128, 512], F32, "hp_f")
            for tt in range(16):
                for grp in range(2):
                    ps, ptr = pspool.get()
                    for k in range(2):
                        P.op("pe", lambda e, ps=ps, k=k, grp=grp, tt=tt: e.matmul(ps[:, :], gT[:, grp * 2 + k, tt * 128:(tt + 1) * 128],
                                                                                  cswv[:, k, :], start=(k == 0), stop=(k == 1)),
                             r=[t_g, t_c], w=[ptr])
                    evac_copy(P, ab[:, tt, grp, :], ps[:, :], [ptr], [t_ab])
            rblk = Pool(nc, es2, "sb", 4, [128, 1024], BF16, "h_frb")
            ost = Pool(nc, es2, "sb", 3, [128, 512], BF16, "h_fo")
            for th in range(2):
                accs = [pspool.get() for _ in range(8)]
                for tc_ in range(16):
                    rc, rct = rblk.get()
                    rs_, rst = rblk.get()
                    P.dma("sp", rc[:], cst["FC"][tc_ * 128:(tc_ + 1) * 128, th * 1024:(th + 1) * 1024], w=[rct])
                    P.dma("sp", rs_[:], cst["FS"][tc_ * 128:(tc_ + 1) * 128, th * 1024:(th + 1) * 1024], w=[rst])
                    for ct in range(4):
                        grp, half = ct // 2, ct % 2
                        for tq in range(2):
                            ps, ptr = accs[ct * 2 + tq]
                            P.op("pe", lambda e, ps=ps, rc=rc, tc_=tc_, grp=grp, half=half, tq=tq: e.matmul(
                                ps[:, :], ab[:, tc_, grp, half * 128:(half + 1) * 128], rc[:, tq * 512:(tq + 1) * 512],
                                start=(tc_ == 0), stop=False), r=[t_ab, rct], w=[ptr])
                            P.op("pe", lambda e, ps=ps, rs_=rs_, tc_=tc_, grp=grp, half=half, tq=tq: e.matmul(
                                ps[:, :], ab[:, tc_, grp, 256 + half * 128:256 + (half + 1) * 128], rs_[:, tq * 512:(tq + 1) * 512],
                                start=False, stop=(tc_ == 15)), r=[t_ab, rst], w=[ptr])
                for ct in range(4):
                    for tq in range(2):
                        ps, ptr = accs[ct * 2 + tq]
                        o_, o_t = ost.get()
                        evac_copy(P, o_[:], ps[:, :], [ptr], [o_t])
                        P.dma("sp", cdT[512 + ct * 128:512 + (ct + 1) * 128, th * 1024 + tq * 512:th * 1024 + (tq + 1) * 512], o_[:],
                              r=[o_t], w=[t_out])
            P.barrier()
        es_g.close()
        hd2 = sb("hd2", [64, S], F32)
        t_hd2 = Tr()
        with ExitStack() as es3:
            zf = es3.enter_context(SB(nc, "h_zf", [33, S], F32))
            fw1_sb = es3.enter_context(SB(nc, "h_fw1", [33, 64], F32))
            fw2_sb = es3.enter_context(SB(nc, "h_fw2", [64, 64], F32))
            fv = es3.enter_context(SB(nc, "h_fv", [64, 8], F32))
            hd1 = es3.enter_context(SB(nc, "h_hd1", [64, S], F32))
            tmp = es3.enter_context(SB(nc, "h_ftmp", [64, S], F32))
            t_f = Tr()
            t_hd1 = Tr()
            t_tmp = Tr()
            P.dma("sp", zf[:], cst["zfeatT"][:, :], w=[t_f])
            P.dma("sp", fw1_sb[:], fw1[:, :], w=[t_f])
            P.dma("sp", fw2_sb[:], fw2[:, :], w=[t_f])
            P.dma("sp", fv[:, 0:4], fvec[:, :], w=[t_f])
            for (a, bcol, so, bo) in ((1, 0, 4, 5), (3, 2, 6, 7)):
                P.op("dve", lambda e, a=a, so=so: e.tensor_scalar(out=fv[:, so:so + 1], in0=fv[:, a:a + 1], scalar1=1.0 / 3, scalar2=None,
                                                                  op0=ALU.mult), r=[t_f], w=[t_f])
                P.op("dve", lambda e, so=so, bcol=bcol, bo=bo: e.tensor_tensor(out=fv[:, bo:bo + 1], in0=fv[:, so:so + 1],
                                                                               in1=fv[:, bcol:bcol + 1], op=ALU.mult), r=[t_f], w=[t_f])
            pspool = Pool(nc, es3, "ps", 4, [128, 512], F32, "hp_m")
            for (wsb, src, t_src, dst, t_dst, so, bo, kk) in ((fw1_sb, zf, t_f, hd1, t_hd1, 4, 5, 33), (fw2_sb, hd1, t_hd1, hd2, t_hd2, 6, 7, 64)):
                for t0 in range(0, S, 512):
                    ps, ptr = pspool.get()
                    P.op("pe", lambda e, ps=ps, wsb=wsb, src=src, t0=t0, kk=kk: e.matmul(ps[0:64, :], wsb[0:kk, :], src[0:kk, t0:t0 + 512],
                                                                                        start=True, stop=True), r=[t_f, t_src], w=[ptr])
                    P.op("act", lambda e, ps=ps, t0=t0, so=so, bo=bo: e.activation(out=tmp[:, t0:t0 + 512], in_=ps[0:64, :], func=AF.Sin,
                                                                                   bias=fv[:, bo:bo + 1], scale=fv[:, so:so + 1]),
                         r=[ptr, t_f], w=[t_tmp])
                    P.op("dve", lambda e, dst=dst, t0=t0: e.tensor_tensor(out=dst[:, t0:t0 + 512], in0=tmp[:, t0:t0 + 512],
                                                                          in1=tmp[:, t0:t0 + 512], op=ALU.mult), r=[t_tmp], w=[t_dst])
                    P.op("dve", lambda e, dst=dst, t0=t0: e.tensor_scalar(out=dst[:, t0:t0 + 512], in0=dst[:, t0:t0 + 512], scalar1=-4.0,
                                                                          scalar2=3.0, op0=ALU.mult, op1=ALU.add), r=[t_dst], w=[t_dst])
                    P.op("dve", lambda e, dst=dst, t0=t0: e.tensor_tensor(out=dst[:, t0:t0 + 512], in0=dst[:, t0:t0 + 512],
                                                                          in1=tmp[:, t0:t0 + 512], op=ALU.mult), r=[t_dst, t_tmp], w=[t_dst])
            P.barrier()
        fw3_sb = sb("fw3", [64, 2048], F32)
        P.dma("sp", fw3_sb[:], fw3_c[:, :], w=[t_c])
        dec_t = sb("dec", [128, 16 * 512], BF16)
        dec = dec_t[:].rearrange("p (t c) -> p t c", c=512)
        t_dec = Tr()
        for tt in range(16):
            P.op("act", lambda e, tt=tt: e.activation(out=dec[:, tt, :], in_=dlt[:], func=AF.Exp, scale=t01n[:, tt:tt + 1]),
                 r=[t_c], w=[t_dec])
        zcur = xs[0]
        t_zcur = t_xs[0]
        for o in range(2):
            with ExitStack() as es4:
                hc_t = es4.enter_context(SB(nc, "h_hc%d" % o, [128, 16 * 2 * 512], BF16))
                hc = hc_t[:].rearrange("p (t k c) -> p t k c", k=2, c=512)
                t_hc = Tr()
                zt_t = es4.enter_context(SB(nc, "h_zt%d" % o, [128, 16 * 512], BF16))
                zt = zt_t[:].rearrange("p (t c) -> p t c", c=512)
                t_zt = Tr()
                yy_t = es4.enter_context(SB(nc, "h_yy%d" % o, [128, NF * 2 * 512], BF16))
                yy = yy_t[:].rearrange("p (f k c) -> p f k c", k=2, c=512)
                t_yy = Tr()
                with ExitStack() as es5:
                    pspool = Pool(nc, es5, "ps", 8, [128, 512], F32, "hp_h")
                    ftmp = Pool(nc, es5, "sb", 4, [128, 512], F32, "h_hft")
                    for tt in range(16):
                        pf, pft = pspool.get()
                        pb, pbt = pspool.get()
                        P.op("pe", lambda e, pf=pf, tt=tt: e.matmul(pf[:, :], hd2[:, tt * 128:(tt + 1) * 128],
                                                                    fw3_sb[:, (o * 2) * 512:(o * 2 + 1) * 512], start=True, stop=True),
                             r=[t_hd2, t_c], w=[pft])
                        P.op("pe", lambda e, pb=pb, tt=tt: e.matmul(pb[:, :], hd2[:, tt * 128:(tt + 1) * 128],
                                                                    fw3_sb[:, (o * 2 + 1) * 512:(o * 2 + 2) * 512], start=True, stop=True),
                             r=[t_hd2, t_c], w=[pbt])
                        f1, f1t = ftmp.get()
                        f2, f2t = ftmp.get()
                        P.op("dve", lambda e, f1=f1, pf=pf, tt=tt: e.tensor_tensor(out=f1[:], in0=pf[:, :], in1=dec[:, tt, :], op=ALU.mult),
                             r=[pft, t_dec], w=[f1t])
                        if tt == 0:
                            P.op("dve", lambda e, f2=f2, pb=pb, tt=tt: e.scalar_tensor_tensor(out=f2[:], in0=pb[:, :], scalar=mask0[:, 0:1],
                                                                                              in1=dec[:, tt, :], op0=ALU.mult, op1=ALU.mult),
                                 r=[pbt, t_dec, t_c], w=[f2t])
                        else:
                            P.op("dve", lambda e, f2=f2, pb=pb, tt=tt: e.tensor_tensor(out=f2[:], in0=pb[:, :], in1=dec[:, tt, :], op=ALU.mult),
                                 r=[pbt, t_dec], w=[f2t])
                        P.op("pool", lambda e, f1=f1, f2=f2, tt=tt: e.tensor_tensor(out=hc[:, tt, 0, :], in0=f1[:], in1=f2[:], op=ALU.add),
                             r=[f1t, f2t], w=[t_hc])
                        P.op("pool", lambda e, f1=f1, f2=f2, tt=tt: e.tensor_tensor(out=hc[:, tt, 1, :], in0=f2[:], in1=f1[:], op=ALU.subtract),
                             r=[f1t, f2t], w=[t_hc])
                    P.barrier()
                with ExitStack() as es5:
                    pstr = Pool(nc, es5, "ps", 2, [128, 1024], BF16, "hp_t")
                    for tt in range(16):
                        pt, ptt = pstr.get()
                        for ct in range(4):
                            P.op("pe", lambda e, pt=pt, ct=ct, tt=tt: e.transpose(pt[:, ct * 128:(ct + 1) * 128],
                                                                                  zcur[:, ct, tt * 128:(tt + 1) * 128], ident_b[:]),
                                 r=[t_zcur, t_c], w=[ptt])
                        evac_copy(P, zt[:, tt, :], pt[:, 0:512], [ptt], [t_zt])
                    P.barrier()
                with ExitStack() as es5:
                    pspool = Pool(nc, es5, "ps", 8, [128, 512], F32, "hp_w")
                    cblk = Pool(nc, es5, "sb", 4, [128, 16 * 128], BF16, "h_cb")
                    hsb = Pool(nc, es5, "sb", 4, [128, 512], F32, "h_hsb")
                    ttmp = Pool(nc, es5, "sb", 4, [128, 512], F32, "h_tt")
                    for fc in range(NF):
                        cb, cbt = cblk.get()
                        sbk, sbt = cblk.get()
                        cbv = cb[:].rearrange("p (t f) -> p t f", f=128)
                        sbv = sbk[:].rearrange("p (t f) -> p t f", f=128)
                        P.dma("sp", cbv, cst["CC"][0:S, fc * 128:(fc + 1) * 128].rearrange("(t p) f -> p t f", p=128), w=[cbt])
                        P.dma("sp", sbv, cst["SS"][0:S, fc * 128:(fc + 1) * 128].rearrange("(t p) f -> p t f", p=128), w=[sbt])
                        pHr, pHrt = pspool.get()
                        pHi, pHit = pspool.get()
                        pZr, pZrt = pspool.get()
                        pZs, pZst = pspool.get()
                        for tc_ in range(16):
                            st_, sp_ = (tc_ == 0), (tc_ == 15)
                            P.op("pe", lambda e, pHr=pHr, cbv=cbv, tc_=tc_, st_=st_, sp_=sp_: e.matmul(pHr[:, :], cbv[:, tc_, :], hc[:, tc_, 0, :],
                                                                                                      start=st_, stop=sp_), r=[cbt, t_hc], w=[pHrt])
                            P.op("pe", lambda e, pHi=pHi, sbv=sbv, tc_=tc_, st_=st_, sp_=sp_: e.matmul(pHi[:, :], sbv[:, tc_, :], hc[:, tc_, 1, :],
                                                                                                      start=st_, stop=sp_), r=[sbt, t_hc], w=[pHit])
                            P.op("pe", lambda e, pZr=pZr, cbv=cbv, tc_=tc_, st_=st_, sp_=sp_: e.matmul(pZr[:, :], cbv[:, tc_, :], zt[:, tc_, :],
                                                                                                      start=st_, stop=sp_), r=[cbt, t_zt], w=[pZrt])
                            P.op("pe", lambda e, pZs=pZs, sbv=sbv, tc_=tc_, st_=st_, sp_=sp_: e.matmul(pZs[:, :], sbv[:, tc_, :], zt[:, tc_, :],
                                                                                                      start=st_, stop=sp_), r=[sbt, t_zt], w=[pZst])
                        hr, hrt = hsb.get()
                        hi, hit = hsb.get()
                        P.op("act", lambda e, hr=hr, pHr=pHr, fc=fc: e.activation(out=hr[:], in_=pHr[:, :], func=AF.Identity, scale=wN[:, fc:fc + 1]),
                             r=[pHrt, t_c], w=[hrt])
                        P.op("act", lambda e, hi=hi, pHi=pHi, fc=fc: e.activation(out=hi[:], in_=pHi[:, :], func=AF.Identity, scale=wN[:, fc:fc + 1]),
                             r=[pHit, t_c], w=[hit])
                        a1, a1t = ttmp.get()
                        a2, a2t = ttmp.get()
                        P.op("dve", lambda e, a1=a1, pZr=pZr, hr=hr: e.tensor_tensor(out=a1[:], in0=pZr[:, :], in1=hr[:], op=ALU.mult),
                             r=[pZrt, hrt], w=[a1t])
                        P.op("dve", lambda e, a2=a2, pZs=pZs, hi=hi: e.tensor_tensor(out=a2[:], in0=pZs[:, :], in1=hi[:], op=ALU.mult),
                             r=[pZst, hit], w=[a2t])
                        P.op("pool", lambda e, a1=a1, a2=a2, fc=fc: e.tensor_tensor(out=yy[:, fc, 0, :], in0=a1[:], in1=a2[:], op=ALU.add),
                             r=[a1t, a2t], w=[t_yy])
                        a3, a3t = ttmp.get()
                        a4, a4t = ttmp.get()
                        P.op("dve", lambda e, a3=a3, pZs=pZs, hr=hr: e.tensor_tensor(out=a3[:], in0=pZs[:, :], in1=hr[:], op=ALU.mult),
                             r=[pZst, hrt], w=[a3t])
                        P.op("dve", lambda e, a4=a4, pZr=pZr, hi=hi: e.tensor_tensor(out=a4[:], in0=pZr[:, :], in1=hi[:], op=ALU.mult),
                             r=[pZrt, hit], w=[a4t])
                        P.op("pool", lambda e, a3=a3, a4=a4, fc=fc: e.tensor_tensor(out=yy[:, fc, 1, :], in0=a3[:], in1=a4[:], op=ALU.subtract),
                             r=[a3t, a4t], w=[t_yy])
                    P.barrier()
                with ExitStack() as es5:
                    pspool = Pool(nc, es5, "ps", 8, [128, 512], F32, "hp_i")
                    rblk = Pool(nc, es5, "sb", 4, [128, 1024], BF16, "h_rb")
                    ev = Pool(nc, es5, "sb", 3, [128, 512], F32, "h_ev")
                    znew = xs[o + 1]
                    t_zn = t_xs[o + 1]
                    ost = Pool(nc, es5, "sb", 3, [128, 512], BF16, "h_ho")
                    for th in range(2):
                        accs = [pspool.get() for _ in range(8)]
                        for fc in range(NF):
                            rc, rct = rblk.get()
                            rs_, rst = rblk.get()
                            P.dma("sp", rc[:], cst["CC"][fc * 128:(fc + 1) * 128, th * 1024:(th + 1) * 1024], w=[rct])
                            P.dma("sp", rs_[:], cst["SS"][fc * 128:(fc + 1) * 128, th * 1024:(th + 1) * 1024], w=[rst])
                            for ct in range(4):
                                for tq in range(2):
                                    ps, ptr = accs[ct * 2 + tq]
                                    P.op("pe", lambda e, ps=ps, rc=rc, fc=fc, ct=ct, tq=tq: e.matmul(
                                        ps[:, :], yy[:, fc, 0, ct * 128:(ct + 1) * 128], rc[:, tq * 512:(tq + 1) * 512],
                                        start=(fc == 0), stop=False), r=[t_yy, rct], w=[ptr])
                                    P.op("pe", lambda e, ps=ps, rs_=rs_, fc=fc, ct=ct, tq=tq: e.matmul(
                                        ps[:, :], yy[:, fc, 1, ct * 128:(ct + 1) * 128], rs_[:, tq * 512:(tq + 1) * 512],
                                        start=False, stop=(fc == NF - 1)), r=[t_yy, rst], w=[ptr])
                        for ct in range(4):
                            for tq in range(2):
                                ps, ptr = accs[ct * 2 + tq]
                                t0 = th * 1024 + tq * 512
                                e_, e_t = ev.get()
                                j = 48 + o * 4 + ct
                                P.op("dve", lambda e, e_=e_, ps=ps, ct=ct, t0=t0, j=j: e.scalar_tensor_tensor(
                                    out=e_[:], in0=zcur[:, ct, t0:t0 + 512], scalar=cvec_sb[:, j:j + 1], in1=ps[:, :],
                                    op0=ALU.mult, op1=ALU.add), r=[ptr, t_zcur, t_c], w=[e_t])
                                if o == 0:
                                    P.op("pool", lambda e, e_=e_, ct=ct, t0=t0: e.tensor_tensor(
                                        out=znew[:, ct, t0:t0 + 512], in0=znew[:, ct, t0:t0 + 512], in1=e_[:], op=ALU.mult),
                                        r=[e_t, t_zn], w=[t_zn])
                                else:
                                    o_, o_t = ost.get()
                                    P.op("pool", lambda e, e_=e_, o_=o_, ct=ct, t0=t0: e.tensor_tensor(
                                        out=o_[:], in0=znew[:, ct, t0:t0 + 512], in1=e_[:], op=ALU.mult), r=[e_t, t_zn], w=[o_t])
                                    P.dma("sp", cdT[ct * 128:(ct + 1) * 128, t0:t0 + 512], o_[:], r=[o_t], w=[t_out])
                    P.barrier()
            zcur = xs[o + 1]
            t_zcur = t_xs[o + 1]
        P.barrier()


def cd_consts():
    c = {}
    L = S
    t01 = np.linspace(0.0, 1.0, L, dtype=np.float32)
    w = (2.0 * np.pi * np.arange(L, dtype=np.float32) / L).astype(np.float32)
    bands = np.linspace(1e-4, 15, 16, dtype=np.float32)
    z = np.concatenate([t01[:, None], np.cos(w[:, None] * bands), -np.sin(w[:, None] * bands)], -1).astype(np.float32)
    c["zfeatT"] = np.ascontiguousarray(z.T)
    n = NF * 128
    a = np.arange(n, dtype=np.int64)
    prod = (a[:, None] * a[None, :]) % 4096
    ang = prod.astype(np.float64) * (2.0 * np.pi / 4096)
    valid = (a[:, None] <= 2048) & (a[None, :] <= 2048)
    c["CC"] = np.where(valid, np.cos(ang), 0.0).astype(np.float32).astype(ml_dtypes.bfloat16)
    c["SS"] = np.where(valid, np.sin(ang), 0.0).astype(np.float32).astype(ml_dtypes.bfloat16)
    wf = np.where((a == 0) | (a == 2048), 1.0, 2.0) / 4096.0
    wf = np.where(a <= 2048, wf, 0.0)
    c["wN"] = np.ascontiguousarray(wf.reshape(NF, 128).T).astype(np.float32)
    c["t01n"] = np.ascontiguousarray((-t01).reshape(16, 128).T).astype(np.float32)
    m0 = np.ones((128, 1), dtype=np.float32)
    m0[0, 0] = 0.0
    c["mask0"] = m0
    t = np.arange(L, dtype=np.int64)
    pf = ((t[:, None] * t[None, :]) % L).astype(np.float64) * (2.0 * np.pi / L)
    c["FC"] = np.cos(pf).astype(np.float32).astype(ml_dtypes.bfloat16)
    c["FS"] = np.sin(pf).astype(np.float32).astype(ml_dtypes.bfloat16)
    k = np.arange(256, dtype=np.int64)
    pw = ((k[:, None] * k[None, :]) % 256).astype(np.float64) * (2.0 * np.pi / 256)
    sc = 1.0 / np.sqrt(float(L) * 256.0)
    c["CSW"] = np.concatenate([np.cos(pw) * sc, -np.sin(pw) * sc], axis=1).astype(np.float32).astype(ml_dtypes.bfloat16)
    return c


def cd_host_inputs(inp, m1, b, hf):
    def fm(v, n=16):
        return np.ascontiguousarray(v.reshape(n, 128).T)
    w = inp["cd_w_in"][0]
    cols = []
    for o in range(3):
        cols += list(range(o * 1024 + 512 * hf, o * 1024 + 512 * hf + 512))
    cols += list(range(3072 + 512 * hf, 3072 + 512 * hf + 512))
    cw = inp["cd_conv_w"][0]
    cb = inp["cd_conv_b"][0]
    cvec = np.zeros((128, 56), dtype=np.float32)
    for o in range(3):
        for ct in range(4):
            ch = o * 1024 + 512 * hf + ct * 128 + np.arange(128)
            j = (o * 4 + ct) * 4
            cvec[:, j] = cw[0, ch]
            cvec[:, j + 1] = cw[1, ch]
            cvec[:, j + 2] = cw[2, ch]
            cvec[:, j + 3] = cb[ch]
    sk = inp["cd_skip"][0]
    for o in range(2):
        for ct in range(4):
            cvec[:, 48 + o * 4 + ct] = sk[o, 512 * hf + ct * 128 + np.arange(128)]
    fw3 = inp["cd_filt_w3"][0]
    f3c = []
    for o in range(2):
        for dr in range(2):
            f3c += list(range(o * 2048 + dr * 1024 + 512 * hf, o * 2048 + dr * 1024 + 512 * hf + 512))
    fvec = np.stack([inp["cd_filt_b1"][0], inp["cd_filt_freq1"][0], inp["cd_filt_b2"][0], inp["cd_filt_freq2"][0]], axis=1)
    import math
    mn = math.log(1e-2) / 1.5
    mx = math.log(1e-2) / 0.3
    deltas = np.abs(np.linspace(mn, mx, 1024, dtype=np.float32))[512 * hf:512 * hf + 512]
    mb = m1[b]
    return {"modv": np.concatenate([fm(mb[0:2048]), fm(mb[2048:4096])], axis=1), "w_in_c": np.ascontiguousarray(w[:, cols]),
            "cvec": cvec, "fw1": np.ascontiguousarray(inp["cd_filt_w1"][0]), "fw2": np.ascontiguousarray(inp["cd_filt_w2"][0]),
            "fvec": np.ascontiguousarray(fvec.astype(np.float32)), "fw3_c": np.ascontiguousarray(fw3[:, f3c]),
            "delta_b": np.tile(deltas[None, :], (128, 1)).astype(np.float32)}


def build_cd_prog():
    nc = bass.Bass("TRN2", target_bir_lowering=False)
    consts = make_consts()
    consts.update(cd_consts())
    with ExitStack() as es:
        P = Prog(nc, es)
        cst = declare_consts(nc, consts)
        hT = dram_in(nc, "hT", [D, S])
        modv = dram_in(nc, "modv", [128, 32])
        w_in_c = dram_in(nc, "w_in_c", [D, 2048])
        cvec = dram_in(nc, "cvec", [128, 56])
        fw1 = dram_in(nc, "fw1", [33, 64])
        fw2 = dram_in(nc, "fw2", [64, 64])
        fvec = dram_in(nc, "fvec", [64, 4])
        fw3_c = dram_in(nc, "fw3_c", [64, 2048])
        delta_b = dram_in(nc, "delta_b", [128, 512])
        cdT = dram_out(nc, "cdT", [1024, S], BF16)
        stage_cd(P, cst, hT, modv, w_in_c, cvec, fw1, fw2, fvec, fw3_c, delta_b, cdT, Tr(), Tr())
        P.barrier()
    return nc, consts


ADA_BF16 = True


def stage_ada(P, csT, adaw, adab, mT, t_out):
    nc = P.nc
    with ExitStack() as es:
        cs = es.enter_context(SB(nc, "d_cs", [128, 80], F32))
        sT = es.enter_context(SB(nc, "d_sT", [128, 80], F32))
        bia = es.enter_context(SB(nc, "d_b", [128, 24], F32))
        res = es.enter_context(SB(nc, "d_res", [128, 120], F32))
        t_c = Tr()
        t_res = Tr()
        P.dma("sp", cs[:], csT[:, :], w=[t_c])
        P.dma("sp", bia[:], adab[:, :], w=[t_c])
        sTb = es.enter_context(SB(nc, "d_sTb", [128, 80], BF16))
        P.op("act", lambda e: e.activation(out=sT[:], in_=cs[:], func=AF.Silu), r=[t_c], w=[t_c])
        P.op("dve", lambda e: e.tensor_copy(out=sTb[:], in_=sT[:]), r=[t_c], w=[t_c])
        wb = Pool(nc, es, "sb", 3, [128, 1536], BF16 if ADA_BF16 else F32, "d_w")
        ps = [es.enter_context(PSM(nc, "dp_%d" % i, [128, 512], F32)) for i in range(2)]
        t_ps = [Tr(), Tr()]
        for l in range(2):
            for k in range(16):
                w_, w_t = wb.get()
                P.dma("pool" if ADA_BF16 else "sp", w_[:], adaw[l, k * 128:(k + 1) * 128, :], w=[w_t])
                for tl in range(12):
                    P.op("pe", lambda e, w_=w_, l=l, k=k, tl=tl: e.matmul(ps[l][:, tl * 5:(tl + 1) * 5], w_[:, tl * 128:(tl + 1) * 128],
                                                                          (sTb if ADA_BF16 else sT)[:, k * 5:(k + 1) * 5], start=(k == 0 and tl == 0),
                                                                          stop=(k == 15), skip_group_check=True),
                         r=[w_t, t_c], w=[t_ps[l]])
            P.op("dve", lambda e, l=l: e.tensor_tensor(out=res[:, l * 60:(l + 1) * 60].rearrange("p (t j) -> p t j", j=5),
                                                       in0=ps[l][:, 0:60].rearrange("p (t j) -> p t j", j=5),
                                                       in1=bia[:, l * 12:(l + 1) * 12].unsqueeze(2).to_broadcast([128, 12, 5]), op=ALU.add),
                 r=[t_ps[l], t_c], w=[t_res])
        P.dma("sp", mT[:, :], res[:], r=[t_res], w=[t_out])
        P.barrier()


def _new():
    return bass.Bass("TRN2", target_bir_lowering=False)


def build_ada_prog():
    nc = _new()
    with ExitStack() as es:
        P = Prog(nc, es)
        csT = dram_in(nc, "csT", [128, 80])
        adaw = dram_in(nc, "adaw", [2, D, 1536])
        adab = dram_in(nc, "adab", [128, 24])
        mT = dram_out(nc, "mT", [128, 120])
        stage_ada(P, csT, adaw, adab, mT, Tr())
    return nc, {}


def build_route_prog():
    nc = _new()
    consts = make_consts()
    with ExitStack() as es:
        P = Prog(nc, es)
        cst = declare_consts(nc, consts)
        hT = dram_in(nc, "hT", [D, S])
        modv = dram_in(nc, "modv", [128, 32])
        router = dram_in(nc, "router", [D, 16])
        xe_out = dram_out(nc, "xe_out", [8, 128, 16 * CAP], BF16)
        pos_out = dram_out(nc, "pos_out", [16, S])
        gate_out = dram_out(nc, "gate_out", [16, S])
        stage_moe_route(P, cst, hT, modv, router, xe_out, pos_out, gate_out, Tr(), Tr())
    return nc, consts


def build_ffn_prog():
    nc = _new()
    with ExitStack() as es:
        P = Prog(nc, es)
        xe_in = dram_in(nc, "xe_in", [2, 4, 128, 16 * CAP], BF16)
        w1 = dram_in(nc, "w1", [2, D, FF])
        w3 = dram_in(nc, "w3", [2, D, FF])
        w2 = dram_in(nc, "w2", [2, FF, D])
        ye_out = dram_out(nc, "ye_out", [2, 8, 128, D], BF16)
        stage_moe_ffn(P, xe_in, w1, w3, w2, ye_out, Tr(), Tr())
    return nc, {}


def build_scat_prog():
    nc = _new()
    consts = make_consts()
    with ExitStack() as es:
        P = Prog(nc, es)
        cst = declare_consts(nc, consts)
        ye_in = dram_in(nc, "ye_in", [16, 2, 128, D], BF16)
        pos_in = dram_in(nc, "pos_in", [16, 1024])
        gate_in = dram_in(nc, "gate_in", [16, 1024])
        hT = dram_in(nc, "hT", [D, 1024])
        vecs = dram_in(nc, "vecs", [128, 48])
        outT = dram_out(nc, "outT", [D, 1024])
        stage_scatter_ln(P, cst, ye_in, pos_in, gate_in, hT, vecs, outT, Tr(), Tr())
    return nc, consts


def build_oproj_prog():
    nc = _new()
    consts = make_consts()
    with ExitStack() as es:
        P = Prog(nc, es)
        cst = declare_consts(nc, consts)
        ao_in = dram_in(nc, "ao_in", [2048, 1024], BF16)
        w_out = dram_in(nc, "w_out", [2048, D])
        hT = dram_in(nc, "hT", [D, 1024])
        vecs = dram_in(nc, "vecs", [128, 48])
        outT = dram_out(nc, "outT", [D, 1024])
        stage_oproj_ln(P, cst, ao_in, w_out, hT, vecs, outT, Tr(), Tr())
    return nc, consts


def _fm(v, n=16):
    return np.ascontiguousarray(np.asarray(v, dtype=np.float32).reshape(n, 128).T)


def _run(nc, consts, in_maps):
    for im in in_maps:
        for k, v in consts.items():
            im["c_" + k] = v
    res = run_bass_kernel_spmd(nc, in_maps, core_ids=list(range(len(in_maps))))
    return res.results


DEBUG = {}


def _dbg(name, arr):
    if "ref" in DEBUG:
        ref = DEBUG["ref"][name]
        err = float(np.sqrt(((arr - ref) ** 2).mean() / (ref ** 2).mean()))
        print("DBG %s relerr %.5f" % (name, err), flush=True)


def kernel_unfused(**inp):
    inp = {k: np.asarray(v) for k, v in inp.items()}
    progs = {}

    def prog(name, fn):
        if name not in progs:
            progs[name] = fn()
        return progs[name]

    nc, consts = prog("ada", build_ada_prog)
    cvs = np.concatenate([inp["c"], inp["c_ctx"][None, :]], axis=0)
    csT = np.ascontiguousarray(cvs.reshape(5, 16, 128).transpose(2, 1, 0).reshape(128, 80))
    ims = []
    for c in range(8):
        ab = inp["ada_b"][:, c * 1536:(c + 1) * 1536].reshape(2, 12, 128)
        ims.append({"csT": csT, "adaw": np.ascontiguousarray(inp["ada_w"][:, :, c * 1536:(c + 1) * 1536]),
                    "adab": np.ascontiguousarray(ab.transpose(2, 0, 1).reshape(128, 24))})
    r = _run(nc, consts, ims)
    m = np.zeros((2, 5, 12288), dtype=np.float32)
    for c in range(8):
        o = np.asarray(r[c]["mT"]).reshape(128, 2, 12, 5)
        m[:, :, c * 1536:(c + 1) * 1536] = o.transpose(1, 3, 2, 0).reshape(2, 5, 1536)
    if "ref" in DEBUG:
        _dbg("m0", m[0, 0:4])
        _dbg("m1", m[1, 0:4])

    def vecs(l, b, which):
        mb = m[l, b]
        g = mb[2 * D:3 * D] if which == 1 else mb[5 * D:6 * D]
        lg = inp["ln1_g" if which == 1 else "ln2_g"][l]
        lb = inp["ln1_b" if which == 1 else "ln2_b"][l]
        return np.concatenate([_fm(g), _fm(lg), _fm(lb)], axis=1)

    def oproj(ao_full, w_out, hT_halves, l):
        nc, consts = prog("oproj", build_oproj_prog)
        ims = []
        for core in range(8):
            b, th = core // 2, core % 2
            ims.append({"ao_in": np.ascontiguousarray(ao_full[b][:, th * 1024:(th + 1) * 1024]), "w_out": w_out,
                        "hT": hT_halves[core], "vecs": vecs(l, b, 1)})
        r = _run(nc, consts, ims)
        return [np.asarray(r[c]["outT"]) for c in range(8)]

    def moe(hT_halves, l):
        hfull = [np.ascontiguousarray(np.concatenate([hT_halves[2 * b], hT_halves[2 * b + 1]], axis=1)) for b in range(4)]
        nc, consts = prog("route", build_route_prog)
        ims = []
        perms = []
        for core in range(8):
            b, hf = core // 2, core % 2
            perm = np.r_[np.arange(8 * hf, 8 * hf + 8), np.arange(8 * (1 - hf), 8 * (1 - hf) + 8)]
            perms.append(perm)
            mb = m[l, b]
            ims.append({"hT": hfull[b], "modv": np.concatenate([_fm(mb[3 * D:4 * D]), _fm(mb[4 * D:5 * D])], axis=1),
                        "router": np.ascontiguousarray(inp["router"][l][:, perm])})
        rr = _run(nc, consts, ims)
        nc, consts = prog("ffn", build_ffn_prog)
        ims = []
        for c in range(8):
            xe = np.stack([np.stack([np.asarray(rr[2 * b + (2 * c + i) // 8]["xe_out"])[(2 * c + i) % 8] for b in range(4)]) for i in range(2)])
            ims.append({"xe_in": xe, "w1": inp["exp_w1"][l, 2 * c:2 * c + 2], "w3": inp["exp_w3"][l, 2 * c:2 * c + 2],
                        "w2": inp["exp_w2"][l, 2 * c:2 * c + 2]})
        rf = _run(nc, consts, ims)
        nc, consts = prog("scat", build_scat_prog)
        ims = []
        for core in range(8):
            b, th = core // 2, core % 2
            perm = perms[core]
            ye = np.stack([np.asarray(rf[e // 2]["ye_out"])[e % 2, 2 * b:2 * b + 2] for e in perm])
            ims.append({"ye_in": ye, "pos_in": np.ascontiguousarray(np.asarray(rr[core]["pos_out"])[:, th * 1024:(th + 1) * 1024]),
                        "gate_in": np.ascontiguousarray(np.asarray(rr[core]["gate_out"])[:, th * 1024:(th + 1) * 1024]),
                        "hT": hT_halves[core], "vecs": vecs(l, b, 2)})
        rs = _run(nc, consts, ims)
        return [np.asarray(rs[c]["outT"]) for c in range(8)]

    def dbg_h(name, halves):
        if "ref" in DEBUG:
            full = np.stack([np.concatenate([halves[2 * b], halves[2 * b + 1]], axis=1).T for b in range(4)])
            _dbg(name, full)

    nc, consts = prog("attn", build_attn_prog)
    ims = [attn_host_inputs(inp, m[0], core // 2, core % 2) for core in range(8)]
    ra = _run(nc, consts, ims)
    ao_full = []
    for b in range(4):
        a0 = np.asarray(ra[2 * b]["aoT"])
        a1 = np.asarray(ra[2 * b + 1]["aoT"])
        ao_full.append(np.concatenate([a0[0:512], a1[0:512], a0[512:1024], a1[512:1024]], axis=0))
    xT_halves = [np.ascontiguousarray(inp["x"][core // 2][(core % 2) * 1024:(core % 2 + 1) * 1024].T) for core in range(8)]
    h = oproj(ao_full, np.ascontiguousarray(inp["ab_w_out"][0]), xT_halves, 0)
    dbg_h("h0a", h)
    h = moe(h, 0)
    dbg_h("h0b", h)
    nc, consts = prog("cd", build_cd_prog)
    ims = []
    for core in range(8):
        b, hf = core // 2, core % 2
        im = cd_host_inputs(inp, m[1], b, hf)
        im["hT"] = np.ascontiguousarray(np.concatenate([h[2 * b], h[2 * b + 1]], axis=1))
        ims.append(im)
    rc = _run(nc, consts, ims)
    cd_full = []
    for b in range(4):
        a0 = np.asarray(rc[2 * b]["cdT"])
        a1 = np.asarray(rc[2 * b + 1]["cdT"])
        cd_full.append(np.concatenate([a0[0:512], a1[0:512], a0[512:1024], a1[512:1024]], axis=0))
    h = oproj(cd_full, np.ascontiguousarray(inp["cd_w_out"][0]), h, 1)
    dbg_h("h1a", h)
    h = moe(h, 1)
    dbg_h("h1b", h)
    out = np.zeros((NB, S, D), dtype=np.float32)
    for core in range(8):
        b, th = core // 2, core % 2
        out[b, th * 1024:(th + 1) * 1024, :] = h[core].T
    return out


U32 = mybir.dt.uint32


def build_fused_prog():
    nc = _new()
    consts = make_consts()
    consts.update(cd_consts())
    with ExitStack() as es:
        P = Prog(nc, es)
        cst = declare_consts(nc, consts)
        I = {}

        def din(name, shape, dt=F32):
            I[name] = dram_in(nc, name, shape, dt)
            return I[name]
        csT = din("csT", [128, 80]); adaw = din("adaw", [2, D, 1536]); adab = din("adab", [128, 24]); oh5 = din("oh5", [128, 5])
        xT = din("xT", [D, S]); ctxT = din("ctxT", [D, LC]); w_in_r = din("w_in_r", [D, 3328]); kvn = din("kvn", [128, 4])
        w_ukv_r = din("w_ukv_r", [512, 1024]); cosT = din("cosT", [128, S]); sinT = din("sinT", [128, S])
        na_T = din("na_T", [128, 4 * TB_N * 64]); na_RM = din("na_RM", [128, 256])
        xown = din("xown", [D, 1024]); w_out0 = din("w_out0", [2048, D]); w_out1 = din("w_out1", [2048, D])
        lnv = din("lnv", [128, 128])
        router = [din("router0", [D, 16]), din("router1", [D, 16])]
        w1 = [din("w1_0", [2, D, FF]), din("w1_1", [2, D, FF])]
        w3 = [din("w3_0", [2, D, FF]), din("w3_1", [2, D, FF])]
        w2 = [din("w2_0", [2, FF, D]), din("w2_1", [2, FF, D])]
        w_in_c = din("w_in_c", [D, 2048]); cvec = din("cvec", [128, 56]); fw1 = din("fw1", [33, 64]); fw2 = din("fw2", [64, 64])
        fvec = din("fvec", [64, 4]); fw3_c = din("fw3_c", [64, 2048]); delta_b = din("delta_b", [128, 512])
        idx_ao = din("idx_ao", [128, 16], U32); idx_h = din("idx_h", [128, 32], U32); idx_xe = din("idx_xe", [128, 8], U32)
        idx_ye = din("idx_ye", [128, 64], U32); oh2 = din("oh2", [128, 2])
        outT = dram_out(nc, "outT", [D, 1024])
        idx_sb = es.enter_context(SB(nc, "x_idx", [128, 120], U32))
        oh2_sb = es.enter_context(SB(nc, "x_oh2", [128, 2], F32))
        t_pg = Tr()
        t_i = Tr()
        P.dma("sp", idx_sb[:, 0:16], idx_ao[:, :], w=[t_i])
        P.dma("sp", idx_sb[:, 16:48], idx_h[:, :], w=[t_i])
        P.dma("sp", idx_sb[:, 48:56], idx_xe[:, :], w=[t_i])
        P.dma("sp", idx_sb[:, 56:120], idx_ye[:, :], w=[t_i])
        P.dma("sp", oh2_sb[:], oh2[:, :], w=[t_i])
        IA, IH, IX, IY = 0, 16, 48, 56

        def ds(name, shape, dt=F32):
            return dram_scratch(nc, name, shape, dt)
        mT_d = ds("x_mT", [128, 120]); gat_m = ds("x_gm", [8 * 128, 120])
        mod_d = ds("x_mod", [128, 2 * 96]); ctx_d = ds("x_ctx", [128, 2 * 96])
        stage_ada(P, csT, adaw, adab, mT_d, Tr())
        P.barrier()
        P.allgather(mT_d[:, :], gat_m[:, :])
        P.barrier()
        with ExitStack() as es0:
            mall = es0.enter_context(SB(nc, "x_mall", [128, 8 * 120], F32))
            tmp = es0.enter_context(SB(nc, "x_mtmp", [128, 8 * 120], F32))
            msel = es0.enter_context(SB(nc, "x_msel", [128, 2 * 192], F32))
            oh5_sb = es0.enter_context(SB(nc, "x_oh5", [128, 5], F32))
            t_m = Tr()
            P.dma("sp", mall[:].rearrange("p (c x) -> p c x", x=120), gat_m.rearrange("(c p) x -> p c x", p=128), w=[t_m])
            P.dma("sp", oh5_sb[:], oh5[:, :], w=[t_m])
            P.op("dve", lambda e: e.tensor_tensor(out=tmp[:].rearrange("p (q j) -> p q j", j=5), in0=mall[:].rearrange("p (q j) -> p q j", j=5),
                                                  in1=oh5_sb[:].unsqueeze(1).to_broadcast([128, 192, 5]), op=ALU.mult), r=[t_m], w=[t_m])
            P.op("dve", lambda e: e.tensor_reduce(out=msel[:, 0:192], in_=tmp[:].rearrange("p (q j) -> p q j", j=5), axis=AX.X, op=ALU.add),
                 r=[t_m], w=[t_m])
            P.op("dve", lambda e: e.tensor_copy(out=msel[:, 192:384], in_=mall[:].rearrange("p (q j) -> p q j", j=5)[:, :, 4]), r=[t_m], w=[t_m])
            for l in range(2):
                for (src0, dst) in ((0, mod_d), (192, ctx_d)):
                    P.dma("sp", dst[:, l * 96:(l + 1) * 96].rearrange("p (c t) -> p c t", t=12),
                          msel[:, src0:src0 + 192].rearrange("p (c l t) -> p c l t", l=2, t=12)[:, :, l, :], r=[t_m], w=[t_m])
            P.barrier()

        def modp(l, a, b_):
            return mod_d[:, l * 96 + a:l * 96 + b_]

        def lnp(l, k):
            return lnv[:, (l * 4 + k) * 16:(l * 4 + k + 1) * 16]
        ao_d = ds("x_ao", [1024, S], BF16); gat_ao = ds("x_gao", [8 * 1024, S], BF16)
        stage_attn(P, cst, xT, ctxT, [modp(0, 0, 32), ctx_d[:, 0:32]], w_in_r, kvn, w_ukv_r, cosT, sinT, na_T, na_RM, ao_d, Tr())
        P.barrier()
        P.allgather(ao_d[:, :], gat_ao[:, :])
        P.barrier()

        def mk_load_ao(gat):
            rows = gat.rearrange("r (h t) -> (r h) t", h=2)

            def load_ao(dst, w):
                for c in range(16):
                    P.idma(dst[:, c, :], rows, idx_sb[:, IA + c:IA + c + 1], r=[t_i], w=w)
            return load_ao

        def mk_load_h(gat):
            def load_h(dst, c, w):
                for half in range(2):
                    P.idma(dst[:, half * 1024:(half + 1) * 1024], gat[:, :], idx_sb[:, IH + c * 2 + half:IH + c * 2 + half + 1], r=[t_i], w=w)
            return load_h

        def layer_moe(l, hA_d, h_out):
            gat_h = ds("x_gh%d" % l, [8 * D, 1024])
            P.allgather(hA_d[:, :], gat_h[:, :])
            P.barrier()
            xe_d = ds("x_xe%d" % l, [8, 128, 16 * CAP], BF16); gat_xe = ds("x_gxe%d" % l, [8 * 8 * 128, 16 * CAP], BF16)
            pos_d = ds("x_pos%d" % l, [16, S]); gate_d = ds("x_gate%d" % l, [16, S])
            stage_moe_route(P, cst, None, [modp(l, 48, 80)], router[l], xe_d, pos_d, gate_d, Tr(), Tr(), load_h=mk_load_h(gat_h))
            P.barrier()
            P.allgather(xe_d.rearrange("e p x -> (e p) x"), gat_xe[:, :])
            P.barrier()
            ye_d = ds("x_ye%d" % l, [2, 8, 128, D], BF16); gat_ye = ds("x_gye%d" % l, [8 * 16 * 128, D], BF16)
            gxe3 = gat_xe.rearrange("r (c s) -> r c s", s=CAP)

            def load_xe(dst, ex, b, w):
                P.idma(dst, gxe3, idx_sb[:, IX + ex * 4 + b:IX + ex * 4 + b + 1], r=[t_i], w=w)
            stage_moe_ffn(P, None, w1[l], w3[l], w2[l], ye_d, Tr(), Tr(), load_xe=load_xe)
            P.barrier()
            P.allgather(ye_d.rearrange("e g p d -> (e g p) d"), gat_ye[:, :])
            P.barrier()
            gye_rows = gat_ye.rearrange("r (h d) -> (r h) d", h=2)

            def load_ye(dst, q, dh, w):
                for st in range(2):
                    col = IY + (q * 2 + st) * 2 + dh
                    P.idma(dst[:, st, :], gye_rows, idx_sb[:, col:col + 1], r=[t_i], w=w)

            pgt = []

            def load_pg(pos, gate, w):
                for (src, dstp) in ((pos_d, pos), (gate_d, gate)):
                    P.dma("sp", pgt[0][:], src[:, :], w=[t_pg])
                    P.op("dve", lambda e, dstp=dstp: e.tensor_scalar(out=dstp, in0=pgt[0][:, 0:1024], scalar1=oh2_sb[0:16, 0:1], scalar2=None,
                                                                     op0=ALU.mult), r=[t_pg, t_i], w=w)
                    P.op("dve", lambda e, dstp=dstp: e.scalar_tensor_tensor(out=dstp, in0=pgt[0][:, 1024:2048], scalar=oh2_sb[0:16, 1:2],
                                                                            in1=dstp, op0=ALU.mult, op1=ALU.add), r=[t_pg, t_i], w=w)
            with ExitStack() as es_pg:
                pgt.append(es_pg.enter_context(SB(nc, "x_pgt", [16, S], F32)))
                stage_scatter_ln(P, cst, None, None, None, hA_d, [modp(l, 80, 96), lnp(l, 2), lnp(l, 3)], h_out, Tr(), Tr(),
                                 load_ye=load_ye, load_pg=load_pg)
                P.barrier()
                pgt.pop()

        hA0 = ds("x_hA0", [D, 1024])
        stage_oproj_ln(P, cst, None, w_out0, xown, [modp(0, 32, 48), lnp(0, 0), lnp(0, 1)], hA0, Tr(), Tr(), load_ao=mk_load_ao(gat_ao))
        P.barrier()
        hB0 = ds("x_hB0", [D, 1024])
        layer_moe(0, hA0, hB0)
        gat_hb = ds("x_ghb", [8 * D, 1024])
        P.allgather(hB0[:, :], gat_hb[:, :])
        P.barrier()
        cd_d = ds("x_cd", [1024, S], BF16); gat_cd = ds("x_gcd", [8 * 1024, S], BF16)
        stage_cd(P, cst, None, [modp(1, 0, 32)], w_in_c, cvec, fw1, fw2, fvec, fw3_c, delta_b, cd_d, Tr(), Tr(), load_h=mk_load_h(gat_hb))
        P.barrier()
        P.allgather(cd_d[:, :], gat_cd[:, :])
        P.barrier()
        hA1 = ds("x_hA1", [D, 1024])
        stage_oproj_ln(P, cst, None, w_out1, hB0, [modp(1, 32, 48), lnp(1, 0), lnp(1, 1)], hA1, Tr(), Tr(), load_ao=mk_load_ao(gat_cd))
        P.barrier()
        layer_moe(1, hA1, outT)
        P.barrier()
    return nc, consts


def fused_host_inputs(inp, consts):
    ims = []
    cvs = np.concatenate([inp["c"], inp["c_ctx"][None, :]], axis=0)
    csT = np.ascontiguousarray(cvs.reshape(5, 16, 128).transpose(2, 1, 0).reshape(128, 80))
    cd_in = {k: inp[k] for k in inp if k.startswith("cd_")}
    m_dummy = np.zeros((5, 12288), dtype=np.float32)
    p = np.arange(128)
    for core in range(8):
        b, hf = core // 2, core % 2
        im = {}
        ab = inp["ada_b"][:, core * 1536:(core + 1) * 1536].reshape(2, 12, 128)
        im["csT"] = csT
        im["adaw"] = np.ascontiguousarray(inp["ada_w"][:, :, core * 1536:(core + 1) * 1536])
        im["adab"] = np.ascontiguousarray(ab.transpose(2, 0, 1).reshape(128, 24))
        oh5 = np.zeros((128, 5), dtype=np.float32); oh5[:, b] = 1.0
        im["oh5"] = oh5
        a = attn_host_inputs(inp, m_dummy, b, hf)
        a.pop("modv")
        im.update(a)
        im["xown"] = np.ascontiguousarray(inp["x"][b][hf * 1024:(hf + 1) * 1024].T)
        im["w_out0"] = np.ascontiguousarray(inp["ab_w_out"][0])
        im["w_out1"] = np.ascontiguousarray(inp["cd_w_out"][0])
        lnv = np.zeros((128, 128), dtype=np.float32)
        for l in range(2):
            for k, nm in enumerate(("ln1_g", "ln1_b", "ln2_g", "ln2_b")):
                lnv[:, (l * 4 + k) * 16:(l * 4 + k + 1) * 16] = _fm(inp[nm][l])
        im["lnv"] = lnv
        perm = np.r_[np.arange(8 * hf, 8 * hf + 8), np.arange(8 * (1 - hf), 8 * (1 - hf) + 8)]
        for l in range(2):
            im["router%d" % l] = np.ascontiguousarray(inp["router"][l][:, perm])
            im["w1_%d" % l] = inp["exp_w1"][l, 2 * core:2 * core + 2]
            im["w3_%d" % l] = inp["exp_w3"][l, 2 * core:2 * core + 2]
            im["w2_%d" % l] = inp["exp_w2"][l, 2 * core:2 * core + 2]
        cdh = cd_host_inputs(cd_in, np.zeros((4, 12288), dtype=np.float32), b, hf)
        cdh.pop("modv")
        im.update(cdh)
        idx_ao = np.zeros((128, 16), dtype=np.uint32)
        for c in range(16):
            part = c // 4
            rank = 2 * b + (part % 2)
            row = (part // 2) * 512 + (c % 4) * 128 + p
            idx_ao[:, c] = (rank * 1024 + row) * 2 + hf
        idx_h = np.zeros((128, 32), dtype=np.uint32)
        for c in range(16):
            for half in range(2):
                idx_h[:, c * 2 + half] = (2 * b + half) * D + c * 128 + p
        idx_xe = np.zeros((128, 8), dtype=np.uint32)
        for ex in range(2):
            e = 2 * core + ex
            for bb in range(4):
                rank = 2 * bb + e // 8
                idx_xe[:, ex * 4 + bb] = (rank * 8 + e % 8) * 128 + p
        idx_ye = np.zeros((128, 64), dtype=np.uint32)
        for q in range(16):
            e = perm[q]
            for st in range(2):
                row = ((e // 2) * 2 + e % 2) * 8 + (2 * b + st)
                for dh in range(2):
                    idx_ye[:, (q * 2 + st) * 2 + dh] = (row * 128 + p) * 2 + dh
        oh2 = np.zeros((128, 2), dtype=np.float32); oh2[:, hf] = 1.0
        im.update({"idx_ao": idx_ao, "idx_h": idx_h, "idx_xe": idx_xe, "idx_ye": idx_ye, "oh2": oh2})
        ims.append(im)
    return ims


def kernel_fused(**inp):
    inp = {k: np.asarray(v) for k, v in inp.items()}
    nc, consts = build_fused_prog()
    ims = fused_host_inputs(inp, consts)
    r = _run(nc, consts, ims)
    out = np.zeros((NB, S, D), dtype=np.float32)
    for core in range(8):
        b, th = core // 2, core % 2
        out[b, th * 1024:(th + 1) * 1024, :] = np.asarray(r[core]["outT"]).T
    return out


def build_solo_prog(l0_only=False):
    nc = _new()
    consts = make_consts()
    consts.update(cd_consts())
    with ExitStack() as es:
        P = Prog(nc, es)
        cst = declare_consts(nc, consts)

        def din(name, shape, dt=F32):
            return dram_in(nc, name, shape, dt)

        def ds(name, shape, dt=F32):
            return dram_scratch(nc, name, shape, dt)
        csT = din("csT", [128, 80]); adaw = din("adaw", [2, D, 12288]); adab = din("adab", [128, 8 * 24]); oh5 = din("oh5", [128, 5])
        xT = din("xT", [D, S]); ctxT = din("ctxT", [D, LC]); kvn = din("kvn", [128, 4])
        w_in_r = [din("w_in_r%d" % h, [D, 3328]) for h in range(2)]
        w_ukv_r = [din("w_ukv_r%d" % h, [512, 1024]) for h in range(2)]
        na_T = [din("na_T%d" % h, [128, 4 * TB_N * 64]) for h in range(2)]
        cosT = din("cosT", [128, S]); sinT = din("sinT", [128, S]); na_RM = din("na_RM", [128, 256])
        w_out = [din("w_out0", [2048, D]), din("w_out1", [2048, D])]
        lnv = din("lnv", [128, 128])
        router = [din("router%d" % l, [D, 16]) for l in range(2)]
        nl = 1 if l0_only else 2
        w1 = [din("w1_%d" % l, [16, D, FF]) for l in range(nl)]
        w3 = [din("w3_%d" % l, [16, D, FF]) for l in range(nl)]
        w2 = [din("w2_%d" % l, [16, FF, D]) for l in range(nl)]
        w_in_c = [din("w_in_c%d" % h, [D, 2048]) for h in range(2)]
        cvec = [din("cvec%d" % h, [128, 56]) for h in range(2)]
        fw3_c = [din("fw3_c%d" % h, [64, 2048]) for h in range(2)]
        delta_b = [din("delta_b%d" % h, [128, 512]) for h in range(2)]
        fw1 = din("fw1", [33, 64]); fw2 = din("fw2", [64, 64]); fvec = din("fvec", [64, 4])
        outT = dram_out(nc, "outT", [D, S])
        m_d = ds("y_m", [8 * 128, 120])
        for sl in range(8):
            stage_ada(P, csT, adaw[:, :, sl * 1536:(sl + 1) * 1536], adab[:, sl * 24:(sl + 1) * 24], m_d[sl * 128:(sl + 1) * 128, :], Tr())
            P.barrier()
        mod_d = ds("y_mod", [128, 2 * 96]); ctx_d = ds("y_ctx", [128, 2 * 96])
        with ExitStack() as es0:
            mall = es0.enter_context(SB(nc, "y_mall", [128, 8 * 120], F32))
            tmp = es0.enter_context(SB(nc, "y_mtmp", [128, 8 * 120], F32))
            msel = es0.enter_context(SB(nc, "y_msel", [128, 2 * 192], F32))
            oh5_sb = es0.enter_context(SB(nc, "y_oh5", [128, 5], F32))
            t_m = Tr()
            P.dma("sp", mall[:].rearrange("p (c x) -> p c x", x=120), m_d.rearrange("(c p) x -> p c x", p=128), w=[t_m])
            P.dma("sp", oh5_sb[:], oh5[:, :], w=[t_m])
            P.op("dve", lambda e: e.tensor_tensor(out=tmp[:].rearrange("p (q j) -> p q j", j=5), in0=mall[:].rearrange("p (q j) -> p q j", j=5),
                                                  in1=oh5_sb[:].unsqueeze(1).to_broadcast([128, 192, 5]), op=ALU.mult), r=[t_m], w=[t_m])
            P.op("dve", lambda e: e.tensor_reduce(out=msel[:, 0:192], in_=tmp[:].rearrange("p (q j) -> p q j", j=5), axis=AX.X, op=ALU.add),
                 r=[t_m], w=[t_m])
            P.op("dve", lambda e: e.tensor_copy(out=msel[:, 192:384], in_=mall[:].rearrange("p (q j) -> p q j", j=5)[:, :, 4]), r=[t_m], w=[t_m])
            for l in range(2):
                for (src0, dst) in ((0, mod_d), (192, ctx_d)):
                    P.dma("sp", dst[:, l * 96:(l + 1) * 96].rearrange("p (c t) -> p c t", t=12),
                          msel[:, src0:src0 + 192].rearrange("p (c l t) -> p c l t", l=2, t=12)[:, :, l, :], r=[t_m], w=[t_m])
            P.barrier()

        def modp(l, a, b_):
            return mod_d[:, l * 96 + a:l * 96 + b_]

        def lnp(l, k):
            return lnv[:, (l * 4 + k) * 16:(l * 4 + k + 1) * 16]

        def mk_load_ao(parts, th):
            def load_ao(dst, w):
                for c in range(16):
                    part = c // 4
                    src = parts[part % 2]
                    r0 = (part // 2) * 512 + (c % 4) * 128
                    P.dma("sp", dst[:, c, :], src[r0:r0 + 128, th * 1024:(th + 1) * 1024], w=w)
            return load_ao

        def mk_load_h(halves):
            def load_h(dst, c, w):
                for half in range(2):
                    P.dma("sp", dst[:, half * 1024:(half + 1) * 1024], halves[half][c * 128:(c + 1) * 128, :], w=w)
            return load_h

        def oproj_both(l, parts, resid_halves, name):
            outs = []
            for th in range(2):
                o = ds("y_%s%d" % (name, th), [D, 1024])
                stage_oproj_ln(P, cst, None, w_out[l], resid_halves[th], [modp(l, 32, 48), lnp(l, 0), lnp(l, 1)], o, Tr(), Tr(),
                               load_ao=mk_load_ao(parts, th))
                P.barrier()
                outs.append(o)
            return outs

        def moe_both(l, hA, out_halves):
            xe_d = ds("y_xe", [16, 128, 16 * CAP], BF16); pos_d = ds("y_pos", [16, S]); gate_d = ds("y_gate", [16, S])
            stage_moe_route16(P, cst, None, [modp(l, 48, 80)], router[l], xe_d, pos_d, gate_d, Tr(), Tr(), load_h=mk_load_h(hA))
            P.barrier()
            ye_d = ds("y_ye", [16, 2, 128, D], BF16)

            def load_xe(dst, ex, b, w):
                P.dma("sp", dst, xe_d[ex].rearrange("p (c s) -> p c s", s=CAP), w=w)
            stage_moe_ffn(P, None, w1[l], w3[l], w2[l], ye_d, Tr(), Tr(), load_xe=load_xe, nb=1, n_exp=16)
            P.barrier()
            for th in range(2):
                def load_ye(dst, q, dh, w):
                    P.dma("sp", dst, ye_d[q, :, :, dh * 1024:(dh + 1) * 1024].rearrange("s p d -> p s d"), w=w)

                def load_pg(pos_sb, gate_sb, w, th=th):
                    P.dma("sp", pos_sb, pos_d[:, th * 1024:(th + 1) * 1024], w=w)
                    P.dma("sp", gate_sb, gate_d[:, th * 1024:(th + 1) * 1024], w=w)
                stage_scatter_ln(P, cst, None, None, None, hA[th], [modp(l, 80, 96), lnp(l, 2), lnp(l, 3)], out_halves[th], Tr(), Tr(),
                                 load_ye=load_ye, load_pg=load_pg)
                P.barrier()

        ao = []
        for hf in range(2):
            a_d = ds("y_ao", [1024, S], BF16)
            stage_attn(P, cst, xT, ctxT, [modp(0, 0, 32), ctx_d[:, 0:32]], w_in_r[hf], kvn, w_ukv_r[hf], cosT, sinT, na_T[hf], na_RM, a_d, Tr())
            P.barrier()
            ao.append(a_d)
        hA0 = oproj_both(0, ao, [xT[:, 0:1024], xT[:, 1024:2048]], "hA0")
        if l0_only:
            moe_both(0, hA0, [outT[:, 0:1024], outT[:, 1024:2048]])
            P.barrier()
            return nc, consts
        hB0 = [ds("y_hB0_%d" % th, [D, 1024]) for th in range(2)]
        moe_both(0, hA0, hB0)
        cd = []
        for hf in range(2):
            c_d = ds("y_cd", [1024, S], BF16)
            stage_cd(P, cst, None, [modp(1, 0, 32)], w_in_c[hf], cvec[hf], fw1, fw2, fvec, fw3_c[hf], delta_b[hf], c_d, Tr(), Tr(),
                     load_h=mk_load_h(hB0))
            P.barrier()
            cd.append(c_d)
        hA1 = oproj_both(1, cd, hB0, "hA1")
        moe_both(1, hA1, [outT[:, 0:1024], outT[:, 1024:2048]])
        P.barrier()
    return nc, consts


def solo_host_inputs(inp):
    ims = []
    cvs = np.concatenate([inp["c"], inp["c_ctx"][None, :]], axis=0)
    csT = np.ascontiguousarray(cvs.reshape(5, 16, 128).transpose(2, 1, 0).reshape(128, 80))
    adab = np.concatenate([np.ascontiguousarray(inp["ada_b"][:, c * 1536:(c + 1) * 1536].reshape(2, 12, 128).transpose(2, 0, 1).reshape(128, 24))
                           for c in range(8)], axis=1)
    cd_in = {k: inp[k] for k in inp if k.startswith("cd_")}
    lnv = np.zeros((128, 128), dtype=np.float32)
    for l in range(2):
        for k, nm in enumerate(("ln1_g", "ln1_b", "ln2_g", "ln2_b")):
            lnv[:, (l * 4 + k) * 16:(l * 4 + k + 1) * 16] = _fm(inp[nm][l])
    zero5 = np.zeros((5, 12288), dtype=np.float32)
    per_b = {}
    for b in range(4):
        im = {"csT": csT, "adaw": inp["ada_w"], "adab": np.ascontiguousarray(adab), "lnv": lnv}
        oh5 = np.zeros((128, 5), dtype=np.float32); oh5[:, b] = 1.0
        im["oh5"] = oh5
        for hf in range(2):
            a = attn_host_inputs(inp, zero5, b, hf)
            for k in ("w_in_r", "w_ukv_r", "na_T"):
                im["%s%d" % (k, hf)] = a[k]
            if hf == 0:
                for k in ("xT", "ctxT", "kvn", "cosT", "sinT", "na_RM"):
                    im[k] = a[k]
            cdh = cd_host_inputs(cd_in, zero5[:4], b, hf)
            for k in ("w_in_c", "cvec", "fw3_c", "delta_b"):
                im["%s%d" % (k, hf)] = cdh[k]
            if hf == 0:
                for k in ("fw1", "fw2", "fvec"):
                    im[k] = cdh[k]
        for l in range(2):
            im["router%d" % l] = np.ascontiguousarray(inp["router"][l])
        im["w_out0"] = np.ascontiguousarray(inp["ab_w_out"][0])
        im["w_out1"] = np.ascontiguousarray(inp["cd_w_out"][0])
        for l in range(2):
            im["w1_%d" % l] = inp["exp_w1"][l]
            im["w3_%d" % l] = inp["exp_w3"][l]
            im["w2_%d" % l] = inp["exp_w2"][l]
        per_b[b] = im
    for b in range(4):
        ims.append(per_b[b])
    return ims


def kernel(**inp):
    inp = {k: np.asarray(v) for k, v in inp.items()}
    nc, consts = build_solo_prog()
    ims = solo_host_inputs(inp)
    r = _run(nc, consts, ims)
    out = np.zeros((NB, S, D), dtype=np.float32)
    for b in range(4):
        out[b] = np.asarray(r[b]["outT"]).T
    return out
```

```python
import numpy as np
from contextlib import ExitStack
import ml_dtypes
import concourse.bass as bass
import concourse.mybir as mybir
from concourse.bass_utils import run_bass_kernel_spmd

F32 = mybir.dt.float32
BF16 = mybir.dt.bfloat16
AF = mybir.ActivationFunctionType
ALU = mybir.AluOpType
AX = mybir.AxisListType

D = 2048
S = 2048
NB = 4
LC = 256
NE = 16
FF = 1408
CAP = 256
ALPHA = (2.0 * 2) ** 0.25
LN_EPS = 1e-6


_cnt = [0]


def SB(nc, name, shape, dt):
    _cnt[0] += 1
    return nc.sbuf_tensor("%s_u%d" % (name, _cnt[0]), shape, dt)


def PSM(nc, name, shape, dt):
    _cnt[0] += 1
    return nc.psum_tensor("%s_u%d" % (name, _cnt[0]), shape, dt)


def load_pieces(P, dst, pieces, w):
    if not isinstance(pieces, (list, tuple)):
        pieces = [pieces]
    off = 0
    for ap in pieces:
        n = ap.shape[-1]
        P.dma("sp", dst[:, off:off + n], ap, w=w)
        off += n


class Tr:
    __slots__ = ("w", "r")

    def __init__(self):
        self.w = None
        self.r = {}


class Prog:
    ENG = ("pe", "act", "dve", "pool", "sp")

    def __init__(self, nc, es):
        self.nc = nc
        self.eobj = {"pe": nc.tensor, "act": nc.scalar, "dve": nc.vector, "pool": nc.gpsimd, "sp": nc.sync}
        self.es = es
        self.gen = {e: 0 for e in ("pe", "act", "dve", "pool")}
        self.esem = {(e, 0): es.enter_context(nc.semaphore("s_" + e)) for e in ("pe", "act", "dve", "pool")}
        self.cnt = {e: 0 for e in self.gen}
        nds = {"sp": 40, "pool": 32, "act": 1}
        self.dsem = {e: [es.enter_context(nc.semaphore("d_%s%d" % (e, i))) for i in range(n)] for e, n in nds.items()}
        self.dcnt = {e: [0] * n for e, n in nds.items()}
        self.dnext = {e: 0 for e in nds}
        self.seen = {e: {} for e in self.ENG}

    def _sem(self, k):
        return self.esem[(k[1], k[2])] if k[0] == "e" else self.dsem[k[1]][k[2]]

    def _sync(self, eng, r, w, mykey, myval):
        need = {}
        for t in r:
            if t.w is not None and need.get(t.w[0], 0) < t.w[1]:
                need[t.w[0]] = t.w[1]
        for t in w:
            if t.w is not None and need.get(t.w[0], 0) < t.w[1]:
                need[t.w[0]] = t.w[1]
            for k, v in t.r.items():
                if need.get(k, 0) < v:
                    need[k] = v
        seen = self.seen[eng]
        e = self.eobj[eng]
        for k, v in need.items():
            if eng == "pe" and k[0] == "e" and k[1] == "pe":
                continue
            if seen.get(k, 0) < v:
                seen[k] = v
                e.wait_ge(self._sem(k), v)
        for t in r:
            if t.r.get(mykey, 0) < myval:
                t.r[mykey] = myval
        for t in w:
            t.w = (mykey, myval)
            t.r = {}

    def op(self, eng, fn, r=(), w=()):
        self.cnt[eng] += 1
        g = self.gen[eng]
        self._sync(eng, r, w, ("e", eng, g), self.cnt[eng])
        fn(self.eobj[eng]).then_inc(self.esem[(eng, g)], 1)

    def dma(self, eng, out, in_, r=(), w=()):
        i = self.dnext[eng]
        self.dnext[eng] = (i + 1) % len(self.dsem[eng])
        key = ("d", eng, i)
        prev = self.dcnt[eng][i]
        self.dcnt[eng][i] = prev + 16
        if prev > 0 and self.seen[eng].get(key, 0) < prev:
            self.seen[eng][key] = prev
            self.eobj[eng].wait_ge(self.dsem[eng][i], prev)
        self._sync(eng, r, w, key, prev + 16)
        self.eobj[eng].dma_start(out=out, in_=in_).then_inc(self.dsem[eng][i], 16)

    def idma(self, out, in_, idx, r=(), w=()):
        eng = "pool"
        i = self.dnext[eng]
        self.dnext[eng] = (i + 1) % len(self.dsem[eng])
        key = ("d", eng, i)
        prev = self.dcnt[eng][i]
        self.dcnt[eng][i] = prev + 16
        if prev > 0 and self.seen[eng].get(key, 0) < prev:
            self.seen[eng][key] = prev
            self.eobj[eng].wait_ge(self.dsem[eng][i], prev)
        self._sync(eng, r, w, key, prev + 16)
        self.nc.gpsimd.indirect_dma_start(out=out, out_offset=None, in_=in_,
                                          in_offset=bass.IndirectOffsetOnAxis(ap=idx, axis=0)).then_inc(self.dsem[eng][i], 16)

    def allgather(self, src, dst, r=(), w=()):
        eng = "pool"
        i = self.dnext[eng]
        self.dnext[eng] = (i + 1) % len(self.dsem[eng])
        key = ("d", eng, i)
        prev = self.dcnt[eng][i]
        self.dcnt[eng][i] = prev + 16
        if prev > 0 and self.seen[eng].get(key, 0) < prev:
            self.seen[eng][key] = prev
            self.eobj[eng].wait_ge(self.dsem[eng][i], prev)
        self._sync(eng, r, w, key, prev + 16)
        self.nc.gpsimd.collective_compute("AllGather", ALU.bypass, replica_groups=[list(range(8))], ins=[src],
                                          outs=[dst]).then_inc(self.dsem[eng][i], 16)

    def barrier(self):
        for eng in self.ENG:
            seen = self.seen[eng]
            e = self.eobj[eng]
            for k2, c in self.cnt.items():
                k = ("e", k2, self.gen[k2])
                if c > 0 and seen.get(k, 0) < c:
                    seen[k] = c
                    e.wait_ge(self.esem[(k2, self.gen[k2])], c)
            for q, lst in self.dcnt.items():
                for i, c in enumerate(lst):
                    k = ("d", q, i)
                    if c > 0 and seen.get(k, 0) < c:
                        seen[k] = c
                        e.wait_ge(self.dsem[q][i], c)
        for k2 in self.cnt:
            if self.cnt[k2] > 24000:
                self.gen[k2] += 1
                self.esem[(k2, self.gen[k2])] = self.es.enter_context(self.nc.semaphore("s_%s_g%d" % (k2, self.gen[k2])))
                self.cnt[k2] = 0


def make_consts():
    c = {}
    c["iota_row"] = np.tile(np.arange(256, dtype=np.float32)[None, :], (128, 1))
    pid = np.arange(128, dtype=np.float32)
    c["pidx"] = np.stack([pid, pid + 128], axis=1).astype(np.float32)
    c["ident_f"] = np.eye(128, dtype=np.float32)
    c["ident_b"] = np.eye(128, dtype=np.float32).astype(ml_dtypes.bfloat16)
    c["ones_f"] = np.ones((128, 128), dtype=np.float32)
    sel = np.zeros((16, 8, 128), dtype=np.float32)
    for e in range(8):
        sel[e, e, :] = 1.0
    c["selrow"] = sel
    thl = np.zeros((128, 16, 2), dtype=np.float32)
    thl[:, :, 0] = np.arange(16)[None, :]
    thl[:, :, 1] = np.arange(128)[:, None]
    c["tokhl"] = thl.reshape(128, 32).astype(ml_dtypes.bfloat16)
    sel16 = np.zeros((16, 16, 128), dtype=np.float32)
    for e in range(16):
        sel16[e, e, :] = 1.0
    c["selrow16"] = sel16
    return c


def stage_moe_route(P, cst, hT, modv, router, xe_out, pos_out, gate_out, t_in, t_out, load_h=None):
    nc = P.nc
    if load_h is None:
        def load_h(dst, c, w):
            P.dma("sp", dst, hT[c * 128:(c + 1) * 128, :], r=[t_in], w=w)
    NDC = D // 128
    NTT = S // 128
    NFT = FF // 128
    with ExitStack() as es:
        def sb(name, shape, dt):
            return es.enter_context(SB(nc, "m_" + name, shape, dt))

        def pst(name, shape, dt):
            return es.enter_context(PSM(nc, "mp_" + name, shape, dt))

        regA = sb("regA", [128, NTT * D], BF16)
        modv_sb = sb("modv", [128, 32], F32)
        sc1p = sb("sc1p", [128, 16], F32)
        router_sb = sb("router", [128, NDC * 16], F32)
        iota_row = sb("iota_row", [128, 256], F32)
        pidx = sb("pidx", [128, 2], F32)
        ident_f = sb("ident_f", [128, 128], F32)
        ident_b = sb("ident_b", [128, 128], BF16)
        ones_f = sb("ones_f", [128, 128], F32)
        selrow = sb("selrow", [16, 8 * 128], F32)
        t_c = Tr()
        load_pieces(P, modv_sb, modv, [t_c])
        P.dma("sp", router_sb[:].rearrange("p (k e) -> p k e", e=16), router.rearrange("(k p) e -> p k e", p=128), w=[t_c])
        P.dma("sp", iota_row[:], cst["iota_row"][:, :], w=[t_c])
        P.dma("sp", pidx[:], cst["pidx"][:, :], w=[t_c])
        P.dma("sp", ident_f[:], cst["ident_f"][:, :], w=[t_c])
        P.dma("sp", ident_b[:], cst["ident_b"][:, :], w=[t_c])
        P.dma("sp", ones_f[:], cst["ones_f"][:, :], w=[t_c])
        P.dma("sp", selrow[:].rearrange("p (e m) -> p e m", m=128), cst["selrow"][:, :, :], w=[t_c])
        P.op("dve", lambda e: e.tensor_scalar(out=sc1p[:], in0=modv_sb[:, 16:32], scalar1=1.0, scalar2=None, op0=ALU.add),
             r=[t_c], w=[t_c])

        u2tok = regA[:].rearrange("p (t d) -> p t d", d=D)
        pos = sb("pos", [16, S], F32)
        gate = sb("gate", [16, S], F32)
        posT = sb("posT", [128, NTT * 16], F32)
        with ExitStack() as es1:
            hbuf = [es1.enter_context(SB(nc, "m_hbuf%d" % i, [128, S], F32)) for i in range(2)]
            u2f = [es1.enter_context(SB(nc, "m_u2f%d" % i, [128, S], F32)) for i in range(2)]
            u2b = [es1.enter_context(SB(nc, "m_u2b%d" % i, [128, S], BF16)) for i in range(2)]
            psL = [es1.enter_context(PSM(nc, "mp_L%d" % i, [128, 512], F32)) for i in range(4)]
            psT = [es1.enter_context(PSM(nc, "mp_T%d" % i, [128, 1024], BF16)) for i in range(2)]
            max8 = es1.enter_context(SB(nc, "m_max8", [16, 8], F32))
            t_h = [Tr(), Tr()]
            t_uf = [Tr(), Tr()]
            t_ub = [Tr(), Tr()]
            t_L = Tr()
            t_T = [Tr(), Tr()]
            t_u2tok = Tr()
            nT = 0
            for c in range(NDC):
                i = c % 2
                load_h(hbuf[i][:], c, [t_h[i]])
                P.op("act", lambda e, i=i, c=c: e.activation(out=u2f[i][:], in_=hbuf[i][:], func=AF.Identity,
                                                             bias=modv_sb[:, c:c + 1], scale=sc1p[:, c:c + 1]),
                     r=[t_h[i], t_c], w=[t_uf[i]])
                for tq in range(4):
                    P.op("pe", lambda e, i=i, c=c, tq=tq: e.matmul(psL[tq][0:16, :], router_sb[:, c * 16:(c + 1) * 16],
                                                                   u2f[i][:, tq * 512:(tq + 1) * 512],
                                                                   start=(c == 0), stop=(c == NDC - 1)),
                         r=[t_uf[i], t_c], w=[t_L])
                P.op("dve", lambda e, i=i: e.tensor_copy(out=u2b[i][:], in_=u2f[i][:]), r=[t_uf[i]], w=[t_ub[i]])
                for g in range(4):
                    j = nT % 2
                    nT += 1
                    for k in range(4):
                        t = g * 4 + k
                        P.op("pe", lambda e, i=i, j=j, k=k, t=t: e.transpose(psT[j][:, k * 128:(k + 1) * 128],
                                                                             u2b[i][:, t * 128:(t + 1) * 128], ident_b[:]),
                             r=[t_ub[i], t_c], w=[t_T[j]])
                    P.op("dve" if g % 2 == 0 else "act",
                         (lambda e, j=j, g=g, c=c: e.tensor_copy(out=u2tok[:, g * 4:(g + 1) * 4, c * 128:(c + 1) * 128],
                                                                 in_=psT[j][:, 0:512].rearrange("p (k d) -> p k d", d=128)))
                         if g % 2 == 0 else
                         (lambda e, j=j, g=g, c=c: e.copy(out=u2tok[:, g * 4:(g + 1) * 4, c * 128:(c + 1) * 128],
                                                          in_=psT[j][:, 0:512].rearrange("p (k d) -> p k d", d=128))),
                         r=[t_T[j]], w=[t_u2tok])
            ex = hbuf[0]
            work = hbuf[1]
            t_ex = t_h[0]
            t_work = t_h[1]
            aff = u2f[0][0:16, :]
            mask = u2f[1][0:16, :]
            onesr = ex[0:16, :]
            t_aff = t_uf[0]
            t_mask = t_uf[1]
            t_ones = t_ex
            for tq in range(4):
                P.op("act", lambda e, tq=tq: e.activation(out=ex[0:16, tq * 512:(tq + 1) * 512], in_=psL[tq][0:16, :], func=AF.Exp),
                     r=[t_L], w=[t_ex])
            t_L2 = Tr()
            for tq in range(4):
                P.op("pe", lambda e, tq=tq: e.matmul(psL[tq][0:16, :], ones_f[0:16, 0:16], ex[0:16, tq * 512:(tq + 1) * 512],
                                                     start=True, stop=True), r=[t_ex, t_c, t_L], w=[t_L])
            for tq in range(4):
                P.op("dve", lambda e, tq=tq: e.reciprocal(out=work[0:16, tq * 512:(tq + 1) * 512], in_=psL[tq][0:16, :]),
                     r=[t_L], w=[t_work])
            P.op("dve", lambda e: e.tensor_tensor(out=aff, in0=ex[0:16, :], in1=work[0:16, :], op=ALU.mult),
                 r=[t_ex, t_work], w=[t_aff])
            P.op("dve", lambda e: e.tensor_copy(out=work[0:16, :], in_=aff), r=[t_aff], w=[t_work])
            t_m8 = Tr()
            for it in range(CAP // 8):
                P.op("dve", lambda e: e.max(out=max8[:], in_=work[0:16, :]), r=[t_work], w=[t_m8])
                P.op("dve", lambda e: e.match_replace(out=work[0:16, :], in_to_replace=max8[:], in_values=work[0:16, :],
                                                      imm_value=-1.0), r=[t_m8, t_work], w=[t_work])
            t_pos = Tr(); t_gate = Tr()
            P.op("pool", lambda e: e.memset(onesr, 1.0), w=[t_ones])
            P.op("dve", lambda e: e.tensor_tensor(out=mask, in0=work[0:16, :], in1=aff, op=ALU.not_equal),
                 r=[t_work, t_aff], w=[t_mask])
            P.op("dve", lambda e: e.tensor_tensor_scan(out=pos[:], data0=onesr, data1=mask, initial=0.0,
                                                       op0=ALU.mult, op1=ALU.add), r=[t_ones, t_mask], w=[t_pos])
            P.op("dve", lambda e: e.tensor_tensor(out=pos[:], in0=pos[:], in1=mask, op=ALU.mult), r=[t_mask, t_pos], w=[t_pos])
            P.op("dve", lambda e: e.tensor_scalar(out=pos[:], in0=pos[:], scalar1=-1.0, scalar2=None, op0=ALU.add),
                 r=[t_pos], w=[t_pos])
            P.op("dve", lambda e: e.tensor_tensor(out=gate[:], in0=aff, in1=mask, op=ALU.mult), r=[t_aff, t_mask], w=[t_gate])
            t_posT = Tr()
            psP = psL[0]
            for t in range(NTT):
                P.op("pe", lambda e, t=t: e.transpose(psP[:, t * 16:(t + 1) * 16], pos[:, t * 128:(t + 1) * 128], ident_f[0:16, 0:16]),
                     r=[t_pos, t_c, t_L], w=[t_L])
            P.op("dve", lambda e: e.tensor_copy(out=posT[:], in_=psP[:, 0:NTT * 16]), r=[t_L], w=[t_posT])
            P.barrier()
        regB = sb("regB", [128, 8 * NDC * CAP], BF16)
        xeT = regB[:].rearrange("p (e c s) -> p e c s", c=NDC, s=CAP)
        t_xeT = [Tr() for _ in range(8)]
        with ExitStack() as es2:
            selb = [es2.enter_context(SB(nc, "m_sel%d" % i, [128, NTT * CAP], BF16)) for i in range(2)]
            psG = [es2.enter_context(PSM(nc, "mp_G%d" % i, [128, 512], F32)) for i in range(4)]
            t_sel = [Tr(), Tr()]
            t_G = [Tr() for _ in range(4)]
            nG = 0
            for ex_i in range(8):
                i = ex_i % 2
                for t in range(NTT):
                    P.op("dve" if t % 2 == 0 else "pool",
                         lambda e, i=i, t=t, ex_i=ex_i: e.tensor_scalar(out=selb[i][:, t * CAP:(t + 1) * CAP], in0=iota_row[:],
                                                                        scalar1=posT[:, t * 16 + ex_i:t * 16 + ex_i + 1],
                                                                        scalar2=None, op0=ALU.is_equal),
                         r=[t_posT, t_c], w=[t_sel[i]])
                for dc in range(NDC):
                    g = nG % 4
                    nG += 1
                    for t in range(NTT):
                        P.op("pe", lambda e, g=g, t=t, dc=dc, i=i: e.matmul(psG[g][:, 0:CAP], u2tok[:, t, dc * 128:(dc + 1) * 128],
                                                                            selb[i][:, t * CAP:(t + 1) * CAP],
                                                                            start=(t == 0), stop=(t == NTT - 1)),
                             r=[t_u2tok, t_sel[i]], w=[t_G[g]])
                    if dc % 2 == 0:
                        P.op("act", lambda e, g=g, dc=dc, ex_i=ex_i: e.copy(out=xeT[:, ex_i, dc, :], in_=psG[g][:, 0:CAP]),
                             r=[t_G[g]], w=[t_xeT[ex_i]])
                    else:
                        P.op("dve", lambda e, g=g, dc=dc, ex_i=ex_i: e.tensor_copy(out=xeT[:, ex_i, dc, :], in_=psG[g][:, 0:CAP]),
                             r=[t_G[g]], w=[t_xeT[ex_i]])
            P.barrier()
        for ex_i in range(8):
            P.dma("sp", xe_out[ex_i], regB[:, ex_i * NDC * CAP:(ex_i + 1) * NDC * CAP], r=[t_xeT[ex_i]], w=[t_out])
        P.dma("sp", pos_out[:, :], pos[:], r=[t_pos], w=[t_out])
        P.dma("sp", gate_out[:, :], gate[:], r=[t_gate], w=[t_out])
        P.barrier()


def stage_moe_route16(P, cst, hT, modv, router, xe_out, pos_out, gate_out, t_in, t_out, load_h=None):
    nc = P.nc
    if load_h is None:
        def load_h(dst, c, w):
            P.dma("sp", dst, hT[c * 128:(c + 1) * 128, :], r=[t_in], w=w)
    NDC = D // 128
    NTT = S // 128
    NFT = FF // 128
    with ExitStack() as es:
        def sb(name, shape, dt):
            return es.enter_context(SB(nc, "m_" + name, shape, dt))

        def pst(name, shape, dt):
            return es.enter_context(PSM(nc, "mp_" + name, shape, dt))

        regA = sb("regA", [128, NTT * D], BF16)
        modv_sb = sb("modv", [128, 32], F32)
        sc1p = sb("sc1p", [128, 16], F32)
        router_sb = sb("router", [128, NDC * 16], F32)
        iota_row = sb("iota_row", [128, 256], F32)
        pidx = sb("pidx", [128, 2], F32)
        ident_f = sb("ident_f", [128, 128], F32)
        ident_b = sb("ident_b", [128, 128], BF16)
        ones_f = sb("ones_f", [128, 128], F32)
        selrow = sb("selrow", [16, 8 * 128], F32)
        t_c = Tr()
        load_pieces(P, modv_sb, modv, [t_c])
        P.dma("sp", router_sb[:].rearrange("p (k e) -> p k e", e=16), router.rearrange("(k p) e -> p k e", p=128), w=[t_c])
        P.dma("sp", iota_row[:], cst["iota_row"][:, :], w=[t_c])
        P.dma("sp", pidx[:], cst["pidx"][:, :], w=[t_c])
        P.dma("sp", ident_f[:], cst["ident_f"][:, :], w=[t_c])
        P.dma("sp", ident_b[:], cst["ident_b"][:, :], w=[t_c])
        P.dma("sp", ones_f[:], cst["ones_f"][:, :], w=[t_c])
        P.dma("sp", selrow[:].rearrange("p (e m) -> p e m", m=128), cst["selrow"][:, :, :], w=[t_c])
        P.op("dve", lambda e: e.tensor_scalar(out=sc1p[:], in0=modv_sb[:, 16:32], scalar1=1.0, scalar2=None, op0=ALU.add),
             r=[t_c], w=[t_c])

        u2tok = regA[:].rearrange("p (t d) -> p t d", d=D)
        pos = sb("pos", [16, S], F32)
        gate = sb("gate", [16, S], F32)
        posT = sb("posT", [128, NTT * 16], F32)
        with ExitStack() as es1:
            hbuf = [es1.enter_context(SB(nc, "m_hbuf%d" % i, [128, S], F32)) for i in range(2)]
            u2f = [es1.enter_context(SB(nc, "m_u2f%d" % i, [128, S], F32)) for i in range(2)]
            u2b = [es1.enter_context(SB(nc, "m_u2b%d" % i, [128, S], BF16)) for i in range(2)]
            psL = [es1.enter_context(PSM(nc, "mp_L%d" % i, [128, 512], F32)) for i in range(4)]
            psT = [es1.enter_context(PSM(nc, "mp_T%d" % i, [128, 1024], BF16)) for i in range(2)]
            max8 = es1.enter_context(SB(nc, "m_max8", [16, 8], F32))
            t_h = [Tr(), Tr()]
            t_uf = [Tr(), Tr()]
            t_ub = [Tr(), Tr()]
            t_L = Tr()
            t_T = [Tr(), Tr()]
            t_u2tok = Tr()
            nT = 0
            for c in range(NDC):
                i = c % 2
                load_h(hbuf[i][:], c, [t_h[i]])
                P.op("act", lambda e, i=i, c=c: e.activation(out=u2f[i][:], in_=hbuf[i][:], func=AF.Identity,
                                                             bias=modv_sb[:, c:c + 1], scale=sc1p[:, c:c + 1]),
                     r=[t_h[i], t_c], w=[t_uf[i]])
                for tq in range(4):
                    P.op("pe", lambda e, i=i, c=c, tq=tq: e.matmul(psL[tq][0:16, :], router_sb[:, c * 16:(c + 1) * 16],
                                                                   u2f[i][:, tq * 512:(tq + 1) * 512],
                                                                   start=(c == 0), stop=(c == NDC - 1)),
                         r=[t_uf[i], t_c], w=[t_L])
                P.op("dve", lambda e, i=i: e.tensor_copy(out=u2b[i][:], in_=u2f[i][:]), r=[t_uf[i]], w=[t_ub[i]])
                for g in range(4):
                    j = nT % 2
                    nT += 1
                    for k in range(4):
                        t = g * 4 + k
                        P.op("pe", lambda e, i=i, j=j, k=k, t=t: e.transpose(psT[j][:, k * 128:(k + 1) * 128],
                                                                             u2b[i][:, t * 128:(t + 1) * 128], ident_b[:]),
                             r=[t_ub[i], t_c], w=[t_T[j]])
                    P.op("dve" if g % 2 == 0 else "act",
                         (lambda e, j=j, g=g, c=c: e.tensor_copy(out=u2tok[:, g * 4:(g + 1) * 4, c * 128:(c + 1) * 128],
                                                                 in_=psT[j][:, 0:512].rearrange("p (k d) -> p k d", d=128)))
                         if g % 2 == 0 else
                         (lambda e, j=j, g=g, c=c: e.copy(out=u2tok[:, g * 4:(g + 1) * 4, c * 128:(c + 1) * 128],
                                                          in_=psT[j][:, 0:512].rearrange("p (k d) -> p k d", d=128))),
                         r=[t_T[j]], w=[t_u2tok])
            ex = hbuf[0]
            work = hbuf[1]
            t_ex = t_h[0]
            t_work = t_h[1]
            aff = u2f[0][0:16, :]
            mask = u2f[1][0:16, :]
            onesr = ex[0:16, :]
            t_aff = t_uf[0]
            t_mask = t_uf[1]
            t_ones = t_ex
            for tq in range(4):
                P.op("act", lambda e, tq=tq: e.activation(out=ex[0:16, tq * 512:(tq + 1) * 512], in_=psL[tq][0:16, :], func=AF.Exp),
                     r=[t_L], w=[t_ex])
            t_L2 = Tr()
            for tq in range(4):
                P.op("pe", lambda e, tq=tq: e.matmul(psL[tq][0:16, :], ones_f[0:16, 0:16], ex[0:16, tq * 512:(tq + 1) * 512],
                                                     start=True, stop=True), r=[t_ex, t_c, t_L], w=[t_L])
            for tq in range(4):
                P.op("dve", lambda e, tq=tq: e.reciprocal(out=work[0:16, tq * 512:(tq + 1) * 512], in_=psL[tq][0:16, :]),
                     r=[t_L], w=[t_work])
            P.op("dve", lambda e: e.tensor_tensor(out=aff, in0=ex[0:16, :], in1=work[0:16, :], op=ALU.mult),
                 r=[t_ex, t_work], w=[t_aff])
            P.op("dve", lambda e: e.tensor_copy(out=work[0:16, :], in_=aff), r=[t_aff], w=[t_work])
            t_m8 = Tr()
            for it in range(CAP // 8):
                P.op("dve", lambda e: e.max(out=max8[:], in_=work[0:16, :]), r=[t_work], w=[t_m8])
                P.op("dve", lambda e: e.match_replace(out=work[0:16, :], in_to_replace=max8[:], in_values=work[0:16, :],
                                                      imm_value=-1.0), r=[t_m8, t_work], w=[t_work])
            t_pos = Tr(); t_gate = Tr()
            P.op("pool", lambda e: e.memset(onesr, 1.0), w=[t_ones])
            P.op("dve", lambda e: e.tensor_tensor(out=mask, in0=work[0:16, :], in1=aff, op=ALU.not_equal),
                 r=[t_work, t_aff], w=[t_mask])
            P.op("dve", lambda e: e.tensor_tensor_scan(out=pos[:], data0=onesr, data1=mask, initial=0.0,
                                                       op0=ALU.mult, op1=ALU.add), r=[t_ones, t_mask], w=[t_pos])
            P.op("dve", lambda e: e.tensor_tensor(out=pos[:], in0=pos[:], in1=mask, op=ALU.mult), r=[t_mask, t_pos], w=[t_pos])
            P.op("dve", lambda e: e.tensor_scalar(out=pos[:], in0=pos[:], scalar1=-1.0, scalar2=None, op0=ALU.add),
                 r=[t_pos], w=[t_pos])
            P.op("dve", lambda e: e.tensor_tensor(out=gate[:], in0=aff, in1=mask, op=ALU.mult), r=[t_aff, t_mask], w=[t_gate])
            t_posT = Tr()
            psP = psL[0]
            for t in range(NTT):
                P.op("pe", lambda e, t=t: e.transpose(psP[:, t * 16:(t + 1) * 16], pos[:, t * 128:(t + 1) * 128], ident_f[0:16, 0:16]),
                     r=[t_pos, t_c, t_L], w=[t_L])
            P.op("dve", lambda e: e.tensor_copy(out=posT[:], in_=psP[:, 0:NTT * 16]), r=[t_L], w=[t_posT])
            P.barrier()
        u2_d = dram_scratch(nc, "m_u2d", [S, D], BF16)
        t_u2d = Tr()
        for t in range(NTT):
            P.dma("sp", u2_d[t * 128:(t + 1) * 128, :], u2tok[:, t, :], r=[t_u2tok], w=[t_u2d])
        with ExitStack() as es2:
            selb = [es2.enter_context(SB(nc, "m_sel%d" % i, [128, NTT * CAP], BF16)) for i in range(2)]
            tokhl = es2.enter_context(SB(nc, "m_tokhl", [128, 32], BF16))
            P.dma("sp", tokhl[:], cst["tokhl"][:, :], w=[t_c])
            psI = Pool(nc, es2, "ps", 2, [128, 512], F32, "mp_I")
            psX = Pool(nc, es2, "ps", 4, [128, 1024], BF16, "mp_X")
            xst = Pool(nc, es2, "sb", 2, [128, NDC * CAP], BF16, "m_xst")
            xtm = Pool(nc, es2, "sb", 4, [128, D], BF16, "m_xtm")
            idxf = Pool(nc, es2, "sb", 4, [128, 1], F32, "m_idxf")
            hlp = Pool(nc, es2, "sb", 4, [128, 2], F32, "m_hl")
            idxu = Pool(nc, es2, "sb", 4, [128, 1], mybir.dt.uint32, "m_idxu")
            t_sel = [Tr(), Tr()]
            posTv = posT[:].rearrange("p (t e) -> p t e", e=16)
            for ex_i in range(16):
                i = ex_i % 2
                P.op("dve", lambda e, i=i, ex_i=ex_i: e.tensor_tensor(
                    out=selb[i][:].rearrange("p (t s) -> p t s", s=CAP), in0=iota_row[:].unsqueeze(1).to_broadcast([128, NTT, CAP]),
                    in1=posTv[:, :, ex_i:ex_i + 1].to_broadcast([128, NTT, CAP]), op=ALU.is_equal), r=[t_posT, t_c], w=[t_sel[i]])
                xs_, xs_t = xst.get()
                xsv = xs_[:].rearrange("p (c s) -> p c s", s=CAP)
                for half in range(2):
                    pi, pit = psI.get()
                    for t in range(NTT):
                        P.op("pe", lambda e, pi=pi, t=t, half=half, i=i: e.matmul(
                            pi[:, 0:2], selb[i][:, t * CAP + half * 128:t * CAP + (half + 1) * 128], tokhl[:, t * 2:t * 2 + 2],
                            start=(t == 0), stop=(t == NTT - 1)), r=[t_sel[i], t_c], w=[pit])
                    f_, f_t = idxf.get()
                    u_, u_t = idxu.get()
                    hl_, hl_t = hlp.get()
                    P.op("act", lambda e, hl_=hl_, pi=pi: e.copy(out=hl_[:], in_=pi[:, 0:2]), r=[pit], w=[hl_t])
                    P.op("dve", lambda e, f_=f_, hl_=hl_: e.tensor_scalar(out=f_[:], in0=hl_[:, 0:1], scalar1=128.0, scalar2=hl_[:, 1:2],
                                                                          op0=ALU.mult, op1=ALU.add), r=[hl_t], w=[f_t])
                    P.op("dve", lambda e, f_=f_, u_=u_: e.tensor_copy(out=u_[:], in_=f_[:]), r=[f_t], w=[u_t])
                    x_, x_t = xtm.get()
                    P.idma(x_[:], u2_d[:, :], u_[:, 0:1], r=[u_t, t_u2d], w=[x_t])
                    for c0 in range(0, NDC, 8):
                        px, pxt = psX.get()
                        for k in range(8):
                            P.op("pe", lambda e, px=px, x_=x_, c0=c0, k=k: e.transpose(px[:, k * 128:(k + 1) * 128],
                                                                                      x_[:, (c0 + k) * 128:(c0 + k + 1) * 128], ident_b[:]),
                                 r=[x_t, t_c], w=[pxt])
                        evac_copy(P, xsv[:, c0:c0 + 8, half * 128:(half + 1) * 128], px[:, :].rearrange("p (k s) -> p k s", s=128),
                                  [pxt], [xs_t])
                P.dma("sp", xe_out[ex_i], xs_[:], r=[xs_t], w=[t_out])
            P.barrier()
        P.dma("sp", pos_out[:, :], pos[:], r=[t_pos], w=[t_out])
        P.dma("sp", gate_out[:, :], gate[:], r=[t_gate], w=[t_out])
        P.barrier()


def dram_in(nc, name, shape, dt=F32):
    return nc.dram_tensor(name, list(shape), dt, kind="ExternalInput").ap()


def dram_out(nc, name, shape, dt=F32):
    return nc.dram_tensor(name, list(shape), dt, kind="ExternalOutput").ap()


def declare_consts(nc, consts):
    out = {}
    for k, v in consts.items():
        dt = BF16 if v.dtype == ml_dtypes.bfloat16 else F32
        out[k] = dram_in(nc, "c_" + k, v.shape, dt)
    return out


_uid = [0]


class Pool:
    def __init__(self, nc, es, kind, n, shape, dt, name):
        mk = (lambda *a: SB(nc, *a)) if kind == "sb" else (lambda *a: PSM(nc, *a))
        _uid[0] += 1
        self.t = [es.enter_context(mk("%s_%d_%d" % (name, _uid[0], i), shape, dt)) for i in range(n)]
        self.tr = [Tr() for _ in range(n)]
        self.i = 0

    def get(self):
        i = self.i
        self.i = (i + 1) % len(self.t)
        return self.t[i], self.tr[i]


def dram_scratch(nc, name, shape, dt):
    _cnt[0] += 1
    return nc.dram_tensor("%s_u%d" % (name, _cnt[0]), list(shape), dt, kind="Internal").ap()


_alt = [0]


def evac_copy(P, out, in_, r, w):
    _alt[0] ^= 1
    if _alt[0]:
        P.op("act", lambda e: e.copy(out=out, in_=in_), r=r, w=w)
    else:
        P.op("dve", lambda e: e.tensor_copy(out=out, in_=in_), r=r, w=w)


def load_wgroup(P, wpool, wsrc, c0, ncols, nk=16):
    wt, wtr = wpool.get()
    view = wt[:, 0:nk * ncols].rearrange("p (k c) -> p k c", c=ncols)
    P.dma("pool", view, wsrc[:, c0:c0 + ncols].rearrange("(k p) c -> p k c", p=128), w=[wtr])
    return view, wtr


def proj_fm(P, pspool, wv, wtr, ft, xT, t_x, t0, n, nk=16):
    ps, ptr = pspool.get()
    for k in range(nk):
        P.op("pe", lambda e, k=k: e.matmul(ps[:, 0:n], wv[:, k, ft * 128:(ft + 1) * 128], xT[:, k, t0:t0 + n],
                                           start=(k == 0), stop=(k == nk - 1)), r=[wtr, t_x], w=[ptr])
    return ps, ptr


def proj_tm(P, pspool, wv, wtr, c0, ncols, xT, t_x, tt, nk=16):
    ps, ptr = pspool.get()
    for k in range(nk):
        P.op("pe", lambda e, k=k: e.matmul(ps[:, 0:ncols], xT[:, k, tt * 128:(tt + 1) * 128], wv[:, k, c0:c0 + ncols],
                                           start=(k == 0), stop=(k == nk - 1)), r=[wtr, t_x], w=[ptr])
    return ps, ptr


def ln_apply(P, es, z, t_z, T, g_sb, b_sb, t_gb, ones_f, t_c, outT, t_out, pspool, name):
    nc = P.nc
    sq = Pool(nc, es, "sb", 3, [128, 512], BF16, name + "_sq")
    zb = Pool(nc, es, "sb", 3, [128, 512], BF16, name + "_zb")
    ones_b = es.enter_context(SB(nc, name + "_1b", [128, 128], BF16))
    P.op("dve", lambda e: e.tensor_copy(out=ones_b[:], in_=ones_f[:]), r=[t_c], w=[t_c])
    st = Pool(nc, es, "sb", 2, [128, 4 * 512], F32, name + "_st")
    ot = Pool(nc, es, "sb", 3, [128, 512], F32, name + "_ot")
    for t0 in range(0, T, 512):
        p1, p1t = pspool.get()
        p2, p2t = pspool.get()
        for c in range(16):
            s, s_t = sq.get()
            P.op("act", lambda e, c=c, s=s: e.activation(out=s[:], in_=z[:, c, t0:t0 + 512], func=AF.Square), r=[t_z], w=[s_t])
            b_, b_t = zb.get()
            P.op("pool" if c % 2 else "dve", lambda e, c=c, b_=b_: e.tensor_copy(out=b_[:], in_=z[:, c, t0:t0 + 512]), r=[t_z], w=[b_t])
            P.op("pe", lambda e, c=c, b_=b_: e.matmul(p1[:, :], ones_b[:, :], b_[:], start=(c == 0), stop=(c == 15)),
                 r=[b_t, t_c], w=[p1t])
            P.op("pe", lambda e, c=c, s=s: e.matmul(p2[:, :], ones_b[:, :], s[:], start=(c == 0), stop=(c == 15)),
                 r=[s_t, t_c], w=[p2t])
        stt, st_t = st.get()
        mean = stt[:, 0:512]
        rstd = stt[:, 512:1024]
        tmp = stt[:, 1024:1536]
        P.op("dve", lambda e: e.tensor_scalar(out=mean, in0=p1[:, :], scalar1=1.0 / D, scalar2=None, op0=ALU.mult), r=[p1t], w=[st_t])
        P.op("dve", lambda e: e.tensor_tensor(out=tmp, in0=mean, in1=mean, op=ALU.mult), r=[st_t], w=[st_t])
        P.op("dve", lambda e: e.scalar_tensor_tensor(out=tmp, in0=p2[:, :], scalar=1.0 / D, in1=tmp, op0=ALU.mult, op1=ALU.subtract),
             r=[p2t, st_t], w=[st_t])
        P.op("dve", lambda e: e.tensor_scalar(out=tmp, in0=tmp, scalar1=LN_EPS, scalar2=None, op0=ALU.add), r=[st_t], w=[st_t])
        P.op("act", lambda e: e.activation(out=tmp, in_=tmp, func=AF.Sqrt), r=[st_t], w=[st_t])
        P.op("dve", lambda e: e.reciprocal(out=rstd, in_=tmp), r=[st_t], w=[st_t])
        for c in range(16):
            o, o_t = ot.get()
            P.op("dve", lambda e, c=c, o=o: e.tensor_tensor(out=o[:], in0=z[:, c, t0:t0 + 512], in1=mean, op=ALU.subtract),
                 r=[t_z, st_t], w=[o_t])
            P.op("dve", lambda e, o=o: e.tensor_tensor(out=o[:], in0=o[:], in1=rstd, op=ALU.mult), r=[st_t, o_t], w=[o_t])
            P.op("act", lambda e, c=c, o=o: e.activation(out=o[:], in_=o[:], func=AF.Identity, bias=b_sb[:, c:c + 1],
                                                         scale=g_sb[:, c:c + 1]), r=[o_t, t_gb], w=[o_t])
            P.dma("sp", outT[c * 128:(c + 1) * 128, t0:t0 + 512], o[:], r=[o_t], w=[t_out])


NA_J = [(0, 6), (2, 10), (6, 14), (10, 16)]
TB_OFF = 3
TB_N = 22
LT = S + LC


def stage_attn(P, cst, xT, ctxT, modv, w_in_r, kvn, w_ukv_r, cosT, sinT, na_T, na_RM, aoT, t_out):
    nc = P.nc
    C_QN, C_QP, C_QPS, C_CKV, C_KP, C_KPS, C_QA, C_KA, C_VA = 0, 512, 768, 1024, 1536, 1664, 1792, 2304, 2816
    qn_d = dram_scratch(nc, "a_qn", [4, 128, S], BF16)
    qp_d = dram_scratch(nc, "a_qp", [2, 128, S], BF16)
    kn_d = dram_scratch(nc, "a_kn", [4, 128, LT], BF16)
    kp_d = dram_scratch(nc, "a_kp", [128, LT], BF16)
    qa_d = dram_scratch(nc, "a_qa", [4, 128, S], BF16)
    ka_d = dram_scratch(nc, "a_ka", [4, 128, LT], BF16)
    vm_d = dram_scratch(nc, "a_vm", [LT, 512], BF16)
    va_d = dram_scratch(nc, "a_va", [LT, 512], BF16)
    t_d = {k: Tr() for k in ("qn", "qp", "kn", "kp", "qa", "ka", "vm", "va")}
    with ExitStack() as es:
        def sb(name, shape, dt):
            return es.enter_context(SB(nc, "a_" + name, shape, dt))
        modv_sb = sb("modv", [128, 64], F32)
        sc1p = sb("sc1p", [128, 32], F32)
        kvn_sb = sb("kvn", [128, 4], F32)
        ones_f = sb("ones_f", [128, 128], F32)
        ones_b = sb("ones_b", [128, 128], BF16)
        t_c = Tr()
        load_pieces(P, modv_sb, modv, [t_c])
        P.dma("sp", kvn_sb[:], kvn[:, :], w=[t_c])
        P.dma("sp", ones_f[:], cst["ones_f"][:, :], w=[t_c])
        P.op("dve", lambda e: e.tensor_scalar(out=sc1p[:, 0:16], in0=modv_sb[:, 16:32], scalar1=1.0, scalar2=None, op0=ALU.add),
             r=[t_c], w=[t_c])
        P.op("dve", lambda e: e.tensor_scalar(out=sc1p[:, 16:32], in0=modv_sb[:, 48:64], scalar1=1.0, scalar2=None, op0=ALU.add),
             r=[t_c], w=[t_c])
        P.op("dve", lambda e: e.tensor_copy(out=ones_b[:], in_=ones_f[:]), r=[t_c], w=[t_c])
        with ExitStack() as es1:
            uall_t = es1.enter_context(SB(nc, "a_uall", [128, 16 * LT], BF16))
            uall = uall_t[:].rearrange("p (k t) -> p k t", t=LT)
            t_u = Tr()
            es_x = ExitStack()
            xin = Pool(nc, es_x, "sb", 2, [128, S], F32, "a_xin")
            for c in range(16):
                xt, xtr = xin.get()
                P.dma("sp", xt[:], xT[c * 128:(c + 1) * 128, :], w=[xtr])
                P.op("act", lambda e, c=c, xt=xt: e.activation(out=uall[:, c, 0:S], in_=xt[:], func=AF.Identity,
                                                               bias=modv_sb[:, c:c + 1], scale=sc1p[:, c:c + 1]),
                     r=[xtr, t_c], w=[t_u])
                xt2, xtr2 = xin.get()
                P.dma("sp", xt2[:, 0:LC], ctxT[c * 128:(c + 1) * 128, :], w=[xtr2])
                P.op("act", lambda e, c=c, xt2=xt2: e.activation(out=uall[:, c, S:LT], in_=xt2[:, 0:LC], func=AF.Identity,
                                                                 bias=modv_sb[:, 32 + c:33 + c], scale=sc1p[:, 16 + c:17 + c]),
                     r=[xtr2, t_c], w=[t_u])
            P.barrier()
            es_x.close()
            wpool = Pool(nc, es1, "sb", 2, [128, 16 * 512], BF16, "a_w")
            pspool = Pool(nc, es1, "ps", 4, [128, 512], F32, "ap_p")
            stg = Pool(nc, es1, "sb", 4, [128, 512], BF16, "a_stg")
            cs = es1.enter_context(SB(nc, "a_cos", [128, S], F32))
            sn = es1.enter_context(SB(nc, "a_sin", [128, S], F32))
            P.dma("sp", cs[:], cosT[:, :], w=[t_c])
            P.dma("sp", sn[:], sinT[:, :], w=[t_c])

            def plain(c0, ntile, ntok, dst, t_dst):
                wv, wtr = load_wgroup(P, wpool, w_in_r, c0, ntile * 128)
                for ft in range(ntile):
                    for t0 in range(0, ntok, 512):
                        ps, ptr = proj_fm(P, pspool, wv, wtr, ft, uall, t_u, t0, 512)
                        s_, s_t = stg.get()
                        evac_copy(P, s_[:], ps[:, :], [ptr], [s_t])
                        P.dma("sp", dst[ft, :, t0:t0 + 512], s_[:], r=[s_t], w=[t_dst])
            plain(C_QN, 4, S, qn_d, t_d["qn"])
            plain(C_QA, 4, S, qa_d, t_d["qa"])
            plain(C_KA, 4, LT // 512 * 512, ka_d, t_d["ka"])
            wv, wtr = load_wgroup(P, wpool, w_in_r, C_KA, 512)
            for ft in range(4):
                ps, ptr = proj_fm(P, pspool, wv, wtr, ft, uall, t_u, S, LC)
                s_, s_t = stg.get()
                evac_copy(P, s_[:, 0:LC], ps[:, 0:LC], [ptr], [s_t])
                P.dma("sp", ka_d[ft, :, S:LT], s_[:, 0:LC], r=[s_t], w=[t_d["ka"]])
            rtmp = Pool(nc, es1, "sb", 2, [128, 512], F32, "a_rtmp")

            def roped(c_p, c_s, ntile, dst_of, t_dst, with_ctx):
                wv, wtr = load_wgroup(P, wpool, w_in_r, c_p, ntile * 128)
                wv2, wtr2 = load_wgroup(P, wpool, w_in_r, c_s, ntile * 128)
                for ft in range(ntile):
                    for t0 in range(0, S, 512):
                        ps, ptr = proj_fm(P, pspool, wv, wtr, ft, uall, t_u, t0, 512)
                        ps2, ptr2 = proj_fm(P, pspool, wv2, wtr2, ft, uall, t_u, t0, 512)
                        r1, r1t = rtmp.get()
                        r2, r2t = rtmp.get()
                        P.op("dve", lambda e, r1=r1, ps=ps, t0=t0: e.tensor_tensor(out=r1[:], in0=ps[:, :], in1=cs[:, t0:t0 + 512], op=ALU.mult),
                             r=[ptr, t_c], w=[r1t])
                        P.op("dve", lambda e, r2=r2, ps2=ps2, t0=t0: e.tensor_tensor(out=r2[:], in0=ps2[:, :], in1=sn[:, t0:t0 + 512], op=ALU.mult),
                             r=[ptr2, t_c], w=[r2t])
                        s_, s_t = stg.get()
                        P.op("pool", lambda e, r1=r1, r2=r2, s_=s_: e.tensor_tensor(out=s_[:], in0=r1[:], in1=r2[:], op=ALU.add),
                             r=[r1t, r2t], w=[s_t])
                        P.dma("sp", dst_of(ft)[:, t0:t0 + 512], s_[:], r=[s_t], w=[t_dst])
                    if with_ctx:
                        ps, ptr = proj_fm(P, pspool, wv, wtr, ft, uall, t_u, S, LC)
                        s_, s_t = stg.get()
                        evac_copy(P, s_[:, 0:LC], ps[:, 0:LC], [ptr], [s_t])
                        P.dma("sp", dst_of(ft)[:, S:LT], s_[:, 0:LC], r=[s_t], w=[t_dst])
            roped(C_QP, C_QPS, 2, lambda ft: qp_d[ft], t_d["qp"], False)
            roped(C_KP, C_KPS, 1, lambda ft: kp_d, t_d["kp"], True)
            wv, wtr = load_wgroup(P, wpool, w_in_r, C_VA, 512)
            for tt in range(LT // 128):
                ps, ptr = proj_tm(P, pspool, wv, wtr, 0, 512, uall, t_u, tt)
                s_, s_t = stg.get()
                evac_copy(P, s_[:], ps[:, :], [ptr], [s_t])
                P.dma("sp", va_d[tt * 128:(tt + 1) * 128, :], s_[:], r=[s_t], w=[t_d["va"]])
            ckf_t = es1.enter_context(SB(nc, "a_ckf", [128, 4 * LT], F32))
            ckf = ckf_t[:].rearrange("p (k t) -> p k t", t=LT)
            ckn_t = es1.enter_context(SB(nc, "a_ckn", [128, 4 * LT], BF16))
            ckn = ckn_t[:].rearrange("p (k t) -> p k t", t=LT)
            t_ckf = Tr()
            t_ckn = Tr()
            wv, wtr = load_wgroup(P, wpool, w_in_r, C_CKV, 512)
            chunks = [(t0, 512) for t0 in range(0, S, 512)] + [(S, LC)]
            for ft in range(4):
                for (t0, n) in chunks:
                    ps, ptr = proj_fm(P, pspool, wv, wtr, ft, uall, t_u, t0, n)
                    evac_copy(P, ckf[:, ft, t0:t0 + n], ps[:, 0:n], [ptr], [t_ckf])
            rs = Pool(nc, es1, "sb", 2, [128, 512], F32, "a_rs")
            for (t0, n) in chunks:
                pss, psst = pspool.get()
                for k in range(4):
                    q_, q_t = rtmp.get()
                    P.op("act", lambda e, q_=q_, k=k, t0=t0, n=n: e.activation(out=q_[:, 0:n], in_=ckf[:, k, t0:t0 + n], func=AF.Square),
                         r=[t_ckf], w=[q_t])
                    P.op("pe", lambda e, q_=q_, k=k, n=n: e.matmul(pss[:, 0:n], ones_f[:, :], q_[:, 0:n], start=(k == 0), stop=(k == 3)),
                         r=[q_t, t_c], w=[psst])
                r_, r_t = rs.get()
                P.op("dve", lambda e, r_=r_, n=n: e.tensor_scalar(out=r_[:, 0:n], in0=pss[:, 0:n], scalar1=1.0 / 512, scalar2=LN_EPS,
                                                                  op0=ALU.mult, op1=ALU.add), r=[psst], w=[r_t])
                P.op("act", lambda e, r_=r_, n=n: e.activation(out=r_[:, 0:n], in_=r_[:, 0:n], func=AF.Sqrt), r=[r_t], w=[r_t])
                P.op("dve", lambda e, r_=r_, n=n: e.reciprocal(out=r_[:, 0:n], in_=r_[:, 0:n]), r=[r_t], w=[r_t])
                for k in range(4):
                    P.op("dve", lambda e, r_=r_, k=k, t0=t0, n=n: e.scalar_tensor_tensor(
                        out=ckn[:, k, t0:t0 + n], in0=ckf[:, k, t0:t0 + n], scalar=kvn_sb[:, k:k + 1], in1=r_[:, 0:n],
                        op0=ALU.mult, op1=ALU.mult), r=[t_ckf, r_t, t_c], w=[t_ckn])
            wv, wtr = load_wgroup(P, wpool, w_ukv_r, 0, 512, nk=4)
            for ft in range(4):
                for (t0, n) in chunks:
                    ps, ptr = proj_fm(P, pspool, wv, wtr, ft, ckn, t_ckn, t0, n, nk=4)
                    s_, s_t = stg.get()
                    evac_copy(P, s_[:, 0:n], ps[:, 0:n], [ptr], [s_t])
                    P.dma("sp", kn_d[ft, :, t0:t0 + n], s_[:, 0:n], r=[s_t], w=[t_d["kn"]])
            wv, wtr = load_wgroup(P, wpool, w_ukv_r, 512, 512, nk=4)
            for tt in range(LT // 128):
                ps, ptr = proj_tm(P, pspool, wv, wtr, 0, 512, ckn, t_ckn, tt, nk=4)
                s_, s_t = stg.get()
                evac_copy(P, s_[:], ps[:, :], [ptr], [s_t])
                P.dma("sp", vm_d[tt * 128:(tt + 1) * 128, :], s_[:], r=[s_t], w=[t_d["vm"]])
            P.barrier()
        with ExitStack() as es2:
            NKT = LT // 128
            qb = Pool(nc, es2, "sb", 2, [128, S], BF16, "a_q")
            qpb = Pool(nc, es2, "sb", 2, [128, S], BF16, "a_qp")
            kb = Pool(nc, es2, "sb", 2, [128, LT], BF16, "a_k")
            vb = Pool(nc, es2, "sb", 2, [128, NKT * 128], BF16, "a_v")
            kpb = es2.enter_context(SB(nc, "a_kpb", [128, LT], BF16))
            t_kpb = Tr()
            P.dma("sp", kpb[:], kp_d[:, :], r=[t_d["kp"]], w=[t_kpb])
            naT = es2.enter_context(SB(nc, "a_naT", [128, 4 * TB_N * 64], F32))
            naRM = es2.enter_context(SB(nc, "a_naRM", [128, 4 * 8 * 8], F32))
            P.dma("sp", naT[:], na_T[:, :], w=[t_c])
            P.dma("sp", naRM[:], na_RM[:, :], w=[t_c])
            naTv = naT[:].rearrange("p (h b q) -> p h b q", b=TB_N, q=64)
            naRMv = naRM[:].rearrange("p (c j r) -> p c j r", j=8, r=8)
            psS = Pool(nc, es2, "ps", 4, [128, 512], F32, "ap_S")
            psO = Pool(nc, es2, "ps", 2, [128, 512], F32, "ap_O")
            psD = Pool(nc, es2, "ps", 2, [128, 512], F32, "ap_D")
            pT = Pool(nc, es2, "sb", 6, [128, 512], BF16, "a_pT")
            sT = Pool(nc, es2, "sb", 4, [128, 512], F32, "a_sT")
            LOOK = 2
            rd = Pool(nc, es2, "sb", 2, [128, 512], F32, "a_rd")
            ao = Pool(nc, es2, "sb", 2, [128, 512], BF16, "a_ao")

            def finish(po, pot, pd, pdt, row0, t0):
                r_, r_t = rd.get()
                P.op("dve", lambda e: e.reciprocal(out=r_[:], in_=pd[:, :]), r=[pdt], w=[r_t])
                a_, a_t = ao.get()
                P.op("dve", lambda e: e.tensor_tensor(out=a_[:], in0=po[:, :], in1=r_[:], op=ALU.mult), r=[pot, r_t], w=[a_t])
                P.dma("sp", aoT[row0:row0 + 128, t0:t0 + 512], a_[:], r=[a_t], w=[t_out])

            SC_MLA = 192 ** -0.5
            SC_NA = 128 ** -0.5
            for h in range(4):
                q_, q_t = qb.get()
                P.dma("sp", q_[:], qn_d[h], r=[t_d["qn"]], w=[q_t])
                if h % 2 == 0:
                    qp_, qp_t = qpb.get()
                    P.dma("sp", qp_[:], qp_d[h // 2], r=[t_d["qp"]], w=[qp_t])
                k_, k_t = kb.get()
                P.dma("sp", k_[:], kn_d[h], r=[t_d["kn"]], w=[k_t])
                v_, v_t = vb.get()
                P.dma("sp", v_[:].rearrange("p (t d) -> p t d", d=128),
                      vm_d[:, h * 128:(h + 1) * 128].rearrange("(t p) d -> p t d", p=128), r=[t_d["vm"]], w=[v_t])
                pb = 64 * (h % 2)
                for t0 in range(0, S, 512):
                    po, pot = psO.get()
                    pd, pdt = psD.get()

                    def issue_s(kt, t0=t0):
                        ps, pst = psS.get()
                        P.op("pe", lambda e, ps=ps, kt=kt: e.matmul(ps[:, :], k_[:, kt * 128:(kt + 1) * 128], q_[:, t0:t0 + 512],
                                                                    start=True, stop=False), r=[k_t, q_t], w=[pst])
                        P.op("pe", lambda e, ps=ps, kt=kt: e.matmul(ps[:, :], kpb[pb:pb + 64, kt * 128:(kt + 1) * 128],
                                                                    qp_[pb:pb + 64, t0:t0 + 512], start=False, stop=True),
                             r=[t_kpb, qp_t], w=[pst])
                        return ps, pst
                    pend = [issue_s(kt) for kt in range(min(LOOK, NKT))]
                    for kt in range(NKT):
                        ps, pst = pend.pop(0)
                        p_, p_t = pT.get()
                        P.op("act", lambda e, ps=ps, p_=p_: e.activation(out=p_[:], in_=ps[:, :], func=AF.Exp, scale=SC_MLA),
                             r=[pst], w=[p_t])
                        if kt + LOOK < NKT:
                            pend.append(issue_s(kt + LOOK))
                        P.op("pe", lambda e, p_=p_, kt=kt: e.matmul(po[:, :], v_[:, kt * 128:(kt + 1) * 128], p_[:],
                                                                    start=(kt == 0), stop=(kt == NKT - 1)), r=[v_t, p_t], w=[pot])
                        P.op("pe", lambda e, p_=p_, kt=kt: e.matmul(pd[:, :], ones_b[:, :], p_[:],
                                                                    start=(kt == 0), stop=(kt == NKT - 1)), r=[t_c, p_t], w=[pdt])
                    finish(po, pot, pd, pdt, h * 128, t0)
            for h in range(4):
                q_, q_t = qb.get()
                P.dma("sp", q_[:], qa_d[h], r=[t_d["qa"]], w=[q_t])
                k_, k_t = kb.get()
                P.dma("sp", k_[:], ka_d[h], r=[t_d["ka"]], w=[k_t])
                v_, v_t = vb.get()
                P.dma("sp", v_[:].rearrange("p (t d) -> p t d", d=128),
                      va_d[:, h * 128:(h + 1) * 128].rearrange("(t p) d -> p t d", p=128), r=[t_d["va"]], w=[v_t])
                for qc in range(4):
                    t0 = qc * 512
                    jlo, jhi = NA_J[qc]
                    kts = list(range(jlo, jhi)) + [16, 17]
                    po, pot = psO.get()
                    pd, pdt = psD.get()

                    def issue_s(kt, t0=t0):
                        ps, pst = psS.get()
                        P.op("pe", lambda e, ps=ps, kt=kt: e.matmul(ps[:, :], k_[:, kt * 128:(kt + 1) * 128], q_[:, t0:t0 + 512],
                                                                    start=True, stop=True), r=[k_t, q_t], w=[pst])
                        return ps, pst
                    pend = [issue_s(kt) for kt in kts[:LOOK]]
                    for idx, kt in enumerate(kts):
                        ps, pst = pend.pop(0)
                        p_, p_t = pT.get()
                        if kt < 16:
                            b0 = 8 * qc - 2 * kt + 7 + TB_OFF
                            s_, s_t = sT.get()
                            P.op("dve", lambda e, ps=ps, s_=s_, b0=b0: e.scalar_tensor_tensor(
                                out=s_[:].rearrange("p (r q) -> p r q", q=64), in0=ps[:, :].rearrange("p (r q) -> p r q", q=64),
                                scalar=SC_NA, in1=naTv[:, h, b0:b0 + 8, :], op0=ALU.mult, op1=ALU.add), r=[pst, t_c], w=[s_t])
                            jj = kt - jlo
                            P.op("pool" if idx % 3 == 2 else "dve", lambda e, s_=s_, jj=jj: e.tensor_tensor(
                                out=s_[:].rearrange("p (r q) -> p r q", q=64), in0=s_[:].rearrange("p (r q) -> p r q", q=64),
                                in1=naRMv[:, qc, jj, :].unsqueeze(2).to_broadcast([128, 8, 64]), op=ALU.add), r=[s_t, t_c], w=[s_t])
                            P.op("act", lambda e, s_=s_, p_=p_: e.activation(out=p_[:], in_=s_[:], func=AF.Exp), r=[s_t], w=[p_t])
                        else:
                            P.op("act", lambda e, ps=ps, p_=p_: e.activation(out=p_[:], in_=ps[:, :], func=AF.Exp, scale=SC_NA),
                                 r=[pst], w=[p_t])
                        if idx + LOOK < len(kts):
                            pend.append(issue_s(kts[idx + LOOK]))
                        last = (idx == len(kts) - 1)
                        P.op("pe", lambda e, p_=p_, kt=kt, idx=idx, last=last: e.matmul(
                            po[:, :], v_[:, kt * 128:(kt + 1) * 128], p_[:], start=(idx == 0), stop=last), r=[v_t, p_t], w=[pot])
                        P.op("pe", lambda e, p_=p_, idx=idx, last=last: e.matmul(pd[:, :], ones_b[:, :], p_[:], start=(idx == 0), stop=last),
                             r=[t_c, p_t], w=[pdt])
                    finish(po, pot, pd, pdt, 512 + h * 128, t0)
            P.barrier()


def na_tables_host(rpb4):
    NEG = np.float32(-1e30)
    qcol = np.arange(64)
    kc = np.arange(64)
    cs = np.clip(qcol - 8, 0, 48)
    col_ok = (kc[None, :] >= cs[:, None]) & (kc[None, :] < cs[:, None] + 16)
    dc = np.clip(kc[None, :] - qcol[:, None], -15, 15) + 15
    T = np.full((128, 4, TB_N, 64), NEG, dtype=np.float32)
    for rk in range(2):
        for bi in range(TB_N):
            b = bi - TB_OFF
            a = 14 - b + rk
            if 0 <= a <= 14:
                for h in range(4):
                    vals = rpb4[h, a][dc]
                    blk = np.where(col_ok, vals, NEG)
                    T[rk * 64:(rk + 1) * 64, h, bi, :] = blk.T
    RM = np.full((128, 4, 8, 8), NEG, dtype=np.float32)
    for qc in range(4):
        jlo, jhi = NA_J[qc]
        for jj in range(jhi - jlo):
            j = jlo + jj
            for rq in range(8):
                r = 8 * qc + rq
                rs = min(max(r - 4, 0), 24)
                for rk in range(2):
                    kr = 2 * j + rk
                    if rs <= kr <= rs + 7:
                        RM[rk * 64:(rk + 1) * 64, qc, jj, rq] = 0.0
    return T.reshape(128, -1), RM.reshape(128, -1)


def rope_tables_host():
    t = np.arange(S)
    row = (t // 64).astype(np.float32)
    col = (t % 64).astype(np.float32)
    n_freq = 16
    inv = (10000.0 ** (-np.arange(n_freq, dtype=np.float32) / n_freq)).astype(np.float32)
    ang = np.concatenate([row[:, None] * inv, col[:, None] * inv], axis=1)
    cos = np.cos(ang).astype(np.float32)
    sin = np.sin(ang).astype(np.float32)
    i = np.arange(128)
    f = (i % 64) % 32
    sign = np.where((i % 64) < 32, -1.0, 1.0).astype(np.float32)
    cosT = np.ascontiguousarray(cos[:, f].T)
    sinT = np.ascontiguousarray((sin[:, f] * sign[None, :]).T)
    return cosT, sinT


def attn_host_inputs(inp, m0, b, hf):
    def fm(v):
        return np.ascontiguousarray(v.reshape(16, 128).T)
    w = inp["ab_w_in"][0]
    hs = [4 * hf + i for i in range(4)]
    cols = []
    for h in hs:
        cols += list(range(h * 192, h * 192 + 128))
    for h in hs:
        cols += list(range(h * 192 + 128, h * 192 + 192))
    for h in hs:
        cols += list(range(h * 192 + 160, h * 192 + 192)) + list(range(h * 192 + 128, h * 192 + 160))
    cols += list(range(1536, 2048))
    kp = list(range(2048, 2112))
    kps = list(range(2080, 2112)) + list(range(2048, 2080))
    cols += kp + kp + kps + kps
    for off in (2112, 3136, 4160):
        for h in hs:
            cols += list(range(off + h * 128, off + (h + 1) * 128))
    w_in_r = np.ascontiguousarray(w[:, cols])
    wu = inp["ab_w_ukv"][0]
    ucols = []
    for h in hs:
        ucols += list(range(h * 256, h * 256 + 128))
    for h in hs:
        ucols += list(range(h * 256 + 128, h * 256 + 256))
    w_ukv_r = np.ascontiguousarray(wu[:, ucols])
    naT, naRM = na_tables_host(inp["ab_rpb"][0][4 * hf:4 * hf + 4])
    cosT, sinT = rope_tables_host()
    mb = m0[b]
    mc = m0[4]
    modv = np.concatenate([fm(mb[0:2048]), fm(mb[2048:4096]), fm(mc[0:2048]), fm(mc[2048:4096])], axis=1)
    return {"xT": np.ascontiguousarray(inp["x"][b].T), "ctxT": np.ascontiguousarray(inp["ctx"][b].T), "modv": modv,
            "w_in_r": w_in_r, "kvn": np.ascontiguousarray(inp["ab_kv_norm"][0].reshape(4, 128).T), "w_ukv_r": w_ukv_r,
            "cosT": cosT, "sinT": sinT, "na_T": naT, "na_RM": naRM}


def build_attn_prog():
    nc = bass.Bass("TRN2", target_bir_lowering=False)
    consts = make_consts()
    with ExitStack() as es:
        P = Prog(nc, es)
        cst = declare_consts(nc, consts)
        xT = dram_in(nc, "xT", [D, S])
        ctxT = dram_in(nc, "ctxT", [D, LC])
        modv = dram_in(nc, "modv", [128, 64])
        w_in_r = dram_in(nc, "w_in_r", [D, 3328])
        kvn = dram_in(nc, "kvn", [128, 4])
        w_ukv_r = dram_in(nc, "w_ukv_r", [512, 1024])
        cosT = dram_in(nc, "cosT", [128, S])
        sinT = dram_in(nc, "sinT", [128, S])
        na_T = dram_in(nc, "na_T", [128, 4 * TB_N * 64])
        na_RM = dram_in(nc, "na_RM", [128, 256])
        aoT = dram_out(nc, "aoT", [1024, S], BF16)
        stage_attn(P, cst, xT, ctxT, modv, w_in_r, kvn, w_ukv_r, cosT, sinT, na_T, na_RM, aoT, Tr())
        P.barrier()
    return nc, consts


def stage_moe_ffn(P, xe_in, w1, w3, w2, ye_out, t_in, t_out, load_xe=None, nb=4, n_exp=2):
    nc = P.nc
    if load_xe is None:
        def load_xe(dst, ex, b, w):
            P.dma("sp", dst, xe_in[ex, b].rearrange("p (c s) -> p c s", s=CAP), r=[t_in], w=w)
    NFT = FF // 128
    NT = nb * CAP
    TCH = min(512, NT)
    with ExitStack() as es:
        xe_t = [es.enter_context(SB(nc, "f_xe%d" % i, [128, 16 * NT], BF16)) for i in range(2)]
        t_xe = [Tr(), Tr()]
        wbuf = Pool(nc, es, "sb", 2, [128, 16 * FF], BF16, "f_w")
        w2buf = Pool(nc, es, "sb", 2, [128, NFT * 512], BF16, "f_w2")
        h1_t = es.enter_context(SB(nc, "f_h1", [128, NFT * NT], BF16))
        h1 = h1_t[:].rearrange("p (f t) -> p f t", t=NT)
        t_h1 = Tr()
        stg = Pool(nc, es, "sb", 3, [128, 512], BF16, "f_stg")
        pspool = Pool(nc, es, "ps", 6, [128, 512], F32, "fp_p")
        def fetch(ex):
            xv = xe_t[ex % 2][:].rearrange("p (c b s) -> p c b s", b=nb, s=CAP)
            for b in range(nb):
                load_xe(xv[:, :, b, :], ex, b, [t_xe[ex % 2]])
        fetch(0)
        for ex in range(n_exp):
            if ex + 1 < n_exp:
                fetch(ex + 1)
            xf = xe_t[ex % 2][:].rearrange("p (c t) -> p c t", t=NT)
            for which, wsrc in ((0, w1), (1, w3)):
                wt, wtr = wbuf.get()
                wv = wt[:].rearrange("p (k c) -> p k c", c=FF)
                for half in range(2):
                    P.dma("pool", wv[:, half * 8:(half + 1) * 8, :],
                          wsrc[ex, half * 1024:(half + 1) * 1024, :].rearrange("(k p) c -> p k c", p=128), w=[wtr])
                for f in range(NFT):
                    for t0 in range(0, NT, TCH):
                        ps, ptr = pspool.get()
                        for k in range(16):
                            P.op("pe", lambda e, ps=ps, k=k, f=f, t0=t0: e.matmul(ps[:, 0:TCH], wv[:, k, f * 128:(f + 1) * 128],
                                                                                  xf[:, k, t0:t0 + TCH], start=(k == 0), stop=(k == 15)),
                                 r=[wtr, t_xe[ex % 2]], w=[ptr])
                        if which == 0:
                            P.op("act", lambda e, ps=ps, f=f, t0=t0: e.activation(out=h1[:, f, t0:t0 + TCH], in_=ps[:, 0:TCH], func=AF.Silu),
                                 r=[ptr], w=[t_h1])
                        else:
                            P.op("dve", lambda e, ps=ps, f=f, t0=t0: e.tensor_tensor(out=h1[:, f, t0:t0 + TCH], in0=h1[:, f, t0:t0 + TCH],
                                                                                     in1=ps[:, 0:TCH], op=ALU.mult), r=[ptr, t_h1], w=[t_h1])
            for dq in range(4):
                w2t, w2tr = w2buf.get()
                w2v = w2t[:].rearrange("p (f d) -> p f d", d=512)
                P.dma("pool", w2v, w2[ex, :, dq * 512:(dq + 1) * 512].rearrange("(f p) d -> p f d", p=128), w=[w2tr])
                for g in range(2 * nb):
                    ps, ptr = pspool.get()
                    for f in range(NFT):
                        P.op("pe", lambda e, ps=ps, f=f, g=g: e.matmul(ps[:, :], h1[:, f, g * 128:(g + 1) * 128], w2v[:, f, :],
                                                                       start=(f == 0), stop=(f == NFT - 1)), r=[t_h1, w2tr], w=[ptr])
                    s_, s_t = stg.get()
                    evac_copy(P, s_[:], ps[:, :], [ptr], [s_t])
                    P.dma("sp", ye_out[ex, g, :, dq * 512:(dq + 1) * 512], s_[:], r=[s_t], w=[t_out])
        P.barrier()


def stage_scatter_ln(P, cst, ye_in, pos_in, gate_in, hT, vecs, outT, t_in, t_out, load_ye=None, load_pg=None):
    nc = P.nc
    if load_ye is None:
        def load_ye(dst, ex, dh, w):
            P.dma("sp", dst, ye_in[ex, :, :, dh * 1024:(dh + 1) * 1024].rearrange("s p d -> p s d"), r=[t_in], w=w)
    if load_pg is None:
        def load_pg(pos, gate, w):
            P.dma("sp", pos, pos_in[:, :], r=[t_in], w=w)
            P.dma("sp", gate, gate_in[:, :], r=[t_in], w=w)
    T = 1024
    z_d = dram_scratch(nc, "s_z%d" % id(outT), [D, T], F32)
    t_z = Tr()
    with ExitStack() as es:
        def sb(name, shape, dt):
            return es.enter_context(SB(nc, "s_" + name, shape, dt))
        vec_sb = sb("vec", [128, 48], F32)
        ones_f = sb("ones_f", [128, 128], F32)
        t_c = Tr()
        load_pieces(P, vec_sb, vecs, [t_c])
        P.dma("sp", ones_f[:], cst["ones_f"][:, :], w=[t_c])
        with ExitStack() as es1:
            pidx = es1.enter_context(SB(nc, "s_pidx", [128, 2], F32))
            selrow = es1.enter_context(SB(nc, "s_selrow", [16, 16 * 128], F32))
            pos = es1.enter_context(SB(nc, "s_pos", [16, T], F32))
            gate = es1.enter_context(SB(nc, "s_gate", [16, T], F32))
            P.dma("sp", pidx[:], cst["pidx"][:, :], w=[t_c])
            P.dma("sp", selrow[:].rearrange("p (e m) -> p e m", m=128), cst["selrow16"][:, :, :], w=[t_c])
            load_pg(pos[:], gate[:], [t_c])
            selrow_b = es1.enter_context(SB(nc, "s_selrowb", [16, 16 * 128], BF16))
            pos_b = es1.enter_context(SB(nc, "s_posb", [16, T], BF16))
            gate_b = es1.enter_context(SB(nc, "s_gateb", [16, T], BF16))
            P.op("dve", lambda e: e.tensor_copy(out=selrow_b[:], in_=selrow[:]), r=[t_c], w=[t_c])
            P.op("dve", lambda e: e.tensor_copy(out=pos_b[:], in_=pos[:]), r=[t_c], w=[t_c])
            P.op("dve", lambda e: e.tensor_copy(out=gate_b[:], in_=gate[:]), r=[t_c], w=[t_c])
            selg_t = es1.enter_context(SB(nc, "s_selg", [128, 32 * T], BF16))
            selg = selg_t[:].rearrange("p (g t) -> p g t", t=T)
            t_selg = Tr()
            psB = Pool(nc, es1, "ps", 4, [128, 512], F32, "sp_B")
            psO = Pool(nc, es1, "ps", 2, [128, 512], F32, "sp_O")
            gb = Pool(nc, es1, "sb", 2, [128, 512], F32, "s_gb")
            for ex in range(16):
                for tq in range(T // 512):
                    pb, pbt = psB.get()
                    pg, pgt = psB.get()
                    P.op("pe", lambda e, pb=pb, ex=ex, tq=tq: e.matmul(pb[:, :], selrow_b[:, ex * 128:(ex + 1) * 128],
                                                                       pos_b[:, tq * 512:(tq + 1) * 512], start=True, stop=True),
                         r=[t_c], w=[pbt])
                    P.op("pe", lambda e, pg=pg, ex=ex, tq=tq: e.matmul(pg[:, :], selrow_b[:, ex * 128:(ex + 1) * 128],
                                                                       gate_b[:, tq * 512:(tq + 1) * 512], start=True, stop=True),
                         r=[t_c], w=[pgt])
                    g_, g_t = gb.get()
                    P.op("act", lambda e, g_=g_, pg=pg: e.copy(out=g_[:], in_=pg[:, :]), r=[pgt], w=[g_t])
                    for st in range(2):
                        P.op("dve", lambda e, pb=pb, g_=g_, st=st, ex=ex, tq=tq: e.scalar_tensor_tensor(
                            out=selg[:, ex * 2 + st, tq * 512:(tq + 1) * 512], in0=pb[:, :], scalar=pidx[:, st:st + 1],
                            in1=g_[:], op0=ALU.is_equal, op1=ALU.mult), r=[pbt, g_t, t_c], w=[t_selg])
            ye_t = es1.enter_context(SB(nc, "s_ye", [128, 32 * 1024], BF16))
            ye = ye_t[:].rearrange("p (g d) -> p g d", d=1024)
            t_ye = Tr()
            hin = Pool(nc, es1, "sb", 2, [128, 512], F32, "s_hin")
            yg = Pool(nc, es1, "sb", 2, [128, 512], F32, "s_yg")
            for dh in range(2):
                for ex in range(16):
                    load_ye(ye[:, ex * 2:ex * 2 + 2, :], ex, dh, [t_ye])
                for dt in range(8):
                    c = dh * 8 + dt
                    for tq in range(T // 512):
                        po, pot = psO.get()
                        for g in range(32):
                            P.op("pe", lambda e, po=po, g=g, dt=dt, tq=tq: e.matmul(po[:, :], ye[:, g, dt * 128:(dt + 1) * 128],
                                                                                    selg[:, g, tq * 512:(tq + 1) * 512],
                                                                                    start=(g == 0), stop=(g == 31)),
                                 r=[t_ye, t_selg], w=[pot])
                        h_, h_t = hin.get()
                        P.dma("sp", h_[:], hT[c * 128:(c + 1) * 128, tq * 512:(tq + 1) * 512], r=[t_in], w=[h_t])
                        y_, y_t = yg.get()
                        P.op("act", lambda e, y_=y_, po=po, c=c: e.activation(out=y_[:], in_=po[:, :], func=AF.Identity,
                                                                              scale=vec_sb[:, c:c + 1]), r=[pot, t_c], w=[y_t])
                        P.op("dve", lambda e, y_=y_, h_=h_: e.scalar_tensor_tensor(out=y_[:], in0=h_[:], scalar=ALPHA, in1=y_[:],
                                                                                   op0=ALU.mult, op1=ALU.add), r=[h_t, y_t], w=[y_t])
                        P.dma("sp", z_d[c * 128:(c + 1) * 128, tq * 512:(tq + 1) * 512], y_[:], r=[y_t], w=[t_z])
            P.barrier()
        z_t = sb("z", [128, 16 * T], F32)
        z = z_t[:].rearrange("p (c t) -> p c t", t=T)
        t_zs = Tr()
        P.dma("sp", z, z_d.rearrange("(c p) t -> p c t", p=128), r=[t_z], w=[t_zs])
        pspool = Pool(nc, es, "ps", 4, [128, 512], F32, "sp_L")
        ln_apply(P, es, z, t_zs, T, vec_sb[:, 16:32], vec_sb[:, 32:48], t_c, ones_f, t_c, outT, t_out, pspool, "s_ln")
        P.barrier()


def stage_oproj_ln(P, cst, ao_in, w_out, hT, vecs, outT, t_in, t_out, load_ao=None):
    nc = P.nc
    if load_ao is None:
        def load_ao(dst, w):
            P.dma("sp", dst, ao_in.rearrange("(c p) t -> p c t", p=128), r=[t_in], w=w)
    T = 1024
    with ExitStack() as es:
        def sb(name, shape, dt):
            return es.enter_context(SB(nc, "o_" + name, shape, dt))
        vec_sb = sb("vec", [128, 48], F32)
        ones_f = sb("ones_f", [128, 128], F32)
        t_c = Tr()
        load_pieces(P, vec_sb, vecs, [t_c])
        P.dma("sp", ones_f[:], cst["ones_f"][:, :], w=[t_c])
        z_t = sb("z", [128, 16 * T], F32)
        z = z_t[:].rearrange("p (c t) -> p c t", t=T)
        t_z = Tr()
        with ExitStack() as es1:
            ao_t = es1.enter_context(SB(nc, "o_ao", [128, 16 * T], BF16))
            ao = ao_t[:].rearrange("p (c t) -> p c t", t=T)
            t_ao = Tr()
            load_ao(ao, [t_ao])
            wpool = Pool(nc, es1, "sb", 2, [128, 16 * 512], BF16, "o_w")
            pspool = Pool(nc, es1, "ps", 4, [128, 512], F32, "op_p")
            hin = Pool(nc, es1, "sb", 2, [128, 512], F32, "o_hin")
            yg = Pool(nc, es1, "sb", 2, [128, 512], F32, "o_yg")
            for grp in range(4):
                wv, wtr = load_wgroup(P, wpool, w_out, grp * 512, 512)
                for ft in range(4):
                    c = grp * 4 + ft
                    for t0 in range(0, T, 512):
                        ps, ptr = proj_fm(P, pspool, wv, wtr, ft, ao, t_ao, t0, 512)
                        h_, h_t = hin.get()
                        P.dma("sp", h_[:], hT[c * 128:(c + 1) * 128, t0:t0 + 512], r=[t_in], w=[h_t])
                        y_, y_t = yg.get()
                        P.op("act", lambda e, y_=y_, ps=ps, c=c: e.activation(out=y_[:], in_=ps[:, :], func=AF.Identity,
                                                                              scale=vec_sb[:, c:c + 1]), r=[ptr, t_c], w=[y_t])
                        P.op("dve", lambda e, y_=y_, h_=h_, c=c, t0=t0: e.scalar_tensor_tensor(
                            out=z[:, c, t0:t0 + 512], in0=h_[:], scalar=ALPHA, in1=y_[:], op0=ALU.mult, op1=ALU.add),
                            r=[h_t, y_t], w=[t_z])
            P.barrier()
        pspool2 = Pool(nc, es, "ps", 4, [128, 512], F32, "op_L")
        ln_apply(P, es, z, t_z, T, vec_sb[:, 16:32], vec_sb[:, 32:48], t_c, ones_f, t_c, outT, t_out, pspool2, "o_ln")
        P.barrier()


NF = 17


def stage_cd(P, cst, hT, modv, w_in_c, cvec, fw1, fw2, fvec, fw3_c, delta_b, cdT, t_in, t_out, load_h=None):
    nc = P.nc
    if load_h is None:
        def load_h(dst, c, w):
            P.dma("sp", dst, hT[c * 128:(c + 1) * 128, :], r=[t_in], w=w)
    with ExitStack() as es:
        def sb(name, shape, dt):
            return es.enter_context(SB(nc, "h_" + name, shape, dt))
        modv_sb = sb("modv", [128, 32], F32)
        sc1p = sb("sc1p", [128, 16], F32)
        cvec_sb = sb("cvec", [128, 56], F32)
        ident_b = sb("ident_b", [128, 128], BF16)
        wN = sb("wN", [128, NF], F32)
        t01n = sb("t01n", [128, 16], F32)
        mask0 = sb("mask0", [128, 1], F32)
        dlt = sb("dlt", [128, 512], F32)
        t_c = Tr()
        load_pieces(P, modv_sb, modv, [t_c])
        P.dma("sp", cvec_sb[:], cvec[:, :], w=[t_c])
        P.dma("sp", ident_b[:], cst["ident_b"][:, :], w=[t_c])
        P.dma("sp", wN[:], cst["wN"][:, :], w=[t_c])
        P.dma("sp", t01n[:], cst["t01n"][:, :], w=[t_c])
        P.dma("sp", mask0[:], cst["mask0"][:, :], w=[t_c])
        P.dma("sp", dlt[:], delta_b[:, :], w=[t_c])
        P.op("dve", lambda e: e.tensor_scalar(out=sc1p[:], in0=modv_sb[:, 16:32], scalar1=1.0, scalar2=None, op0=ALU.add),
             r=[t_c], w=[t_c])
        xs_t = [sb("xs%d" % o, [128, 4 * S], BF16) for o in range(3)]
        xs = [t[:].rearrange("p (c t) -> p c t", t=S) for t in xs_t]
        t_xs = [Tr(), Tr(), Tr()]
        es_g = ExitStack()
        g_t = es_g.enter_context(SB(nc, "h_g", [128, 4 * S], BF16))
        gT = g_t[:].rearrange("p (c t) -> p c t", t=S)
        t_g = Tr()
        with ExitStack() as es1:
            u_t = es1.enter_context(SB(nc, "h_u", [128, 16 * S], BF16))
            u = u_t[:].rearrange("p (k t) -> p k t", t=S)
            t_u = Tr()
            es_x = ExitStack()
            xin = Pool(nc, es_x, "sb", 2, [128, S], F32, "h_xin")
            for c in range(16):
                xt, xtr = xin.get()
                load_h(xt[:], c, [xtr])
                P.op("act", lambda e, c=c, xt=xt: e.activation(out=u[:, c, :], in_=xt[:], func=AF.Identity,
                                                               bias=modv_sb[:, c:c + 1], scale=sc1p[:, c:c + 1]),
                     r=[xtr, t_c], w=[t_u])
            P.barrier()
            es_x.close()
            wpool = Pool(nc, es1, "sb", 2, [128, 16 * 512], BF16, "h_w")
            pspool = Pool(nc, es1, "ps", 4, [128, 512], F32, "hp_p")
            prow = Pool(nc, es1, "sb", 2, [128, S + 2], F32, "h_prow")
            acc = Pool(nc, es1, "sb", 2, [128, S], F32, "h_acc")
            for o in range(3):
                wv, wtr = load_wgroup(P, wpool, w_in_c, o * 512, 512)
                for ct in range(4):
                    pr, prt = prow.get()
                    P.op("pool", lambda e, pr=pr: e.memset(pr[:, 0:1], 0.0), w=[prt])
                    P.op("pool", lambda e, pr=pr: e.memset(pr[:, S + 1:S + 2], 0.0), w=[prt])
                    for t0 in range(0, S, 512):
                        ps, ptr = proj_fm(P, pspool, wv, wtr, ct, u, t_u, t0, 512)
                        evac_copy(P, pr[:, 1 + t0:1 + t0 + 512], ps[:, :], [ptr], [prt])
                    a_, a_t = acc.get()
                    j = (o * 4 + ct) * 4
                    P.op("act", lambda e, a_=a_, pr=pr, j=j: e.activation(out=a_[:], in_=pr[:, 1:S + 1], func=AF.Identity,
                                                                          bias=cvec_sb[:, j + 3:j + 4], scale=cvec_sb[:, j + 1:j + 2]),
                         r=[prt, t_c], w=[a_t])
                    P.op("dve", lambda e, a_=a_, pr=pr, j=j: e.scalar_tensor_tensor(out=a_[:], in0=pr[:, 0:S], scalar=cvec_sb[:, j:j + 1],
                                                                                    in1=a_[:], op0=ALU.mult, op1=ALU.add),
                         r=[prt, a_t, t_c], w=[a_t])
                    P.op("dve", lambda e, a_=a_, pr=pr, j=j, o=o, ct=ct: e.scalar_tensor_tensor(
                        out=xs[o][:, ct, :], in0=pr[:, 2:S + 2], scalar=cvec_sb[:, j + 2:j + 3], in1=a_[:], op0=ALU.mult, op1=ALU.add),
                        r=[prt, a_t, t_c], w=[t_xs[o]])
            wv, wtr = load_wgroup(P, wpool, w_in_c, 1536, 512)
            for ct in range(4):
                for t0 in range(0, S, 512):
                    ps, ptr = proj_fm(P, pspool, wv, wtr, ct, u, t_u, t0, 512)
                    evac_copy(P, gT[:, ct, t0:t0 + 512], ps[:, :], [ptr], [t_g])
            P.barrier()
        with ExitStack() as es2:
            csw = es2.enter_context(SB(nc, "h_csw", [128, 2 * 512], BF16))
            P.dma("sp", csw[:].rearrange("p (k c) -> p k c", c=512), cst["CSW"].rearrange("(k p) c -> p k c", p=128), w=[t_c])
            cswv = csw[:].rearrange("p (k c) -> p k c", c=512)
            ab_t = es2.enter_context(SB(nc, "h_ab", [128, 16 * 2 * 512], BF16))
            ab = ab_t[:].rearrange("p (t g c) -> p t g c", g=2, c=512)
            t_ab = Tr()
            pspool = Pool(nc, es2, "ps", 8, [128, 512], F32, "hp_f")
            for tt in range(16):
                for grp in range(2):
                    ps, ptr = pspool.get()
                    for k in range(2):
                        P.op("pe", lambda e, ps=ps, k=k, grp=grp, tt=tt: e.matmul(ps[:, :], gT[:, grp * 2 + k, tt * 128:(tt + 1) * 128],
                                                                                  cswv[:, k, :], start=(k == 0), stop=(k == 1)),
                             r=[t_g, t_c], w=[ptr])
                    evac_copy(P, ab[:, tt, grp, :], ps[:, :], [ptr], [t_ab])
            rblk = Pool(nc, es2, "sb", 4, [128, 1024], BF16, "h_frb")
            ost = Pool(nc, es2, "sb", 3, [128, 512], BF16, "h_fo")
            for th in range(2):
                accs = [pspool.get() for _ in range(8)]
                for tc_ in range(16):
                    rc, rct = rblk.get()
                    rs_, rst = rblk.get()
                    P.dma("sp", rc[:], cst["FC"][tc_ * 128:(tc_ + 1) * 128, th * 1024:(th + 1) * 1024], w=[rct])
                    P.dma("sp", rs_[:], cst["FS"][tc_ * 128:(tc_ + 1) * 128, th * 1024:(th + 1) * 1024], w=[rst])
                    for ct in range(4):
                        grp, half = ct // 2, ct % 2
                        for tq in range(2):
                            ps, ptr = accs[ct * 2 + tq]
                            P.op("pe", lambda e, ps=ps, rc=rc, tc_=tc_, grp=grp, half=half, tq=tq: e.matmul(
                                ps[:, :], ab[:, tc_, grp, half * 128:(half + 1) * 128], rc[:, tq * 512:(tq + 1) * 512],
                                start=(tc_ == 0), stop=False), r=[t_ab, rct], w=[ptr])
                            P.op("pe", lambda e, ps=ps, rs_=rs_, tc_=tc_, grp=grp, half=half, tq=tq: e.matmul(
                                ps[:, :], ab[:, tc_, grp, 256 + half * 128:256 + (half + 1) * 128], rs_[:, tq * 512:(tq + 1) * 512],
                                start=False, stop=(tc_ == 15)), r=[t_ab, rst], w=[ptr])
                for ct in range(4):
                    for tq in range(2):
                        ps, ptr = accs[ct * 2 + tq]
                        o_, o_t = ost.get()
                        evac_copy(P, o_[:], ps[:, :], [ptr], [o_t])
                        P.dma("sp", cdT[512 + ct * 128:512 + (ct + 1) * 128, th * 1024 + tq * 512:th * 1024 + (tq + 1) * 512], o_[:],
                              r=[o_t], w=[t_out])
            P.barrier()
        es_g.close()
        hd2 = sb("hd2", [64, S], F32)
        t_hd2 = Tr()
        with ExitStack() as es3:
            zf = es3.enter_context(SB(nc, "h_zf", [33, S], F32))
            fw1_sb = es3.enter_context(SB(nc, "h_fw1", [33, 64], F32))
            fw2_sb = es3.enter_context(SB(nc, "h_fw2", [64, 64], F32))
            fv = es3.enter_context(SB(nc, "h_fv", [64, 8], F32))
            hd1 = es3.enter_context(SB(nc, "h_hd1", [64, S], F32))
            tmp = es3.enter_context(SB(nc, "h_ftmp", [64, S], F32))
            t_f = Tr()
            t_hd1 = Tr()
            t_tmp = Tr()
            P.dma("sp", zf[:], cst["zfeatT"][:, :], w=[t_f])
            P.dma("sp", fw1_sb[:], fw1[:, :], w=[t_f])
            P.dma("sp", fw2_sb[:], fw2[:, :], w=[t_f])
            P.dma("sp", fv[:, 0:4], fvec[:, :], w=[t_f])
            for (a, bcol, so, bo) in ((1, 0, 4, 5), (3, 2, 6, 7)):
                P.op("dve", lambda e, a=a, so=so: e.tensor_scalar(out=fv[:, so:so + 1], in0=fv[:, a:a + 1], scalar1=1.0 / 3, scalar2=None,
                                                                  op0=ALU.mult), r=[t_f], w=[t_f])
                P.op("dve", lambda e, so=so, bcol=bcol, bo=bo: e.tensor_tensor(out=fv[:, bo:bo + 1], in0=fv[:, so:so + 1],
                                                                               in1=fv[:, bcol:bcol + 1], op=ALU.mult), r=[t_f], w=[t_f])
            pspool = Pool(nc, es3, "ps", 4, [128, 512], F32, "hp_m")
            for (wsb, src, t_src, dst, t_dst, so, bo, kk) in ((fw1_sb, zf, t_f, hd1, t_hd1, 4, 5, 33), (fw2_sb, hd1, t_hd1, hd2, t_hd2, 6, 7, 64)):
                for t0 in range(0, S, 512):
                    ps, ptr = pspool.get()
                    P.op("pe", lambda e, ps=ps, wsb=wsb, src=src, t0=t0, kk=kk: e.matmul(ps[0:64, :], wsb[0:kk, :], src[0:kk, t0:t0 + 512],
                                                                                        start=True, stop=True), r=[t_f, t_src], w=[ptr])
                    P.op("act", lambda e, ps=ps, t0=t0, so=so, bo=bo: e.activation(out=tmp[:, t0:t0 + 512], in_=ps[0:64, :], func=AF.Sin,
                                                                                   bias=fv[:, bo:bo + 1], scale=fv[:, so:so + 1]),
                         r=[ptr, t_f], w=[t_tmp])
                    P.op("dve", lambda e, dst=dst, t0=t0: e.tensor_tensor(out=dst[:, t0:t0 + 512], in0=tmp[:, t0:t0 + 512],
                                                                          in1=tmp[:, t0:t0 + 512], op=ALU.mult), r=[t_tmp], w=[t_dst])
                    P.op("dve", lambda e, dst=dst, t0=t0: e.tensor_scalar(out=dst[:, t0:t0 + 512], in0=dst[:, t0:t0 + 512], scalar1=-4.0,
                                                                          scalar2=3.0, op0=ALU.mult, op1=ALU.add), r=[t_dst], w=[t_dst])
                    P.op("dve", lambda e, dst=dst, t0=t0: e.tensor_tensor(out=dst[:, t0:t0 + 512], in0=dst[:, t0:t0 + 512],
                                                                          in1=tmp[:, t0:t0 + 512], op=ALU.mult), r=[t_dst, t_tmp], w=[t_dst])
            P.barrier()
        fw3_sb = sb("fw3", [64, 2048], F32)
        P.dma("sp", fw3_sb[:], fw3_c[:, :], w=[t_c])
        dec_t = sb("dec", [128, 16 * 512], BF16)
        dec = dec_t[:].rearrange("p (t c) -> p t c", c=512)
        t_dec = Tr()
        for tt in range(16):
            P.op("act", lambda e, tt=tt: e.activation(out=dec[:, tt, :], in_=dlt[:], func=AF.Exp, scale=t01n[:, tt:tt + 1]),
                 r=[t_c], w=[t_dec])
        zcur = xs[0]
        t_zcur = t_xs[0]
        for o in range(2):
            with ExitStack() as es4:
                hc_t = es4.enter_context(SB(nc, "h_hc%d" % o, [128, 16 * 2 * 512], BF16))
                hc = hc_t[:].rearrange("p (t k c) -> p t k c", k=2, c=512)
                t_hc = Tr()
                zt_t = es4.enter_context(SB(nc, "h_zt%d" % o, [128, 16 * 512], BF16))
                zt = zt_t[:].rearrange("p (t c) -> p t c", c=512)
                t_zt = Tr()
                yy_t = es4.enter_context(SB(nc, "h_yy%d" % o, [128, NF * 2 * 512], BF16))
                yy = yy_t[:].rearrange("p (f k c) -> p f k c", k=2, c=512)
                t_yy = Tr()
                with ExitStack() as es5:
                    pspool = Pool(nc, es5, "ps", 8, [128, 512], F32, "hp_h")
                    ftmp = Pool(nc, es5, "sb", 4, [128, 512], F32, "h_hft")
                    for tt in range(16):
                        pf, pft = pspool.get()
                        pb, pbt = pspool.get()
                        P.op("pe", lambda e, pf=pf, tt=tt: e.matmul(pf[:, :], hd2[:, tt * 128:(tt + 1) * 128],
                                                                    fw3_sb[:, (o * 2) * 512:(o * 2 + 1) * 512], start=True, stop=True),
                             r=[t_hd2, t_c], w=[pft])
                        P.op("pe", lambda e, pb=pb, tt=tt: e.matmul(pb[:, :], hd2[:, tt * 128:(tt + 1) * 128],
                                                                    fw3_sb[:, (o * 2 + 1) * 512:(o * 2 + 2) * 512], start=True, stop=True),
                             r=[t_hd2, t_c], w=[pbt])
                        f1, f1t = ftmp.get()
                        f2, f2t = ftmp.get()
                        P.op("dve", lambda e, f1=f1, pf=pf, tt=tt: e.tensor_tensor(out=f1[:], in0=pf[:, :], in1=dec[:, tt, :], op=ALU.mult),
                             r=[pft, t_dec], w=[f1t])
                        if tt == 0:
                            P.op("dve", lambda e, f2=f2, pb=pb, tt=tt: e.scalar_tensor_tensor(out=f2[:], in0=pb[:, :], scalar=mask0[:, 0:1],
                                                                                              in1=dec[:, tt, :], op0=ALU.mult, op1=ALU.mult),
                                 r=[pbt, t_dec, t_c], w=[f2t])
                        else:
                            P.op("dve", lambda e, f2=f2, pb=pb, tt=tt: e.tensor_tensor(out=f2[:], in0=pb[:, :], in1=dec[:, tt, :], op=ALU.mult),
                                 r=[pbt, t_dec], w=[f2t])
                        P.op("pool", lambda e, f1=f1, f2=f2, tt=tt: e.tensor_tensor(out=hc[:, tt, 0, :], in0=f1[:], in1=f2[:], op=ALU.add),
                             r=[f1t, f2t], w=[t_hc])
                        P.op("pool", lambda e, f1=f1, f2=f2, tt=tt: e.tensor_tensor(out=hc[:, tt, 1, :], in0=f2[:], in1=f1[:], op=ALU.subtract),
                             r=[f1t, f2t], w=[t_hc])
                    P.barrier()
                with ExitStack() as es5:
                    pstr = Pool(nc, es5, "ps", 2, [128, 1024], BF16, "hp_t")
                    for tt in range(16):
                        pt, ptt = pstr.get()
                        for ct in range(4):
                            P.op("pe", lambda e, pt=pt, ct=ct, tt=tt: e.transpose(pt[:, ct * 128:(ct + 1) * 128],
                                                                                  zcur[:, ct, tt * 128:(tt + 1) * 128], ident_b[:]),
                                 r=[t_zcur, t_c], w=[ptt])
                        evac_copy(P, zt[:, tt, :], pt[:, 0:512], [ptt], [t_zt])
                    P.barrier()
                with ExitStack() as es5:
                    pspool = Pool(nc, es5, "ps", 8, [128, 512], F32, "hp_w")
                    cblk = Pool(nc, es5, "sb", 4, [128, 16 * 128], BF16, "h_cb")
                    hsb = Pool(nc, es5, "sb", 4, [128, 512], F32, "h_hsb")
                    ttmp = Pool(nc, es5, "sb", 4, [128, 512], F32, "h_tt")
                    for fc in range(NF):
                        cb, cbt = cblk.get()
                        sbk, sbt = cblk.get()
                        cbv = cb[:].rearrange("p (t f) -> p t f", f=128)
                        sbv = sbk[:].rearrange("p (t f) -> p t f", f=128)
                        P.dma("sp", cb[:], cst["CCt"][fc], w=[cbt])
                        P.dma("sp", sbk[:], cst["SSt"][fc], w=[sbt])
                        pHr, pHrt = pspool.get()
                        pHi, pHit = pspool.get()
                        pZr, pZrt = pspool.get()
                        pZs, pZst = pspool.get()
                        for tc_ in range(16):
                            st_, sp_ = (tc_ == 0), (tc_ == 15)
                            P.op("pe", lambda e, pHr=pHr, cbv=cbv, tc_=tc_, st_=st_, sp_=sp_: e.matmul(pHr[:, :], cbv[:, tc_, :], hc[:, tc_, 0, :],
                                                                                                      start=st_, stop=sp_), r=[cbt, t_hc], w=[pHrt])
                            P.op("pe", lambda e, pHi=pHi, sbv=sbv, tc_=tc_, st_=st_, sp_=sp_: e.matmul(pHi[:, :], sbv[:, tc_, :], hc[:, tc_, 1, :],
                                                                                                      start=st_, stop=sp_), r=[sbt, t_hc], w=[pHit])
                            P.op("pe", lambda e, pZr=pZr, cbv=cbv, tc_=tc_, st_=st_, sp_=sp_: e.matmul(pZr[:, :], cbv[:, tc_, :], zt[:, tc_, :],
                                                                                                      start=st_, stop=sp_), r=[cbt, t_zt], w=[pZrt])
                            P.op("pe", lambda e, pZs=pZs, sbv=sbv, tc_=tc_, st_=st_, sp_=sp_: e.matmul(pZs[:, :], sbv[:, tc_, :], zt[:, tc_, :],
                                                                                                      start=st_, stop=sp_), r=[sbt, t_zt], w=[pZst])
                        hr, hrt = hsb.get()
                        hi, hit = hsb.get()
                        P.op("act", lambda e, hr=hr, pHr=pHr, fc=fc: e.activation(out=hr[:], in_=pHr[:, :], func=AF.Identity, scale=wN[:, fc:fc + 1]),
                             r=[pHrt, t_c], w=[hrt])
                        P.op("act", lambda e, hi=hi, pHi=pHi, fc=fc: e.activation(out=hi[:], in_=pHi[:, :], func=AF.Identity, scale=wN[:, fc:fc + 1]),
                             r=[pHit, t_c], w=[hit])
                        a1, a1t = ttmp.get()
                        a2, a2t = ttmp.get()
                        P.op("dve", lambda e, a1=a1, pZr=pZr, hr=hr: e.tensor_tensor(out=a1[:], in0=pZr[:, :], in1=hr[:], op=ALU.mult),
                             r=[pZrt, hrt], w=[a1t])
                        P.op("dve", lambda e, a2=a2, pZs=pZs, hi=hi: e.tensor_tensor(out=a2[:], in0=pZs[:, :], in1=hi[:], op=ALU.mult),
                             r=[pZst, hit], w=[a2t])
                        P.op("pool", lambda e, a1=a1, a2=a2, fc=fc: e.tensor_tensor(out=yy[:, fc, 0, :], in0=a1[:], in1=a2[:], op=ALU.add),
                             r=[a1t, a2t], w=[t_yy])
                        a3, a3t = ttmp.get()
                        a4, a4t = ttmp.get()
                        P.op("dve", lambda e, a3=a3, pZs=pZs, hr=hr: e.tensor_tensor(out=a3[:], in0=pZs[:, :], in1=hr[:], op=ALU.mult),
                             r=[pZst, hrt], w=[a3t])
                        P.op("dve", lambda e, a4=a4, pZr=pZr, hi=hi: e.tensor_tensor(out=a4[:], in0=pZr[:, :], in1=hi[:], op=ALU.mult),
                             r=[pZrt, hit], w=[a4t])
                        P.op("pool", lambda e, a3=a3, a4=a4, fc=fc: e.tensor_tensor(out=yy[:, fc, 1, :], in0=a3[:], in1=a4[:], op=ALU.subtract),
                             r=[a3t, a4t], w=[t_yy])
                    P.barrier()
                with ExitStack() as es5:
                    pspool = Pool(nc, es5, "ps", 8, [128, 512], F32, "hp_i")
                    rblk = Pool(nc, es5, "sb", 4, [128, 1024], BF16, "h_rb")
                    ev = Pool(nc, es5, "sb", 3, [128, 512], F32, "h_ev")
                    znew = xs[o + 1]
                    t_zn = t_xs[o + 1]
                    ost = Pool(nc, es5, "sb", 3, [128, 512], BF16, "h_ho")
                    for th in range(2):
                        accs = [pspool.get() for _ in range(8)]
                        for fc in range(NF):
                            rc, rct = rblk.get()
                            rs_, rst = rblk.get()
                            P.dma("sp", rc[:], cst["CC"][fc * 128:(fc + 1) * 128, th * 1024:(th + 1) * 1024], w=[rct])
                            P.dma("sp", rs_[:], cst["SS"][fc * 128:(fc + 1) * 128, th * 1024:(th + 1) * 1024], w=[rst])
                            for ct in range(4):
                                for tq in range(2):
                                    ps, ptr = accs[ct * 2 + tq]
                                    P.op("pe", lambda e, ps=ps, rc=rc, fc=fc, ct=ct, tq=tq: e.matmul(
                                        ps[:, :], yy[:, fc, 0, ct * 128:(ct + 1) * 128], rc[:, tq * 512:(tq + 1) * 512],
                                        start=(fc == 0), stop=False), r=[t_yy, rct], w=[ptr])
                                    P.op("pe", lambda e, ps=ps, rs_=rs_, fc=fc, ct=ct, tq=tq: e.matmul(
                                        ps[:, :], yy[:, fc, 1, ct * 128:(ct + 1) * 128], rs_[:, tq * 512:(tq + 1) * 512],
                                        start=False, stop=(fc == NF - 1)), r=[t_yy, rst], w=[ptr])
                        for ct in range(4):
                            for tq in range(2):
                                ps, ptr = accs[ct * 2 + tq]
                                t0 = th * 1024 + tq * 512
                                e_, e_t = ev.get()
                                j = 48 + o * 4 + ct
                                P.op("dve", lambda e, e_=e_, ps=ps, ct=ct, t0=t0, j=j: e.scalar_tensor_tensor(
                                    out=e_[:], in0=zcur[:, ct, t0:t0 + 512], scalar=cvec_sb[:, j:j + 1], in1=ps[:, :],
                                    op0=ALU.mult, op1=ALU.add), r=[ptr, t_zcur, t_c], w=[e_t])
                                if o == 0:
                                    P.op("pool", lambda e, e_=e_, ct=ct, t0=t0: e.tensor_tensor(
                                        out=znew[:, ct, t0:t0 + 512], in0=znew[:, ct, t0:t0 + 512], in1=e_[:], op=ALU.mult),
                                        r=[e_t, t_zn], w=[t_zn])
                                else:
                                    o_, o_t = ost.get()
                                    P.op("pool", lambda e, e_=e_, o_=o_, ct=ct, t0=t0: e.tensor_tensor(
                                        out=o_[:], in0=znew[:, ct, t0:t0 + 512], in1=e_[:], op=ALU.mult), r=[e_t, t_zn], w=[o_t])
                                    P.dma("sp", cdT[ct * 128:(ct + 1) * 128, t0:t0 + 512], o_[:], r=[o_t], w=[t_out])
                    P.barrier()
            zcur = xs[o + 1]
            t_zcur = t_xs[o + 1]
        P.barrier()


def cd_consts():
    c = {}
    L = S
    t01 = np.linspace(0.0, 1.0, L, dtype=np.float32)
    w = (2.0 * np.pi * np.arange(L, dtype=np.float32) / L).astype(np.float32)
    bands = np.linspace(1e-4, 15, 16, dtype=np.float32)
    z = np.concatenate([t01[:, None], np.cos(w[:, None] * bands), -np.sin(w[:, None] * bands)], -1).astype(np.float32)
    c["zfeatT"] = np.ascontiguousarray(z.T)
    n = NF * 128
    a = np.arange(n, dtype=np.int64)
    prod = (a[:, None] * a[None, :]) % 4096
    ang = prod.astype(np.float64) * (2.0 * np.pi / 4096)
    valid = (a[:, None] <= 2048) & (a[None, :] <= 2048)
    c["CC"] = np.where(valid, np.cos(ang), 0.0).astype(np.float32).astype(ml_dtypes.bfloat16)
    c["SS"] = np.where(valid, np.sin(ang), 0.0).astype(np.float32).astype(ml_dtypes.bfloat16)
    for nm in ("CC", "SS"):
        m = np.asarray(c[nm])[0:S, :].reshape(16, 128, NF, 128)
        c[nm + "t"] = np.ascontiguousarray(m.transpose(2, 1, 0, 3).reshape(NF, 128, 16 * 128))
    wf = np.where((a == 0) | (a == 2048), 1.0, 2.0) / 4096.0
    wf = np.where(a <= 2048, wf, 0.0)
    c["wN"] = np.ascontiguousarray(wf.reshape(NF, 128).T).astype(np.float32)
    c["t01n"] = np.ascontiguousarray((-t01).reshape(16, 128).T).astype(np.float32)
    m0 = np.ones((128, 1), dtype=np.float32)
    m0[0, 0] = 0.0
    c["mask0"] = m0
    t = np.arange(L, dtype=np.int64)
    pf = ((t[:, None] * t[None, :]) % L).astype(np.float64) * (2.0 * np.pi / L)
    c["FC"] = np.cos(pf).astype(np.float32).astype(ml_dtypes.bfloat16)
    c["FS"] = np.sin(pf).astype(np.float32).astype(ml_dtypes.bfloat16)
    k = np.arange(256, dtype=np.int64)
    pw = ((k[:, None] * k[None, :]) % 256).astype(np.float64) * (2.0 * np.pi / 256)
    sc = 1.0 / np.sqrt(float(L) * 256.0)
    c["CSW"] = np.concatenate([np.cos(pw) * sc, -np.sin(pw) * sc], axis=1).astype(np.float32).astype(ml_dtypes.bfloat16)
    return c


def cd_host_inputs(inp, m1, b, hf):
    def fm(v, n=16):
        return np.ascontiguousarray(v.reshape(n, 128).T)
    w = inp["cd_w_in"][0]
    cols = []
    for o in range(3):
        cols += list(range(o * 1024 + 512 * hf, o * 1024 + 512 * hf + 512))
    cols += list(range(3072 + 512 * hf, 3072 + 512 * hf + 512))
    cw = inp["cd_conv_w"][0]
    cb = inp["cd_conv_b"][0]
    cvec = np.zeros((128, 56), dtype=np.float32)
    for o in range(3):
        for ct in range(4):
            ch = o * 1024 + 512 * hf + ct * 128 + np.arange(128)
            j = (o * 4 + ct) * 4
            cvec[:, j] = cw[0, ch]
            cvec[:, j + 1] = cw[1, ch]
            cvec[:, j + 2] = cw[2, ch]
            cvec[:, j + 3] = cb[ch]
    sk = inp["cd_skip"][0]
    for o in range(2):
        for ct in range(4):
            cvec[:, 48 + o * 4 + ct] = sk[o, 512 * hf + ct * 128 + np.arange(128)]
    fw3 = inp["cd_filt_w3"][0]
    f3c = []
    for o in range(2):
        for dr in range(2):
            f3c += list(range(o * 2048 + dr * 1024 + 512 * hf, o * 2048 + dr * 1024 + 512 * hf + 512))
    fvec = np.stack([inp["cd_filt_b1"][0], inp["cd_filt_freq1"][0], inp["cd_filt_b2"][0], inp["cd_filt_freq2"][0]], axis=1)
    import math
    mn = math.log(1e-2) / 1.5
    mx = math.log(1e-2) / 0.3
    deltas = np.abs(np.linspace(mn, mx, 1024, dtype=np.float32))[512 * hf:512 * hf + 512]
    mb = m1[b]
    return {"modv": np.concatenate([fm(mb[0:2048]), fm(mb[2048:4096])], axis=1), "w_in_c": np.ascontiguousarray(w[:, cols]),
            "cvec": cvec, "fw1": np.ascontiguousarray(inp["cd_filt_w1"][0]), "fw2": np.ascontiguousarray(inp["cd_filt_w2"][0]),
            "fvec": np.ascontiguousarray(fvec.astype(np.float32)), "fw3_c": np.ascontiguousarray(fw3[:, f3c]),
            "delta_b": np.tile(deltas[None, :], (128, 1)).astype(np.float32)}


def build_cd_prog():
    nc = bass.Bass("TRN2", target_bir_lowering=False)
    consts = make_consts()
    consts.update(cd_consts())
    with ExitStack() as es:
        P = Prog(nc, es)
        cst = declare_consts(nc, consts)
        hT = dram_in(nc, "hT", [D, S])
        modv = dram_in(nc, "modv", [128, 32])
        w_in_c = dram_in(nc, "w_in_c", [D, 2048])
        cvec = dram_in(nc, "cvec", [128, 56])
        fw1 = dram_in(nc, "fw1", [33, 64])
        fw2 = dram_in(nc, "fw2", [64, 64])
        fvec = dram_in(nc, "fvec", [64, 4])
        fw3_c = dram_in(nc, "fw3_c", [64, 2048])
        delta_b = dram_in(nc, "delta_b", [128, 512])
        cdT = dram_out(nc, "cdT", [1024, S], BF16)
        stage_cd(P, cst, hT, modv, w_in_c, cvec, fw1, fw2, fvec, fw3_c, delta_b, cdT, Tr(), Tr())
        P.barrier()
    return nc, consts


ADA_BF16 = True


def stage_ada(P, csT, adaw, adab, mT, t_out):
    nc = P.nc
    with ExitStack() as es:
        cs = es.enter_context(SB(nc, "d_cs", [128, 80], F32))
        sT = es.enter_context(SB(nc, "d_sT", [128, 80], F32))
        bia = es.enter_context(SB(nc, "d_b", [128, 24], F32))
        res = es.enter_context(SB(nc, "d_res", [128, 120], F32))
        t_c = Tr()
        t_res = Tr()
        P.dma("sp", cs[:], csT[:, :], w=[t_c])
        P.dma("sp", bia[:], adab[:, :], w=[t_c])
        sTb = es.enter_context(SB(nc, "d_sTb", [128, 80], BF16))
        P.op("act", lambda e: e.activation(out=sT[:], in_=cs[:], func=AF.Silu), r=[t_c], w=[t_c])
        P.op("dve", lambda e: e.tensor_copy(out=sTb[:], in_=sT[:]), r=[t_c], w=[t_c])
        wb = Pool(nc, es, "sb", 3, [128, 1536], BF16 if ADA_BF16 else F32, "d_w")
        ps = [es.enter_context(PSM(nc, "dp_%d" % i, [128, 512], F32)) for i in range(2)]
        t_ps = [Tr(), Tr()]
        for l in range(2):
            for k in range(16):
                w_, w_t = wb.get()
                P.dma("pool" if ADA_BF16 else "sp", w_[:], adaw[l, k * 128:(k + 1) * 128, :], w=[w_t])
                for tl in range(12):
                    P.op("pe", lambda e, w_=w_, l=l, k=k, tl=tl: e.matmul(ps[l][:, tl * 5:(tl + 1) * 5], w_[:, tl * 128:(tl + 1) * 128],
                                                                          (sTb if ADA_BF16 else sT)[:, k * 5:(k + 1) * 5], start=(k == 0 and tl == 0),
                                                                          stop=(k == 15), skip_group_check=True),
                         r=[w_t, t_c], w=[t_ps[l]])
            P.op("dve", lambda e, l=l: e.tensor_tensor(out=res[:, l * 60:(l + 1) * 60].rearrange("p (t j) -> p t j", j=5),
                                                       in0=ps[l][:, 0:60].rearrange("p (t j) -> p t j", j=5),
                                                       in1=bia[:, l * 12:(l + 1) * 12].unsqueeze(2).to_broadcast([128, 12, 5]), op=ALU.add),
                 r=[t_ps[l], t_c], w=[t_res])
        P.dma("sp", mT[:, :], res[:], r=[t_res], w=[t_out])
        P.barrier()


def _new():
    return bass.Bass("TRN2", target_bir_lowering=False)


def build_ada_prog():
    nc = _new()
    with ExitStack() as es:
        P = Prog(nc, es)
        csT = dram_in(nc, "csT", [128, 80])
        adaw = dram_in(nc, "adaw", [2, D, 1536])
        adab = dram_in(nc, "adab", [128, 24])
        mT = dram_out(nc, "mT", [128, 120])
        stage_ada(P, csT, adaw, adab, mT, Tr())
    return nc, {}


def build_route_prog():
    nc = _new()
    consts = make_consts()
    with ExitStack() as es:
        P = Prog(nc, es)
        cst = declare_consts(nc, consts)
        hT = dram_in(nc, "hT", [D, S])
        modv = dram_in(nc, "modv", [128, 32])
        router = dram_in(nc, "router", [D, 16])
        xe_out = dram_out(nc, "xe_out", [8, 128, 16 * CAP], BF16)
        pos_out = dram_out(nc, "pos_out", [16, S])
        gate_out = dram_out(nc, "gate_out", [16, S])
        stage_moe_route(P, cst, hT, modv, router, xe_out, pos_out, gate_out, Tr(), Tr())
    return nc, consts


def build_ffn_prog():
    nc = _new()
    with ExitStack() as es:
        P = Prog(nc, es)
        xe_in = dram_in(nc, "xe_in", [2, 4, 128, 16 * CAP], BF16)
        w1 = dram_in(nc, "w1", [2, D, FF])
        w3 = dram_in(nc, "w3", [2, D, FF])
        w2 = dram_in(nc, "w2", [2, FF, D])
        ye_out = dram_out(nc, "ye_out", [2, 8, 128, D], BF16)
        stage_moe_ffn(P, xe_in, w1, w3, w2, ye_out, Tr(), Tr())
    return nc, {}


def build_scat_prog():
    nc = _new()
    consts = make_consts()
    with ExitStack() as es:
        P = Prog(nc, es)
        cst = declare_consts(nc, consts)
        ye_in = dram_in(nc, "ye_in", [16, 2, 128, D], BF16)
        pos_in = dram_in(nc, "pos_in", [16, 1024])
        gate_in = dram_in(nc, "gate_in", [16, 1024])
        hT = dram_in(nc, "hT", [D, 1024])
        vecs = dram_in(nc, "vecs", [128, 48])
        outT = dram_out(nc, "outT", [D, 1024])
        stage_scatter_ln(P, cst, ye_in, pos_in, gate_in, hT, vecs, outT, Tr(), Tr())
    return nc, consts


def build_oproj_prog():
    nc = _new()
    consts = make_consts()
    with ExitStack() as es:
        P = Prog(nc, es)
        cst = declare_consts(nc, consts)
        ao_in = dram_in(nc, "ao_in", [2048, 1024], BF16)
        w_out = dram_in(nc, "w_out", [2048, D])
        hT = dram_in(nc, "hT", [D, 1024])
        vecs = dram_in(nc, "vecs", [128, 48])
        outT = dram_out(nc, "outT", [D, 1024])
        stage_oproj_ln(P, cst, ao_in, w_out, hT, vecs, outT, Tr(), Tr())
    return nc, consts


def _fm(v, n=16):
    return np.ascontiguousarray(np.asarray(v, dtype=np.float32).reshape(n, 128).T)


def _run(nc, consts, in_maps):
    for im in in_maps:
        for k, v in consts.items():
            im["c_" + k] = v
    res = run_bass_kernel_spmd(nc, in_maps, core_ids=list(range(len(in_maps))))
    return res.results


DEBUG = {}


def _dbg(name, arr):
    if "ref" in DEBUG:
        ref = DEBUG["ref"][name]
        err = float(np.sqrt(((arr - ref) ** 2).mean() / (ref ** 2).mean()))
        print("DBG %s relerr %.5f" % (name, err), flush=True)


def kernel_unfused(**inp):
    inp = {k: np.asarray(v) for k, v in inp.items()}
    progs = {}

    def prog(name, fn):
        if name not in progs:
            progs[name] = fn()
        return progs[name]

    nc, consts = prog("ada", build_ada_prog)
    cvs = np.concatenate([inp["c"], inp["c_ctx"][None, :]], axis=0)
    csT = np.ascontiguousarray(cvs.reshape(5, 16, 128).transpose(2, 1, 0).reshape(128, 80))
    ims = []
    for c in range(8):
        ab = inp["ada_b"][:, c * 1536:(c + 1) * 1536].reshape(2, 12, 128)
        ims.append({"csT": csT, "adaw": np.ascontiguousarray(inp["ada_w"][:, :, c * 1536:(c + 1) * 1536]),
                    "adab": np.ascontiguousarray(ab.transpose(2, 0, 1).reshape(128, 24))})
    r = _run(nc, consts, ims)
    m = np.zeros((2, 5, 12288), dtype=np.float32)
    for c in range(8):
        o = np.asarray(r[c]["mT"]).reshape(128, 2, 12, 5)
        m[:, :, c * 1536:(c + 1) * 1536] = o.transpose(1, 3, 2, 0).reshape(2, 5, 1536)
    if "ref" in DEBUG:
        _dbg("m0", m[0, 0:4])
        _dbg("m1", m[1, 0:4])

    def vecs(l, b, which):
        mb = m[l, b]
        g = mb[2 * D:3 * D] if which == 1 else mb[5 * D:6 * D]
        lg = inp["ln1_g" if which == 1 else "ln2_g"][l]
        lb = inp["ln1_b" if which == 1 else "ln2_b"][l]
        return np.concatenate([_fm(g), _fm(lg), _fm(lb)], axis=1)

    def oproj(ao_full, w_out, hT_halves, l):
        nc, consts = prog("oproj", build_oproj_prog)
        ims = []
        for core in range(8):
            b, th = core // 2, core % 2
            ims.append({"ao_in": np.ascontiguousarray(ao_full[b][:, th * 1024:(th + 1) * 1024]), "w_out": w_out,
                        "hT": hT_halves[core], "vecs": vecs(l, b, 1)})
        r = _run(nc, consts, ims)
        return [np.asarray(r[c]["outT"]) for c in range(8)]

    def moe(hT_halves, l):
        hfull = [np.ascontiguousarray(np.concatenate([hT_halves[2 * b], hT_halves[2 * b + 1]], axis=1)) for b in range(4)]
        nc, consts = prog("route", build_route_prog)
        ims = []
        perms = []
        for core in range(8):
            b, hf = core // 2, core % 2
            perm = np.r_[np.arange(8 * hf, 8 * hf + 8), np.arange(8 * (1 - hf), 8 * (1 - hf) + 8)]
            perms.append(perm)
            mb = m[l, b]
            ims.append({"hT": hfull[b], "modv": np.concatenate([_fm(mb[3 * D:4 * D]), _fm(mb[4 * D:5 * D])], axis=1),
                        "router": np.ascontiguousarray(inp["router"][l][:, perm])})
        rr = _run(nc, consts, ims)
        nc, consts = prog("ffn", build_ffn_prog)
        ims = []
        for c in range(8):
            xe = np.stack([np.stack([np.asarray(rr[2 * b + (2 * c + i) // 8]["xe_out"])[(2 * c + i) % 8] for b in range(4)]) for i in range(2)])
            ims.append({"xe_in": xe, "w1": inp["exp_w1"][l, 2 * c:2 * c + 2], "w3": inp["exp_w3"][l, 2 * c:2 * c + 2],
                        "w2": inp["exp_w2"][l, 2 * c:2 * c + 2]})
        rf = _run(nc, consts, ims)
        nc, consts = prog("scat", build_scat_prog)
        ims = []
        for core in range(8):
            b, th = core // 2, core % 2
            perm = perms[core]
            ye = np.stack([np.asarray(rf[e // 2]["ye_out"])[e % 2, 2 * b:2 * b + 2] for e in perm])
            ims.append({"ye_in": ye, "pos_in": np.ascontiguousarray(np.asarray(rr[core]["pos_out"])[:, th * 1024:(th + 1) * 1024]),
                        "gate_in": np.ascontiguousarray(np.asarray(rr[core]["gate_out"])[:, th * 1024:(th + 1) * 1024]),
                        "hT": hT_halves[core], "vecs": vecs(l, b, 2)})
        rs = _run(nc, consts, ims)
        return [np.asarray(rs[c]["outT"]) for c in range(8)]

    def dbg_h(name, halves):
        if "ref" in DEBUG:
            full = np.stack([np.concatenate([halves[2 * b], halves[2 * b + 1]], axis=1).T for b in range(4)])
            _dbg(name, full)

    nc, consts = prog("attn", build_attn_prog)
    ims = [attn_host_inputs(inp, m[0], core // 2, core % 2) for core in range(8)]
    ra = _run(nc, consts, ims)
    ao_full = []
    for b in range(4):
        a0 = np.asarray(ra[2 * b]["aoT"])
        a1 = np.asarray(ra[2 * b + 1]["aoT"])
        ao_full.append(np.concatenate([a0[0:512], a1[0:512], a0[512:1024], a1[512:1024]], axis=0))
    xT_halves = [np.ascontiguousarray(inp["x"][core // 2][(core % 2) * 1024:(core % 2 + 1) * 1024].T) for core in range(8)]
    h = oproj(ao_full, np.ascontiguousarray(inp["ab_w_out"][0]), xT_halves, 0)
    dbg_h("h0a", h)
    h = moe(h, 0)
    dbg_h("h0b", h)
    nc, consts = prog("cd", build_cd_prog)
    ims = []
    for core in range(8):
        b, hf = core // 2, core % 2
        im = cd_host_inputs(inp, m[1], b, hf)
        im["hT"] = np.ascontiguousarray(np.concatenate([h[2 * b], h[2 * b + 1]], axis=1))
        ims.append(im)
    rc = _run(nc, consts, ims)
    cd_full = []
    for b in range(4):
        a0 = np.asarray(rc[2 * b]["cdT"])
        a1 = np.asarray(rc[2 * b + 1]["cdT"])
        cd_full.append(np.concatenate([a0[0:512], a1[0:512], a0[512:1024], a1[512:1024]], axis=0))
    h = oproj(cd_full, np.ascontiguousarray(inp["cd_w_out"][0]), h, 1)
    dbg_h("h1a", h)
    h = moe(h, 1)
    dbg_h("h1b", h)
    out = np.zeros((NB, S, D), dtype=np.float32)
    for core in range(8):
        b, th = core // 2, core % 2
        out[b, th * 1024:(th + 1) * 1024, :] = h[core].T
    return out


U32 = mybir.dt.uint32


def build_fused_prog():
    nc = _new()
    consts = make_consts()
    consts.update(cd_consts())
    with ExitStack() as es:
        P = Prog(nc, es)
        cst = declare_consts(nc, consts)
        I = {}

        def din(name, shape, dt=F32):
            I[name] = dram_in(nc, name, shape, dt)
            return I[name]
        csT = din("csT", [128, 80]); adaw = din("adaw", [2, D, 1536]); adab = din("adab", [128, 24]); oh5 = din("oh5", [128, 5])
        xT = din("xT", [D, S]); ctxT = din("ctxT", [D, LC]); w_in_r = din("w_in_r", [D, 3328]); kvn = din("kvn", [128, 4])
        w_ukv_r = din("w_ukv_r", [512, 1024]); cosT = din("cosT", [128, S]); sinT = din("sinT", [128, S])
        na_T = din("na_T", [128, 4 * TB_N * 64]); na_RM = din("na_RM", [128, 256])
        xown = din("xown", [D, 1024]); w_out0 = din("w_out0", [2048, D]); w_out1 = din("w_out1", [2048, D])
        lnv = din("lnv", [128, 128])
        router = [din("router0", [D, 16]), din("router1", [D, 16])]
        w1 = [din("w1_0", [2, D, FF]), din("w1_1", [2, D, FF])]
        w3 = [din("w3_0", [2, D, FF]), din("w3_1", [2, D, FF])]
        w2 = [din("w2_0", [2, FF, D]), din("w2_1", [2, FF, D])]
        w_in_c = din("w_in_c", [D, 2048]); cvec = din("cvec", [128, 56]); fw1 = din("fw1", [33, 64]); fw2 = din("fw2", [64, 64])
        fvec = din("fvec", [64, 4]); fw3_c = din("fw3_c", [64, 2048]); delta_b = din("delta_b", [128, 512])
        idx_ao = din("idx_ao", [128, 16], U32); idx_h = din("idx_h", [128, 32], U32); idx_xe = din("idx_xe", [128, 8], U32)
        idx_ye = din("idx_ye", [128, 64], U32); oh2 = din("oh2", [128, 2])
        outT = dram_out(nc, "outT", [D, 1024])
        idx_sb = es.enter_context(SB(nc, "x_idx", [128, 120], U32))
        oh2_sb = es.enter_context(SB(nc, "x_oh2", [128, 2], F32))
        t_pg = Tr()
        t_i = Tr()
        P.dma("sp", idx_sb[:, 0:16], idx_ao[:, :], w=[t_i])
        P.dma("sp", idx_sb[:, 16:48], idx_h[:, :], w=[t_i])
        P.dma("sp", idx_sb[:, 48:56], idx_xe[:, :], w=[t_i])
        P.dma("sp", idx_sb[:, 56:120], idx_ye[:, :], w=[t_i])
        P.dma("sp", oh2_sb[:], oh2[:, :], w=[t_i])
        IA, IH, IX, IY = 0, 16, 48, 56

        def ds(name, shape, dt=F32):
            return dram_scratch(nc, name, shape, dt)
        mT_d = ds("x_mT", [128, 120]); gat_m = ds("x_gm", [8 * 128, 120])
        mod_d = ds("x_mod", [128, 2 * 96]); ctx_d = ds("x_ctx", [128, 2 * 96])
        stage_ada(P, csT, adaw, adab, mT_d, Tr())
        P.barrier()
        P.allgather(mT_d[:, :], gat_m[:, :])
        P.barrier()
        with ExitStack() as es0:
            mall = es0.enter_context(SB(nc, "x_mall", [128, 8 * 120], F32))
            tmp = es0.enter_context(SB(nc, "x_mtmp", [128, 8 * 120], F32))
            msel = es0.enter_context(SB(nc, "x_msel", [128, 2 * 192], F32))
            oh5_sb = es0.enter_context(SB(nc, "x_oh5", [128, 5], F32))
            t_m = Tr()
            P.dma("sp", mall[:].rearrange("p (c x) -> p c x", x=120), gat_m.rearrange("(c p) x -> p c x", p=128), w=[t_m])
            P.dma("sp", oh5_sb[:], oh5[:, :], w=[t_m])
            P.op("dve", lambda e: e.tensor_tensor(out=tmp[:].rearrange("p (q j) -> p q j", j=5), in0=mall[:].rearrange("p (q j) -> p q j", j=5),
                                                  in1=oh5_sb[:].unsqueeze(1).to_broadcast([128, 192, 5]), op=ALU.mult), r=[t_m], w=[t_m])
            P.op("dve", lambda e: e.tensor_reduce(out=msel[:, 0:192], in_=tmp[:].rearrange("p (q j) -> p q j", j=5), axis=AX.X, op=ALU.add),
                 r=[t_m], w=[t_m])
            P.op("dve", lambda e: e.tensor_copy(out=msel[:, 192:384], in_=mall[:].rearrange("p (q j) -> p q j", j=5)[:, :, 4]), r=[t_m], w=[t_m])
            for l in range(2):
                for (src0, dst) in ((0, mod_d), (192, ctx_d)):
                    P.dma("sp", dst[:, l * 96:(l + 1) * 96].rearrange("p (c t) -> p c t", t=12),
                          msel[:, src0:src0 + 192].rearrange("p (c l t) -> p c l t", l=2, t=12)[:, :, l, :], r=[t_m], w=[t_m])
            P.barrier()

        def modp(l, a, b_):
            return mod_d[:, l * 96 + a:l * 96 + b_]

        def lnp(l, k):
            return lnv[:, (l * 4 + k) * 16:(l * 4 + k + 1) * 16]
        ao_d = ds("x_ao", [1024, S], BF16); gat_ao = ds("x_gao", [8 * 1024, S], BF16)
        stage_attn(P, cst, xT, ctxT, [modp(0, 0, 32), ctx_d[:, 0:32]], w_in_r, kvn, w_ukv_r, cosT, sinT, na_T, na_RM, ao_d, Tr())
        P.barrier()
        P.allgather(ao_d[:, :], gat_ao[:, :])
        P.barrier()

        def mk_load_ao(gat):
            rows = gat.rearrange("r (h t) -> (r h) t", h=2)

            def load_ao(dst, w):
                for c in range(16):
                    P.idma(dst[:, c, :], rows, idx_sb[:, IA + c:IA + c + 1], r=[t_i], w=w)
            return load_ao

        def mk_load_h(gat):
            def load_h(dst, c, w):
                for half in range(2):
                    P.idma(dst[:, half * 1024:(half + 1) * 1024], gat[:, :], idx_sb[:, IH + c * 2 + half:IH + c * 2 + half + 1], r=[t_i], w=w)
            return load_h

        def layer_moe(l, hA_d, h_out):
            gat_h = ds("x_gh%d" % l, [8 * D, 1024])
            P.allgather(hA_d[:, :], gat_h[:, :])
            P.barrier()
            xe_d = ds("x_xe%d" % l, [8, 128, 16 * CAP], BF16); gat_xe = ds("x_gxe%d" % l, [8 * 8 * 128, 16 * CAP], BF16)
            pos_d = ds("x_pos%d" % l, [16, S]); gate_d = ds("x_gate%d" % l, [16, S])
            stage_moe_route(P, cst, None, [modp(l, 48, 80)], router[l], xe_d, pos_d, gate_d, Tr(), Tr(), load_h=mk_load_h(gat_h))
            P.barrier()
            P.allgather(xe_d.rearrange("e p x -> (e p) x"), gat_xe[:, :])
            P.barrier()
            ye_d = ds("x_ye%d" % l, [2, 8, 128, D], BF16); gat_ye = ds("x_gye%d" % l, [8 * 16 * 128, D], BF16)
            gxe3 = gat_xe.rearrange("r (c s) -> r c s", s=CAP)

            def load_xe(dst, ex, b, w):
                P.idma(dst, gxe3, idx_sb[:, IX + ex * 4 + b:IX + ex * 4 + b + 1], r=[t_i], w=w)
            stage_moe_ffn(P, None, w1[l], w3[l], w2[l], ye_d, Tr(), Tr(), load_xe=load_xe)
            P.barrier()
            P.allgather(ye_d.rearrange("e g p d -> (e g p) d"), gat_ye[:, :])
            P.barrier()
            gye_rows = gat_ye.rearrange("r (h d) -> (r h) d", h=2)

            def load_ye(dst, q, dh, w):
                for st in range(2):
                    col = IY + (q * 2 + st) * 2 + dh
                    P.idma(dst[:, st, :], gye_rows, idx_sb[:, col:col + 1], r=[t_i], w=w)

            pgt = []

            def load_pg(pos, gate, w):
                for (src, dstp) in ((pos_d, pos), (gate_d, gate)):
                    P.dma("sp", pgt[0][:], src[:, :], w=[t_pg])
                    P.op("dve", lambda e, dstp=dstp: e.tensor_scalar(out=dstp, in0=pgt[0][:, 0:1024], scalar1=oh2_sb[0:16, 0:1], scalar2=None,
                                                                     op0=ALU.mult), r=[t_pg, t_i], w=w)
                    P.op("dve", lambda e, dstp=dstp: e.scalar_tensor_tensor(out=dstp, in0=pgt[0][:, 1024:2048], scalar=oh2_sb[0:16, 1:2],
                                                                            in1=dstp, op0=ALU.mult, op1=ALU.add), r=[t_pg, t_i], w=w)
            with ExitStack() as es_pg:
                pgt.append(es_pg.enter_context(SB(nc, "x_pgt", [16, S], F32)))
                stage_scatter_ln(P, cst, None, None, None, hA_d, [modp(l, 80, 96), lnp(l, 2), lnp(l, 3)], h_out, Tr(), Tr(),
                                 load_ye=load_ye, load_pg=load_pg)
                P.barrier()
                pgt.pop()

        hA0 = ds("x_hA0", [D, 1024])
        stage_oproj_ln(P, cst, None, w_out0, xown, [modp(0, 32, 48), lnp(0, 0), lnp(0, 1)], hA0, Tr(), Tr(), load_ao=mk_load_ao(gat_ao))
        P.barrier()
        hB0 = ds("x_hB0", [D, 1024])
        layer_moe(0, hA0, hB0)
        gat_hb = ds("x_ghb", [8 * D, 1024])
        P.allgather(hB0[:, :], gat_hb[:, :])
        P.barrier()
        cd_d = ds("x_cd", [1024, S], BF16); gat_cd = ds("x_gcd", [8 * 1024, S], BF16)
        stage_cd(P, cst, None, [modp(1, 0, 32)], w_in_c, cvec, fw1, fw2, fvec, fw3_c, delta_b, cd_d, Tr(), Tr(), load_h=mk_load_h(gat_hb))
        P.barrier()
        P.allgather(cd_d[:, :], gat_cd[:, :])
        P.barrier()
        hA1 = ds("x_hA1", [D, 1024])
        stage_oproj_ln(P, cst, None, w_out1, hB0, [modp(1, 32, 48), lnp(1, 0), lnp(1, 1)], hA1, Tr(), Tr(), load_ao=mk_load_ao(gat_cd))
        P.barrier()
        layer_moe(1, hA1, outT)
        P.barrier()
    return nc, consts


def fused_host_inputs(inp, consts):
    ims = []
    cvs = np.concatenate([inp["c"], inp["c_ctx"][None, :]], axis=0)
    csT = np.ascontiguousarray(cvs.reshape(5, 16, 128).transpose(2, 1, 0).reshape(128, 80))
    cd_in = {k: inp[k] for k in inp if k.startswith("cd_")}
    m_dummy = np.zeros((5, 12288), dtype=np.float32)
    p = np.arange(128)
    for core in range(8):
        b, hf = core // 2, core % 2
        im = {}
        ab = inp["ada_b"][:, core * 1536:(core + 1) * 1536].reshape(2, 12, 128)
        im["csT"] = csT
        im["adaw"] = np.ascontiguousarray(inp["ada_w"][:, :, core * 1536:(core + 1) * 1536])
        im["adab"] = np.ascontiguousarray(ab.transpose(2, 0, 1).reshape(128, 24))
        oh5 = np.zeros((128, 5), dtype=np.float32); oh5[:, b] = 1.0
        im["oh5"] = oh5
        a = attn_host_inputs(inp, m_dummy, b, hf)
        a.pop("modv")
        im.update(a)
        im["xown"] = np.ascontiguousarray(inp["x"][b][hf * 1024:(hf + 1) * 1024].T)
        im["w_out0"] = np.ascontiguousarray(inp["ab_w_out"][0])
        im["w_out1"] = np.ascontiguousarray(inp["cd_w_out"][0])
        lnv = np.zeros((128, 128), dtype=np.float32)
        for l in range(2):
            for k, nm in enumerate(("ln1_g", "ln1_b", "ln2_g", "ln2_b")):
                lnv[:, (l * 4 + k) * 16:(l * 4 + k + 1) * 16] = _fm(inp[nm][l])
        im["lnv"] = lnv
        perm = np.r_[np.arange(8 * hf, 8 * hf + 8), np.arange(8 * (1 - hf), 8 * (1 - hf) + 8)]
        for l in range(2):
            im["router%d" % l] = np.ascontiguousarray(inp["router"][l][:, perm])
            im["w1_%d" % l] = inp["exp_w1"][l, 2 * core:2 * core + 2]
            im["w3_%d" % l] = inp["exp_w3"][l, 2 * core:2 * core + 2]
            im["w2_%d" % l] = inp["exp_w2"][l, 2 * core:2 * core + 2]
        cdh = cd_host_inputs(cd_in, np.zeros((4, 12288), dtype=np.float32), b, hf)
        cdh.pop("modv")
        im.update(cdh)
        idx_ao = np.zeros((128, 16), dtype=np.uint32)
        for c in range(16):
            part = c // 4
            rank = 2 * b + (part % 2)
            row = (part // 2) * 512 + (c % 4) * 128 + p
            idx_ao[:, c] = (rank * 1024 + row) * 2 + hf
        idx_h = np.zeros((128, 32), dtype=np.uint32)
        for c in range(16):
            for half in range(2):
                idx_h[:, c * 2 + half] = (2 * b + half) * D + c * 128 + p
        idx_xe = np.zeros((128, 8), dtype=np.uint32)
        for ex in range(2):
            e = 2 * core + ex
            for bb in range(4):
                rank = 2 * bb + e // 8
                idx_xe[:, ex * 4 + bb] = (rank * 8 + e % 8) * 128 + p
        idx_ye = np.zeros((128, 64), dtype=np.uint32)
        for q in range(16):
            e = perm[q]
            for st in range(2):
                row = ((e // 2) * 2 + e % 2) * 8 + (2 * b + st)
                for dh in range(2):
                    idx_ye[:, (q * 2 + st) * 2 + dh] = (row * 128 + p) * 2 + dh
        oh2 = np.zeros((128, 2), dtype=np.float32); oh2[:, hf] = 1.0
        im.update({"idx_ao": idx_ao, "idx_h": idx_h, "idx_xe": idx_xe, "idx_ye": idx_ye, "oh2": oh2})
        ims.append(im)
    return ims


def kernel_fused(**inp):
    inp = {k: np.asarray(v) for k, v in inp.items()}
    nc, consts = build_fused_prog()
    ims = fused_host_inputs(inp, consts)
    r = _run(nc, consts, ims)
    out = np.zeros((NB, S, D), dtype=np.float32)
    for core in range(8):
        b, th = core // 2, core % 2
        out[b, th * 1024:(th + 1) * 1024, :] = np.asarray(r[core]["outT"]).T
    return out


def build_solo_prog(l0_only=False):
    nc = _new()
    consts = make_consts()
    consts.update(cd_consts())
    with ExitStack() as es:
        P = Prog(nc, es)
        cst = declare_consts(nc, consts)

        def din(name, shape, dt=F32):
            return dram_in(nc, name, shape, dt)

        def ds(name, shape, dt=F32):
            return dram_scratch(nc, name, shape, dt)
        csT = din("csT", [128, 80]); adaw = din("adaw", [2, D, 12288]); adab = din("adab", [128, 8 * 24]); oh5 = din("oh5", [128, 5])
        xT = din("xT", [D, S]); ctxT = din("ctxT", [D, LC]); kvn = din("kvn", [128, 4])
        w_in_r = [din("w_in_r%d" % h, [D, 3328]) for h in range(2)]
        w_ukv_r = [din("w_ukv_r%d" % h, [512, 1024]) for h in range(2)]
        na_T = [din("na_T%d" % h, [128, 4 * TB_N * 64]) for h in range(2)]
        cosT = din("cosT", [128, S]); sinT = din("sinT", [128, S]); na_RM = din("na_RM", [128, 256])
        w_out = [din("w_out0", [2048, D]), din("w_out1", [2048, D])]
        lnv = din("lnv", [128, 128])
        router = [din("router%d" % l, [D, 16]) for l in range(2)]
        nl = 1 if l0_only else 2
        w1 = [din("w1_%d" % l, [16, D, FF]) for l in range(nl)]
        w3 = [din("w3_%d" % l, [16, D, FF]) for l in range(nl)]
        w2 = [din("w2_%d" % l, [16, FF, D]) for l in range(nl)]
        w_in_c = [din("w_in_c%d" % h, [D, 2048]) for h in range(2)]
        cvec = [din("cvec%d" % h, [128, 56]) for h in range(2)]
        fw3_c = [din("fw3_c%d" % h, [64, 2048]) for h in range(2)]
        delta_b = [din("delta_b%d" % h, [128, 512]) for h in range(2)]
        fw1 = din("fw1", [33, 64]); fw2 = din("fw2", [64, 64]); fvec = din("fvec", [64, 4])
        outT = dram_out(nc, "outT", [D, S])
        m_d = ds("y_m", [8 * 128, 120])
        for sl in range(8):
            stage_ada(P, csT, adaw[:, :, sl * 1536:(sl + 1) * 1536], adab[:, sl * 24:(sl + 1) * 24], m_d[sl * 128:(sl + 1) * 128, :], Tr())
            P.barrier()
        mod_d = ds("y_mod", [128, 2 * 96]); ctx_d = ds("y_ctx", [128, 2 * 96])
        with ExitStack() as es0:
            mall = es0.enter_context(SB(nc, "y_mall", [128, 8 * 120], F32))
            tmp = es0.enter_context(SB(nc, "y_mtmp", [128, 8 * 120], F32))
            msel = es0.enter_context(SB(nc, "y_msel", [128, 2 * 192], F32))
            oh5_sb = es0.enter_context(SB(nc, "y_oh5", [128, 5], F32))
            t_m = Tr()
            P.dma("sp", mall[:].rearrange("p (c x) -> p c x", x=120), m_d.rearrange("(c p) x -> p c x", p=128), w=[t_m])
            P.dma("sp", oh5_sb[:], oh5[:, :], w=[t_m])
            P.op("dve", lambda e: e.tensor_tensor(out=tmp[:].rearrange("p (q j) -> p q j", j=5), in0=mall[:].rearrange("p (q j) -> p q j", j=5),
                                                  in1=oh5_sb[:].unsqueeze(1).to_broadcast([128, 192, 5]), op=ALU.mult), r=[t_m], w=[t_m])
            P.op("dve", lambda e: e.tensor_reduce(out=msel[:, 0:192], in_=tmp[:].rearrange("p (q j) -> p q j", j=5), axis=AX.X, op=ALU.add),
                 r=[t_m], w=[t_m])
            P.op("dve", lambda e: e.tensor_copy(out=msel[:, 192:384], in_=mall[:].rearrange("p (q j) -> p q j", j=5)[:, :, 4]), r=[t_m], w=[t_m])
            for l in range(2):
                for (src0, dst) in ((0, mod_d), (192, ctx_d)):
                    P.dma("sp", dst[:, l * 96:(l + 1) * 96].rearrange("p (c t) -> p c t", t=12),
                          msel[:, src0:src0 + 192].rearrange("p (c l t) -> p c l t", l=2, t=12)[:, :, l, :], r=[t_m], w=[t_m])
            P.barrier()

        def modp(l, a, b_):
            return mod_d[:, l * 96 + a:l * 96 + b_]

        def lnp(l, k):
            return lnv[:, (l * 4 + k) * 16:(l * 4 + k + 1) * 16]

        def mk_load_ao(parts, th):
            def load_ao(dst, w):
                for c in range(16):
                    part = c // 4
                    src = parts[part % 2]
                    r0 = (part // 2) * 512 + (c % 4) * 128
                    P.dma("sp", dst[:, c, :], src[r0:r0 + 128, th * 1024:(th + 1) * 1024], w=w)
            return load_ao

        def mk_load_h(halves):
            def load_h(dst, c, w):
                for half in range(2):
                    P.dma("sp", dst[:, half * 1024:(half + 1) * 1024], halves[half][c * 128:(c + 1) * 128, :], w=w)
            return load_h

        def oproj_both(l, parts, resid_halves, name):
            outs = []
            for th in range(2):
                o = ds("y_%s%d" % (name, th), [D, 1024])
                stage_oproj_ln(P, cst, None, w_out[l], resid_halves[th], [modp(l, 32, 48), lnp(l, 0), lnp(l, 1)], o, Tr(), Tr(),
                               load_ao=mk_load_ao(parts, th))
                P.barrier()
                outs.append(o)
            return outs

        def moe_both(l, hA, out_halves):
            xe_d = ds("y_xe", [16, 128, 16 * CAP], BF16); pos_d = ds("y_pos", [16, S]); gate_d = ds("y_gate", [16, S])
            stage_moe_route16(P, cst, None, [modp(l, 48, 80)], router[l], xe_d, pos_d, gate_d, Tr(), Tr(), load_h=mk_load_h(hA))
            P.barrier()
            ye_d = ds("y_ye", [16, 2, 128, D], BF16)

            def load_xe(dst, ex, b, w):
                P.dma("sp", dst, xe_d[ex].rearrange("p (c s) -> p c s", s=CAP), w=w)
            stage_moe_ffn(P, None, w1[l], w3[l], w2[l], ye_d, Tr(), Tr(), load_xe=load_xe, nb=1, n_exp=16)
            P.barrier()
            for th in range(2):
                def load_ye(dst, q, dh, w):
                    P.dma("sp", dst, ye_d[q, :, :, dh * 1024:(dh + 1) * 1024].rearrange("s p d -> p s d"), w=w)

                def load_pg(pos_sb, gate_sb, w, th=th):
                    P.dma("sp", pos_sb, pos_d[:, th * 1024:(th + 1) * 1024], w=w)
                    P.dma("sp", gate_sb, gate_d[:, th * 1024:(th + 1) * 1024], w=w)
                stage_scatter_ln(P, cst, None, None, None, hA[th], [modp(l, 80, 96), lnp(l, 2), lnp(l, 3)], out_halves[th], Tr(), Tr(),
                                 load_ye=load_ye, load_pg=load_pg)
                P.barrier()

        ao = []
        for hf in range(2):
            a_d = ds("y_ao", [1024, S], BF16)
            stage_attn(P, cst, xT, ctxT, [modp(0, 0, 32), ctx_d[:, 0:32]], w_in_r[hf], kvn, w_ukv_r[hf], cosT, sinT, na_T[hf], na_RM, a_d, Tr())
            P.barrier()
            ao.append(a_d)
        hA0 = oproj_both(0, ao, [xT[:, 0:1024], xT[:, 1024:2048]], "hA0")
        if l0_only:
            moe_both(0, hA0, [outT[:, 0:1024], outT[:, 1024:2048]])
            P.barrier()
            return nc, consts
        hB0 = [ds("y_hB0_%d" % th, [D, 1024]) for th in range(2)]
        moe_both(0, hA0, hB0)
        cd = []
        for hf in range(2):
            c_d = ds("y_cd", [1024, S], BF16)
            stage_cd(P, cst, None, [modp(1, 0, 32)], w_in_c[hf], cvec[hf], fw1, fw2, fvec, fw3_c[hf], delta_b[hf], c_d, Tr(), Tr(),
                     load_h=mk_load_h(hB0))
            P.barrier()
            cd.append(c_d)
        hA1 = oproj_both(1, cd, hB0, "hA1")
        moe_both(1, hA1, [outT[:, 0:1024], outT[:, 1024:2048]])
        P.barrier()
    return nc, consts


def solo_host_inputs(inp):
    ims = []
    cvs = np.concatenate([inp["c"], inp["c_ctx"][None, :]], axis=0)
    csT = np.ascontiguousarray(cvs.reshape(5, 16, 128).transpose(2, 1, 0).reshape(128, 80))
    adab = np.concatenate([np.ascontiguousarray(inp["ada_b"][:, c * 1536:(c + 1) * 1536].reshape(2, 12, 128).transpose(2, 0, 1).reshape(128, 24))
                           for c in range(8)], axis=1)
    cd_in = {k: inp[k] for k in inp if k.startswith("cd_")}
    lnv = np.zeros((128, 128), dtype=np.float32)
    for l in range(2):
        for k, nm in enumerate(("ln1_g", "ln1_b", "ln2_g", "ln2_b")):
            lnv[:, (l * 4 + k) * 16:(l * 4 + k + 1) * 16] = _fm(inp[nm][l])
    zero5 = np.zeros((5, 12288), dtype=np.float32)
    per_b = {}
    for b in range(4):
        im = {"csT": csT, "adaw": inp["ada_w"], "adab": np.ascontiguousarray(adab), "lnv": lnv}
        oh5 = np.zeros((128, 5), dtype=np.float32); oh5[:, b] = 1.0
        im["oh5"] = oh5
        for hf in range(2):
            a = attn_host_inputs(inp, zero5, b, hf)
            for k in ("w_in_r", "w_ukv_r", "na_T"):
                im["%s%d" % (k, hf)] = a[k]
            if hf == 0:
                for k in ("xT", "ctxT", "kvn", "cosT", "sinT", "na_RM"):
                    im[k] = a[k]
            cdh = cd_host_inputs(cd_in, zero5[:4], b, hf)
            for k in ("w_in_c", "cvec", "fw3_c", "delta_b"):
                im["%s%d" % (k, hf)] = cdh[k]
            if hf == 0:
                for k in ("fw1", "fw2", "fvec"):
                    im[k] = cdh[k]
        for l in range(2):
            im["router%d" % l] = np.ascontiguousarray(inp["router"][l])
        im["w_out0"] = np.ascontiguousarray(inp["ab_w_out"][0])
        im["w_out1"] = np.ascontiguousarray(inp["cd_w_out"][0])
        for l in range(2):
            im["w1_%d" % l] = inp["exp_w1"][l]
            im["w3_%d" % l] = inp["exp_w3"][l]
            im["w2_%d" % l] = inp["exp_w2"][l]
        per_b[b] = im
    for b in range(4):
        ims.append(per_b[b])
    return ims


def kernel(**inp):
    inp = {k: np.asarray(v) for k, v in inp.items()}
    nc, consts = build_solo_prog()
    ims = solo_host_inputs(inp)
    r = _run(nc, consts, ims)
    out = np.zeros((NB, S, D), dtype=np.float32)
    for b in range(4):
        out[b] = np.asarray(r[b]["outT"]).T
    return out
```
